# Optimizing a Trainium2 kernel written in Bass

```python
import math
import jax
import jax.numpy as jnp
from jax import lax
import numpy as np


D_MODEL = 1024
BATCH = 8
SEQ = 2048
DEPTH = 2

CTX_LEN = 256
GRID_W = 64
Q_BLOCK = 128
ROPE_THETA = 10000.0
EPS = 1e-6
NEG_BIG = -1e30

MIX_W = D_MODEL // 2
A_HEAD_DIM = 64
A_HEADS = MIX_W // (2 * A_HEAD_DIM)
B_HEAD_DIM = 64
B_Q_HEADS = MIX_W // B_HEAD_DIM
B_KV_HEADS = 2
C_HEAD_DIM = 128
C_HEADS = MIX_W // C_HEAD_DIM
C_CONV = 3
C_CHUNK = 128
N_BRANCH = 3
IN_WIDTHS = (MIX_W, MIX_W, MIX_W, MIX_W, B_KV_HEADS * B_HEAD_DIM, B_KV_HEADS * B_HEAD_DIM,
             MIX_W, MIX_W, MIX_W, MIX_W, 4 * C_HEADS, N_BRANCH * D_MODEL)
IN_COLS = sum(IN_WIDTHS)
N_EXPERTS = 16
EXPERT_FF = 2 * D_MODEL
EC_CAPACITY_FACTOR = 2

kernel_name = 'hybrid_diffusion_block'


def rmsnorm(x, w):
    xf = x.astype(jnp.float32)
    y = xf * lax.rsqrt(jnp.mean(xf * xf, axis=-1, keepdims=True) + EPS)
    return (y * w.astype(jnp.float32)).astype(x.dtype)


def modulate(x, shift, scale):
    return x * (1.0 + scale) + shift


def split_columns(p):
    return jnp.split(p, np.cumsum(IN_WIDTHS)[:-1].tolist(), axis=-1)


def diff_lambda_init(layer):
    return 0.8 - 0.6 * math.exp(-0.3 * layer)


def axial_rope_tables(n_tokens, head_dim):
    n_rows = n_tokens // GRID_W
    rows = jnp.repeat(jnp.arange(n_rows, dtype=jnp.float32), GRID_W)
    cols = jnp.tile(jnp.arange(GRID_W, dtype=jnp.float32), n_rows)
    n_freq = head_dim // 4
    inv_freq = ROPE_THETA ** (-jnp.arange(n_freq, dtype=jnp.float32) / n_freq)
    ang = jnp.concatenate([rows[:, None] * inv_freq, cols[:, None] * inv_freq], axis=-1)
    return jnp.cos(ang), jnp.sin(ang)


def apply_rope(x, cos, sin):
    shape = (1, x.shape[1]) + (1,) * (x.ndim - 3) + (x.shape[-1] // 2,)
    c = cos.reshape(shape).astype(x.dtype)
    s = sin.reshape(shape).astype(x.dtype)
    x1, x2 = jnp.split(x, 2, axis=-1)
    return jnp.concatenate([x1 * c - x2 * s, x1 * s + x2 * c], axis=-1)


def sweep_query_blocks(fn, q):
    B, L = q.shape[:2]
    nb = L // Q_BLOCK
    qb = jnp.moveaxis(q.reshape((B, nb, Q_BLOCK) + q.shape[2:]), 1, 0)
    out = lax.map(fn, qb)
    return jnp.moveaxis(out, 0, 1).reshape((B, L) + out.shape[3:])


def diff_attention(q, k, v, lam, subln_w, lam_init):
    s = jnp.einsum('bqhjd,bkhjd->bhjqk', q, k).astype(jnp.float32) * (A_HEAD_DIM ** -0.5)
    p = jax.nn.softmax(s, axis=-1)
    p = p[:, :, 0] - lam * p[:, :, 1]
    o = jnp.einsum('bhqk,bkhe->bqhe', p.astype(v.dtype), v)
    o = rmsnorm(o, subln_w) * (1.0 - lam_init)
    return o.reshape(o.shape[:2] + (-1,))


def gqa_attention(q, k, v):
    B, Lq = q.shape[:2]
    qg = q.reshape(B, Lq, B_KV_HEADS, B_Q_HEADS // B_KV_HEADS, B_HEAD_DIM)
    s = jnp.einsum('bqhgd,bkhd->bhgqk', qg, k).astype(jnp.float32) * (B_HEAD_DIM ** -0.5)
    p = jax.nn.softmax(s, axis=-1)
    o = jnp.einsum('bhgqk,bkhd->bqhgd', p.astype(v.dtype), v)
    return o.reshape(B, Lq, B_Q_HEADS * B_HEAD_DIM)


def centred_depthwise_conv(x, w):
    K = w.shape[0]
    pad = K // 2
    L = x.shape[1]
    xp = jnp.pad(x, ((0, 0), (pad, pad), (0, 0)))
    return sum(xp[:, j:j + L] * w[j] for j in range(K))


def mlstm_chunkwise(q, k, v, ig, lf, state):
    B, L, H, _ = q.shape
    nc = L // C_CHUNK

    def chunks(a):
        return jnp.moveaxis(a.reshape((B, nc, C_CHUNK) + a.shape[2:]), 1, 0)

    tril = jnp.tril(jnp.ones((C_CHUNK, C_CHUNK), dtype=bool))

    def step(carry, xs):
        C, n, m = carry
        qc, kc, vc, ic, fc = xs
        b = jnp.cumsum(fc, axis=1).transpose(0, 2, 1)
        i = ic.transpose(0, 2, 1)
        log_d = jnp.where(tril, b[..., :, None] - b[..., None, :] + i[..., None, :], NEG_BIG)
        log_inter = b + m[..., None]
        m_t = jnp.maximum(log_inter, log_d.max(axis=-1))
        d = jnp.exp(log_d - m_t[..., None])
        w_inter = jnp.exp(log_inter - m_t)
        s = jnp.einsum('bthd,bshd->bhts', qc, kc).astype(jnp.float32) * d
        num = (jnp.einsum('bhts,bshv->bhtv', s, vc)
               + w_inter[..., None] * jnp.einsum('bhvd,bthd->bhtv', C, qc))
        den = s.sum(axis=-1) + w_inter * jnp.einsum('bhd,bthd->bht', n, qc)
        h = num / jnp.maximum(jnp.abs(den), jnp.exp(-m_t))[..., None]
        b_last = b[..., -1]
        log_w = b_last[..., None] - b + i
        m_new = jnp.maximum(b_last + m, log_w.max(axis=-1))
        w = jnp.exp(log_w - m_new[..., None])
        decay = jnp.exp(b_last + m - m_new)
        C_new = decay[..., None, None] * C + jnp.einsum('bhs,bshv,bshd->bhvd', w, vc, kc)
        n_new = decay[..., None] * n + jnp.einsum('bhs,bshd->bhd', w, kc)
        return (C_new, n_new, m_new), h.transpose(0, 2, 1, 3)

    state, hs = lax.scan(step, state, (chunks(q), chunks(k), chunks(v), chunks(ig), chunks(lf)))
    return state, jnp.moveaxis(hs, 0, 1).reshape(B, L, H, -1)


def mlstm_inputs(q, k, v, g, conv_w, gate_b):
    qk = jax.nn.silu(centred_depthwise_conv(jnp.concatenate([q, k], axis=-1), conv_w))
    q, k = jnp.split(qk, 2, axis=-1)
    heads = lambda a: a.reshape(a.shape[:2] + (C_HEADS, C_HEAD_DIM))
    g = g.astype(jnp.float32) + gate_b.astype(jnp.float32)
    i_f, f_f, i_b, f_b = jnp.split(g, 4, axis=-1)
    qkv = (heads(q), heads(k) * (C_HEAD_DIM ** -0.5), heads(v))
    gates = ((i_f, jax.nn.log_sigmoid(f_f)), (i_b, jax.nn.log_sigmoid(f_b)))
    return qkv, gates


def mlstm_bidirectional(ctx_in, lat_in):
    (qc, kc, vc), gates_c = ctx_in
    (ql, kl, vl), gates_l = lat_in
    B = ql.shape[0]
    outs_c, outs_l = [], []
    for direction in range(2):
        flip = (lambda a: a[:, ::-1]) if direction == 1 else (lambda a: a)
        state0 = (jnp.zeros((B, C_HEADS, C_HEAD_DIM, C_HEAD_DIM), jnp.float32),
                  jnp.zeros((B, C_HEADS, C_HEAD_DIM), jnp.float32),
                  jnp.zeros((B, C_HEADS), jnp.float32))
        ic, fc = gates_c[direction]
        il, fl = gates_l[direction]
        st, hc = mlstm_chunkwise(flip(qc), flip(kc), flip(vc), flip(ic), flip(fc), state0)
        _, hl = mlstm_chunkwise(flip(ql), flip(kl), flip(vl), flip(il), flip(fl), st)
        outs_c.append(flip(hc))
        outs_l.append(flip(hl))
    return outs_c[0] + outs_c[1], outs_l[0] + outs_l[1]


def mlstm_output(h, o, norm_w):
    h = rmsnorm(h.astype(o.dtype), norm_w.reshape(C_HEADS, C_HEAD_DIM))
    return jax.nn.sigmoid(o) * h.reshape(o.shape)


def hybrid_mixer(h_c, h_l, cos, sin, lam_init, w_in, conv_w, gate_b, c_norm_w, lam_vecs, subln_w,
                 q_norm_w, k_norm_w, w_br_a, w_br_b, w_br_c, w_out, with_ctx):
    p_c = split_columns(h_c @ w_in)
    p_l = split_columns(h_l @ w_in)

    lv = lam_vecs.astype(jnp.float32)
    lam = jnp.exp(jnp.sum(lv[0] * lv[1])) - jnp.exp(jnp.sum(lv[2] * lv[3])) + lam_init
    heads_a = lambda a: a.reshape(a.shape[:2] + (A_HEADS, 2, A_HEAD_DIM))
    vheads_a = lambda a: a.reshape(a.shape[:2] + (A_HEADS, 2 * A_HEAD_DIM))
    qa_c, ka_c, va_c = heads_a(p_c[0]), heads_a(p_c[1]), vheads_a(p_c[2])
    qa_l = apply_rope(heads_a(p_l[0]), cos, sin)
    ka_all = jnp.concatenate([ka_c, apply_rope(heads_a(p_l[1]), cos, sin)], axis=1)
    va_all = jnp.concatenate([va_c, vheads_a(p_l[2])], axis=1)
    oa_l = sweep_query_blocks(lambda qblk: diff_attention(qblk, ka_all, va_all, lam, subln_w, lam_init), qa_l)

    heads_b = lambda a, nh: a.reshape(a.shape[:2] + (nh, B_HEAD_DIM))
    qb_c = rmsnorm(heads_b(p_c[3], B_Q_HEADS), q_norm_w)
    kb_c = rmsnorm(heads_b(p_c[4], B_KV_HEADS), k_norm_w)
    vb_c = heads_b(p_c[5], B_KV_HEADS)
    qb_l = apply_rope(rmsnorm(heads_b(p_l[3], B_Q_HEADS), q_norm_w), cos, sin)
    kb_all = jnp.concatenate([kb_c, apply_rope(rmsnorm(heads_b(p_l[4], B_KV_HEADS), k_norm_w), cos, sin)], axis=1)
    vb_all = jnp.concatenate([vb_c, heads_b(p_l[5], B_KV_HEADS)], axis=1)
    ob_l = sweep_query_blocks(lambda qblk: gqa_attention(qblk, kb_all, vb_all), qb_l)

    ctx_in = mlstm_inputs(p_c[6], p_c[7], p_c[8], p_c[10], conv_w, gate_b)
    lat_in = mlstm_inputs(p_l[6], p_l[7], p_l[8], p_l[10], conv_w, gate_b)
    hc_sum, hl_sum = mlstm_bidirectional(ctx_in, lat_in)
    oc_l = mlstm_output(hl_sum, p_l[9], c_norm_w)

    def merge(oa, ob, oc, g):
        gates = jax.nn.sigmoid(g).reshape(g.shape[:2] + (N_BRANCH, D_MODEL))
        merged = (gates[..., 0, :] * (oa @ w_br_a) + gates[..., 1, :] * (ob @ w_br_b)
                  + gates[..., 2, :] * (oc @ w_br_c))
        return merged @ w_out

    y_l = merge(oa_l, ob_l, oc_l, p_l[11])
    y_c = None
    if with_ctx:
        oa_c = diff_attention(qa_c, ka_c, va_c, lam, subln_w, lam_init)
        ob_c = gqa_attention(qb_c, kb_c, vb_c)
        oc_c = mlstm_output(hc_sum, p_c[9], c_norm_w)
        y_c = merge(oa_c, ob_c, oc_c, p_c[11])
    return y_c, y_l


def expert_choice_ffn(h, w_router, w_gate, w_up, w_down):
    B, N, D = h.shape
    cap = EC_CAPACITY_FACTOR * N // N_EXPERTS
    logits = jnp.einsum('bnd,de->bne', h, w_router).astype(jnp.float32)
    aff = jax.nn.softmax(logits, axis=-1).transpose(0, 2, 1)
    g, idx = lax.top_k(aff, cap)
    xe = jax.vmap(lambda hb, ib: hb[ib])(h, idx)
    a = jnp.einsum('becd,edf->becf', xe, w_gate)
    u = jnp.einsum('becd,edf->becf', xe, w_up)
    ye = jnp.einsum('becf,efd->becd', jax.nn.silu(a) * u, w_down) * g[..., None].astype(h.dtype)
    return jax.vmap(lambda yb, ib: jnp.zeros((N, D), yb.dtype).at[ib.reshape(-1)].add(yb.reshape(-1, D)))(ye, idx)


def setup_inputs(seed: int = 0) -> dict:
    key = jax.random.key(seed)
    ks = jax.random.split(key, 32)
    D = D_MODEL
    nrm = lambda k, shape, scale: jax.random.normal(k, shape, jnp.float32) * scale
    gate_b = jnp.concatenate([
        nrm(ks[10], (DEPTH, C_HEADS), 0.1),
        jax.random.uniform(ks[11], (DEPTH, C_HEADS), jnp.float32, 3.0, 6.0),
        nrm(ks[12], (DEPTH, C_HEADS), 0.1),
        jax.random.uniform(ks[13], (DEPTH, C_HEADS), jnp.float32, 3.0, 6.0)], axis=-1)
    return {
        'x': nrm(ks[0], (BATCH, SEQ, D), 1.0),
        'c': nrm(ks[1], (BATCH, D), 1.0),
        'ctx': nrm(ks[2], (BATCH, CTX_LEN, D), 1.0),
        'c_ctx': nrm(ks[3], (D,), 1.0),
        'w_ada': nrm(ks[4], (DEPTH, D, 6 * D), 0.5 * D ** -0.5),
        'b_ada': nrm(ks[5], (DEPTH, 6 * D), 0.02),
        'norm1_w': 1.0 + nrm(ks[6], (DEPTH, D), 0.05),
        'norm2_w': 1.0 + nrm(ks[7], (DEPTH, D), 0.05),
        'w_in': nrm(ks[8], (DEPTH, D, IN_COLS), D ** -0.5),
        'mlstm_conv_w': nrm(ks[9], (DEPTH, C_CONV, 2 * MIX_W), C_CONV ** -0.5),
        'mlstm_gate_b': gate_b,
        'mlstm_norm_w': 1.0 + nrm(ks[14], (DEPTH, MIX_W), 0.05),
        'diff_lambda': nrm(ks[15], (DEPTH, 4, A_HEAD_DIM), 0.1),
        'diff_subln_w': 1.0 + nrm(ks[16], (DEPTH, 2 * A_HEAD_DIM), 0.05),
        'gqa_qnorm_w': 1.0 + nrm(ks[17], (DEPTH, B_HEAD_DIM), 0.05),
        'gqa_knorm_w': 1.0 + nrm(ks[18], (DEPTH, B_HEAD_DIM), 0.05),
        'w_branch_a': nrm(ks[19], (DEPTH, MIX_W, D), MIX_W ** -0.5),
        'w_branch_b': nrm(ks[20], (DEPTH, MIX_W, D), MIX_W ** -0.5),
        'w_branch_c': nrm(ks[21], (DEPTH, MIX_W, D), MIX_W ** -0.5),
        'w_out': nrm(ks[22], (DEPTH, D, D), D ** -0.5),
        'w_router': nrm(ks[23], (DEPTH, D, N_EXPERTS), D ** -0.5),
        'w_exp_gate': nrm(ks[24], (DEPTH, N_EXPERTS, D, EXPERT_FF), D ** -0.5),
        'w_exp_up': nrm(ks[25], (DEPTH, N_EXPERTS, D, EXPERT_FF), D ** -0.5),
        'w_exp_down': nrm(ks[26], (DEPTH, N_EXPERTS, EXPERT_FF, D), EXPERT_FF ** -0.5),
        'final_norm_w': 1.0 + nrm(ks[27], (D,), 0.05),
    }


def reference(x, c, ctx, c_ctx, w_ada, b_ada, norm1_w, norm2_w, w_in, mlstm_conv_w, mlstm_gate_b,
              mlstm_norm_w, diff_lambda, diff_subln_w, gqa_qnorm_w, gqa_knorm_w, w_branch_a, w_branch_b,
              w_branch_c, w_out, w_router, w_exp_gate, w_exp_up, w_exp_down, final_norm_w):
    cos, sin = axial_rope_tables(x.shape[1], A_HEAD_DIM)
    for layer in range(DEPTH):
        with_ctx = layer < DEPTH - 1
        mod_l = (jax.nn.silu(c) @ w_ada[layer] + b_ada[layer])[:, None, :]
        mod_c = (jax.nn.silu(c_ctx) @ w_ada[layer] + b_ada[layer])[None, None, :]
        sh1_l, sc1_l, g1_l, sh2_l, sc2_l, g2_l = jnp.split(mod_l, 6, axis=-1)
        sh1_c, sc1_c, g1_c, sh2_c, sc2_c, g2_c = jnp.split(mod_c, 6, axis=-1)

        h_l = modulate(rmsnorm(x, norm1_w[layer]), sh1_l, sc1_l)
        h_c = modulate(rmsnorm(ctx, norm1_w[layer]), sh1_c, sc1_c)
        y_c, y_l = hybrid_mixer(h_c, h_l, cos, sin, diff_lambda_init(layer), w_in[layer],
                                mlstm_conv_w[layer], mlstm_gate_b[layer], mlstm_norm_w[layer],
                                diff_lambda[layer], diff_subln_w[layer], gqa_qnorm_w[layer],
                                gqa_knorm_w[layer], w_branch_a[layer], w_branch_b[layer],
                                w_branch_c[layer], w_out[layer], with_ctx)
        x = x + g1_l * y_l
        h_l = modulate(rmsnorm(x, norm2_w[layer]), sh2_l, sc2_l)
        x = x + g2_l * expert_choice_ffn(h_l, w_router[layer], w_exp_gate[layer], w_exp_up[layer], w_exp_down[layer])
        if with_ctx:
            ctx = ctx + g1_c * y_c
            h_c = modulate(rmsnorm(ctx, norm2_w[layer]), sh2_c, sc2_c)
            ctx = ctx + g2_c * expert_choice_ffn(h_c, w_router[layer], w_exp_gate[layer], w_exp_up[layer], w_exp_down[layer])
    return rmsnorm(x, final_norm_w)
```

```python
import math
from contextlib import ExitStack

import numpy as np
import concourse.bass as bass
import concourse.mybir as mybir
from concourse.bass_utils import run_bass_kernel_spmd

F32 = mybir.dt.float32
BF16 = mybir.dt.bfloat16
AF = mybir.ActivationFunctionType
ALU = mybir.AluOpType
AX = mybir.AxisListType
EPS = 1e-6


class Sched:
    def __init__(self, nc, n_dma_sems=32, same_engine_sync=True):
        self.nc = nc
        self.eng = {"pe": nc.tensor, "dve": nc.vector, "act": nc.scalar, "pool": nc.gpsimd, "sp": nc.sync}
        self.sem = {k: nc.alloc_semaphore(name="s_" + k) for k in self.eng}
        self.cnt = {k: 0 for k in self.eng}
        self.waited = {k: {} for k in self.eng}
        self.dsem = [nc.alloc_semaphore(name="d%d" % i) for i in range(2 * n_dma_sems)]
        self.dcnt = [0] * (2 * n_dma_sems)
        self.nds = n_dma_sems
        self.drr = [0, 0]
        self.tok = {}
        self.same = same_engine_sync
        self.n_inst = 0
        self.n_wait = 0

    def _st(self, t):
        s = self.tok.get(t)
        if s is None:
            s = self.tok[t] = [None, []]
        return s

    def _wait(self, engname, deps):
        need = {}
        for ev, skip_same in deps:
            if ev is None:
                continue
            sem, val, src = ev
            if src == engname and (skip_same or not self.same or engname == "pe"):
                continue
            k = sem.num
            if k not in need or need[k][1] < val:
                need[k] = (sem, val)
        e = self.eng[engname]
        w = self.waited[engname]
        for k, (sem, val) in need.items():
            if w.get(k, 0) < val:
                e.wait_ge(sem, val)
                w[k] = val
                self.n_wait += 1

    def _deps(self, reads, writes, excl):
        deps = []
        for t in reads:
            deps.append((self._st(t)[0], False))
        for t in writes:
            s = self._st(t)
            deps.append((s[0], False))
            deps.extend((r, False) for r in s[1])
        for t in excl:
            s = self._st(t)
            deps.append((s[0], True))
        return deps

    def _commit(self, ev, reads, writes, excl):
        for t in reads:
            self._st(t)[1].append(ev)
        for t in writes:
            s = self._st(t)
            s[0] = ev
            s[1] = []
        for t in excl:
            s = self._st(t)
            s[0] = ev
            s[1] = []

    def op(self, engname, fn, reads=(), writes=(), excl=()):
        self._wait(engname, self._deps(reads, writes, excl))
        inst = fn(self.eng[engname])
        self.cnt[engname] += 1
        ev = (self.sem[engname], self.cnt[engname], engname)
        inst.then_inc(ev[0], 1)
        self._commit(ev, reads, writes, excl)
        self.n_inst += 1
        return ev

    def dma(self, queue, out, in_, reads=(), writes=(), **kw):
        self._wait(queue, self._deps(reads, writes, ()))
        q = 1 if queue == "pool" else 0
        i = q * self.nds + self.drr[q]
        self.drr[q] = (self.drr[q] + 1) % self.nds
        inst = self.eng[queue].dma_start(out=out, in_=in_, **kw)
        self.dcnt[i] += 16
        ev = (self.dsem[i], self.dcnt[i], "dma")
        inst.then_inc(ev[0], 16)
        self._commit(ev, reads, writes, ())
        self.n_inst += 1
        return ev

    def wait_all(self, engname):
        deps = [((self.sem[k], self.cnt[k], k), False) for k in self.eng if self.cnt[k] > 0 and k != engname]
        deps += [((self.dsem[i], self.dcnt[i], "dma"), False) for i in range(len(self.dsem)) if self.dcnt[i] > 0]
        self._wait(engname, deps)

    def barrier(self):
        for k in ("pe", "dve", "act", "pool", "sp"):
            self.wait_all(k)
        self.tok = {}


class Cfg:
    def __init__(s, D=1024, L=2048, LC=256, E=16, FF=2048, DEPTH=2, GW=64):
        s.D, s.L, s.LC, s.E, s.FF, s.DEPTH, s.GW = D, L, LC, E, FF, DEPTH, GW
        s.KC = D // 128
        s.NT = L // 128
        s.NTC = LC // 128
        s.NTT = s.NT + s.NTC
        s.NTOK = s.NTT * 128
        MW = s.MW = D // 2
        s.HA = MW // 128
        s.HBQ = MW // 64
        s.GRP = s.HBQ // 2
        s.HC = MW // 128
        s.FC = FF // 128
        s.CAPL = 2 * L // E
        s.CAPC = 2 * LC // E
        s.oAq, s.oAk, s.oAv, s.oBq = 0, MW, 2 * MW, 3 * MW
        s.oBk, s.oBv = 4 * MW, 4 * MW + 128
        s.oCq, s.oCk, s.oCv, s.oCo = 4 * MW + 256, 5 * MW + 256, 6 * MW + 256, 7 * MW + 256
        s.oG = 8 * MW + 256
        s.oMG = s.oG + 4 * s.HC
        s.INC = s.oMG + 3 * D
        off = {}
        n = 0

        def add(name, w):
            nonlocal n
            off[name] = (n, w)
            n += w
        add("c", s.KC)
        add("cctx", s.KC)
        for l in range(DEPTH):
            add("bada%d" % l, 6 * s.KC)
            add("n1%d" % l, s.KC)
            add("n2%d" % l, s.KC)
            add("conv%d" % l, 3 * 2 * s.HC)
            add("qnc%d" % l, 2)
            add("knc%d" % l, 2)
        add("cosT", 0)
        add("iop", 1)
        add("eps", 1)
        s.coff, s.NCOL = off, n
        roff = {}
        n = 0

        def addr(name, w):
            nonlocal n
            roff[name] = (n, w)
            n += w
        addr("sub", 128)
        addr("cn", MW)
        addr("gb", 4 * s.HC)
        addr("lam", 256)
        s.NROWL = n
        n = 0
        addr("iof", 256)
        s.roff, s.NROWG = roff, n


def rope_tables(cfg):
    L, GW = cfg.L, cfg.GW
    t = np.arange(L)
    rows = (t // GW).astype(np.float32)
    cols = (t % GW).astype(np.float32)
    nf = 16
    inv = (10000.0 ** (-np.arange(nf, dtype=np.float32) / nf)).astype(np.float32)
    ang = np.concatenate([rows[:, None] * inv, cols[:, None] * inv], axis=-1).astype(np.float32)
    cos = np.cos(ang).astype(np.float32)
    sin = np.sin(ang).astype(np.float32)
    cosT = np.zeros((128, L), np.float32)
    sinT = np.zeros((128, L), np.float32)
    for p in range(128):
        d = p % 64
        f = d % 32
        cosT[p] = cos[:, f]
        sinT[p] = -sin[:, f] if d < 32 else sin[:, f]
    return cosT, sinT


def const_pack():
    r = np.arange(128)
    ident = np.eye(128, dtype=np.float32)
    triU = (r[:, None] <= r[None, :]).astype(np.float32)
    triL = (r[:, None] >= r[None, :]).astype(np.float32)
    sU = (r[:, None] < r[None, :]).astype(np.float32)
    sL = (r[:, None] > r[None, :]).astype(np.float32)
    ones = np.ones((128, 128), np.float32)
    blk = (r[:, None] // 64 == r[None, :] // 64).astype(np.float32)
    return np.concatenate([ident, triU, triL, sU, sL, ones, blk], axis=1)


CI, CTU, CTL, CSU, CSL, CON, CBK = range(7)


class Builder:
    def __init__(self, cfg, taps=None, stop_after=None):
        self.cfg = cfg
        self.taps = taps or {}
        self.stop_after = stop_after
        self.nc = bass.Bass("TRN2", target_bir_lowering=False)
        self.S = None

    def sb(self, es, name, shape, dt):
        self._uid = getattr(self, "_uid", 0) + 1
        return es.enter_context(self.nc.sbuf_tensor("%s_%d" % (name, self._uid), list(shape), dt))

    def V(self, fn, r=(), w=(), x=()):
        return self.S.op("dve", fn, r, w, x)

    def A(self, fn, r=(), w=(), x=()):
        return self.S.op("act", fn, r, w, x)

    def G(self, fn, r=(), w=(), x=()):
        return self.S.op("pool", fn, r, w, x)

    def P(self, fn, r=(), w=(), x=()):
        return self.S.op("pe", fn, r, w, x)

    def bk(self, i):
        return "pb%d" % i

    def cst(self, k):
        return self.consts[:, k * 128:(k + 1) * 128]

    def col(self, name, a=0, b=None):
        o, w = self.cfg.coff[name]
        if b is None:
            b = w
        return self.cols[:, o + a:o + b]

    def row(self, name, a=0, b=None):
        o, w = self.cfg.roff[name]
        if b is None:
            b = w
        t = self.rowsG if name in ("iof",) else self.rowsL
        return t[:, o + a:o + b]

    def tap(self, name, ap_sb, reads):
        if name not in self.taps:
            return
        shape = list(ap_sb.shape)
        d = self.nc.dram_tensor("tap_" + name, shape, ap_sb.dtype, kind="ExternalOutput").ap()
        self.S.dma("sp", d, ap_sb, reads=reads, writes=["tapd_" + name])

    def build(self):
        cfg, nc = self.cfg, self.nc
        D, L, LC, E, FF, DEPTH = cfg.D, cfg.L, cfg.LC, cfg.E, cfg.FF, cfg.DEPTH
        KC, NT, NTC, NTT = cfg.KC, cfg.NT, cfg.NTC, cfg.NTT
        dr = {}

        def din(name, shape, dt=F32):
            dr[name] = nc.dram_tensor(name, list(shape), dt, kind="ExternalInput").ap()
            return dr[name]
        din("x", [L, D])
        din("ctx", [LC, D])
        din("cols", [128, cfg.NCOL])
        din("rowsL", [DEPTH, 128, cfg.NROWL])
        din("rowsG", [128, cfg.NROWG])
        din("brows", [DEPTH, 128, 6 * D])
        din("fnrow", [128, D])
        din("consts", [128, 7 * 128])
        din("ropeT", [128, 2 * L])
        din("esel", [E, E * 128])
        din("w_ada", [DEPTH, D, 6 * D])
        din("w_in", [DEPTH, D, cfg.INC])
        din("w_branch_a", [DEPTH, cfg.MW, D])
        din("w_branch_b", [DEPTH, cfg.MW, D])
        din("w_branch_c", [DEPTH, cfg.MW, D])
        din("w_out", [DEPTH, D, D])
        din("w_router", [DEPTH, D, E])
        din("w_exp_gate", [DEPTH, E, D, FF])
        din("w_exp_up", [DEPTH, E, D, FF])
        din("w_exp_down", [DEPTH, E, FF, D])
        self.dr = dr
        self.y = nc.dram_tensor("y", [L, D], F32, kind="ExternalOutput").ap()
        self.S = Sched(nc)
        S = self.S
        with ExitStack() as es:
            self.pb = [es.enter_context(nc.psum_tensor("pb%d" % i, [128, 512], F32)) for i in range(8)]
            self.x_sb = self.sb(es, "x_sb", [128, NT, D], F32)
            self.c_sb = self.sb(es, "c_sb", [128, NTC, D], F32)
            self.cols = self.sb(es, "cols_sb", [128, cfg.NCOL], F32)
            self.rowsG = self.sb(es, "rowsG_sb", [128, cfg.NROWG], F32)
            self.consts = self.sb(es, "consts_sb", [128, 7 * 128], F32)
            self.identb = self.sb(es, "identb", [128, 128], BF16)
            self.silc = self.sb(es, "silc", [128, KC, 2], F32)
            self.modc = self.sb(es, "modc", [128, 6 * KC, 2], F32)
            self.modA = self.sb(es, "modA", [128, 2, KC, 2], F32)
            self.grow = self.sb(es, "grow", [128, 2, D], F32)
            S.dma("sp", self.cols[:], dr["cols"], writes=["cols"])
            S.dma("sp", self.rowsG[:], dr["rowsG"], writes=["rowsG"])
            S.dma("sp", self.consts[:], dr["consts"], writes=["consts"])
            for i in range(NT):
                S.dma("sp", self.x_sb[:, i, :], dr["x"][i * 128:(i + 1) * 128, :], writes=["x%d" % i])
            for i in range(NTC):
                S.dma("sp", self.c_sb[:, i, :], dr["ctx"][i * 128:(i + 1) * 128, :], writes=["x%d" % (NT + i)])
            self.V(lambda e: e.tensor_copy(out=self.identb[:], in_=self.cst(CI)), r=["consts"], w=["identb"])
            self.A(lambda e: e.activation(out=self.silc[:, :, 0], in_=self.col("c"), func=AF.Silu), r=["cols"], w=["silc"])
            self.A(lambda e: e.activation(out=self.silc[:, :, 1], in_=self.col("cctx"), func=AF.Silu), r=["cols"], w=["silc"])
            for l in range(DEPTH):
                self.layer(l)
                if self.stop_after is not None and self.stop_after[0] == l:
                    break
            self.final()
            S.barrier()
        return nc

    def src_tile(self, i):
        return self.x_sb[:, i, :] if i < self.cfg.NT else self.c_sb[:, i - self.cfg.NT, :]

    def phase_mod(self, l, rowsec, do_cols):
        cfg, S = self.cfg, self.S
        D, KC = cfg.D, cfg.KC
        wa_d = self.dr["w_ada"][l].rearrange("(kc p) f -> p kc f", p=128)
        npiece = 6 * D // 512
        with ExitStack() as es:
            wa = [self.sb(es, "wa%d" % i, [128, KC, 512], F32) for i in range(2)]
            rep = self.sb(es, "rep", [128, KC, 2, 128], F32)
            brow = self.sb(es, "brow", [128, D], F32)
            S.dma("sp", brow[:], self.dr["brows"][l][:, rowsec * D:(rowsec + 1) * D], writes=["brow"])
            for kc in range(KC):
                for w_ in range(2):
                    self.V(lambda e: e.tensor_copy(out=rep[:, kc, w_, :], in_=self.silc[:, kc, w_:w_ + 1].to_broadcast([128, 128])),
                           r=["silc"], w=["rep"])
            jj = 0
            for j in range(npiece):
                sec = (j * 512) // D
                off = j * 512 - sec * D
                if not do_cols and sec != rowsec:
                    continue
                buf = wa[jj % 2]
                tk = "wa%d" % (jj % 2)
                pbk = self.pb[jj % 2]
                bkt = self.bk(jj % 2)
                jj += 1
                S.dma("sp", buf[:], wa_d[:, :, j * 512:(j + 1) * 512], writes=[tk])
                if do_cols:
                    for s_ in range(4):
                        for kc in range(KC):
                            self.P(lambda e: e.matmul(pbk[:, s_ * 2:s_ * 2 + 2], lhsT=buf[:, kc, s_ * 128:(s_ + 1) * 128],
                                                      rhs=self.silc[:, kc, :], start=(kc == 0), stop=(kc == KC - 1)),
                                   r=[tk, "silc"], x=[bkt])
                    o, _ = cfg.coff["bada%d" % l]
                    self.V(lambda e: e.tensor_tensor(
                        out=self.modc[:, j * 4:(j + 1) * 4, :],
                        in0=pbk[:, 0:8].rearrange("p (a b) -> p a b", b=2),
                        in1=self.cols[:, o + j * 4:o + (j + 1) * 4].unsqueeze(2).to_broadcast([128, 4, 2]),
                        op=ALU.add), r=["cols"], w=["modc"], x=[bkt])
                if sec == rowsec:
                    for w_ in range(2):
                        pb2 = self.pb[2 + w_]
                        for kc in range(KC):
                            self.P(lambda e: e.matmul(pb2[:, :], lhsT=rep[:, kc, w_, :], rhs=buf[:, kc, :],
                                                      start=(kc == 0), stop=(kc == KC - 1)),
                                   r=[tk, "rep"], x=[self.bk(2 + w_)])
                        self.V(lambda e: e.tensor_tensor(out=self.grow[:, w_, off:off + 512], in0=pb2[:, :],
                                                         in1=brow[:, off:off + 512], op=ALU.add),
                               r=["brow"], w=["grow"], x=[self.bk(2 + w_)])
            if do_cols:
                for ni, (nname, scsec) in enumerate((("n1%d" % l, 1), ("n2%d" % l, 4))):
                    self.V(lambda e: e.scalar_tensor_tensor(
                        out=self.modA[:, ni, :, :], in0=self.modc[:, scsec * KC:(scsec + 1) * KC, :], scalar=1.0,
                        in1=self.col(nname).unsqueeze(2).to_broadcast([128, KC, 2]), op0=ALU.add, op1=ALU.mult),
                        r=["modc", "cols"], w=["modA"])
            S.barrier()

    def phase_norm(self, es, l, ni, xs, hT, with_ctx=True):
        cfg, S = self.cfg, self.S
        D, KC, NT, NTT = cfg.D, cfg.KC, cfg.NT, cfg.NTT
        shsec = 0 if ni == 0 else 3
        ntl = NTT if with_ctx else NT
        with ExitStack() as es2:
            ss = self.sb(es2, "nss", [128, NTT], F32)
            rstd = self.sb(es2, "nrstd", [128, NTT], F32)
            junk = self.sb(es2, "njunk", [128, D], BF16)
            for i in range(ntl):
                self.A(lambda e: e.activation(out=junk[:], in_=self.src_tile(i), func=AF.Square,
                                              accum_out=ss[:, i:i + 1]), r=["x%d" % i], w=["njunk", "nss"])
            self.V(lambda e: e.tensor_scalar(out=rstd[:, :ntl], in0=ss[:, :ntl], scalar1=1.0 / D, scalar2=EPS,
                                             op0=ALU.mult, op1=ALU.add), r=["nss"], w=["nrstd"])
            self.A(lambda e: e.activation(out=rstd[:, :ntl], in_=rstd[:, :ntl], func=AF.Sqrt), r=[], w=["nrstd"])
            self.V(lambda e: e.reciprocal(out=rstd[:, :ntl], in_=rstd[:, :ntl]), r=[], w=["nrstd"])
            for i in range(ntl):
                self.V(lambda e: e.tensor_scalar(out=xs[:, i, :], in0=self.src_tile(i), scalar1=rstd[:, i:i + 1],
                                                 scalar2=None, op0=ALU.mult), r=["x%d" % i, "nrstd"], w=["xs%d" % i])
            groups = [(g, min(4, NT - g), 0) for g in range(0, NT, 4)]
            if with_ctx:
                groups += [(NT + g, min(4, cfg.NTC - g), 1) for g in range(0, cfg.NTC, 4)]
            n = 0
            for fc in range(KC):
                for (t0, nt_, w_) in groups:
                    b = n % 2
                    n += 1
                    pv = self.pb[b][:].bitcast(BF16)
                    for k in range(nt_):
                        self.P(lambda e: e.transpose(out=pv[:, k * 128:(k + 1) * 128],
                                                     in_=xs[:, t0 + k, fc * 128:(fc + 1) * 128], identity=self.identb[:]),
                               r=["xs%d" % (t0 + k), "identb"], x=[self.bk(b)])
                    dst = hT[:, fc, t0 * 128:(t0 + nt_) * 128]
                    sc = self.modA[:, ni, fc, w_:w_ + 1]
                    bi = self.modc[:, shsec * KC + fc, w_:w_ + 1]
                    if n % 2 == 0:
                        self.A(lambda e: e.activation(out=dst, in_=pv[:, :nt_ * 128], func=AF.Identity, scale=sc, bias=bi),
                               r=["modA", "modc"], w=["hT%d" % fc], x=[self.bk(b)])
                    else:
                        self.V(lambda e: e.tensor_scalar(out=dst, in0=pv[:, :nt_ * 128], scalar1=sc, scalar2=bi,
                                                         op0=ALU.mult, op1=ALU.add),
                               r=["modA", "modc"], w=["hT%d" % fc], x=[self.bk(b)])
            S.barrier()

    def layer(self, l):
        cfg = self.cfg
        with_ctx = l < cfg.DEPTH - 1
        self.phase_mod(l, 2, True)
        if self.stop_after == (l, "mod"):
            return
        with ExitStack() as es:
            hT = self.sb(es, "hT", [128, cfg.KC, cfg.NTOK], BF16)
            with ExitStack() as e0:
                xs = self.sb(e0, "xs", [128, cfg.NTT, cfg.D], BF16)
                self.phase_norm(e0, l, 0, xs, hT, True)
            self.tap("hT%d" % l, hT[:], ["hT%d" % fc for fc in range(cfg.KC)])
            if self.stop_after == (l, "norm1"):
                return
            self.phase_mix(l, hT, with_ctx)
        if self.stop_after is not None and self.stop_after[0] == l and self.stop_after[1] != "ffn":
            return
        self.phase_ffn(l, with_ctx)

    def wload(self, dst, win, slices, tok):
        a = 0
        for (c0, n) in slices:
            self.S.dma("pool", dst[:, :, a:a + n], win[:, :, c0:c0 + n], writes=[tok])
            a += n

    def proj_fm(self, W, wtok, hT, col0, ncols, banks, consume):
        KC = self.cfg.KC
        gi = 0
        for g0 in range(col0, col0 + ncols, 512):
            gn = min(512, col0 + ncols - g0)
            b = banks[gi % len(banks)]
            gi += 1
            for kc in range(KC):
                self.P(lambda e: e.matmul(self.pb[b][:, :gn], lhsT=W[:, kc, :], rhs=hT[:, kc, g0:g0 + gn],
                                          start=(kc == 0), stop=(kc == KC - 1)),
                       r=[wtok, "hT%d" % kc], x=[self.bk(b)])
            consume(self.pb[b][:, :gn], g0, gn, b)

    def proj_tm(self, W, wtok, hT, ti, ncols, b):
        KC = self.cfg.KC
        for kc in range(KC):
            self.P(lambda e: e.matmul(self.pb[b][:, :ncols], lhsT=hT[:, kc, ti * 128:(ti + 1) * 128], rhs=W[:, kc, :ncols],
                                      start=(kc == 0), stop=(kc == KC - 1)),
                   r=[wtok, "hT%d" % kc], x=[self.bk(b)])

    def qk_chunk(self, l, es, hT, win, nat, dst, dtok, nrm, nq_cols, pf):
        cfg = self.cfg
        L, KC = cfg.L, cfg.KC
        if "qkW" not in es:
            es["qkW"] = self.sb(es["es"], "qkW", [128, KC, 128], BF16)
            es["qkWp"] = self.sb(es["es"], "qkWp", [128, KC, 128], BF16)
            for nm in ("qk_t1", "qk_t2", "qk_sq", "qk_rs"):
                es[nm] = self.sb(es["es"], nm, [128, 512], F32)
        pf = ""
        W, Wp = es["qkW"], es["qkWp"]
        perm = []
        for (c0, n) in nat:
            for a in range(0, n, 64):
                perm += [(c0 + a + 32, 32), (c0 + a, 32)]
        self.wload(W, win, nat, pf + "qkW")
        self.wload(Wp, win, perm, pf + "qkWp")
        t1, t2, sq, rs = es["qk_t1"], es["qk_t2"], es["qk_sq"], es["qk_rs"]
        cosT, sinT = self.ropeT[:, 0:L], self.ropeT[:, L:2 * L]
        for g0 in range(0, nq_cols, 512):
            gn = min(512, nq_cols - g0)
            lat = g0 < L
            gi_ = g0 // 512
            bq, bp, bs_ = [0, 2, 4][gi_ % 3], [1, 3, 5][gi_ % 3], 6 + gi_ % 2
            for kc in range(KC):
                self.P(lambda e: e.matmul(self.pb[bq][:, :gn], lhsT=W[:, kc, :], rhs=hT[:, kc, g0:g0 + gn],
                                          start=(kc == 0), stop=(kc == KC - 1)), r=[pf + "qkW", "hT%d" % kc], x=[self.bk(bq)])
            if lat:
                for kc in range(KC):
                    self.P(lambda e: e.matmul(self.pb[bp][:, :gn], lhsT=Wp[:, kc, :], rhs=hT[:, kc, g0:g0 + gn],
                                              start=(kc == 0), stop=(kc == KC - 1)), r=[pf + "qkWp", "hT%d" % kc], x=[self.bk(bp)])
            pq, pp = self.pb[bq][:, :gn], self.pb[bp][:, :gn]
            if nrm is not None:
                self.A(lambda e: e.activation(out=sq[:, :gn], in_=pq, func=AF.Square), w=[pf + "qk_sq"], x=[self.bk(bq)])
                self.P(lambda e: e.matmul(self.pb[bs_][:, :gn], lhsT=self.cst(CBK), rhs=sq[:, :gn], start=True, stop=True),
                       r=["consts", pf + "qk_sq"], x=[self.bk(bs_)])
                self.V(lambda e: e.tensor_scalar(out=rs[:, :gn], in0=self.pb[bs_][:, :gn], scalar1=1.0 / 64, scalar2=EPS,
                                                 op0=ALU.mult, op1=ALU.add), w=[pf + "qk_rs"], x=[self.bk(bs_)])
                self.A(lambda e: e.activation(out=rs[:, :gn], in_=rs[:, :gn], func=AF.Sqrt), w=[pf + "qk_rs"])
                self.V(lambda e: e.reciprocal(out=rs[:, :gn], in_=rs[:, :gn]), w=[pf + "qk_rs"])
                wc = self.col(nrm + "%d" % l)
                if lat:
                    self.V(lambda e: e.scalar_tensor_tensor(out=t1[:, :gn], in0=pq, scalar=wc[:, 0:1], in1=cosT[:, g0:g0 + gn],
                                                            op0=ALU.mult, op1=ALU.mult), r=["cols", "ropeT"], w=[pf + "qk_t1"], x=[self.bk(bq)])
                    self.V(lambda e: e.scalar_tensor_tensor(out=t2[:, :gn], in0=pp, scalar=wc[:, 1:2], in1=sinT[:, g0:g0 + gn],
                                                            op0=ALU.mult, op1=ALU.mult), r=["cols", "ropeT"], w=[pf + "qk_t2"], x=[self.bk(bp)])
                    self.G(lambda e: e.tensor_tensor(out=t1[:, :gn], in0=t1[:, :gn], in1=t2[:, :gn], op=ALU.add),
                           r=[pf + "qk_t2"], w=[pf + "qk_t1"])
                    self.V(lambda e: e.tensor_tensor(out=dst[:, g0:g0 + gn], in0=t1[:, :gn], in1=rs[:, :gn], op=ALU.mult),
                           r=[pf + "qk_t1", pf + "qk_rs"], w=[dtok])
                else:
                    self.V(lambda e: e.scalar_tensor_tensor(out=dst[:, g0:g0 + gn], in0=pq, scalar=wc[:, 0:1], in1=rs[:, :gn],
                                                            op0=ALU.mult, op1=ALU.mult), r=["cols", pf + "qk_rs"], w=[dtok], x=[self.bk(bq)])
            else:
                if lat:
                    self.V(lambda e: e.tensor_tensor(out=t1[:, :gn], in0=pq, in1=cosT[:, g0:g0 + gn], op=ALU.mult),
                           r=["ropeT"], w=[pf + "qk_t1"], x=[self.bk(bq)])
                    self.V(lambda e: e.tensor_tensor(out=t2[:, :gn], in0=pp, in1=sinT[:, g0:g0 + gn], op=ALU.mult),
                           r=["ropeT"], w=[pf + "qk_t2"], x=[self.bk(bp)])
                    self.G(lambda e: e.tensor_tensor(out=dst[:, g0:g0 + gn], in0=t1[:, :gn], in1=t2[:, :gn], op=ALU.add),
                           r=[pf + "qk_t1", pf + "qk_t2"], w=[dtok])
                else:
                    self.A(lambda e: e.copy(out=dst[:, g0:g0 + gn], in_=pq), w=[dtok], x=[self.bk(bq)])

    def attention(self, PT, QT, KT, Vaug, vw, qcol0, nq, ktiles, finish, tokQ, tokK, tokV, sbanks, accsets, dist, state):
        spb = 512 // (vw + 1)
        for g0 in range(qcol0, qcol0 + nq, 512):
            gn = min(512, qcol0 + nq - g0)
            nqt = gn // 128
            aset = accsets[state["gi"] % len(accsets)]
            state["gi"] += 1

            def acc(j, qt, aset=aset, nqt=nqt):
                s_ = j * nqt + qt
                bnk = aset[s_ // spb]
                return self.pb[bnk][:, (s_ % spb) * (vw + 1):(s_ % spb + 1) * (vw + 1)], bnk
            nb = (2 * nqt + spb - 1) // spb
            for b in range(nb):
                self.V(lambda e: e.memset(self.pb[aset[b]][:], 0.0), w=[self.bk(aset[b])])
            steps = [(kt, j) for kt in ktiles for j in range(2)]
            n = len(steps)

            def issue_S(i):
                kt, j = steps[i]
                sbk = sbanks[i % len(sbanks)]
                pbuf = i % len(PT)
                self.P(lambda e: e.matmul(self.pb[sbk][:, :gn], lhsT=KT[:, j, kt * 128:(kt + 1) * 128],
                                          rhs=QT[:, g0:g0 + gn], start=True, stop=True),
                       r=[tokQ, tokK], x=[self.bk(sbk)])
                self.A(lambda e: e.activation(out=PT[pbuf][:, :gn], in_=self.pb[sbk][:, :gn], func=AF.Exp, scale=0.125),
                       w=["PT%d" % pbuf], x=[self.bk(sbk)])

            def issue_PV(i):
                kt, j = steps[i]
                pbuf = i % len(PT)
                for qt in range(nqt):
                    ap_, bnk = acc(j, qt)
                    self.P(lambda e: e.matmul(ap_, lhsT=PT[pbuf][:, qt * 128:(qt + 1) * 128], rhs=Vaug(kt),
                                              start=False, stop=False, skip_group_check=True),
                           r=["PT%d" % pbuf, tokV], x=[self.bk(bnk)])
            for i in range(min(dist, n)):
                issue_S(i)
            for i in range(n):
                if i + dist < n:
                    issue_S(i + dist)
                issue_PV(i)
            if state.get("pending") is not None:
                state["pending"]()

            def fin(g0=g0, nqt=nqt, acc=acc):
                for qt in range(nqt):
                    (a0, b0), (a1, b1) = acc(0, qt), acc(1, qt)
                    finish(g0 // 128 + qt, a0, a1, [self.bk(b0), self.bk(b1)])
            state["pending"] = fin

    def pad_k(self, es, KT):
        NTOK = self.cfg.NTOK
        KTz = self.sb(es, "KTz", [128, 2, NTOK], BF16)
        self.G(lambda e: e.memset(KTz[64:128, 0, :], 0.0), w=["KTz"])
        self.G(lambda e: e.memset(KTz[0:64, 1, :], 0.0), w=["KTz"])
        self.A(lambda e: e.copy(out=KTz[0:64, 0, :], in_=KT[0:64, :]), r=["KT"], w=["KTz"])
        self.G(lambda e: e.tensor_copy(out=KTz[64:128, 1, :], in_=KT[64:128, :]), r=["KT"], w=["KTz"])
        return KTz

    def attention_flush(self, state):
        if state.get("pending") is not None:
            state["pending"]()
            state["pending"] = None

    def out_transpose(self, tok_ap, ttok, oT, chunk, ti, bank=7):
        pv = self.pb[bank][:].bitcast(BF16)
        k = self._otn % 3
        self._otn += 1
        st = self.otst[k]
        self.P(lambda e: e.transpose(out=pv[:, 0:128], in_=tok_ap, identity=self.identb[:]), r=[ttok, "identb"], x=[self.bk(bank)])
        self.A(lambda e: e.copy(out=st[:], in_=pv[:, 0:128]), w=["otst%d" % k], x=[self.bk(bank)])
        self.S.dma("sp", self.oTd[chunk, :, ti * 128:(ti + 1) * 128], st[:], reads=["otst%d" % k], writes=["oTd"])

    def rstd_col(self, ss, n, rs_tok, r):
        self.V(lambda e: e.tensor_scalar(out=ss, in0=ss, scalar1=1.0 / n, scalar2=EPS, op0=ALU.mult, op1=ALU.add), r=r, w=[rs_tok])
        self.A(lambda e: e.activation(out=ss, in_=ss, func=AF.Sqrt), w=[rs_tok])
        self.V(lambda e: e.reciprocal(out=ss, in_=ss), w=[rs_tok])

    def phase_mix(self, l, hT, with_ctx):
        cfg, S = self.cfg, self.S
        D, L, LC, KC, NT, NTC, NTT, NTOK = cfg.D, cfg.L, cfg.LC, cfg.KC, cfg.NT, cfg.NTC, cfg.NTT, cfg.NTOK
        NB = cfg.HA
        win = self.dr["w_in"][l].rearrange("(kc p) c -> p kc c", p=128)
        lam_init = 0.8 - 0.6 * math.exp(-0.3 * l)
        nqc = NTOK if with_ctx else L
        allk = list(range(NTT))
        ctxk = list(range(NT, NTT))
        hTt = ["hT%d" % kc for kc in range(KC)]
        with ExitStack() as es:
            oT = None
            self.oTd = self.nc.dram_tensor("oTd%d" % l, [3 * NB, 128, NTOK], BF16).ap()
            self.otst = [self.sb(es, "otst%d" % i, [128, 128], BF16) for i in range(3)]
            self._otn = 0
            self.rowsL = self.sb(es, "rowsL", [128, cfg.NROWL], F32)
            S.dma("sp", self.rowsL[:], self.dr["rowsL"][l], writes=["rows"])
            lam = self.sb(es, "lam", [128, 4], F32)
            subw = self.sb(es, "subw", [128, 128], F32)
            ljunk = self.sb(es, "ljunk", [128, 64], F32)
            lr = self.row("lam")
            for i in range(2):
                self.V(lambda e: e.tensor_tensor(out=ljunk[:], in0=lr[:, 128 * i:128 * i + 64], in1=lr[:, 128 * i + 64:128 * i + 128],
                                                 op=ALU.mult), r=["rows"], w=["ljunk"])
                self.V(lambda e: e.reduce_sum(out=lam[:, i:i + 1], in_=ljunk[:], axis=AX.X), r=["ljunk"], w=["lam"])
            self.A(lambda e: e.activation(out=lam[:, 0:2], in_=lam[:, 0:2], func=AF.Exp), w=["lam"])
            self.V(lambda e: e.tensor_tensor(out=lam[:, 2:3], in0=lam[:, 1:2], in1=lam[:, 0:1], op=ALU.subtract), w=["lam"])
            self.V(lambda e: e.tensor_scalar(out=lam[:, 2:3], in0=lam[:, 2:3], scalar1=-lam_init, scalar2=None, op0=ALU.add), w=["lam"])
            self.V(lambda e: e.tensor_scalar(out=subw[:], in0=self.row("sub"), scalar1=1.0 - lam_init, scalar2=None,
                                             op0=ALU.mult), r=["rows"], w=["subw"])
            e_rope = ExitStack()
            self.ropeT = self.sb(e_rope, "ropeT", [128, 2 * L], F32)
            S.dma("sp", self.ropeT[:], self.dr["ropeT"], writes=["ropeT"])
            for h in range(cfg.HA):
                with ExitStack() as e2:
                    QT = self.sb(e2, "QT", [128, NTOK], BF16)
                    KT = self.sb(e2, "KT", [128, NTOK], BF16)
                    Va = self.sb(e2, "Va", [128, NTT, 129], BF16)
                    Wv = self.sb(e2, "Wv", [128, KC, 128], BF16)
                    qsc = {"es": e2}
                    self.qk_chunk(l, qsc, hT, win, [(cfg.oAq + h * 128, 128)], QT, "QT", None, nqc, "q")
                    self.qk_chunk(l, qsc, hT, win, [(cfg.oAk + h * 128, 128)], KT, "KT", None, NTOK, "k")
                    self.wload(Wv, win, [(cfg.oAv + h * 128, 128)], "Wv")
                    self.G(lambda e: e.memset(Va[:, :, 128:129], 1.0), w=["Va"])
                    for ti in range(NTT):
                        b = 5 + ti % 2
                        self.proj_tm(Wv, "Wv", hT, ti, 128, b)
                        self.A(lambda e: e.copy(out=Va[:, ti, 0:128], in_=self.pb[b][:, :128]), w=["Va"], x=[self.bk(b)])
                    fs = [self.sb(e2, "fin%d" % i, [128, 132], F32) for i in range(2)]
                    ft = [self.sb(e2, "fint%d" % i, [128, 128], F32) for i in range(2)]
                    fo = [self.sb(e2, "fino%d" % i, [128, 128], BF16) for i in range(2)]
                    fj = self.sb(e2, "finj", [128, 128], F32)
                    fcnt = [0]

                    def finishA(ti, a0, a1, btoks, h=h):
                        k = fcnt[0] % 2
                        fcnt[0] += 1
                        sm, t1, ob = fs[k], ft[k], fo[k]
                        stok, ttok, otok = "fin%d" % k, "fint%d" % k, "fino%d" % k
                        self.V(lambda e: e.reciprocal(out=sm[:, 128:129], in_=a0[:, 128:129]), w=[stok], x=btoks)
                        self.V(lambda e: e.reciprocal(out=sm[:, 129:130], in_=a1[:, 128:129]), w=[stok], x=btoks)
                        self.V(lambda e: e.tensor_tensor(out=sm[:, 129:130], in0=sm[:, 129:130], in1=lam[:, 2:3], op=ALU.mult),
                               r=["lam"], w=[stok])
                        self.V(lambda e: e.tensor_scalar(out=t1[:], in0=a1[:, 0:128], scalar1=sm[:, 129:130], scalar2=None,
                                                         op0=ALU.mult), r=[stok], w=[ttok], x=btoks)
                        self.V(lambda e: e.scalar_tensor_tensor(out=sm[:, 0:128], in0=a0[:, 0:128], scalar=sm[:, 128:129], in1=t1[:],
                                                                op0=ALU.mult, op1=ALU.add), r=[ttok], w=[stok], x=btoks)
                        self.A(lambda e: e.activation(out=fj[:], in_=sm[:, 0:128], func=AF.Square, accum_out=sm[:, 130:131]),
                               r=[stok], w=["finj", stok + "s"])
                        self.rstd_col(sm[:, 130:131], 128, stok + "s", [])
                        self.V(lambda e: e.scalar_tensor_tensor(out=ob[:], in0=sm[:, 0:128], scalar=sm[:, 130:131], in1=subw[:],
                                                                op0=ALU.mult, op1=ALU.mult), r=[stok, stok + "s", "subw"], w=[otok])
                        self.out_transpose(ob[:], otok, oT, h, ti, bank=0)
                    PT = [self.sb(e2, "PT%d" % i, [128, 512], BF16) for i in range(3)]
                    KTz = self.pad_k(e2, KT)
                    ast = {"gi": 0, "pending": None}
                    akw = dict(sbanks=[0, 1], accsets=[[2, 3, 4], [5, 6, 7]], dist=1, state=ast)
                    self.attention(PT, QT, KTz, lambda kt: Va[:, kt, :], 128, 0, L, allk, finishA, "QT", "KTz", "Va", **akw)
                    if with_ctx:
                        self.attention(PT, QT, KTz, lambda kt: Va[:, kt, :], 128, L, LC, ctxk, finishA, "QT", "KTz", "Va", **akw)
                    self.attention_flush(ast)
                    S.barrier()
            self.tap("oTa%d" % l, self.oTd[0:NB], ["oTd"])
            if self.stop_after == (l, "mixA"):
                e_rope.close()
                return
            for c2 in range(NB):
                hk = (2 * c2) // cfg.GRP
                with ExitStack() as e2:
                    QT = self.sb(e2, "QT", [128, NTOK], BF16)
                    KT = self.sb(e2, "KT", [128, NTOK], BF16)
                    Vb = self.sb(e2, "Vb", [128, NTT, 65], BF16)
                    Wv = self.sb(e2, "Wv", [128, KC, 64], BF16)
                    qsc = {"es": e2}
                    self.qk_chunk(l, qsc, hT, win, [(cfg.oBq + c2 * 128, 128)], QT, "QT", "qnc", nqc, "q")
                    self.qk_chunk(l, qsc, hT, win, [(cfg.oBk + hk * 64, 64), (cfg.oBk + hk * 64, 64)], KT, "KT", "knc", NTOK, "k")
                    self.wload(Wv, win, [(cfg.oBv + hk * 64, 64)], "Wv")
                    self.G(lambda e: e.memset(Vb[:, :, 64:65], 1.0), w=["Vb"])
                    for ti in range(NTT):
                        b = 5 + ti % 2
                        self.proj_tm(Wv, "Wv", hT, ti, 64, b)
                        self.A(lambda e: e.copy(out=Vb[:, ti, 0:64], in_=self.pb[b][:, :64]), w=["Vb"], x=[self.bk(b)])
                    fs = [self.sb(e2, "fin%d" % i, [128, 2], F32) for i in range(2)]
                    fo = [self.sb(e2, "fino%d" % i, [128, 128], BF16) for i in range(2)]
                    fcnt = [0]

                    def finishB(ti, a0, a1, btoks, c2=c2):
                        k = fcnt[0] % 2
                        fcnt[0] += 1
                        sm, ob = fs[k], fo[k]
                        stok, otok = "fin%d" % k, "fino%d" % k
                        self.V(lambda e: e.reciprocal(out=sm[:, 0:1], in_=a0[:, 64:65]), w=[stok], x=btoks)
                        self.V(lambda e: e.reciprocal(out=sm[:, 1:2], in_=a1[:, 64:65]), w=[stok], x=btoks)
                        self.V(lambda e: e.tensor_scalar(out=ob[:, 0:64], in0=a0[:, 0:64], scalar1=sm[:, 0:1], scalar2=None,
                                                         op0=ALU.mult), r=[stok], w=[otok], x=btoks)
                        self.V(lambda e: e.tensor_scalar(out=ob[:, 64:128], in0=a1[:, 0:64], scalar1=sm[:, 1:2], scalar2=None,
                                                         op0=ALU.mult), r=[stok], w=[otok], x=btoks)
                        self.out_transpose(ob[:], otok, oT, NB + c2, ti)
                    PT = [self.sb(e2, "PT%d" % i, [128, 512], BF16) for i in range(4)]
                    KTz = self.pad_k(e2, KT)
                    ast = {"gi": 0, "pending": None}
                    akw = dict(sbanks=[0, 1, 6], accsets=[[2, 3], [4, 5]], dist=2, state=ast)
                    self.attention(PT, QT, KTz, lambda kt: Vb[:, kt, :], 64, 0, L, allk, finishB, "QT", "KTz", "Vb", **akw)
                    if with_ctx:
                        self.attention(PT, QT, KTz, lambda kt: Vb[:, kt, :], 64, L, LC, ctxk, finishB, "QT", "KTz", "Vb", **akw)
                    self.attention_flush(ast)
                    S.barrier()
            self.tap("oTb%d" % l, self.oTd[NB:2 * NB], ["oTd"])
            S.barrier()
            e_rope.close()
            if self.stop_after == (l, "mixB"):
                return
            self.mix_mlstm(l, hT, win, oT, with_ctx)
            self.tap("oTc%d" % l, self.oTd[2 * NB:3 * NB], ["oTd"])
            if self.stop_after == (l, "mixC"):
                return
            self.mix_merge(l, hT, win, oT, with_ctx)

    def mix_mlstm(self, l, hT, win, oT, with_ctx):
        cfg, S = self.cfg, self.S
        D, L, LC, KC, NT, NTC, NTT, NTOK, HC = cfg.D, cfg.L, cfg.LC, cfg.KC, cfg.NT, cfg.NTC, cfg.NTT, cfg.NTOK, cfg.HC
        NB = HC
        one_col = self.cst(CON)[:, 0:1]
        with ExitStack() as es:
            Wg = self.sb(es, "Wg", [128, KC, 4 * HC], BF16)
            self.wload(Wg, win, [(cfg.oG, 4 * HC)], "Wg")
            Gt = self.sb(es, "Gt", [128, NTT, 4 * HC], F32)
            LF = self.sb(es, "LF", [128, NTT, 2, HC], F32)
            BC = self.sb(es, "BC", [128, NTT, 2, HC], F32)
            TOT = self.sb(es, "TOT", [128, NTT, 2, HC], F32)
            BIAS = self.sb(es, "BIAS", [128, NTT, 2, HC], F32)
            EB = self.sb(es, "EB", [128, NTT, 2, HC], F32)
            WC = self.sb(es, "WC", [128, NTT, 2, HC], F32)
            AC = self.sb(es, "AC", [128, NTT, 2, HC], F32)
            for ti in range(NTT):
                b = 5 + ti % 2
                self.proj_tm(Wg, "Wg", hT, ti, 4 * HC, b)
                self.V(lambda e: e.tensor_tensor(out=Gt[:, ti, :], in0=self.pb[b][:, :4 * HC], in1=self.row("gb"), op=ALU.add),
                       r=["rows"], w=["Gt"], x=[self.bk(b)])
            Gv = Gt[:].rearrange("p t (q h) -> p t q h", h=HC)
            for d in range(2):
                self.A(lambda e: e.activation(out=LF[:, :, d, :], in_=Gv[:, :, 2 * d + 1, :], func=AF.Exp, scale=-1.0), r=["Gt"], w=["LF"])
            self.A(lambda e: e.activation(out=LF[:], in_=LF[:], func=AF.Ln, bias=one_col), r=["consts"], w=["LF"])
            self.V(lambda e: e.tensor_scalar(out=LF[:], in0=LF[:], scalar1=-1.0, scalar2=None, op0=ALU.mult), w=["LF"])
            for ti in range(NTT):
                b = 5 + ti % 2
                self.P(lambda e: e.matmul(self.pb[b][:, 0:HC], lhsT=self.cst(CTU), rhs=LF[:, ti, 0, :], start=True, stop=True),
                       r=["consts", "LF"], x=[self.bk(b)])
                self.P(lambda e: e.matmul(self.pb[b][:, HC:2 * HC], lhsT=self.cst(CTL), rhs=LF[:, ti, 1, :], start=True, stop=True),
                       r=["consts", "LF"], x=[self.bk(b)])
                self.P(lambda e: e.matmul(self.pb[b][:, 2 * HC:4 * HC], lhsT=self.cst(CON), rhs=LF[:, ti, :, :].rearrange("p a b -> p (a b)"),
                                          start=True, stop=True), r=["consts", "LF"], x=[self.bk(b)])
                self.V(lambda e: e.tensor_copy(out=BC[:, ti, :, :].rearrange("p a b -> p (a b)"), in_=self.pb[b][:, 0:2 * HC]), w=["BC"], x=[self.bk(b)])
                self.V(lambda e: e.tensor_copy(out=TOT[:, ti, :, :].rearrange("p a b -> p (a b)"), in_=self.pb[b][:, 2 * HC:4 * HC]), w=["TOT"], x=[self.bk(b)])
            for d in range(2):
                self.V(lambda e: e.tensor_tensor(out=BIAS[:, :, d, :], in0=Gv[:, :, 2 * d, :], in1=BC[:, :, d, :], op=ALU.subtract),
                       r=["Gt", "BC"], w=["BIAS"])
            self.A(lambda e: e.activation(out=EB[:], in_=BC[:], func=AF.Exp), r=["BC"], w=["EB"])
            self.V(lambda e: e.tensor_tensor(out=WC[:], in0=TOT[:], in1=BIAS[:], op=ALU.add), r=["TOT", "BIAS"], w=["WC"])
            self.A(lambda e: e.activation(out=WC[:], in_=WC[:], func=AF.Exp), w=["WC"])
            self.A(lambda e: e.activation(out=AC[:], in_=TOT[:], func=AF.Exp), r=["TOT"], w=["AC"])
            S.barrier()
            for hc in range(HC):
                with ExitStack() as e2:
                    Ws = {}
                    for nm, o in (("q", cfg.oCq), ("k", cfg.oCk), ("v", cfg.oCv), ("o", cfg.oCo)):
                        Ws[nm] = self.sb(e2, "Wc" + nm, [128, KC, 128], BF16)
                        self.wload(Ws[nm], win, [(o + hc * 128, 128)], "Wc" + nm)
                    qT = self.sb(e2, "cqT", [128, NTOK], BF16)
                    kT = self.sb(e2, "ckT", [128, NTOK], BF16)
                    raw = self.sb(e2, "craw", [128, NTOK], F32)
                    acc = self.sb(e2, "cacc", [128, NTOK], F32)
                    cw = self.col("conv%d" % l)
                    for (nm, dst, dtok, scale, chunk) in (("q", qT, "cqT", 1.0, hc), ("k", kT, "ckT", 128.0 ** -0.5, HC + hc)):
                        def cons(ps_ap, g0, gn, b):
                            self.A(lambda e: e.copy(out=raw[:, g0:g0 + gn], in_=ps_ap), w=["craw"], x=[self.bk(b)])
                        self.proj_fm(Ws[nm], "Wc" + nm, hT, 0, NTOK, [5, 6], cons)
                        w0 = cw[:, 0 * 2 * HC + chunk:0 * 2 * HC + chunk + 1]
                        w1 = cw[:, 1 * 2 * HC + chunk:1 * 2 * HC + chunk + 1]
                        w2 = cw[:, 2 * 2 * HC + chunk:2 * 2 * HC + chunk + 1]
                        for (s0, n) in ((0, L), (L, LC)):
                            self.V(lambda e: e.tensor_scalar(out=acc[:, s0:s0 + n], in0=raw[:, s0:s0 + n], scalar1=w1, scalar2=None, op0=ALU.mult),
                                   r=["craw", "cols"], w=["cacc"])
                            self.V(lambda e: e.scalar_tensor_tensor(out=acc[:, s0 + 1:s0 + n], in0=raw[:, s0:s0 + n - 1], scalar=w0,
                                                                    in1=acc[:, s0 + 1:s0 + n], op0=ALU.mult, op1=ALU.add), r=["craw", "cols"], w=["cacc"])
                            self.V(lambda e: e.scalar_tensor_tensor(out=acc[:, s0:s0 + n - 1], in0=raw[:, s0 + 1:s0 + n], scalar=w2,
                                                                    in1=acc[:, s0:s0 + n - 1], op0=ALU.mult, op1=ALU.add), r=["craw", "cols"], w=["cacc"])
                        self.A(lambda e: e.activation(out=raw[:], in_=acc[:], func=AF.Sigmoid), r=["cacc"], w=["craw"])
                        self.V(lambda e: e.scalar_tensor_tensor(out=dst[:], in0=acc[:], scalar=scale, in1=raw[:], op0=ALU.mult, op1=ALU.mult),
                               r=["cacc", "craw"], w=[dtok])
                    Vc = self.sb(e2, "cVc", [128, NTT, 129], BF16)
                    OG = acc[:].rearrange("p (t d) -> p t d", d=128)
                    Kt = self.sb(e2, "cKt", [128, NTT, 128], BF16)
                    HS = self.sb(e2, "cHS", [128, NTT, 128], F32)
                    self.G(lambda e: e.memset(Vc[:, :, 128:129], 1.0), w=["cVc"])
                    self.G(lambda e: e.memset(HS[:], 0.0), w=["cHS"])
                    pv7 = self.pb[7][:].bitcast(BF16)
                    for ti in range(NTT):
                        self.proj_tm(Ws["v"], "Wcv", hT, ti, 128, 5)
                        self.A(lambda e: e.copy(out=Vc[:, ti, 0:128], in_=self.pb[5][:, :128]), w=["cVc"], x=[self.bk(5)])
                        self.P(lambda e: e.transpose(out=pv7[:, 0:128], in_=kT[:, ti * 128:(ti + 1) * 128], identity=self.identb[:]),
                               r=["ckT", "identb"], x=[self.bk(7)])
                        self.V(lambda e: e.tensor_copy(out=Kt[:, ti, :], in_=pv7[:, 0:128]), w=["cKt"], x=[self.bk(7)])
                    INTRA = [raw[:].rearrange("p (t d) -> p t d", d=128), acc[:].rearrange("p (t d) -> p t d", d=128)]
                    itok = ["craw", "cacc"]
                    DENI = self.sb(e2, "cDENI", [128, NTT, 2], F32)
                    SB = [self.sb(e2, "cSB%d" % d, [128, NTT, 129], BF16) for d in range(2)]
                    st = [self.sb(e2, "st%d" % d, [128, 129], F32) for d in range(2)]
                    rot = dict(Lt=[self.sb(e2, "Lt%d" % i, [128, 128], F32) for i in range(3)],
                               DT=[self.sb(e2, "DT%d" % i, [128, 128], F32) for i in range(3)],
                               SM=[self.sb(e2, "SM%d" % i, [128, 128], BF16) for i in range(3)],
                               VW=[self.sb(e2, "VW%d" % i, [128, 129], BF16) for i in range(3)],
                               nd=[self.sb(e2, "nd%d" % i, [128, 132], F32) for i in range(3)])
                    order = [list(range(NT, NTT)) + list(range(NT)), list(range(NTT - 1, NT - 1, -1)) + list(range(NT - 1, -1, -1))]
                    out_tiles = list(range(NTT)) if with_ctx else list(range(NT))
                    for kk in ("Lt", "DT", "SM", "VW"):
                        rot[kk].append(self.sb(e2, kk + "3", [128, 129 if kk == "VW" else 128], BF16 if kk in ("SM", "VW") else F32))
                    items = [(ti, d) for ti in out_tiles for d in range(2)]
                    nit = len(items)

                    def p1A(i):
                        ti, d = items[i]
                        cs = slice(ti * 128, (ti + 1) * 128)
                        bS = (i // 2) % 2
                        if d == 0:
                            self.P(lambda e: e.matmul(self.pb[bS][:, :128], lhsT=kT[:, cs], rhs=qT[:, cs], start=True, stop=True),
                                   r=["ckT", "cqT"], x=[self.bk(bS)])
                        k4 = i % 4
                        bL = 2 + i % 2
                        Lt = rot["Lt"][k4]
                        self.V(lambda e: e.tensor_scalar(out=Lt[:], in0=self.cst(CSL if d == 0 else CSU), scalar1=LF[:, ti, d, hc:hc + 1],
                                                         scalar2=None, op0=ALU.mult), r=["consts"], w=["Lt%d" % k4])
                        self.P(lambda e: e.matmul(self.pb[bL][:, :128], lhsT=Lt[:], rhs=self.cst(CTU if d == 0 else CTL),
                                                  start=True, stop=True), r=["Lt%d" % k4, "consts"], x=[self.bk(bL)])

                    def p1B(i):
                        ti, d = items[i]
                        bS = (i // 2) % 2
                        k4 = i % 4
                        bL = 2 + i % 2
                        DT, SM = rot["DT"][k4], rot["SM"][k4]
                        self.A(lambda e: e.activation(out=DT[:], in_=self.pb[bL][:, :128], func=AF.Exp,
                                                      bias=Gv[:, ti, 2 * d, hc:hc + 1]), w=["DT%d" % k4], x=[self.bk(bL)])
                        self.G(lambda e: e.tensor_tensor(out=DT[:], in0=DT[:], in1=self.cst(CTU if d == 0 else CTL), op=ALU.mult),
                               r=["consts"], w=["DT%d" % k4])
                        self.V(lambda e: e.tensor_tensor(out=SM[:], in0=DT[:], in1=self.pb[bS][:, :128], op=ALU.mult),
                               r=["DT%d" % k4], w=["SM%d" % k4], x=[self.bk(bS)])

                    def p1C(i):
                        ti, d = items[i]
                        k4 = i % 4
                        bI = 4 + i % 2
                        SM = rot["SM"][k4]
                        self.P(lambda e: e.matmul(self.pb[bI][:, :129], lhsT=SM[:], rhs=Vc[:, ti, :], start=True, stop=True),
                               r=["SM%d" % k4, "cVc"], x=[self.bk(bI)])
                        self.A(lambda e: e.copy(out=INTRA[d][:, ti, :], in_=self.pb[bI][:, :128]), w=[itok[d]], x=[self.bk(bI)])
                        self.V(lambda e: e.tensor_copy(out=DENI[:, ti, d:d + 1], in_=self.pb[bI][:, 128:129]), w=["cDENI"], x=[self.bk(bI)])
                    for i in range(nit + 2):
                        if i < nit:
                            p1A(i)
                        if 0 <= i - 1 < nit:
                            p1B(i - 1)
                        if 0 <= i - 2 < nit:
                            p1C(i - 2)
                    for d in range(2):
                        self.G(lambda e: e.memset(st[d][:], 0.0), w=["st%d" % d])
                        self.G(lambda e: e.memset(SB[d][:, 0, :], 0.0), w=["cSB%d" % d])
                    sitems = [(step, d) for step in range(NTT - 1) for d in range(2)]
                    nsi = len(sitems)
                    ubanks = [6, 7, 0, 1]

                    def p2A(i):
                        step, d = sitems[i]
                        ti = order[d][step]
                        k4 = i % 4
                        bU = ubanks[i % 4]
                        VW = rot["VW"][k4]
                        self.V(lambda e: e.tensor_scalar(out=VW[:], in0=Vc[:, ti, :], scalar1=WC[:, ti, d, hc:hc + 1], scalar2=None,
                                                         op0=ALU.mult), r=["cVc"], w=["VW%d" % k4])
                        self.P(lambda e: e.matmul(self.pb[bU][:, :129], lhsT=Kt[:, ti, :], rhs=VW[:], start=True, stop=True),
                               r=["cKt", "VW%d" % k4], x=[self.bk(bU)])

                    def p2B(i):
                        step, d = sitems[i]
                        ti = order[d][step]
                        bU = ubanks[i % 4]
                        self.V(lambda e: e.scalar_tensor_tensor(out=st[d][:], in0=st[d][:], scalar=AC[:, ti, d, hc:hc + 1],
                                                                in1=self.pb[bU][:, :129], op0=ALU.mult, op1=ALU.add),
                               w=["st%d" % d], x=[self.bk(bU)])
                        self.A(lambda e: e.copy(out=SB[d][:, step + 1, :], in_=st[d][:]), r=["st%d" % d], w=["cSB%d" % d])
                    for i in range(nsi + 2):
                        if i < nsi:
                            p2A(i)
                        if 0 <= i - 2 < nsi:
                            p2B(i - 2)
                    n3 = 0
                    for step in range(NTT):
                        for d in range(2):
                            ti = order[d][step]
                            if not (ti < NT or with_ctx):
                                continue
                            cs = slice(ti * 128, (ti + 1) * 128)
                            k3 = n3 % 3
                            bN = 2 + n3 % 4
                            n3 += 1
                            nd = rot["nd"][k3]
                            ntk = "nd%d" % k3
                            self.P(lambda e: e.matmul(self.pb[bN][:, :129], lhsT=qT[:, cs], rhs=SB[d][:, step, :], start=True, stop=True),
                                   r=["cqT", "cSB%d" % d], x=[self.bk(bN)])
                            self.V(lambda e: e.scalar_tensor_tensor(out=nd[:, 0:128], in0=self.pb[bN][:, 0:128], scalar=EB[:, ti, d, hc:hc + 1],
                                                                    in1=INTRA[d][:, ti, :], op0=ALU.mult, op1=ALU.add),
                                   r=[itok[d]], w=[ntk], x=[self.bk(bN)])
                            self.V(lambda e: e.scalar_tensor_tensor(out=nd[:, 128:129], in0=self.pb[bN][:, 128:129], scalar=EB[:, ti, d, hc:hc + 1],
                                                                    in1=DENI[:, ti, d:d + 1], op0=ALU.mult, op1=ALU.add),
                                   r=["cDENI"], w=[ntk], x=[self.bk(bN)])
                            self.V(lambda e: e.scalar_tensor_tensor(out=nd[:, 129:130], in0=nd[:, 128:129], scalar=-1.0, in1=nd[:, 128:129],
                                                                    op0=ALU.mult, op1=ALU.max), w=[ntk])
                            self.V(lambda e: e.tensor_scalar(out=nd[:, 129:130], in0=nd[:, 129:130], scalar1=1.0, scalar2=None, op0=ALU.max), w=[ntk])
                            self.V(lambda e: e.reciprocal(out=nd[:, 129:130], in_=nd[:, 129:130]), w=[ntk])
                            self.G(lambda e: e.scalar_tensor_tensor(out=HS[:, ti, :], in0=nd[:, 0:128], scalar=nd[:, 129:130],
                                                                    in1=HS[:, ti, :], op0=ALU.mult, op1=ALU.add), r=[ntk], w=["cHS"]) \
                                if False else self.V(lambda e: e.scalar_tensor_tensor(out=HS[:, ti, :], in0=nd[:, 0:128], scalar=nd[:, 129:130],
                                                                                      in1=HS[:, ti, :], op0=ALU.mult, op1=ALU.add), r=[ntk], w=["cHS"])
                    S.barrier()
                    for ti in (range(NTT) if with_ctx else range(NT)):
                        b = 5 + ti % 2
                        self.proj_tm(Ws["o"], "Wco", hT, ti, 128, b)
                        self.A(lambda e: e.activation(out=OG[:, ti, :], in_=self.pb[b][:, :128], func=AF.Sigmoid), w=["cacc"], x=[self.bk(b)])
                    fs = self.sb(e2, "cfs", [128, NTT], F32)
                    fj = self.sb(e2, "cfj", [128, 128], F32)
                    fo = [self.sb(e2, "cfo%d" % i, [128, 128], BF16) for i in range(2)]
                    tiles = list(range(NTT)) if with_ctx else list(range(NT))
                    for ti in tiles:
                        self.A(lambda e: e.activation(out=fj[:], in_=HS[:, ti, :], func=AF.Square, accum_out=fs[:, ti:ti + 1]), w=["cfj", "cfs"])
                    self.rstd_col(fs[:, :len(tiles)], 128, "cfs", [])
                    cn = self.row("cn", hc * 128, (hc + 1) * 128)
                    for n_, ti in enumerate(tiles):
                        k = n_ % 2
                        self.V(lambda e: e.scalar_tensor_tensor(out=HS[:, ti, :], in0=HS[:, ti, :], scalar=fs[:, ti:ti + 1], in1=cn,
                                                                op0=ALU.mult, op1=ALU.mult), r=["cfs", "rows"], w=["cHS"])
                        self.G(lambda e: e.tensor_tensor(out=fo[k][:], in0=HS[:, ti, :], in1=OG[:, ti, :], op=ALU.mult), r=["cHS", "cacc"], w=["cfo%d" % k])
                        self.out_transpose(fo[k][:], "cfo%d" % k, oT, 2 * NB + hc, ti)
                    S.barrier()

    def mix_merge(self, l, hT, win, oT, with_ctx):
        cfg, S = self.cfg, self.S
        D, L, KC, NT, NTT, NTOK, NB = cfg.D, cfg.L, cfg.KC, cfg.NT, cfg.NTT, cfg.NTOK, cfg.HA
        ntok = NTOK if with_ctx else L
        wbr = [self.dr[n][l].rearrange("(c p) d -> p c d", p=128) for n in ("w_branch_a", "w_branch_b", "w_branch_c")]
        S.barrier()
        with ExitStack() as es:
            mT = self.sb(es, "mT", [128, KC, 512], BF16)
            acc = self.sb(es, "macc", [128, 512], F32)
            sgs = [self.sb(es, "msg%d" % i, [128, 512], F32) for i in range(2)]
            Wg = [self.sb(es, "mWg%d" % i, [128, KC, 128], BF16) for i in range(6)]
            Wb = [self.sb(es, "mWb%d" % i, [128, NB, 128], BF16) for i in range(6)]
            oTg = [self.sb(es, "oTg%d" % i, [128, NB, 512], BF16) for i in range(3)]
            Wo = self.sb(es, "mWo", [128, KC, D], BF16)
            tmp = [self.sb(es, "mtmp%d" % i, [128, 512], F32) for i in range(2)]
            S.dma("pool", Wo[:], self.dr["w_out"][l].rearrange("(c p) d -> p c d", p=128), writes=["mWo"])
            n = 0
            gcnt = 0
            n2 = 0
            for g0 in range(0, ntok, 512):
                gn = min(512, ntok - g0)
                for i in range(3):
                    S.dma("sp", oTg[i][:, :, :gn], self.oTd[i * NB:(i + 1) * NB, :, g0:g0 + gn].rearrange("c p t -> p c t"),
                          reads=["oTd"], writes=["oTg%d" % i])
                for dc in range(KC):
                    for i in range(3):
                        k = n % 6
                        n += 1
                        self.wload(Wg[k], win, [(cfg.oMG + i * D + dc * 128, 128)], "mWg%d" % k)
                        S.dma("pool", Wb[k][:], wbr[i][:, :, dc * 128:(dc + 1) * 128], writes=["mWb%d" % k])
                        bg = 5 + gcnt % 2
                        bb = 0 + gcnt % 2
                        sg = sgs[gcnt % 2]
                        stok = "msg%d" % (gcnt % 2)
                        gcnt += 1
                        for kc in range(KC):
                            self.P(lambda e: e.matmul(self.pb[bg][:, :gn], lhsT=Wg[k][:, kc, :], rhs=hT[:, kc, g0:g0 + gn],
                                                      start=(kc == 0), stop=(kc == KC - 1)), r=["mWg%d" % k, "hT%d" % kc], x=[self.bk(bg)])
                        self.A(lambda e: e.activation(out=sg[:, :gn], in_=self.pb[bg][:, :gn], func=AF.Sigmoid), w=[stok], x=[self.bk(bg)])
                        for c in range(NB):
                            self.P(lambda e: e.matmul(self.pb[bb][:, :gn], lhsT=Wb[k][:, c, :], rhs=oTg[i][:, c, :gn],
                                                      start=(c == 0), stop=(c == NB - 1)), r=["mWb%d" % k, "oTg%d" % i], x=[self.bk(bb)])
                        if i == 0:
                            self.V(lambda e: e.tensor_tensor(out=acc[:, :gn], in0=sg[:, :gn], in1=self.pb[bb][:, :gn], op=ALU.mult),
                                   r=[stok], w=["macc"], x=[self.bk(bb)])
                        else:
                            self.V(lambda e: e.tensor_tensor(out=sg[:, :gn], in0=sg[:, :gn], in1=self.pb[bb][:, :gn], op=ALU.mult),
                                   w=[stok], x=[self.bk(bb)])
                            if i == 1:
                                self.G(lambda e: e.tensor_tensor(out=acc[:, :gn], in0=acc[:, :gn], in1=sg[:, :gn], op=ALU.add),
                                       r=[stok], w=["macc"])
                            else:
                                self.G(lambda e: e.tensor_tensor(out=mT[:, dc, :gn], in0=acc[:, :gn], in1=sg[:, :gn], op=ALU.add),
                                       r=[stok, "macc"], w=["mT"])
                for tt in range(gn // 128):
                    ti = g0 // 128 + tt
                    w_ = 0 if ti < NT else 1
                    for hf in range(D // 512):
                        k = n2 % 2
                        n2 += 1
                        b = 2 + k
                        for kc in range(KC):
                            self.P(lambda e: e.matmul(self.pb[b][:, :], lhsT=mT[:, kc, tt * 128:(tt + 1) * 128], rhs=Wo[:, kc, hf * 512:(hf + 1) * 512],
                                                      start=(kc == 0), stop=(kc == KC - 1)), r=["mT", "mWo"], x=[self.bk(b)])
                        self.V(lambda e: e.tensor_tensor(out=tmp[k][:], in0=self.pb[b][:, :], in1=self.grow[:, w_, hf * 512:(hf + 1) * 512], op=ALU.mult),
                               r=["grow"], w=["mtmp%d" % k], x=[self.bk(b)])
                        xt = self.src_tile(ti)
                        self.G(lambda e: e.tensor_tensor(out=xt[:, hf * 512:(hf + 1) * 512], in0=xt[:, hf * 512:(hf + 1) * 512], in1=tmp[k][:], op=ALU.add),
                               r=["mtmp%d" % k], w=["x%d" % ti])
            S.barrier()

    def phase_ffn(self, l, with_ctx):
        cfg, S = self.cfg, self.S
        D, L, LC, E, FF, FC, KC, NT, NTC, NTT, NTOK = cfg.D, cfg.L, cfg.LC, cfg.E, cfg.FF, cfg.FC, cfg.KC, cfg.NT, cfg.NTC, cfg.NTT, cfg.NTOK
        self.phase_mod(l, 5, False)
        sets = [dict(t0=0, nt=NT, cap=cfg.CAPL, w=0, s0=0)]
        if with_ctx:
            sets.append(dict(t0=NT, nt=NTC, cap=cfg.CAPC, w=1, s0=cfg.CAPL))
        NS = sum(st["cap"] for st in sets)
        ntl = NTT if with_ctx else NT
        stiles = []
        for si, st in enumerate(sets):
            assert st["s0"] % 128 == 0
            for a in range(0, st["cap"], 128):
                stiles.append((st["s0"] + a, min(128, st["cap"] - a), si))
        NST = len(stiles)
        NH = D // 512
        assert NST * NH <= 6
        identf = self.cst(CI)
        iof = self.row("iof")
        with ExitStack() as es:
            xs = self.sb(es, "xs2", [128, NTT, D], BF16)
            rankTok = self.sb(es, "rankTok", [128, NTT, E], F32)
            LG = self.sb(es, "LG", [128, NTT, E], F32)
            iopj = self.sb(es, "iopj", [128, NST], F32)
            e_row = ExitStack()
            gT = self.sb(e_row, "gT", [E, NTOK], F32)
            rankT = self.sb(e_row, "rankT", [E, NTOK], F32)
            for k, (s_start, nn, si) in enumerate(stiles):
                self.V(lambda e: e.tensor_scalar(out=iopj[:, k:k + 1], in0=self.col("iop"), scalar1=float(s_start), scalar2=None, op0=ALU.add),
                       r=["cols"], w=["iopj"])
            with ExitStack() as e1:
                hT2 = self.sb(e1, "hT2", [128, KC, NTOK], BF16)
                self.phase_norm(e1, l, 1, xs, hT2, with_ctx)
                Wr = self.sb(e1, "Wr", [128, KC, E], BF16)
                S.dma("pool", Wr[:], self.dr["w_router"][l].rearrange("(c p) e -> p c e", p=128), writes=["Wr"])
                mx = self.sb(e1, "lgmx", [128, NTT], F32)
                for ti in range(ntl):
                    b = 5 + ti % 2
                    self.proj_tm(Wr, "Wr", hT2, ti, E, b)
                    self.V(lambda e: e.tensor_copy(out=LG[:, ti, :], in_=self.pb[b][:, :E]), w=["LG"], x=[self.bk(b)])
                lg = LG[:, :ntl, :]
                self.V(lambda e: e.tensor_reduce(out=mx[:, :ntl], in_=lg, axis=AX.X, op=ALU.max), r=["LG"], w=["lgmx"])
                self.V(lambda e: e.tensor_tensor(out=lg, in0=lg, in1=mx[:, :ntl].unsqueeze(2).to_broadcast([128, ntl, E]), op=ALU.subtract),
                       r=["lgmx"], w=["LG"])
                self.A(lambda e: e.activation(out=lg, in_=lg, func=AF.Exp), w=["LG"])
                self.V(lambda e: e.reduce_sum(out=mx[:, :ntl], in_=lg, axis=AX.X), r=["LG"], w=["lgmx"])
                self.V(lambda e: e.reciprocal(out=mx[:, :ntl], in_=mx[:, :ntl]), w=["lgmx"])
                self.V(lambda e: e.tensor_tensor(out=lg, in0=lg, in1=mx[:, :ntl].unsqueeze(2).to_broadcast([128, ntl, E]), op=ALU.mult),
                       r=["lgmx"], w=["LG"])
                for t0 in range(0, ntl, 4):
                    nt_ = min(4, ntl - t0)
                    b = (t0 // 4) % 2
                    for k in range(nt_):
                        self.P(lambda e: e.transpose(out=self.pb[b][0:E, k * 128:(k + 1) * 128], in_=LG[:, t0 + k, :], identity=identf),
                               r=["LG", "consts"], x=[self.bk(b)])
                    self.A(lambda e: e.copy(out=gT[:, t0 * 128:(t0 + nt_) * 128], in_=self.pb[b][0:E, :nt_ * 128]), w=["gT"], x=[self.bk(b)])
                S.barrier()
            with ExitStack() as e1:
                nmax = max(st["nt"] for st in sets) * 128
                work = self.sb(e1, "tkw", [E, nmax], F32)
                MK = self.sb(e1, "tkm", [E, nmax], F32)
                CS = self.sb(e1, "tkc", [E, nmax], F32)
                ones = self.sb(e1, "tko", [E, nmax], F32)
                mx8 = self.sb(e1, "tk8", [E, 8], F32)
                self.G(lambda e: e.memset(ones[:], 1.0), w=["tko"])
                for st in sets:
                    c0, n, cap = st["t0"] * 128, st["nt"] * 128, st["cap"]
                    assert cap % 8 == 0
                    self.V(lambda e: e.tensor_copy(out=work[:, :n], in_=gT[:, c0:c0 + n]), r=["gT"], w=["tkw"])
                    for r_ in range(cap // 8):
                        self.V(lambda e: e.max(out=mx8[:], in_=work[:, :n]), r=["tkw"], w=["tk8"])
                        if r_ < cap // 8 - 1:
                            self.V(lambda e: e.match_replace(out=work[:, :n], in_to_replace=mx8[:], in_values=work[:, :n], imm_value=-1.0),
                                   r=["tk8"], w=["tkw"])
                    self.V(lambda e: e.tensor_scalar(out=MK[:, :n], in0=gT[:, c0:c0 + n], scalar1=mx8[:, 7:8], scalar2=None, op0=ALU.is_ge),
                           r=["gT", "tk8"], w=["tkm"])
                    self.V(lambda e: e.tensor_tensor_scan(out=CS[:, :n], data0=ones[:, :n], data1=MK[:, :n], initial=0.0, op0=ALU.mult, op1=ALU.add),
                           r=["tko", "tkm"], w=["tkc"])
                    self.V(lambda e: e.scalar_tensor_tensor(out=CS[:, :n], in0=CS[:, :n], scalar=float(st["s0"]), in1=MK[:, :n], op0=ALU.add, op1=ALU.mult),
                           r=["tkm"], w=["tkc"])
                    self.V(lambda e: e.tensor_scalar(out=rankT[:, c0:c0 + n], in0=CS[:, :n], scalar1=-1.0, scalar2=None, op0=ALU.add),
                           r=["tkc"], w=["rankT"])
                for ti in range(ntl):
                    b = ti % 2
                    self.P(lambda e: e.transpose(out=self.pb[b][:, 0:E], in_=rankT[0:E, ti * 128:(ti + 1) * 128], identity=identf[0:E, 0:E]),
                           r=["rankT", "consts"], x=[self.bk(b)])
                    self.V(lambda e: e.tensor_copy(out=rankTok[:, ti, :], in_=self.pb[b][:, 0:E]), w=["rankTok"], x=[self.bk(b)])
                S.barrier()
            self.tap("rankT%d" % l, rankT[:, :ntl * 128], [])
            self.tap("gT%d" % l, gT[:, :ntl * 128], [])
            S.barrier()
            e_row.close()
            with ExitStack() as e1:
                CAPM = max(st["cap"] for st in sets)
                Sel = self.sb(e1, "Sel", [128, NTT, CAPM], BF16)
                SelT = [self.sb(e1, "SelT%d" % i, [128, NST, 512], BF16) for i in range(2)]
                gsb = [self.sb(e1, "gsb%d" % i, [128, 512], F32) for i in range(2)]
                repR = [self.sb(e1, "repR%d" % i, [128, 128], F32) for i in range(2)]
                repG = [self.sb(e1, "repG%d" % i, [128, 128], F32) for i in range(2)]
                xeT = self.sb(e1, "xeT", [128, KC, NS], BF16)
                actT = self.sb(e1, "actT", [128, FC, NS], BF16)
                ye = self.sb(e1, "ye", [128, NST, D], BF16)
                sa = [self.sb(e1, "sa%d" % i, [128, NS], F32) for i in range(2)]
                PW = 256
                DP = 2
                NWB = 3
                Wg = [self.sb(e1, "eWg%d" % i, [128, KC, PW], BF16) for i in range(NWB)]
                Wu = [self.sb(e1, "eWu%d" % i, [128, KC, PW], BF16) for i in range(NWB)]
                Wd = [self.sb(e1, "eWd%d" % i, [128, DP, D], BF16) for i in range(NWB)]
                cn = dict(wcnt=0, dcnt=0, scnt=0, ocnt=0, ecnt=0, rcnt=0)

                def do_gather(ex):
                        for st in sets:
                            for k in range(st["nt"]):
                                ti = st["t0"] + k
                                cap = st["cap"]
                                if st["s0"] == 0:
                                    self.V(lambda e: e.tensor_scalar(out=Sel[:, ti, :cap], in0=iof[:, :cap], scalar1=rankTok[:, ti, ex:ex + 1],
                                                                     scalar2=None, op0=ALU.is_equal), r=["rankTok", "rowsG"], w=["Sel"])
                                else:
                                    self.V(lambda e: e.tensor_scalar(out=Sel[:, ti, :cap], in0=iof[:, :cap], scalar1=float(st["s0"]),
                                                                     scalar2=rankTok[:, ti, ex:ex + 1], op0=ALU.add, op1=ALU.is_equal),
                                           r=["rankTok", "rowsG"], w=["Sel"])
                        for fc in range(KC):
                            b = 6 + fc % 2
                            for st in sets:
                                s0, cap, w_ = st["s0"], st["cap"], st["w"]
                                for k in range(st["nt"]):
                                    ti = st["t0"] + k
                                    self.P(lambda e: e.matmul(self.pb[b][:, s0:s0 + cap], lhsT=xs[:, ti, fc * 128:(fc + 1) * 128], rhs=Sel[:, ti, :cap],
                                                              start=(k == 0), stop=(k == st["nt"] - 1)), r=["xs%d" % ti, "Sel"], x=[self.bk(b)])
                                sc_ = self.modA[:, 1, fc, w_:w_ + 1]
                                bi_ = self.modc[:, 3 * KC + fc, w_:w_ + 1]
                                cn["ecnt"] += 1
                                if cn["ecnt"] % 2 == 0:
                                    self.A(lambda e: e.activation(out=xeT[:, fc, s0:s0 + cap], in_=self.pb[b][:, s0:s0 + cap], func=AF.Identity,
                                                                  scale=sc_, bias=bi_), r=["modA", "modc"], w=["xeT"], x=[self.bk(b)])
                                else:
                                    self.V(lambda e: e.tensor_scalar(out=xeT[:, fc, s0:s0 + cap], in0=self.pb[b][:, s0:s0 + cap], scalar1=sc_, scalar2=bi_,
                                                                     op0=ALU.mult, op1=ALU.add), r=["modA", "modc"], w=["xeT"], x=[self.bk(b)])

                def do_gateup(ex):
                        wg_d = self.dr["w_exp_gate"][l, ex].rearrange("(kc p) f -> p kc f", p=128)
                        wu_d = self.dr["w_exp_up"][l, ex].rearrange("(kc p) f -> p kc f", p=128)
                        for pc in range(FF // PW):
                            kb = cn["wcnt"] % NWB
                            cn["wcnt"] += 1
                            S.dma("pool", Wg[kb][:], wg_d[:, :, pc * PW:(pc + 1) * PW], writes=["eWg%d" % kb])
                            S.dma("pool", Wu[kb][:], wu_d[:, :, pc * PW:(pc + 1) * PW], writes=["eWu%d" % kb])
                            for fo in range(PW // 128):
                                fidx = pc * (PW // 128) + fo
                                ba, bu = (0, 1) if fidx % 2 == 0 else (2, 3)
                                for kc in range(KC):
                                    self.P(lambda e: e.matmul(self.pb[ba][:, :NS], lhsT=Wg[kb][:, kc, fo * 128:(fo + 1) * 128], rhs=xeT[:, kc, :],
                                                              start=(kc == 0), stop=(kc == KC - 1)), r=["eWg%d" % kb, "xeT"], x=[self.bk(ba)])
                                for kc in range(KC):
                                    self.P(lambda e: e.matmul(self.pb[bu][:, :NS], lhsT=Wu[kb][:, kc, fo * 128:(fo + 1) * 128], rhs=xeT[:, kc, :],
                                                              start=(kc == 0), stop=(kc == KC - 1)), r=["eWu%d" % kb, "xeT"], x=[self.bk(bu)])
                                sa_ = sa[fidx % 2]
                                self.A(lambda e: e.activation(out=sa_[:], in_=self.pb[ba][:, :NS], func=AF.Silu), w=["sa%d" % (fidx % 2)], x=[self.bk(ba)])
                                self.V(lambda e: e.tensor_tensor(out=actT[:, fidx, :], in0=sa_[:], in1=self.pb[bu][:, :NS], op=ALU.mult),
                                       r=["sa%d" % (fidx % 2)], w=["actT"], x=[self.bk(bu)])

                def do_rest(ex):
                        wd_d = self.dr["w_exp_down"][l, ex].rearrange("(fc p) d -> p fc d", p=128)
                        for pc in range(FC // DP):
                            kb = cn["dcnt"] % NWB
                            cn["dcnt"] += 1
                            S.dma("pool", Wd[kb][:], wd_d[:, pc * DP:(pc + 1) * DP, :], writes=["eWd%d" % kb])
                            for f2 in range(DP):
                                fc = pc * DP + f2
                                for k_st, (s_start, nn, si) in enumerate(stiles):
                                    for hf in range(NH):
                                        b = k_st * NH + hf
                                        self.P(lambda e: e.matmul(self.pb[b][:nn, :512], lhsT=actT[:, fc, s_start:s_start + nn],
                                                                  rhs=Wd[kb][:, f2, hf * 512:(hf + 1) * 512], start=(fc == 0), stop=(fc == FC - 1)),
                                               r=["actT", "eWd%d" % kb], x=[self.bk(b)])
                        for k_st, (s_start, nn, si) in enumerate(stiles):
                            w_ = sets[si]["w"]
                            for hf in range(NH):
                                b = k_st * NH + hf
                                self.V(lambda e: e.tensor_tensor(out=ye[:nn, k_st, hf * 512:(hf + 1) * 512], in0=self.pb[b][:nn, :512],
                                                                 in1=self.grow[:nn, w_, hf * 512:(hf + 1) * 512], op=ALU.mult),
                                       r=["grow"], w=["ye"], x=[self.bk(b)])
                        for si, st in enumerate(sets):
                            c0, n = st["t0"] * 128, st["nt"] * 128
                            mine = [(k_st, s_start, nn) for k_st, (s_start, nn, sj) in enumerate(stiles) if sj == si]
                            for g0 in range(c0, c0 + n, 512):
                                gn = min(512, c0 + n - g0)
                                kb = cn["scnt"] % 2
                                cn["scnt"] += 1
                                for tt in range(gn // 128):
                                    ti = g0 // 128 + tt
                                    rk = cn["rcnt"] % 2
                                    cn["rcnt"] += 1
                                    self.G(lambda e: e.tensor_copy(out=repR[rk][:], in_=rankTok[:, ti, ex:ex + 1].to_broadcast([128, 128])),
                                           r=["rankTok"], w=["repR%d" % rk])
                                    self.G(lambda e: e.tensor_copy(out=repG[rk][:], in_=LG[:, ti, ex:ex + 1].to_broadcast([128, 128])),
                                           r=["LG"], w=["repG%d" % rk])
                                    self.P(lambda e: e.matmul(self.pb[6][:, tt * 128:(tt + 1) * 128], lhsT=repR[rk][:], rhs=identf, start=True, stop=True),
                                           r=["repR%d" % rk, "consts"], x=[self.bk(6)])
                                    self.P(lambda e: e.matmul(self.pb[7][:, tt * 128:(tt + 1) * 128], lhsT=repG[rk][:], rhs=identf, start=True, stop=True),
                                           r=["repG%d" % rk, "consts"], x=[self.bk(7)])
                                self.A(lambda e: e.copy(out=gsb[kb][:, :gn], in_=self.pb[7][:, :gn]), w=["gsb%d" % kb], x=[self.bk(7)])
                                for (k_st, s_start, nn) in mine:
                                    self.V(lambda e: e.scalar_tensor_tensor(out=SelT[kb][:, k_st, :gn], in0=self.pb[6][:, :gn], scalar=iopj[:, k_st:k_st + 1],
                                                                            in1=gsb[kb][:, :gn], op0=ALU.is_equal, op1=ALU.mult),
                                           r=["iopj", "gsb%d" % kb], w=["SelT%d" % kb], x=[self.bk(6)])
                                for tt in range(gn // 128):
                                    ti = g0 // 128 + tt
                                    xt = self.src_tile(ti)
                                    for hf in range(NH):
                                        b = cn["ocnt"] % 4
                                        cn["ocnt"] += 1
                                        for idx, (k_st, s_start, nn) in enumerate(mine):
                                            self.P(lambda e: e.matmul(self.pb[b][:, :512], lhsT=SelT[kb][:nn, k_st, tt * 128:(tt + 1) * 128],
                                                                      rhs=ye[:nn, k_st, hf * 512:(hf + 1) * 512], start=(idx == 0), stop=(idx == len(mine) - 1)),
                                                   r=["SelT%d" % kb, "ye"], x=[self.bk(b)])
                                        self.V(lambda e: e.tensor_tensor(out=xt[:, hf * 512:(hf + 1) * 512], in0=xt[:, hf * 512:(hf + 1) * 512],
                                                                         in1=self.pb[b][:, :512], op=ALU.add), w=["x%d" % ti], x=[self.bk(b)])

                do_gather(0)
                for ex in range(E):
                    do_gateup(ex)
                    if ex + 1 < E:
                        do_gather(ex + 1)
                    do_rest(ex)
                S.barrier()

    def final(self):
        cfg, S = self.cfg, self.S
        D, NT = cfg.D, cfg.NT
        with ExitStack() as es:
            ss = self.sb(es, "fss", [128, NT], F32)
            rstd = self.sb(es, "frstd", [128, NT], F32)
            junk = self.sb(es, "fjunk", [128, D], BF16)
            ot = [self.sb(es, "fo%d" % i, [128, D], F32) for i in range(2)]
            fnr = self.sb(es, "fnr", [128, D], F32)
            S.dma("sp", fnr[:], self.dr["fnrow"], writes=["fnr"])
            for i in range(NT):
                self.A(lambda e: e.activation(out=junk[:], in_=self.x_sb[:, i, :], func=AF.Square,
                                              accum_out=ss[:, i:i + 1]), r=["x%d" % i], w=["fjunk", "fss"])
            self.V(lambda e: e.tensor_scalar(out=rstd[:], in0=ss[:], scalar1=1.0 / D, scalar2=EPS,
                                             op0=ALU.mult, op1=ALU.add), r=["fss"], w=["frstd"])
            self.A(lambda e: e.activation(out=rstd[:], in_=rstd[:], func=AF.Sqrt), r=[], w=["frstd"])
            self.V(lambda e: e.reciprocal(out=rstd[:], in_=rstd[:]), r=[], w=["frstd"])
            for i in range(NT):
                o = ot[i % 2]
                self.V(lambda e: e.scalar_tensor_tensor(out=o[:], in0=self.x_sb[:, i, :], scalar=rstd[:, i:i + 1],
                                                        in1=fnr[:], op0=ALU.mult, op1=ALU.mult),
                       r=["x%d" % i, "frstd", "fnr"], w=["fo%d" % (i % 2)])
                S.dma("sp", self.y[i * 128:(i + 1) * 128, :], o[:], reads=["fo%d" % (i % 2)], writes=["y%d" % i])
            S.barrier()


def host_packs(cfg, inp, b):
    D, KC, DEPTH = cfg.D, cfg.KC, cfg.DEPTH
    cols = np.zeros((128, cfg.NCOL), np.float32)

    def colset(name, v):
        o, w = cfg.coff[name]
        cols[:, o:o + w] = np.asarray(v, np.float32).reshape(w, 128).T
    colset("c", inp["c"][b])
    colset("cctx", inp["c_ctx"])
    for l in range(DEPTH):
        colset("bada%d" % l, inp["b_ada"][l])
        colset("n1%d" % l, inp["norm1_w"][l])
        colset("n2%d" % l, inp["norm2_w"][l])
        colset("conv%d" % l, np.asarray(inp["mlstm_conv_w"][l]).reshape(-1))
        qn = np.asarray(inp["gqa_qnorm_w"][l], np.float32)
        kn = np.asarray(inp["gqa_knorm_w"][l], np.float32)
        perm = np.concatenate([np.arange(32, 64), np.arange(0, 32)])
        o, _ = cfg.coff["qnc%d" % l]
        cols[:, o] = np.tile(qn, 2)
        cols[:, o + 1] = np.tile(qn[perm], 2)
        o, _ = cfg.coff["knc%d" % l]
        cols[:, o] = np.tile(kn, 2)
        cols[:, o + 1] = np.tile(kn[perm], 2)
    cols[:, cfg.coff["iop"][0]] = np.arange(128)
    cols[:, cfg.coff["eps"][0]] = EPS
    rowsL = np.zeros((DEPTH, 128, cfg.NROWL), np.float32)
    rowsG = np.zeros((128, cfg.NROWG), np.float32)
    brows = np.zeros((DEPTH, 128, 6 * D), np.float32)

    def rowset(arr, name, v):
        o, w = cfg.roff[name]
        arr[:, o:o + w] = np.asarray(v, np.float32).reshape(1, w)
    for l in range(DEPTH):
        rowset(rowsL[l], "sub", inp["diff_subln_w"][l])
        rowset(rowsL[l], "cn", inp["mlstm_norm_w"][l])
        rowset(rowsL[l], "gb", inp["mlstm_gate_b"][l])
        rowset(rowsL[l], "lam", np.asarray(inp["diff_lambda"][l]).reshape(-1))
        brows[l] = np.asarray(inp["b_ada"][l], np.float32).reshape(1, 6 * D)
    rowset(rowsG, "iof", np.arange(256))
    rows = (rowsL, rowsG, brows)
    return cols, rows


def make_in_maps(cfg, inp, cores):
    cosT, sinT = rope_tables(cfg)
    ropeT = np.concatenate([cosT, sinT], axis=1)
    consts = const_pack()
    E = cfg.E
    esel = np.zeros((E, E * 128), np.float32)
    for e in range(E):
        esel[e, e * 128:(e + 1) * 128] = 1.0
    shared = {k: np.ascontiguousarray(np.asarray(inp[k], np.float32)) for k in
              ("w_ada", "w_in", "w_branch_a", "w_branch_b", "w_branch_c", "w_out", "w_router",
               "w_exp_gate", "w_exp_up", "w_exp_down")}
    maps = []
    for b in cores:
        cols, rows = host_packs(cfg, inp, b)
        m = {"x": np.ascontiguousarray(inp["x"][b], np.float32), "ctx": np.ascontiguousarray(inp["ctx"][b], np.float32),
             "cols": cols, "rowsL": rows[0], "rowsG": rows[1], "brows": rows[2], "fnrow": np.ascontiguousarray(np.broadcast_to(np.asarray(inp["final_norm_w"], np.float32).reshape(1, -1), (128, cfg.D))), "consts": consts, "ropeT": ropeT, "esel": esel}
        m.update(shared)
        maps.append(m)
    return maps


_CACHE = {}


def kernel(**inputs):
    cfg = Cfg()
    if "nc" not in _CACHE:
        _CACHE["nc"] = Builder(cfg).build()
    nc = _CACHE["nc"]
    inp = {k: np.asarray(v) for k, v in inputs.items()}
    n = inp["x"].shape[0]
    in_maps = make_in_maps(cfg, inp, list(range(n)))
    res = run_bass_kernel_spmd(nc, in_maps, core_ids=list(range(n)))
    return np.stack([np.asarray(r["y"], np.float32) for r in res.results], axis=0)
```

```python
import math
from contextlib import ExitStack

import numpy as np
import concourse.bass as bass
import concourse.mybir as mybir
from concourse.bass_utils import run_bass_kernel_spmd

F32 = mybir.dt.float32
BF16 = mybir.dt.bfloat16
AF = mybir.ActivationFunctionType
ALU = mybir.AluOpType
AX = mybir.AxisListType
EPS = 1e-6


class Sched:
    def __init__(self, nc, n_dma_sems=32, same_engine_sync=True):
        self.nc = nc
        self.eng = {"pe": nc.tensor, "dve": nc.vector, "act": nc.scalar, "pool": nc.gpsimd, "sp": nc.sync}
        self.sem = {k: nc.alloc_semaphore(name="s_" + k) for k in self.eng}
        self.cnt = {k: 0 for k in self.eng}
        self.waited = {k: {} for k in self.eng}
        self.dsem = [nc.alloc_semaphore(name="d%d" % i) for i in range(2 * n_dma_sems)]
        self.dcnt = [0] * (2 * n_dma_sems)
        self.nds = n_dma_sems
        self.drr = [0, 0]
        self.tok = {}
        self.same = same_engine_sync
        self.n_inst = 0
        self.n_wait = 0

    def _st(self, t):
        s = self.tok.get(t)
        if s is None:
            s = self.tok[t] = [None, []]
        return s

    def _wait(self, engname, deps):
        need = {}
        for ev, skip_same in deps:
            if ev is None:
                continue
            sem, val, src = ev
            if src == engname and (skip_same or not self.same or engname == "pe"):
                continue
            k = sem.num
            if k not in need or need[k][1] < val:
                need[k] = (sem, val)
        e = self.eng[engname]
        w = self.waited[engname]
        for k, (sem, val) in need.items():
            if w.get(k, 0) < val:
                e.wait_ge(sem, val)
                w[k] = val
                self.n_wait += 1

    def _deps(self, reads, writes, excl):
        deps = []
        for t in reads:
            deps.append((self._st(t)[0], False))
        for t in writes:
            s = self._st(t)
            deps.append((s[0], False))
            deps.extend((r, False) for r in s[1])
        for t in excl:
            s = self._st(t)
            deps.append((s[0], True))
        return deps

    def _commit(self, ev, reads, writes, excl):
        for t in reads:
            self._st(t)[1].append(ev)
        for t in writes:
            s = self._st(t)
            s[0] = ev
            s[1] = []
        for t in excl:
            s = self._st(t)
            s[0] = ev
            s[1] = []

    def op(self, engname, fn, reads=(), writes=(), excl=()):
        self._wait(engname, self._deps(reads, writes, excl))
        inst = fn(self.eng[engname])
        self.cnt[engname] += 1
        ev = (self.sem[engname], self.cnt[engname], engname)
        inst.then_inc(ev[0], 1)
        self._commit(ev, reads, writes, excl)
        self.n_inst += 1
        return ev

    def dma(self, queue, out, in_, reads=(), writes=(), **kw):
        self._wait(queue, self._deps(reads, writes, ()))
        q = 1 if queue == "pool" else 0
        i = q * self.nds + self.drr[q]
        self.drr[q] = (self.drr[q] + 1) % self.nds
        inst = self.eng[queue].dma_start(out=out, in_=in_, **kw)
        self.dcnt[i] += 16
        ev = (self.dsem[i], self.dcnt[i], "dma")
        inst.then_inc(ev[0], 16)
        self._commit(ev, reads, writes, ())
        self.n_inst += 1
        return ev

    def wait_all(self, engname):
        deps = [((self.sem[k], self.cnt[k], k), False) for k in self.eng if self.cnt[k] > 0 and k != engname]
        deps += [((self.dsem[i], self.dcnt[i], "dma"), False) for i in range(len(self.dsem)) if self.dcnt[i] > 0]
        self._wait(engname, deps)

    def barrier(self):
        for k in ("pe", "dve", "act", "pool", "sp"):
            self.wait_all(k)
        self.tok = {}


class Cfg:
    def __init__(s, D=1024, L=2048, LC=256, E=16, FF=2048, DEPTH=2, GW=64):
        s.D, s.L, s.LC, s.E, s.FF, s.DEPTH, s.GW = D, L, LC, E, FF, DEPTH, GW
        s.KC = D // 128
        s.NT = L // 128
        s.NTC = LC // 128
        s.NTT = s.NT + s.NTC
        s.NTOK = s.NTT * 128
        MW = s.MW = D // 2
        s.HA = MW // 128
        s.HBQ = MW // 64
        s.GRP = s.HBQ // 2
        s.HC = MW // 128
        s.FC = FF // 128
        s.CAPL = 2 * L // E
        s.CAPC = 2 * LC // E
        s.oAq, s.oAk, s.oAv, s.oBq = 0, MW, 2 * MW, 3 * MW
        s.oBk, s.oBv = 4 * MW, 4 * MW + 128
        s.oCq, s.oCk, s.oCv, s.oCo = 4 * MW + 256, 5 * MW + 256, 6 * MW + 256, 7 * MW + 256
        s.oG = 8 * MW + 256
        s.oMG = s.oG + 4 * s.HC
        s.INC = s.oMG + 3 * D
        off = {}
        n = 0

        def add(name, w):
            nonlocal n
            off[name] = (n, w)
            n += w
        add("c", s.KC)
        add("cctx", s.KC)
        for l in range(DEPTH):
            add("bada%d" % l, 6 * s.KC)
            add("n1%d" % l, s.KC)
            add("n2%d" % l, s.KC)
            add("conv%d" % l, 3 * 2 * s.HC)
            add("qnc%d" % l, 2)
            add("knc%d" % l, 2)
        add("cosT", 0)
        add("iop", 1)
        add("eps", 1)
        s.coff, s.NCOL = off, n
        roff = {}
        n = 0

        def addr(name, w):
            nonlocal n
            roff[name] = (n, w)
            n += w
        addr("sub", 128)
        addr("cn", MW)
        addr("gb", 4 * s.HC)
        addr("lam", 256)
        s.NROWL = n
        n = 0
        addr("iof", 256)
        s.roff, s.NROWG = roff, n


def rope_tables(cfg):
    L, GW = cfg.L, cfg.GW
    t = np.arange(L)
    rows = (t // GW).astype(np.float32)
    cols = (t % GW).astype(np.float32)
    nf = 16
    inv = (10000.0 ** (-np.arange(nf, dtype=np.float32) / nf)).astype(np.float32)
    ang = np.concatenate([rows[:, None] * inv, cols[:, None] * inv], axis=-1).astype(np.float32)
    cos = np.cos(ang).astype(np.float32)
    sin = np.sin(ang).astype(np.float32)
    cosT = np.zeros((128, L), np.float32)
    sinT = np.zeros((128, L), np.float32)
    for p in range(128):
        d = p % 64
        f = d % 32
        cosT[p] = cos[:, f]
        sinT[p] = -sin[:, f] if d < 32 else sin[:, f]
    return cosT, sinT


def const_pack():
    r = np.arange(128)
    ident = np.eye(128, dtype=np.float32)
    triU = (r[:, None] <= r[None, :]).astype(np.float32)
    triL = (r[:, None] >= r[None, :]).astype(np.float32)
    sU = (r[:, None] < r[None, :]).astype(np.float32)
    sL = (r[:, None] > r[None, :]).astype(np.float32)
    ones = np.ones((128, 128), np.float32)
    blk = (r[:, None] // 64 == r[None, :] // 64).astype(np.float32)
    return np.concatenate([ident, triU, triL, sU, sL, ones, blk], axis=1)


CI, CTU, CTL, CSU, CSL, CON, CBK = range(7)


class Builder:
    def __init__(self, cfg, taps=None, stop_after=None):
        self.cfg = cfg
        self.taps = taps or {}
        self.stop_after = stop_after
        self.nc = bass.Bass("TRN2", target_bir_lowering=False)
        self.S = None

    def sb(self, es, name, shape, dt):
        self._uid = getattr(self, "_uid", 0) + 1
        return es.enter_context(self.nc.sbuf_tensor("%s_%d" % (name, self._uid), list(shape), dt))

    def V(self, fn, r=(), w=(), x=()):
        return self.S.op("dve", fn, r, w, x)

    def A(self, fn, r=(), w=(), x=()):
        return self.S.op("act", fn, r, w, x)

    def G(self, fn, r=(), w=(), x=()):
        return self.S.op("pool", fn, r, w, x)

    def P(self, fn, r=(), w=(), x=()):
        return self.S.op("pe", fn, r, w, x)

    def bk(self, i):
        return "pb%d" % i

    def cst(self, k):
        return self.consts[:, k * 128:(k + 1) * 128]

    def col(self, name, a=0, b=None):
        o, w = self.cfg.coff[name]
        if b is None:
            b = w
        return self.cols[:, o + a:o + b]

    def row(self, name, a=0, b=None):
        o, w = self.cfg.roff[name]
        if b is None:
            b = w
        t = self.rowsG if name in ("iof",) else self.rowsL
        return t[:, o + a:o + b]

    def tap(self, name, ap_sb, reads):
        if name not in self.taps:
            return
        shape = list(ap_sb.shape)
        d = self.nc.dram_tensor("tap_" + name, shape, ap_sb.dtype, kind="ExternalOutput").ap()
        self.S.dma("sp", d, ap_sb, reads=reads, writes=["tapd_" + name])

    def build(self):
        cfg, nc = self.cfg, self.nc
        D, L, LC, E, FF, DEPTH = cfg.D, cfg.L, cfg.LC, cfg.E, cfg.FF, cfg.DEPTH
        KC, NT, NTC, NTT = cfg.KC, cfg.NT, cfg.NTC, cfg.NTT
        dr = {}

        def din(name, shape, dt=F32):
            dr[name] = nc.dram_tensor(name, list(shape), dt, kind="ExternalInput").ap()
            return dr[name]
        din("x", [L, D])
        din("ctx", [LC, D])
        din("cols", [128, cfg.NCOL])
        din("rowsL", [DEPTH, 128, cfg.NROWL])
        din("rowsG", [128, cfg.NROWG])
        din("brows", [DEPTH, 128, 6 * D])
        din("fnrow", [128, D])
        din("consts", [128, 7 * 128])
        din("ropeT", [128, 2 * L])
        din("esel", [E, E * 128])
        din("w_ada", [DEPTH, D, 6 * D])
        din("w_in", [DEPTH, D, cfg.INC])
        din("w_branch_a", [DEPTH, cfg.MW, D])
        din("w_branch_b", [DEPTH, cfg.MW, D])
        din("w_branch_c", [DEPTH, cfg.MW, D])
        din("w_out", [DEPTH, D, D])
        din("w_router", [DEPTH, D, E])
        din("w_exp_gate", [DEPTH, E, D, FF])
        din("w_exp_up", [DEPTH, E, D, FF])
        din("w_exp_down", [DEPTH, E, FF, D])
        self.dr = dr
        self.y = nc.dram_tensor("y", [L, D], F32, kind="ExternalOutput").ap()
        self.S = Sched(nc)
        S = self.S
        with ExitStack() as es:
            self.pb = [es.enter_context(nc.psum_tensor("pb%d" % i, [128, 512], F32)) for i in range(8)]
            self.x_sb = self.sb(es, "x_sb", [128, NT, D], F32)
            self.c_sb = self.sb(es, "c_sb", [128, NTC, D], F32)
            self.cols = self.sb(es, "cols_sb", [128, cfg.NCOL], F32)
            self.rowsG = self.sb(es, "rowsG_sb", [128, cfg.NROWG], F32)
            self.consts = self.sb(es, "consts_sb", [128, 7 * 128], F32)
            self.identb = self.sb(es, "identb", [128, 128], BF16)
            self.silc = self.sb(es, "silc", [128, KC, 2], F32)
            self.modc = self.sb(es, "modc", [128, 6 * KC, 2], F32)
            self.modA = self.sb(es, "modA", [128, 2, KC, 2], F32)
            self.grow = self.sb(es, "grow", [128, 2, D], F32)
            S.dma("sp", self.cols[:], dr["cols"], writes=["cols"])
            S.dma("sp", self.rowsG[:], dr["rowsG"], writes=["rowsG"])
            S.dma("sp", self.consts[:], dr["consts"], writes=["consts"])
            for i in range(NT):
                S.dma("sp", self.x_sb[:, i, :], dr["x"][i * 128:(i + 1) * 128, :], writes=["x%d" % i])
            for i in range(NTC):
                S.dma("sp", self.c_sb[:, i, :], dr["ctx"][i * 128:(i + 1) * 128, :], writes=["x%d" % (NT + i)])
            self.V(lambda e: e.tensor_copy(out=self.identb[:], in_=self.cst(CI)), r=["consts"], w=["identb"])
            self.A(lambda e: e.activation(out=self.silc[:, :, 0], in_=self.col("c"), func=AF.Silu), r=["cols"], w=["silc"])
            self.A(lambda e: e.activation(out=self.silc[:, :, 1], in_=self.col("cctx"), func=AF.Silu), r=["cols"], w=["silc"])
            for l in range(DEPTH):
                self.layer(l)
                if self.stop_after is not None and self.stop_after[0] == l:
                    break
            self.final()
            S.barrier()
        return nc

    def src_tile(self, i):
        return self.x_sb[:, i, :] if i < self.cfg.NT else self.c_sb[:, i - self.cfg.NT, :]

    def phase_mod(self, l, rowsec, do_cols):
        cfg, S = self.cfg, self.S
        D, KC = cfg.D, cfg.KC
        wa_d = self.dr["w_ada"][l].rearrange("(kc p) f -> p kc f", p=128)
        npiece = 6 * D // 512
        with ExitStack() as es:
            wa = [self.sb(es, "wa%d" % i, [128, KC, 512], F32) for i in range(2)]
            rep = self.sb(es, "rep", [128, KC, 2, 128], F32)
            brow = self.sb(es, "brow", [128, D], F32)
            S.dma("sp", brow[:], self.dr["brows"][l][:, rowsec * D:(rowsec + 1) * D], writes=["brow"])
            for kc in range(KC):
                for w_ in range(2):
                    self.V(lambda e: e.tensor_copy(out=rep[:, kc, w_, :], in_=self.silc[:, kc, w_:w_ + 1].to_broadcast([128, 128])),
                           r=["silc"], w=["rep"])
            jj = 0
            for j in range(npiece):
                sec = (j * 512) // D
                off = j * 512 - sec * D
                if not do_cols and sec != rowsec:
                    continue
                buf = wa[jj % 2]
                tk = "wa%d" % (jj % 2)
                pbk = self.pb[jj % 2]
                bkt = self.bk(jj % 2)
                jj += 1
                S.dma("sp", buf[:], wa_d[:, :, j * 512:(j + 1) * 512], writes=[tk])
                if do_cols:
                    for s_ in range(4):
                        for kc in range(KC):
                            self.P(lambda e: e.matmul(pbk[:, s_ * 2:s_ * 2 + 2], lhsT=buf[:, kc, s_ * 128:(s_ + 1) * 128],
                                                      rhs=self.silc[:, kc, :], start=(kc == 0), stop=(kc == KC - 1)),
                                   r=[tk, "silc"], x=[bkt])
                    o, _ = cfg.coff["bada%d" % l]
                    self.V(lambda e: e.tensor_tensor(
                        out=self.modc[:, j * 4:(j + 1) * 4, :],
                        in0=pbk[:, 0:8].rearrange("p (a b) -> p a b", b=2),
                        in1=self.cols[:, o + j * 4:o + (j + 1) * 4].unsqueeze(2).to_broadcast([128, 4, 2]),
                        op=ALU.add), r=["cols"], w=["modc"], x=[bkt])
                if sec == rowsec:
                    for w_ in range(2):
                        pb2 = self.pb[2 + w_]
                        for kc in range(KC):
                            self.P(lambda e: e.matmul(pb2[:, :], lhsT=rep[:, kc, w_, :], rhs=buf[:, kc, :],
                                                      start=(kc == 0), stop=(kc == KC - 1)),
                                   r=[tk, "rep"], x=[self.bk(2 + w_)])
                        self.V(lambda e: e.tensor_tensor(out=self.grow[:, w_, off:off + 512], in0=pb2[:, :],
                                                         in1=brow[:, off:off + 512], op=ALU.add),
                               r=["brow"], w=["grow"], x=[self.bk(2 + w_)])
            if do_cols:
                for ni, (nname, scsec) in enumerate((("n1%d" % l, 1), ("n2%d" % l, 4))):
                    self.V(lambda e: e.scalar_tensor_tensor(
                        out=self.modA[:, ni, :, :], in0=self.modc[:, scsec * KC:(scsec + 1) * KC, :], scalar=1.0,
                        in1=self.col(nname).unsqueeze(2).to_broadcast([128, KC, 2]), op0=ALU.add, op1=ALU.mult),
                        r=["modc", "cols"], w=["modA"])
            S.barrier()

    def phase_norm(self, es, l, ni, xs, hT, with_ctx=True):
        cfg, S = self.cfg, self.S
        D, KC, NT, NTT = cfg.D, cfg.KC, cfg.NT, cfg.NTT
        shsec = 0 if ni == 0 else 3
        ntl = NTT if with_ctx else NT
        with ExitStack() as es2:
            ss = self.sb(es2, "nss", [128, NTT], F32)
            rstd = self.sb(es2, "nrstd", [128, NTT], F32)
            junk = self.sb(es2, "njunk", [128, D], BF16)
            for i in range(ntl):
                self.A(lambda e: e.activation(out=junk[:], in_=self.src_tile(i), func=AF.Square,
                                              accum_out=ss[:, i:i + 1]), r=["x%d" % i], w=["njunk", "nss"])
            self.V(lambda e: e.tensor_scalar(out=rstd[:, :ntl], in0=ss[:, :ntl], scalar1=1.0 / D, scalar2=EPS,
                                             op0=ALU.mult, op1=ALU.add), r=["nss"], w=["nrstd"])
            self.A(lambda e: e.activation(out=rstd[:, :ntl], in_=rstd[:, :ntl], func=AF.Sqrt), r=[], w=["nrstd"])
            self.V(lambda e: e.reciprocal(out=rstd[:, :ntl], in_=rstd[:, :ntl]), r=[], w=["nrstd"])
            for i in range(ntl):
                self.V(lambda e: e.tensor_scalar(out=xs[:, i, :], in0=self.src_tile(i), scalar1=rstd[:, i:i + 1],
                                                 scalar2=None, op0=ALU.mult), r=["x%d" % i, "nrstd"], w=["xs%d" % i])
            groups = [(g, min(4, NT - g), 0) for g in range(0, NT, 4)]
            if with_ctx:
                groups += [(NT + g, min(4, cfg.NTC - g), 1) for g in range(0, cfg.NTC, 4)]
            n = 0
            for fc in range(KC):
                for (t0, nt_, w_) in groups:
                    b = n % 2
                    n += 1
                    pv = self.pb[b][:].bitcast(BF16)
                    for k in range(nt_):
                        self.P(lambda e: e.transpose(out=pv[:, k * 128:(k + 1) * 128],
                                                     in_=xs[:, t0 + k, fc * 128:(fc + 1) * 128], identity=self.identb[:]),
                               r=["xs%d" % (t0 + k), "identb"], x=[self.bk(b)])
                    dst = hT[:, fc, t0 * 128:(t0 + nt_) * 128]
                    sc = self.modA[:, ni, fc, w_:w_ + 1]
                    bi = self.modc[:, shsec * KC + fc, w_:w_ + 1]
                    if n % 2 == 0:
                        self.A(lambda e: e.activation(out=dst, in_=pv[:, :nt_ * 128], func=AF.Identity, scale=sc, bias=bi),
                               r=["modA", "modc"], w=["hT%d" % fc], x=[self.bk(b)])
                    else:
                        self.V(lambda e: e.tensor_scalar(out=dst, in0=pv[:, :nt_ * 128], scalar1=sc, scalar2=bi,
                                                         op0=ALU.mult, op1=ALU.add),
                               r=["modA", "modc"], w=["hT%d" % fc], x=[self.bk(b)])
            S.barrier()

    def layer(self, l):
        cfg = self.cfg
        with_ctx = l < cfg.DEPTH - 1
        self.phase_mod(l, 2, True)
        if self.stop_after == (l, "mod"):
            return
        with ExitStack() as es:
            hT = self.sb(es, "hT", [128, cfg.KC, cfg.NTOK], BF16)
            with ExitStack() as e0:
                xs = self.sb(e0, "xs", [128, cfg.NTT, cfg.D], BF16)
                self.phase_norm(e0, l, 0, xs, hT, True)
            self.tap("hT%d" % l, hT[:], ["hT%d" % fc for fc in range(cfg.KC)])
            if self.stop_after == (l, "norm1"):
                return
            self.phase_mix(l, hT, with_ctx)
        if self.stop_after is not None and self.stop_after[0] == l and self.stop_after[1] != "ffn":
            return
        self.phase_ffn(l, with_ctx)

    def wload(self, dst, win, slices, tok):
        a = 0
        for (c0, n) in slices:
            self.S.dma("pool", dst[:, :, a:a + n], win[:, :, c0:c0 + n], writes=[tok])
            a += n

    def proj_fm(self, W, wtok, hT, col0, ncols, banks, consume):
        KC = self.cfg.KC
        gi = 0
        for g0 in range(col0, col0 + ncols, 512):
            gn = min(512, col0 + ncols - g0)
            b = banks[gi % len(banks)]
            gi += 1
            for kc in range(KC):
                self.P(lambda e: e.matmul(self.pb[b][:, :gn], lhsT=W[:, kc, :], rhs=hT[:, kc, g0:g0 + gn],
                                          start=(kc == 0), stop=(kc == KC - 1)),
                       r=[wtok, "hT%d" % kc], x=[self.bk(b)])
            consume(self.pb[b][:, :gn], g0, gn, b)

    def proj_tm(self, W, wtok, hT, ti, ncols, b):
        KC = self.cfg.KC
        for kc in range(KC):
            self.P(lambda e: e.matmul(self.pb[b][:, :ncols], lhsT=hT[:, kc, ti * 128:(ti + 1) * 128], rhs=W[:, kc, :ncols],
                                      start=(kc == 0), stop=(kc == KC - 1)),
                   r=[wtok, "hT%d" % kc], x=[self.bk(b)])

    def qk_chunk(self, l, es, hT, win, nat, dst, dtok, nrm, nq_cols, pf):
        cfg = self.cfg
        L, KC = cfg.L, cfg.KC
        if "qkW" not in es:
            es["qkW"] = self.sb(es["es"], "qkW", [128, KC, 128], BF16)
            es["qkWp"] = self.sb(es["es"], "qkWp", [128, KC, 128], BF16)
            for nm in ("qk_t1", "qk_t2", "qk_sq", "qk_rs"):
                es[nm] = self.sb(es["es"], nm, [128, 512], F32)
        pf = ""
        W, Wp = es["qkW"], es["qkWp"]
        perm = []
        for (c0, n) in nat:
            for a in range(0, n, 64):
                perm += [(c0 + a + 32, 32), (c0 + a, 32)]
        self.wload(W, win, nat, pf + "qkW")
        self.wload(Wp, win, perm, pf + "qkWp")
        t1, t2, sq, rs = es["qk_t1"], es["qk_t2"], es["qk_sq"], es["qk_rs"]
        cosT, sinT = self.ropeT[:, 0:L], self.ropeT[:, L:2 * L]
        for g0 in range(0, nq_cols, 512):
            gn = min(512, nq_cols - g0)
            lat = g0 < L
            gi_ = g0 // 512
            bq, bp, bs_ = [0, 2, 4][gi_ % 3], [1, 3, 5][gi_ % 3], 6 + gi_ % 2
            for kc in range(KC):
                self.P(lambda e: e.matmul(self.pb[bq][:, :gn], lhsT=W[:, kc, :], rhs=hT[:, kc, g0:g0 + gn],
                                          start=(kc == 0), stop=(kc == KC - 1)), r=[pf + "qkW", "hT%d" % kc], x=[self.bk(bq)])
            if lat:
                for kc in range(KC):
                    self.P(lambda e: e.matmul(self.pb[bp][:, :gn], lhsT=Wp[:, kc, :], rhs=hT[:, kc, g0:g0 + gn],
                                              start=(kc == 0), stop=(kc == KC - 1)), r=[pf + "qkWp", "hT%d" % kc], x=[self.bk(bp)])
            pq, pp = self.pb[bq][:, :gn], self.pb[bp][:, :gn]
            if nrm is not None:
                self.A(lambda e: e.activation(out=sq[:, :gn], in_=pq, func=AF.Square), w=[pf + "qk_sq"], x=[self.bk(bq)])
                self.P(lambda e: e.matmul(self.pb[bs_][:, :gn], lhsT=self.cst(CBK), rhs=sq[:, :gn], start=True, stop=True),
                       r=["consts", pf + "qk_sq"], x=[self.bk(bs_)])
                self.V(lambda e: e.tensor_scalar(out=rs[:, :gn], in0=self.pb[bs_][:, :gn], scalar1=1.0 / 64, scalar2=EPS,
                                                 op0=ALU.mult, op1=ALU.add), w=[pf + "qk_rs"], x=[self.bk(bs_)])
                self.A(lambda e: e.activation(out=rs[:, :gn], in_=rs[:, :gn], func=AF.Sqrt), w=[pf + "qk_rs"])
                self.V(lambda e: e.reciprocal(out=rs[:, :gn], in_=rs[:, :gn]), w=[pf + "qk_rs"])
                wc = self.col(nrm + "%d" % l)
                if lat:
                    self.V(lambda e: e.scalar_tensor_tensor(out=t1[:, :gn], in0=pq, scalar=wc[:, 0:1], in1=cosT[:, g0:g0 + gn],
                                                            op0=ALU.mult, op1=ALU.mult), r=["cols", "ropeT"], w=[pf + "qk_t1"], x=[self.bk(bq)])
                    self.V(lambda e: e.scalar_tensor_tensor(out=t2[:, :gn], in0=pp, scalar=wc[:, 1:2], in1=sinT[:, g0:g0 + gn],
                                                            op0=ALU.mult, op1=ALU.mult), r=["cols", "ropeT"], w=[pf + "qk_t2"], x=[self.bk(bp)])
                    self.G(lambda e: e.tensor_tensor(out=t1[:, :gn], in0=t1[:, :gn], in1=t2[:, :gn], op=ALU.add),
                           r=[pf + "qk_t2"], w=[pf + "qk_t1"])
                    self.V(lambda e: e.tensor_tensor(out=dst[:, g0:g0 + gn], in0=t1[:, :gn], in1=rs[:, :gn], op=ALU.mult),
                           r=[pf + "qk_t1", pf + "qk_rs"], w=[dtok])
                else:
                    self.V(lambda e: e.scalar_tensor_tensor(out=dst[:, g0:g0 + gn], in0=pq, scalar=wc[:, 0:1], in1=rs[:, :gn],
                                                            op0=ALU.mult, op1=ALU.mult), r=["cols", pf + "qk_rs"], w=[dtok], x=[self.bk(bq)])
            else:
                if lat:
                    self.V(lambda e: e.tensor_tensor(out=t1[:, :gn], in0=pq, in1=cosT[:, g0:g0 + gn], op=ALU.mult),
                           r=["ropeT"], w=[pf + "qk_t1"], x=[self.bk(bq)])
                    self.V(lambda e: e.tensor_tensor(out=t2[:, :gn], in0=pp, in1=sinT[:, g0:g0 + gn], op=ALU.mult),
                           r=["ropeT"], w=[pf + "qk_t2"], x=[self.bk(bp)])
                    self.G(lambda e: e.tensor_tensor(out=dst[:, g0:g0 + gn], in0=t1[:, :gn], in1=t2[:, :gn], op=ALU.add),
                           r=[pf + "qk_t1", pf + "qk_t2"], w=[dtok])
                else:
                    self.A(lambda e: e.copy(out=dst[:, g0:g0 + gn], in_=pq), w=[dtok], x=[self.bk(bq)])

    def attention(self, PT, QT, KT, Vaug, vw, qcol0, nq, ktiles, finish, tokQ, tokK, tokV, sbanks, accsets, dist, state):
        spb = 512 // (vw + 1)
        for g0 in range(qcol0, qcol0 + nq, 512):
            gn = min(512, qcol0 + nq - g0)
            nqt = gn // 128
            aset = accsets[state["gi"] % len(accsets)]
            state["gi"] += 1

            def acc(j, qt, aset=aset, nqt=nqt):
                s_ = j * nqt + qt
                bnk = aset[s_ // spb]
                return self.pb[bnk][:, (s_ % spb) * (vw + 1):(s_ % spb + 1) * (vw + 1)], bnk
            nb = (2 * nqt + spb - 1) // spb
            for b in range(nb):
                self.V(lambda e: e.memset(self.pb[aset[b]][:], 0.0), w=[self.bk(aset[b])])
            steps = [(kt, j) for kt in ktiles for j in range(2)]
            n = len(steps)

            def issue_S(i):
                kt, j = steps[i]
                sbk = sbanks[i % len(sbanks)]
                pbuf = i % len(PT)
                self.P(lambda e: e.matmul(self.pb[sbk][:, :gn], lhsT=KT[:, j, kt * 128:(kt + 1) * 128],
                                          rhs=QT[:, g0:g0 + gn], start=True, stop=True),
                       r=[tokQ, tokK], x=[self.bk(sbk)])
                self.A(lambda e: e.activation(out=PT[pbuf][:, :gn], in_=self.pb[sbk][:, :gn], func=AF.Exp, scale=0.125),
                       w=["PT%d" % pbuf], x=[self.bk(sbk)])

            def issue_PV(i):
                kt, j = steps[i]
                pbuf = i % len(PT)
                for qt in range(nqt):
                    ap_, bnk = acc(j, qt)
                    self.P(lambda e: e.matmul(ap_, lhsT=PT[pbuf][:, qt * 128:(qt + 1) * 128], rhs=Vaug(kt),
                                              start=False, stop=False, skip_group_check=True),
                           r=["PT%d" % pbuf, tokV], x=[self.bk(bnk)])
            for i in range(min(dist, n)):
                issue_S(i)
            pend = state.get("pending")
            for i in range(n):
                if i + dist < n:
                    issue_S(i + dist)
                issue_PV(i)
                if pend is not None and i == min(1, n - 1):
                    pend(1)
                if pend is not None and i == min(max(n // 2, 2), n - 1):
                    pend(2)

            def fin(phase, g0=g0, nqt=nqt, acc=acc):
                for qt in range(nqt):
                    (a0, b0), (a1, b1) = acc(0, qt), acc(1, qt)
                    finish(g0 // 128 + qt, a0, a1, [self.bk(b0), self.bk(b1)], qt, phase)
            state["pending"] = fin

    def pad_k(self, es, KT):
        NTOK = self.cfg.NTOK
        KTz = self.sb(es, "KTz", [128, 2, NTOK], BF16)
        self.G(lambda e: e.memset(KTz[64:128, 0, :], 0.0), w=["KTz"])
        self.G(lambda e: e.memset(KTz[0:64, 1, :], 0.0), w=["KTz"])
        self.A(lambda e: e.copy(out=KTz[0:64, 0, :], in_=KT[0:64, :]), r=["KT"], w=["KTz"])
        self.G(lambda e: e.tensor_copy(out=KTz[64:128, 1, :], in_=KT[64:128, :]), r=["KT"], w=["KTz"])
        return KTz

    def attention_flush(self, state):
        if state.get("pending") is not None:
            state["pending"](1)
            state["pending"](2)
            state["pending"] = None

    def out_transpose(self, tok_ap, ttok, oT, chunk, ti, bank=7):
        pv = self.pb[bank][:].bitcast(BF16)
        k = self._otn % 3
        self._otn += 1
        st = self.otst[k]
        self.P(lambda e: e.transpose(out=pv[:, 0:128], in_=tok_ap, identity=self.identb[:]), r=[ttok, "identb"], x=[self.bk(bank)])
        self.A(lambda e: e.copy(out=st[:], in_=pv[:, 0:128]), w=["otst%d" % k], x=[self.bk(bank)])
        self.S.dma("sp", self.oTd[chunk, :, ti * 128:(ti + 1) * 128], st[:], reads=["otst%d" % k], writes=["oTd"])

    def rstd_col(self, ss, n, rs_tok, r):
        self.V(lambda e: e.tensor_scalar(out=ss, in0=ss, scalar1=1.0 / n, scalar2=EPS, op0=ALU.mult, op1=ALU.add), r=r, w=[rs_tok])
        self.A(lambda e: e.activation(out=ss, in_=ss, func=AF.Sqrt), w=[rs_tok])
        self.V(lambda e: e.reciprocal(out=ss, in_=ss), w=[rs_tok])

    def phase_mix(self, l, hT, with_ctx):
        cfg, S = self.cfg, self.S
        D, L, LC, KC, NT, NTC, NTT, NTOK = cfg.D, cfg.L, cfg.LC, cfg.KC, cfg.NT, cfg.NTC, cfg.NTT, cfg.NTOK
        NB = cfg.HA
        win = self.dr["w_in"][l].rearrange("(kc p) c -> p kc c", p=128)
        lam_init = 0.8 - 0.6 * math.exp(-0.3 * l)
        nqc = NTOK if with_ctx else L
        allk = list(range(NTT))
        ctxk = list(range(NT, NTT))
        hTt = ["hT%d" % kc for kc in range(KC)]
        with ExitStack() as es:
            oT = None
            self.oTd = self.nc.dram_tensor("oTd%d" % l, [3 * NB, 128, NTOK], BF16).ap()
            self.otst = [self.sb(es, "otst%d" % i, [128, 128], BF16) for i in range(3)]
            self._otn = 0
            self.rowsL = self.sb(es, "rowsL", [128, cfg.NROWL], F32)
            S.dma("sp", self.rowsL[:], self.dr["rowsL"][l], writes=["rows"])
            lam = self.sb(es, "lam", [128, 4], F32)
            subw = self.sb(es, "subw", [128, 128], F32)
            ljunk = self.sb(es, "ljunk", [128, 64], F32)
            lr = self.row("lam")
            for i in range(2):
                self.V(lambda e: e.tensor_tensor(out=ljunk[:], in0=lr[:, 128 * i:128 * i + 64], in1=lr[:, 128 * i + 64:128 * i + 128],
                                                 op=ALU.mult), r=["rows"], w=["ljunk"])
                self.V(lambda e: e.reduce_sum(out=lam[:, i:i + 1], in_=ljunk[:], axis=AX.X), r=["ljunk"], w=["lam"])
            self.A(lambda e: e.activation(out=lam[:, 0:2], in_=lam[:, 0:2], func=AF.Exp), w=["lam"])
            self.V(lambda e: e.tensor_tensor(out=lam[:, 2:3], in0=lam[:, 1:2], in1=lam[:, 0:1], op=ALU.subtract), w=["lam"])
            self.V(lambda e: e.tensor_scalar(out=lam[:, 2:3], in0=lam[:, 2:3], scalar1=-lam_init, scalar2=None, op0=ALU.add), w=["lam"])
            self.V(lambda e: e.tensor_scalar(out=subw[:], in0=self.row("sub"), scalar1=1.0 - lam_init, scalar2=None,
                                             op0=ALU.mult), r=["rows"], w=["subw"])
            e_rope = ExitStack()
            self.ropeT = self.sb(e_rope, "ropeT", [128, 2 * L], F32)
            S.dma("sp", self.ropeT[:], self.dr["ropeT"], writes=["ropeT"])
            for h in range(cfg.HA):
                with ExitStack() as e2:
                    QT = self.sb(e2, "QT", [128, NTOK], BF16)
                    KT = self.sb(e2, "KT", [128, NTOK], BF16)
                    Va = self.sb(e2, "Va", [128, NTT, 129], BF16)
                    Wv = self.sb(e2, "Wv", [128, KC, 128], BF16)
                    qsc = {"es": e2}
                    self.qk_chunk(l, qsc, hT, win, [(cfg.oAq + h * 128, 128)], QT, "QT", None, nqc, "q")
                    self.qk_chunk(l, qsc, hT, win, [(cfg.oAk + h * 128, 128)], KT, "KT", None, NTOK, "k")
                    self.wload(Wv, win, [(cfg.oAv + h * 128, 128)], "Wv")
                    self.G(lambda e: e.memset(Va[:, :, 128:129], 1.0), w=["Va"])
                    for ti in range(NTT):
                        b = 5 + ti % 2
                        self.proj_tm(Wv, "Wv", hT, ti, 128, b)
                        self.A(lambda e: e.copy(out=Va[:, ti, 0:128], in_=self.pb[b][:, :128]), w=["Va"], x=[self.bk(b)])
                    fs = [self.sb(e2, "fin%d" % i, [128, 132], F32) for i in range(4)]
                    ft = [self.sb(e2, "fint%d" % i, [128, 128], F32) for i in range(4)]
                    fo = [self.sb(e2, "fino%d" % i, [128, 128], BF16) for i in range(4)]
                    fj = self.sb(e2, "finj", [128, 128], F32)

                    def finishA(ti, a0, a1, btoks, k, phase, h=h):
                        sm, t1, ob = fs[k], ft[k], fo[k]
                        stok, ttok, otok = "fin%d" % k, "fint%d" % k, "fino%d" % k
                        if phase == 2:
                            self.out_transpose(ob[:], otok, oT, h, ti, bank=0)
                            return
                        self.V(lambda e: e.reciprocal(out=sm[:, 128:129], in_=a0[:, 128:129]), w=[stok], x=btoks)
                        self.V(lambda e: e.reciprocal(out=sm[:, 129:130], in_=a1[:, 128:129]), w=[stok], x=btoks)
                        self.V(lambda e: e.tensor_tensor(out=sm[:, 129:130], in0=sm[:, 129:130], in1=lam[:, 2:3], op=ALU.mult),
                               r=["lam"], w=[stok])
                        self.V(lambda e: e.tensor_scalar(out=t1[:], in0=a1[:, 0:128], scalar1=sm[:, 129:130], scalar2=None,
                                                         op0=ALU.mult), r=[stok], w=[ttok], x=btoks)
                        self.V(lambda e: e.scalar_tensor_tensor(out=sm[:, 0:128], in0=a0[:, 0:128], scalar=sm[:, 128:129], in1=t1[:],
                                                                op0=ALU.mult, op1=ALU.add), r=[ttok], w=[stok], x=btoks)
                        self.A(lambda e: e.activation(out=fj[:], in_=sm[:, 0:128], func=AF.Square, accum_out=sm[:, 130:131]),
                               r=[stok], w=["finj", stok + "s"])
                        self.rstd_col(sm[:, 130:131], 128, stok + "s", [])
                        self.V(lambda e: e.scalar_tensor_tensor(out=ob[:], in0=sm[:, 0:128], scalar=sm[:, 130:131], in1=subw[:],
                                                                op0=ALU.mult, op1=ALU.mult), r=[stok, stok + "s", "subw"], w=[otok])
                    PT = [self.sb(e2, "PT%d" % i, [128, 512], BF16) for i in range(3)]
                    KTz = self.pad_k(e2, KT)
                    ast = {"gi": 0, "pending": None}
                    akw = dict(sbanks=[0, 1], accsets=[[2, 3, 4], [5, 6, 7]], dist=1, state=ast)
                    self.attention(PT, QT, KTz, lambda kt: Va[:, kt, :], 128, 0, L, allk, finishA, "QT", "KTz", "Va", **akw)
                    if with_ctx:
                        self.attention(PT, QT, KTz, lambda kt: Va[:, kt, :], 128, L, LC, ctxk, finishA, "QT", "KTz", "Va", **akw)
                    self.attention_flush(ast)
                    S.barrier()
            self.tap("oTa%d" % l, self.oTd[0:NB], ["oTd"])
            if self.stop_after == (l, "mixA"):
                e_rope.close()
                return
            c2_per_hk = max(1, cfg.GRP // 2)
            for hk in range(2):
                with ExitStack() as e2:
                    QT = self.sb(e2, "QT", [128, NTOK], BF16)
                    KT = self.sb(e2, "KT", [128, NTOK], BF16)
                    Vb = self.sb(e2, "Vb", [128, NTT, 65], BF16)
                    Wv = self.sb(e2, "Wv", [128, KC, 64], BF16)
                    qsc = {"es": e2}
                    self.qk_chunk(l, qsc, hT, win, [(cfg.oBk + hk * 64, 64), (cfg.oBk + hk * 64, 64)], KT, "KT", "knc", NTOK, "k")
                    self.wload(Wv, win, [(cfg.oBv + hk * 64, 64)], "Wv")
                    self.G(lambda e: e.memset(Vb[:, :, 64:65], 1.0), w=["Vb"])
                    for ti in range(NTT):
                        b = 5 + ti % 2
                        self.proj_tm(Wv, "Wv", hT, ti, 64, b)
                        self.A(lambda e: e.copy(out=Vb[:, ti, 0:64], in_=self.pb[b][:, :64]), w=["Vb"], x=[self.bk(b)])
                    fs = [self.sb(e2, "fin%d" % i, [128, 2], F32) for i in range(4)]
                    fo = [self.sb(e2, "fino%d" % i, [128, 128], BF16) for i in range(4)]
                    PT = [self.sb(e2, "PT%d" % i, [128, 512], BF16) for i in range(4)]
                    KTz = self.pad_k(e2, KT)
                    for c2 in range(hk * c2_per_hk, (hk + 1) * c2_per_hk):
                        self.qk_chunk(l, qsc, hT, win, [(cfg.oBq + c2 * 128, 128)], QT, "QT", "qnc", nqc, "q")

                        def finishB(ti, a0, a1, btoks, k, phase, c2=c2):
                            sm, ob = fs[k], fo[k]
                            stok, otok = "fin%d" % k, "fino%d" % k
                            if phase == 2:
                                self.out_transpose(ob[:], otok, oT, NB + c2, ti)
                                return
                            self.V(lambda e: e.reciprocal(out=sm[:, 0:1], in_=a0[:, 64:65]), w=[stok], x=btoks)
                            self.V(lambda e: e.reciprocal(out=sm[:, 1:2], in_=a1[:, 64:65]), w=[stok], x=btoks)
                            self.V(lambda e: e.tensor_scalar(out=ob[:, 0:64], in0=a0[:, 0:64], scalar1=sm[:, 0:1], scalar2=None,
                                                             op0=ALU.mult), r=[stok], w=[otok], x=btoks)
                            self.V(lambda e: e.tensor_scalar(out=ob[:, 64:128], in0=a1[:, 0:64], scalar1=sm[:, 1:2], scalar2=None,
                                                             op0=ALU.mult), r=[stok], w=[otok], x=btoks)
                        ast = {"gi": 0, "pending": None}
                        akw = dict(sbanks=[0, 1, 6], accsets=[[2, 3], [4, 5]], dist=2, state=ast)
                        self.attention(PT, QT, KTz, lambda kt: Vb[:, kt, :], 64, 0, L, allk, finishB, "QT", "KTz", "Vb", **akw)
                        if with_ctx:
                            self.attention(PT, QT, KTz, lambda kt: Vb[:, kt, :], 64, L, LC, ctxk, finishB, "QT", "KTz", "Vb", **akw)
                        self.attention_flush(ast)
                    S.barrier()
            self.tap("oTb%d" % l, self.oTd[NB:2 * NB], ["oTd"])
            S.barrier()
            e_rope.close()
            if self.stop_after == (l, "mixB"):
                return
            self.mix_mlstm(l, hT, win, oT, with_ctx)
            self.tap("oTc%d" % l, self.oTd[2 * NB:3 * NB], ["oTd"])
            if self.stop_after == (l, "mixC"):
                return
            self.mix_merge(l, hT, win, oT, with_ctx)

    def mix_mlstm(self, l, hT, win, oT, with_ctx):
        cfg, S = self.cfg, self.S
        D, L, LC, KC, NT, NTC, NTT, NTOK, HC = cfg.D, cfg.L, cfg.LC, cfg.KC, cfg.NT, cfg.NTC, cfg.NTT, cfg.NTOK, cfg.HC
        NB = HC
        one_col = self.cst(CON)[:, 0:1]
        with ExitStack() as es:
            Wg = self.sb(es, "Wg", [128, KC, 4 * HC], BF16)
            self.wload(Wg, win, [(cfg.oG, 4 * HC)], "Wg")
            Gt = self.sb(es, "Gt", [128, NTT, 4 * HC], F32)
            LF = self.sb(es, "LF", [128, NTT, 2, HC], F32)
            BC = self.sb(es, "BC", [128, NTT, 2, HC], F32)
            TOT = self.sb(es, "TOT", [128, NTT, 2, HC], F32)
            BIAS = self.sb(es, "BIAS", [128, NTT, 2, HC], F32)
            EB = self.sb(es, "EB", [128, NTT, 2, HC], F32)
            WC = self.sb(es, "WC", [128, NTT, 2, HC], F32)
            AC = self.sb(es, "AC", [128, NTT, 2, HC], F32)
            for ti in range(NTT):
                b = 5 + ti % 2
                self.proj_tm(Wg, "Wg", hT, ti, 4 * HC, b)
                self.V(lambda e: e.tensor_tensor(out=Gt[:, ti, :], in0=self.pb[b][:, :4 * HC], in1=self.row("gb"), op=ALU.add),
                       r=["rows"], w=["Gt"], x=[self.bk(b)])
            Gv = Gt[:].rearrange("p t (q h) -> p t q h", h=HC)
            for d in range(2):
                self.A(lambda e: e.activation(out=LF[:, :, d, :], in_=Gv[:, :, 2 * d + 1, :], func=AF.Exp, scale=-1.0), r=["Gt"], w=["LF"])
            self.A(lambda e: e.activation(out=LF[:], in_=LF[:], func=AF.Ln, bias=one_col), r=["consts"], w=["LF"])
            self.V(lambda e: e.tensor_scalar(out=LF[:], in0=LF[:], scalar1=-1.0, scalar2=None, op0=ALU.mult), w=["LF"])
            for ti in range(NTT):
                b = 5 + ti % 2
                self.P(lambda e: e.matmul(self.pb[b][:, 0:HC], lhsT=self.cst(CTU), rhs=LF[:, ti, 0, :], start=True, stop=True),
                       r=["consts", "LF"], x=[self.bk(b)])
                self.P(lambda e: e.matmul(self.pb[b][:, HC:2 * HC], lhsT=self.cst(CTL), rhs=LF[:, ti, 1, :], start=True, stop=True),
                       r=["consts", "LF"], x=[self.bk(b)])
                self.P(lambda e: e.matmul(self.pb[b][:, 2 * HC:4 * HC], lhsT=self.cst(CON), rhs=LF[:, ti, :, :].rearrange("p a b -> p (a b)"),
                                          start=True, stop=True), r=["consts", "LF"], x=[self.bk(b)])
                self.V(lambda e: e.tensor_copy(out=BC[:, ti, :, :].rearrange("p a b -> p (a b)"), in_=self.pb[b][:, 0:2 * HC]), w=["BC"], x=[self.bk(b)])
                self.V(lambda e: e.tensor_copy(out=TOT[:, ti, :, :].rearrange("p a b -> p (a b)"), in_=self.pb[b][:, 2 * HC:4 * HC]), w=["TOT"], x=[self.bk(b)])
            for d in range(2):
                self.V(lambda e: e.tensor_tensor(out=BIAS[:, :, d, :], in0=Gv[:, :, 2 * d, :], in1=BC[:, :, d, :], op=ALU.subtract),
                       r=["Gt", "BC"], w=["BIAS"])
            self.A(lambda e: e.activation(out=EB[:], in_=BC[:], func=AF.Exp), r=["BC"], w=["EB"])
            self.V(lambda e: e.tensor_tensor(out=WC[:], in0=TOT[:], in1=BIAS[:], op=ALU.add), r=["TOT", "BIAS"], w=["WC"])
            self.A(lambda e: e.activation(out=WC[:], in_=WC[:], func=AF.Exp), w=["WC"])
            self.A(lambda e: e.activation(out=AC[:], in_=TOT[:], func=AF.Exp), r=["TOT"], w=["AC"])
            S.barrier()
            for hc in range(HC):
                with ExitStack() as e2:
                    Ws = {}
                    for nm, o in (("q", cfg.oCq), ("k", cfg.oCk), ("v", cfg.oCv), ("o", cfg.oCo)):
                        Ws[nm] = self.sb(e2, "Wc" + nm, [128, KC, 128], BF16)
                        self.wload(Ws[nm], win, [(o + hc * 128, 128)], "Wc" + nm)
                    qT = self.sb(e2, "cqT", [128, NTOK], BF16)
                    kT = self.sb(e2, "ckT", [128, NTOK], BF16)
                    raw = self.sb(e2, "craw", [128, NTOK], F32)
                    acc = self.sb(e2, "cacc", [128, NTOK], F32)
                    cw = self.col("conv%d" % l)
                    for (nm, dst, dtok, scale, chunk) in (("q", qT, "cqT", 1.0, hc), ("k", kT, "ckT", 128.0 ** -0.5, HC + hc)):
                        def cons(ps_ap, g0, gn, b):
                            self.A(lambda e: e.copy(out=raw[:, g0:g0 + gn], in_=ps_ap), w=["craw"], x=[self.bk(b)])
                        self.proj_fm(Ws[nm], "Wc" + nm, hT, 0, NTOK, [5, 6], cons)
                        w0 = cw[:, 0 * 2 * HC + chunk:0 * 2 * HC + chunk + 1]
                        w1 = cw[:, 1 * 2 * HC + chunk:1 * 2 * HC + chunk + 1]
                        w2 = cw[:, 2 * 2 * HC + chunk:2 * 2 * HC + chunk + 1]
                        for (s0, n) in ((0, L), (L, LC)):
                            self.V(lambda e: e.tensor_scalar(out=acc[:, s0:s0 + n], in0=raw[:, s0:s0 + n], scalar1=w1, scalar2=None, op0=ALU.mult),
                                   r=["craw", "cols"], w=["cacc"])
                            self.V(lambda e: e.scalar_tensor_tensor(out=acc[:, s0 + 1:s0 + n], in0=raw[:, s0:s0 + n - 1], scalar=w0,
                                                                    in1=acc[:, s0 + 1:s0 + n], op0=ALU.mult, op1=ALU.add), r=["craw", "cols"], w=["cacc"])
                            self.V(lambda e: e.scalar_tensor_tensor(out=acc[:, s0:s0 + n - 1], in0=raw[:, s0 + 1:s0 + n], scalar=w2,
                                                                    in1=acc[:, s0:s0 + n - 1], op0=ALU.mult, op1=ALU.add), r=["craw", "cols"], w=["cacc"])
                        self.A(lambda e: e.activation(out=raw[:], in_=acc[:], func=AF.Sigmoid), r=["cacc"], w=["craw"])
                        self.V(lambda e: e.scalar_tensor_tensor(out=dst[:], in0=acc[:], scalar=scale, in1=raw[:], op0=ALU.mult, op1=ALU.mult),
                               r=["cacc", "craw"], w=[dtok])
                    Vc = self.sb(e2, "cVc", [128, NTT, 129], BF16)
                    OG = acc[:].rearrange("p (t d) -> p t d", d=128)
                    Kt = self.sb(e2, "cKt", [128, NTT, 128], BF16)
                    HS = self.sb(e2, "cHS", [128, NTT, 128], F32)
                    self.G(lambda e: e.memset(Vc[:, :, 128:129], 1.0), w=["cVc"])
                    self.G(lambda e: e.memset(HS[:], 0.0), w=["cHS"])
                    pv7 = self.pb[7][:].bitcast(BF16)
                    for ti in range(NTT):
                        self.proj_tm(Ws["v"], "Wcv", hT, ti, 128, 5)
                        self.A(lambda e: e.copy(out=Vc[:, ti, 0:128], in_=self.pb[5][:, :128]), w=["cVc"], x=[self.bk(5)])
                        self.P(lambda e: e.transpose(out=pv7[:, 0:128], in_=kT[:, ti * 128:(ti + 1) * 128], identity=self.identb[:]),
                               r=["ckT", "identb"], x=[self.bk(7)])
                        self.V(lambda e: e.tensor_copy(out=Kt[:, ti, :], in_=pv7[:, 0:128]), w=["cKt"], x=[self.bk(7)])
                    INTRA = [raw[:].rearrange("p (t d) -> p t d", d=128), acc[:].rearrange("p (t d) -> p t d", d=128)]
                    itok = ["craw", "cacc"]
                    DENI = self.sb(e2, "cDENI", [128, NTT, 2], F32)
                    SB = [self.sb(e2, "cSB%d" % d, [128, NTT, 129], BF16) for d in range(2)]
                    st = [self.sb(e2, "st%d" % d, [128, 129], F32) for d in range(2)]
                    rot = dict(Lt=[self.sb(e2, "Lt%d" % i, [128, 128], F32) for i in range(3)],
                               DT=[self.sb(e2, "DT%d" % i, [128, 128], F32) for i in range(3)],
                               SM=[self.sb(e2, "SM%d" % i, [128, 128], BF16) for i in range(3)],
                               VW=[self.sb(e2, "VW%d" % i, [128, 129], BF16) for i in range(3)],
                               nd=[self.sb(e2, "nd%d" % i, [128, 132], F32) for i in range(3)])
                    order = [list(range(NT, NTT)) + list(range(NT)), list(range(NTT - 1, NT - 1, -1)) + list(range(NT - 1, -1, -1))]
                    out_tiles = list(range(NTT)) if with_ctx else list(range(NT))
                    for kk in ("Lt", "DT", "SM", "VW"):
                        rot[kk].append(self.sb(e2, kk + "3", [128, 129 if kk == "VW" else 128], BF16 if kk in ("SM", "VW") else F32))
                    items = [(ti, d) for ti in out_tiles for d in range(2)]
                    nit = len(items)

                    def p1A(i):
                        ti, d = items[i]
                        cs = slice(ti * 128, (ti + 1) * 128)
                        bS = (i // 2) % 2
                        if d == 0:
                            self.P(lambda e: e.matmul(self.pb[bS][:, :128], lhsT=kT[:, cs], rhs=qT[:, cs], start=True, stop=True),
                                   r=["ckT", "cqT"], x=[self.bk(bS)])
                        k4 = i % 4
                        bL = 2 + i % 2
                        Lt = rot["Lt"][k4]
                        self.V(lambda e: e.tensor_scalar(out=Lt[:], in0=self.cst(CSL if d == 0 else CSU), scalar1=LF[:, ti, d, hc:hc + 1],
                                                         scalar2=None, op0=ALU.mult), r=["consts"], w=["Lt%d" % k4])
                        self.P(lambda e: e.matmul(self.pb[bL][:, :128], lhsT=Lt[:], rhs=self.cst(CTU if d == 0 else CTL),
                                                  start=True, stop=True), r=["Lt%d" % k4, "consts"], x=[self.bk(bL)])

                    def p1B(i):
                        ti, d = items[i]
                        bS = (i // 2) % 2
                        k4 = i % 4
                        bL = 2 + i % 2
                        DT, SM = rot["DT"][k4], rot["SM"][k4]
                        self.A(lambda e: e.activation(out=DT[:], in_=self.pb[bL][:, :128], func=AF.Exp,
                                                      bias=Gv[:, ti, 2 * d, hc:hc + 1]), w=["DT%d" % k4], x=[self.bk(bL)])
                        self.G(lambda e: e.tensor_tensor(out=DT[:], in0=DT[:], in1=self.cst(CTU if d == 0 else CTL), op=ALU.mult),
                               r=["consts"], w=["DT%d" % k4])
                        self.V(lambda e: e.tensor_tensor(out=SM[:], in0=DT[:], in1=self.pb[bS][:, :128], op=ALU.mult),
                               r=["DT%d" % k4], w=["SM%d" % k4], x=[self.bk(bS)])

                    def p1C(i):
                        ti, d = items[i]
                        k4 = i % 4
                        bI = 4 + i % 2
                        SM = rot["SM"][k4]
                        self.P(lambda e: e.matmul(self.pb[bI][:, :129], lhsT=SM[:], rhs=Vc[:, ti, :], start=True, stop=True),
                               r=["SM%d" % k4, "cVc"], x=[self.bk(bI)])
                        self.A(lambda e: e.copy(out=INTRA[d][:, ti, :], in_=self.pb[bI][:, :128]), w=[itok[d]], x=[self.bk(bI)])
                        self.V(lambda e: e.tensor_copy(out=DENI[:, ti, d:d + 1], in_=self.pb[bI][:, 128:129]), w=["cDENI"], x=[self.bk(bI)])
                    for i in range(nit + 2):
                        if i < nit:
                            p1A(i)
                        if 0 <= i - 1 < nit:
                            p1B(i - 1)
                        if 0 <= i - 2 < nit:
                            p1C(i - 2)
                    for d in range(2):
                        self.G(lambda e: e.memset(st[d][:], 0.0), w=["st%d" % d])
                        self.G(lambda e: e.memset(SB[d][:, 0, :], 0.0), w=["cSB%d" % d])
                    sitems = [(step, d) for step in range(NTT - 1) for d in range(2)]
                    nsi = len(sitems)
                    ubanks = [6, 7, 0, 1]

                    def p2A(i):
                        step, d = sitems[i]
                        ti = order[d][step]
                        k4 = i % 4
                        bU = ubanks[i % 4]
                        VW = rot["VW"][k4]
                        self.V(lambda e: e.tensor_scalar(out=VW[:], in0=Vc[:, ti, :], scalar1=WC[:, ti, d, hc:hc + 1], scalar2=None,
                                                         op0=ALU.mult), r=["cVc"], w=["VW%d" % k4])
                        self.P(lambda e: e.matmul(self.pb[bU][:, :129], lhsT=Kt[:, ti, :], rhs=VW[:], start=True, stop=True),
                               r=["cKt", "VW%d" % k4], x=[self.bk(bU)])

                    def p2B(i):
                        step, d = sitems[i]
                        ti = order[d][step]
                        bU = ubanks[i % 4]
                        self.V(lambda e: e.scalar_tensor_tensor(out=st[d][:], in0=st[d][:], scalar=AC[:, ti, d, hc:hc + 1],
                                                                in1=self.pb[bU][:, :129], op0=ALU.mult, op1=ALU.add),
                               w=["st%d" % d], x=[self.bk(bU)])
                        self.A(lambda e: e.copy(out=SB[d][:, step + 1, :], in_=st[d][:]), r=["st%d" % d], w=["cSB%d" % d])
                    for i in range(nsi + 2):
                        if i < nsi:
                            p2A(i)
                        if 0 <= i - 2 < nsi:
                            p2B(i - 2)
                    n3 = 0
                    for step in range(NTT):
                        for d in range(2):
                            ti = order[d][step]
                            if not (ti < NT or with_ctx):
                                continue
                            cs = slice(ti * 128, (ti + 1) * 128)
                            k3 = n3 % 3
                            bN = 2 + n3 % 4
                            n3 += 1
                            nd = rot["nd"][k3]
                            ntk = "nd%d" % k3
                            self.P(lambda e: e.matmul(self.pb[bN][:, :129], lhsT=qT[:, cs], rhs=SB[d][:, step, :], start=True, stop=True),
                                   r=["cqT", "cSB%d" % d], x=[self.bk(bN)])
                            self.V(lambda e: e.scalar_tensor_tensor(out=nd[:, 0:128], in0=self.pb[bN][:, 0:128], scalar=EB[:, ti, d, hc:hc + 1],
                                                                    in1=INTRA[d][:, ti, :], op0=ALU.mult, op1=ALU.add),
                                   r=[itok[d]], w=[ntk], x=[self.bk(bN)])
                            self.V(lambda e: e.scalar_tensor_tensor(out=nd[:, 128:129], in0=self.pb[bN][:, 128:129], scalar=EB[:, ti, d, hc:hc + 1],
                                                                    in1=DENI[:, ti, d:d + 1], op0=ALU.mult, op1=ALU.add),
                                   r=["cDENI"], w=[ntk], x=[self.bk(bN)])
                            self.V(lambda e: e.scalar_tensor_tensor(out=nd[:, 129:130], in0=nd[:, 128:129], scalar=-1.0, in1=nd[:, 128:129],
                                                                    op0=ALU.mult, op1=ALU.max), w=[ntk])
                            self.V(lambda e: e.tensor_scalar(out=nd[:, 129:130], in0=nd[:, 129:130], scalar1=1.0, scalar2=None, op0=ALU.max), w=[ntk])
                            self.V(lambda e: e.reciprocal(out=nd[:, 129:130], in_=nd[:, 129:130]), w=[ntk])
                            self.G(lambda e: e.scalar_tensor_tensor(out=HS[:, ti, :], in0=nd[:, 0:128], scalar=nd[:, 129:130],
                                                                    in1=HS[:, ti, :], op0=ALU.mult, op1=ALU.add), r=[ntk], w=["cHS"]) \
                                if False else self.V(lambda e: e.scalar_tensor_tensor(out=HS[:, ti, :], in0=nd[:, 0:128], scalar=nd[:, 129:130],
                                                                                      in1=HS[:, ti, :], op0=ALU.mult, op1=ALU.add), r=[ntk], w=["cHS"])
                    S.barrier()
                    for ti in (range(NTT) if with_ctx else range(NT)):
                        b = 5 + ti % 2
                        self.proj_tm(Ws["o"], "Wco", hT, ti, 128, b)
                        self.A(lambda e: e.activation(out=OG[:, ti, :], in_=self.pb[b][:, :128], func=AF.Sigmoid), w=["cacc"], x=[self.bk(b)])
                    fs = self.sb(e2, "cfs", [128, NTT], F32)
                    fj = self.sb(e2, "cfj", [128, 128], F32)
                    fo = [self.sb(e2, "cfo%d" % i, [128, 128], BF16) for i in range(2)]
                    tiles = list(range(NTT)) if with_ctx else list(range(NT))
                    for ti in tiles:
                        self.A(lambda e: e.activation(out=fj[:], in_=HS[:, ti, :], func=AF.Square, accum_out=fs[:, ti:ti + 1]), w=["cfj", "cfs"])
                    self.rstd_col(fs[:, :len(tiles)], 128, "cfs", [])
                    cn = self.row("cn", hc * 128, (hc + 1) * 128)
                    for n_, ti in enumerate(tiles):
                        k = n_ % 2
                        self.V(lambda e: e.scalar_tensor_tensor(out=HS[:, ti, :], in0=HS[:, ti, :], scalar=fs[:, ti:ti + 1], in1=cn,
                                                                op0=ALU.mult, op1=ALU.mult), r=["cfs", "rows"], w=["cHS"])
                        self.G(lambda e: e.tensor_tensor(out=fo[k][:], in0=HS[:, ti, :], in1=OG[:, ti, :], op=ALU.mult), r=["cHS", "cacc"], w=["cfo%d" % k])
                        self.out_transpose(fo[k][:], "cfo%d" % k, oT, 2 * NB + hc, ti)
                    S.barrier()

    def mix_merge(self, l, hT, win, oT, with_ctx):
        cfg, S = self.cfg, self.S
        D, L, KC, NT, NTT, NTOK, NB = cfg.D, cfg.L, cfg.KC, cfg.NT, cfg.NTT, cfg.NTOK, cfg.HA
        ntok = NTOK if with_ctx else L
        wbr = [self.dr[n][l].rearrange("(c p) d -> p c d", p=128) for n in ("w_branch_a", "w_branch_b", "w_branch_c")]
        S.barrier()
        with ExitStack() as es:
            mT = self.sb(es, "mT", [128, KC, 512], BF16)
            acc = self.sb(es, "macc", [128, 512], F32)
            sgs = [self.sb(es, "msg%d" % i, [128, 512], F32) for i in range(2)]
            Wg = [self.sb(es, "mWg%d" % i, [128, KC, 128], BF16) for i in range(6)]
            Wb = [self.sb(es, "mWb%d" % i, [128, NB, 128], BF16) for i in range(6)]
            oTg = [self.sb(es, "oTg%d" % i, [128, NB, 512], BF16) for i in range(3)]
            Wo = self.sb(es, "mWo", [128, KC, D], BF16)
            tmp = [self.sb(es, "mtmp%d" % i, [128, 512], F32) for i in range(2)]
            S.dma("pool", Wo[:], self.dr["w_out"][l].rearrange("(c p) d -> p c d", p=128), writes=["mWo"])
            n = 0
            gcnt = 0
            n2 = 0
            for g0 in range(0, ntok, 512):
                gn = min(512, ntok - g0)
                for i in range(3):
                    S.dma("sp", oTg[i][:, :, :gn], self.oTd[i * NB:(i + 1) * NB, :, g0:g0 + gn].rearrange("c p t -> p c t"),
                          reads=["oTd"], writes=["oTg%d" % i])
                for dc in range(KC):
                    for i in range(3):
                        k = n % 6
                        n += 1
                        self.wload(Wg[k], win, [(cfg.oMG + i * D + dc * 128, 128)], "mWg%d" % k)
                        S.dma("pool", Wb[k][:], wbr[i][:, :, dc * 128:(dc + 1) * 128], writes=["mWb%d" % k])
                        bg = 5 + gcnt % 2
                        bb = 0 + gcnt % 2
                        sg = sgs[gcnt % 2]
                        stok = "msg%d" % (gcnt % 2)
                        gcnt += 1
                        for kc in range(KC):
                            self.P(lambda e: e.matmul(self.pb[bg][:, :gn], lhsT=Wg[k][:, kc, :], rhs=hT[:, kc, g0:g0 + gn],
                                                      start=(kc == 0), stop=(kc == KC - 1)), r=["mWg%d" % k, "hT%d" % kc], x=[self.bk(bg)])
                        self.A(lambda e: e.activation(out=sg[:, :gn], in_=self.pb[bg][:, :gn], func=AF.Sigmoid), w=[stok], x=[self.bk(bg)])
                        for c in range(NB):
                            self.P(lambda e: e.matmul(self.pb[bb][:, :gn], lhsT=Wb[k][:, c, :], rhs=oTg[i][:, c, :gn],
                                                      start=(c == 0), stop=(c == NB - 1)), r=["mWb%d" % k, "oTg%d" % i], x=[self.bk(bb)])
                        if i == 0:
                            self.V(lambda e: e.tensor_tensor(out=acc[:, :gn], in0=sg[:, :gn], in1=self.pb[bb][:, :gn], op=ALU.mult),
                                   r=[stok], w=["macc"], x=[self.bk(bb)])
                        else:
                            self.V(lambda e: e.tensor_tensor(out=sg[:, :gn], in0=sg[:, :gn], in1=self.pb[bb][:, :gn], op=ALU.mult),
                                   w=[stok], x=[self.bk(bb)])
                            if i == 1:
                                self.G(lambda e: e.tensor_tensor(out=acc[:, :gn], in0=acc[:, :gn], in1=sg[:, :gn], op=ALU.add),
                                       r=[stok], w=["macc"])
                            else:
                                self.G(lambda e: e.tensor_tensor(out=mT[:, dc, :gn], in0=acc[:, :gn], in1=sg[:, :gn], op=ALU.add),
                                       r=[stok, "macc"], w=["mT"])
                for tt in range(gn // 128):
                    ti = g0 // 128 + tt
                    w_ = 0 if ti < NT else 1
                    for hf in range(D // 512):
                        k = n2 % 2
                        n2 += 1
                        b = 2 + k
                        for kc in range(KC):
                            self.P(lambda e: e.matmul(self.pb[b][:, :], lhsT=mT[:, kc, tt * 128:(tt + 1) * 128], rhs=Wo[:, kc, hf * 512:(hf + 1) * 512],
                                                      start=(kc == 0), stop=(kc == KC - 1)), r=["mT", "mWo"], x=[self.bk(b)])
                        self.V(lambda e: e.tensor_tensor(out=tmp[k][:], in0=self.pb[b][:, :], in1=self.grow[:, w_, hf * 512:(hf + 1) * 512], op=ALU.mult),
                               r=["grow"], w=["mtmp%d" % k], x=[self.bk(b)])
                        xt = self.src_tile(ti)
                        self.G(lambda e: e.tensor_tensor(out=xt[:, hf * 512:(hf + 1) * 512], in0=xt[:, hf * 512:(hf + 1) * 512], in1=tmp[k][:], op=ALU.add),
                               r=["mtmp%d" % k], w=["x%d" % ti])
            S.barrier()

    def phase_ffn(self, l, with_ctx):
        cfg, S = self.cfg, self.S
        D, L, LC, E, FF, FC, KC, NT, NTC, NTT, NTOK = cfg.D, cfg.L, cfg.LC, cfg.E, cfg.FF, cfg.FC, cfg.KC, cfg.NT, cfg.NTC, cfg.NTT, cfg.NTOK
        self.phase_mod(l, 5, False)
        sets = [dict(t0=0, nt=NT, cap=cfg.CAPL, w=0, s0=0)]
        if with_ctx:
            sets.append(dict(t0=NT, nt=NTC, cap=cfg.CAPC, w=1, s0=cfg.CAPL))
        NS = sum(st["cap"] for st in sets)
        ntl = NTT if with_ctx else NT
        stiles = []
        for si, st in enumerate(sets):
            assert st["s0"] % 128 == 0
            for a in range(0, st["cap"], 128):
                stiles.append((st["s0"] + a, min(128, st["cap"] - a), si))
        NST = len(stiles)
        NH = D // 512
        assert NST * NH <= 6
        identf = self.cst(CI)
        iof = self.row("iof")
        with ExitStack() as es:
            xs = self.sb(es, "xs2", [128, NTT, D], BF16)
            rankTok = self.sb(es, "rankTok", [128, NTT, E], F32)
            LG = self.sb(es, "LG", [128, NTT, E], F32)
            iopj = self.sb(es, "iopj", [128, NST], F32)
            e_row = ExitStack()
            gT = self.sb(e_row, "gT", [E, NTOK], F32)
            rankT = self.sb(e_row, "rankT", [E, NTOK], F32)
            for k, (s_start, nn, si) in enumerate(stiles):
                self.V(lambda e: e.tensor_scalar(out=iopj[:, k:k + 1], in0=self.col("iop"), scalar1=float(s_start), scalar2=None, op0=ALU.add),
                       r=["cols"], w=["iopj"])
            with ExitStack() as e1:
                hT2 = self.sb(e1, "hT2", [128, KC, NTOK], BF16)
                self.phase_norm(e1, l, 1, xs, hT2, with_ctx)
                Wr = self.sb(e1, "Wr", [128, KC, E], BF16)
                S.dma("pool", Wr[:], self.dr["w_router"][l].rearrange("(c p) e -> p c e", p=128), writes=["Wr"])
                mx = self.sb(e1, "lgmx", [128, NTT], F32)
                for ti in range(ntl):
                    b = 5 + ti % 2
                    self.proj_tm(Wr, "Wr", hT2, ti, E, b)
                    self.V(lambda e: e.tensor_copy(out=LG[:, ti, :], in_=self.pb[b][:, :E]), w=["LG"], x=[self.bk(b)])
                lg = LG[:, :ntl, :]
                self.V(lambda e: e.tensor_reduce(out=mx[:, :ntl], in_=lg, axis=AX.X, op=ALU.max), r=["LG"], w=["lgmx"])
                self.V(lambda e: e.tensor_tensor(out=lg, in0=lg, in1=mx[:, :ntl].unsqueeze(2).to_broadcast([128, ntl, E]), op=ALU.subtract),
                       r=["lgmx"], w=["LG"])
                self.A(lambda e: e.activation(out=lg, in_=lg, func=AF.Exp), w=["LG"])
                self.V(lambda e: e.reduce_sum(out=mx[:, :ntl], in_=lg, axis=AX.X), r=["LG"], w=["lgmx"])
                self.V(lambda e: e.reciprocal(out=mx[:, :ntl], in_=mx[:, :ntl]), w=["lgmx"])
                self.V(lambda e: e.tensor_tensor(out=lg, in0=lg, in1=mx[:, :ntl].unsqueeze(2).to_broadcast([128, ntl, E]), op=ALU.mult),
                       r=["lgmx"], w=["LG"])
                for t0 in range(0, ntl, 4):
                    nt_ = min(4, ntl - t0)
                    b = (t0 // 4) % 2
                    for k in range(nt_):
                        self.P(lambda e: e.transpose(out=self.pb[b][0:E, k * 128:(k + 1) * 128], in_=LG[:, t0 + k, :], identity=identf),
                               r=["LG", "consts"], x=[self.bk(b)])
                    self.A(lambda e: e.copy(out=gT[:, t0 * 128:(t0 + nt_) * 128], in_=self.pb[b][0:E, :nt_ * 128]), w=["gT"], x=[self.bk(b)])
                S.barrier()
            with ExitStack() as e1:
                nmax = max(st["nt"] for st in sets) * 128
                work = self.sb(e1, "tkw", [E, nmax], F32)
                MK = self.sb(e1, "tkm", [E, nmax], F32)
                CS = self.sb(e1, "tkc", [E, nmax], F32)
                ones = self.sb(e1, "tko", [E, nmax], F32)
                mx8 = self.sb(e1, "tk8", [E, 8], F32)
                self.G(lambda e: e.memset(ones[:], 1.0), w=["tko"])
                for st in sets:
                    c0, n, cap = st["t0"] * 128, st["nt"] * 128, st["cap"]
                    assert cap % 8 == 0
                    self.V(lambda e: e.tensor_copy(out=work[:, :n], in_=gT[:, c0:c0 + n]), r=["gT"], w=["tkw"])
                    for r_ in range(cap // 8):
                        self.V(lambda e: e.max(out=mx8[:], in_=work[:, :n]), r=["tkw"], w=["tk8"])
                        if r_ < cap // 8 - 1:
                            self.V(lambda e: e.match_replace(out=work[:, :n], in_to_replace=mx8[:], in_values=work[:, :n], imm_value=-1.0),
                                   r=["tk8"], w=["tkw"])
                    self.V(lambda e: e.tensor_scalar(out=MK[:, :n], in0=gT[:, c0:c0 + n], scalar1=mx8[:, 7:8], scalar2=None, op0=ALU.is_ge),
                           r=["gT", "tk8"], w=["tkm"])
                    self.V(lambda e: e.tensor_tensor_scan(out=CS[:, :n], data0=ones[:, :n], data1=MK[:, :n], initial=0.0, op0=ALU.mult, op1=ALU.add),
                           r=["tko", "tkm"], w=["tkc"])
                    self.V(lambda e: e.scalar_tensor_tensor(out=CS[:, :n], in0=CS[:, :n], scalar=float(st["s0"]), in1=MK[:, :n], op0=ALU.add, op1=ALU.mult),
                           r=["tkm"], w=["tkc"])
                    self.V(lambda e: e.tensor_scalar(out=rankT[:, c0:c0 + n], in0=CS[:, :n], scalar1=-1.0, scalar2=None, op0=ALU.add),
                           r=["tkc"], w=["rankT"])
                for ti in range(ntl):
                    b = ti % 2
                    self.P(lambda e: e.transpose(out=self.pb[b][:, 0:E], in_=rankT[0:E, ti * 128:(ti + 1) * 128], identity=identf[0:E, 0:E]),
                           r=["rankT", "consts"], x=[self.bk(b)])
                    self.V(lambda e: e.tensor_copy(out=rankTok[:, ti, :], in_=self.pb[b][:, 0:E]), w=["rankTok"], x=[self.bk(b)])
                S.barrier()
            self.tap("rankT%d" % l, rankT[:, :ntl * 128], [])
            self.tap("gT%d" % l, gT[:, :ntl * 128], [])
            S.barrier()
            e_row.close()
            with ExitStack() as e1:
                CAPM = max(st["cap"] for st in sets)
                Sel = self.sb(e1, "Sel", [128, NTT, CAPM], BF16)
                SelT = [self.sb(e1, "SelT%d" % i, [128, NST, 512], BF16) for i in range(2)]
                gsb = [self.sb(e1, "gsb%d" % i, [128, 512], F32) for i in range(2)]
                repR = [self.sb(e1, "repR%d" % i, [128, 128], F32) for i in range(2)]
                repG = [self.sb(e1, "repG%d" % i, [128, 128], F32) for i in range(2)]
                xeT = self.sb(e1, "xeT", [128, KC, NS], BF16)
                actT = self.sb(e1, "actT", [128, FC, NS], BF16)
                ye = self.sb(e1, "ye", [128, NST, D], BF16)
                sa = [self.sb(e1, "sa%d" % i, [128, NS], F32) for i in range(2)]
                PW = 256
                DP = 2
                NWB = 3
                Wg = [self.sb(e1, "eWg%d" % i, [128, KC, PW], BF16) for i in range(NWB)]
                Wu = [self.sb(e1, "eWu%d" % i, [128, KC, PW], BF16) for i in range(NWB)]
                Wd = [self.sb(e1, "eWd%d" % i, [128, DP, D], BF16) for i in range(NWB)]
                cn = dict(wcnt=0, dcnt=0, scnt=0, ocnt=0, ecnt=0, rcnt=0)

                def do_gather(ex):
                        for st in sets:
                            for k in range(st["nt"]):
                                ti = st["t0"] + k
                                cap = st["cap"]
                                if st["s0"] == 0:
                                    self.V(lambda e: e.tensor_scalar(out=Sel[:, ti, :cap], in0=iof[:, :cap], scalar1=rankTok[:, ti, ex:ex + 1],
                                                                     scalar2=None, op0=ALU.is_equal), r=["rankTok", "rowsG"], w=["Sel"])
                                else:
                                    self.V(lambda e: e.tensor_scalar(out=Sel[:, ti, :cap], in0=iof[:, :cap], scalar1=float(st["s0"]),
                                                                     scalar2=rankTok[:, ti, ex:ex + 1], op0=ALU.add, op1=ALU.is_equal),
                                           r=["rankTok", "rowsG"], w=["Sel"])
                        for fc in range(KC):
                            b = 6 + fc % 2
                            for st in sets:
                                s0, cap, w_ = st["s0"], st["cap"], st["w"]
                                for k in range(st["nt"]):
                                    ti = st["t0"] + k
                                    self.P(lambda e: e.matmul(self.pb[b][:, s0:s0 + cap], lhsT=xs[:, ti, fc * 128:(fc + 1) * 128], rhs=Sel[:, ti, :cap],
                                                              start=(k == 0), stop=(k == st["nt"] - 1)), r=["xs%d" % ti, "Sel"], x=[self.bk(b)])
                                sc_ = self.modA[:, 1, fc, w_:w_ + 1]
                                bi_ = self.modc[:, 3 * KC + fc, w_:w_ + 1]
                                cn["ecnt"] += 1
                                if cn["ecnt"] % 2 == 0:
                                    self.A(lambda e: e.activation(out=xeT[:, fc, s0:s0 + cap], in_=self.pb[b][:, s0:s0 + cap], func=AF.Identity,
                                                                  scale=sc_, bias=bi_), r=["modA", "modc"], w=["xeT"], x=[self.bk(b)])
                                else:
                                    self.V(lambda e: e.tensor_scalar(out=xeT[:, fc, s0:s0 + cap], in0=self.pb[b][:, s0:s0 + cap], scalar1=sc_, scalar2=bi_,
                                                                     op0=ALU.mult, op1=ALU.add), r=["modA", "modc"], w=["xeT"], x=[self.bk(b)])

                def do_gateup(ex):
                        wg_d = self.dr["w_exp_gate"][l, ex].rearrange("(kc p) f -> p kc f", p=128)
                        wu_d = self.dr["w_exp_up"][l, ex].rearrange("(kc p) f -> p kc f", p=128)
                        for pc in range(FF // PW):
                            kb = cn["wcnt"] % NWB
                            cn["wcnt"] += 1
                            S.dma("pool", Wg[kb][:], wg_d[:, :, pc * PW:(pc + 1) * PW], writes=["eWg%d" % kb])
                            S.dma("pool", Wu[kb][:], wu_d[:, :, pc * PW:(pc + 1) * PW], writes=["eWu%d" % kb])
                            for fo in range(PW // 128):
                                fidx = pc * (PW // 128) + fo
                                ba, bu = (0, 1) if fidx % 2 == 0 else (2, 3)
                                for kc in range(KC):
                                    self.P(lambda e: e.matmul(self.pb[ba][:, :NS], lhsT=Wg[kb][:, kc, fo * 128:(fo + 1) * 128], rhs=xeT[:, kc, :],
                                                              start=(kc == 0), stop=(kc == KC - 1)), r=["eWg%d" % kb, "xeT"], x=[self.bk(ba)])
                                for kc in range(KC):
                                    self.P(lambda e: e.matmul(self.pb[bu][:, :NS], lhsT=Wu[kb][:, kc, fo * 128:(fo + 1) * 128], rhs=xeT[:, kc, :],
                                                              start=(kc == 0), stop=(kc == KC - 1)), r=["eWu%d" % kb, "xeT"], x=[self.bk(bu)])
                                sa_ = sa[fidx % 2]
                                self.A(lambda e: e.activation(out=sa_[:], in_=self.pb[ba][:, :NS], func=AF.Silu), w=["sa%d" % (fidx % 2)], x=[self.bk(ba)])
                                self.V(lambda e: e.tensor_tensor(out=actT[:, fidx, :], in0=sa_[:], in1=self.pb[bu][:, :NS], op=ALU.mult),
                                       r=["sa%d" % (fidx % 2)], w=["actT"], x=[self.bk(bu)])

                def do_rest(ex):
                        wd_d = self.dr["w_exp_down"][l, ex].rearrange("(fc p) d -> p fc d", p=128)
                        for pc in range(FC // DP):
                            kb = cn["dcnt"] % NWB
                            cn["dcnt"] += 1
                            S.dma("pool", Wd[kb][:], wd_d[:, pc * DP:(pc + 1) * DP, :], writes=["eWd%d" % kb])
                            for f2 in range(DP):
                                fc = pc * DP + f2
                                for k_st, (s_start, nn, si) in enumerate(stiles):
                                    for hf in range(NH):
                                        b = k_st * NH + hf
                                        self.P(lambda e: e.matmul(self.pb[b][:nn, :512], lhsT=actT[:, fc, s_start:s_start + nn],
                                                                  rhs=Wd[kb][:, f2, hf * 512:(hf + 1) * 512], start=(fc == 0), stop=(fc == FC - 1)),
                                               r=["actT", "eWd%d" % kb], x=[self.bk(b)])
                        for k_st, (s_start, nn, si) in enumerate(stiles):
                            w_ = sets[si]["w"]
                            for hf in range(NH):
                                b = k_st * NH + hf
                                self.V(lambda e: e.tensor_tensor(out=ye[:nn, k_st, hf * 512:(hf + 1) * 512], in0=self.pb[b][:nn, :512],
                                                                 in1=self.grow[:nn, w_, hf * 512:(hf + 1) * 512], op=ALU.mult),
                                       r=["grow"], w=["ye"], x=[self.bk(b)])
                        for si, st in enumerate(sets):
                            c0, n = st["t0"] * 128, st["nt"] * 128
                            mine = [(k_st, s_start, nn) for k_st, (s_start, nn, sj) in enumerate(stiles) if sj == si]
                            for g0 in range(c0, c0 + n, 512):
                                gn = min(512, c0 + n - g0)
                                kb = cn["scnt"] % 2
                                cn["scnt"] += 1
                                for tt in range(gn // 128):
                                    ti = g0 // 128 + tt
                                    rk = cn["rcnt"] % 2
                                    cn["rcnt"] += 1
                                    self.G(lambda e: e.tensor_copy(out=repR[rk][:], in_=rankTok[:, ti, ex:ex + 1].to_broadcast([128, 128])),
                                           r=["rankTok"], w=["repR%d" % rk])
                                    self.G(lambda e: e.tensor_copy(out=repG[rk][:], in_=LG[:, ti, ex:ex + 1].to_broadcast([128, 128])),
                                           r=["LG"], w=["repG%d" % rk])
                                    self.P(lambda e: e.matmul(self.pb[6][:, tt * 128:(tt + 1) * 128], lhsT=repR[rk][:], rhs=identf, start=True, stop=True),
                                           r=["repR%d" % rk, "consts"], x=[self.bk(6)])
                                    self.P(lambda e: e.matmul(self.pb[7][:, tt * 128:(tt + 1) * 128], lhsT=repG[rk][:], rhs=identf, start=True, stop=True),
                                           r=["repG%d" % rk, "consts"], x=[self.bk(7)])
                                self.A(lambda e: e.copy(out=gsb[kb][:, :gn], in_=self.pb[7][:, :gn]), w=["gsb%d" % kb], x=[self.bk(7)])
                                for (k_st, s_start, nn) in mine:
                                    self.V(lambda e: e.scalar_tensor_tensor(out=SelT[kb][:, k_st, :gn], in0=self.pb[6][:, :gn], scalar=iopj[:, k_st:k_st + 1],
                                                                            in1=gsb[kb][:, :gn], op0=ALU.is_equal, op1=ALU.mult),
                                           r=["iopj", "gsb%d" % kb], w=["SelT%d" % kb], x=[self.bk(6)])
                                for tt in range(gn // 128):
                                    ti = g0 // 128 + tt
                                    xt = self.src_tile(ti)
                                    for hf in range(NH):
                                        b = cn["ocnt"] % 4
                                        cn["ocnt"] += 1
                                        for idx, (k_st, s_start, nn) in enumerate(mine):
                                            self.P(lambda e: e.matmul(self.pb[b][:, :512], lhsT=SelT[kb][:nn, k_st, tt * 128:(tt + 1) * 128],
                                                                      rhs=ye[:nn, k_st, hf * 512:(hf + 1) * 512], start=(idx == 0), stop=(idx == len(mine) - 1)),
                                                   r=["SelT%d" % kb, "ye"], x=[self.bk(b)])
                                        self.V(lambda e: e.tensor_tensor(out=xt[:, hf * 512:(hf + 1) * 512], in0=xt[:, hf * 512:(hf + 1) * 512],
                                                                         in1=self.pb[b][:, :512], op=ALU.add), w=["x%d" % ti], x=[self.bk(b)])

                do_gather(0)
                for ex in range(E):
                    do_gateup(ex)
                    if ex + 1 < E:
                        do_gather(ex + 1)
                    do_rest(ex)
                S.barrier()

    def final(self):
        cfg, S = self.cfg, self.S
        D, NT = cfg.D, cfg.NT
        with ExitStack() as es:
            ss = self.sb(es, "fss", [128, NT], F32)
            rstd = self.sb(es, "frstd", [128, NT], F32)
            junk = self.sb(es, "fjunk", [128, D], BF16)
            ot = [self.sb(es, "fo%d" % i, [128, D], F32) for i in range(2)]
            fnr = self.sb(es, "fnr", [128, D], F32)
            S.dma("sp", fnr[:], self.dr["fnrow"], writes=["fnr"])
            for i in range(NT):
                self.A(lambda e: e.activation(out=junk[:], in_=self.x_sb[:, i, :], func=AF.Square,
                                              accum_out=ss[:, i:i + 1]), r=["x%d" % i], w=["fjunk", "fss"])
            self.V(lambda e: e.tensor_scalar(out=rstd[:], in0=ss[:], scalar1=1.0 / D, scalar2=EPS,
                                             op0=ALU.mult, op1=ALU.add), r=["fss"], w=["frstd"])
            self.A(lambda e: e.activation(out=rstd[:], in_=rstd[:], func=AF.Sqrt), r=[], w=["frstd"])
            self.V(lambda e: e.reciprocal(out=rstd[:], in_=rstd[:]), r=[], w=["frstd"])
            for i in range(NT):
                o = ot[i % 2]
                self.V(lambda e: e.scalar_tensor_tensor(out=o[:], in0=self.x_sb[:, i, :], scalar=rstd[:, i:i + 1],
                                                        in1=fnr[:], op0=ALU.mult, op1=ALU.mult),
                       r=["x%d" % i, "frstd", "fnr"], w=["fo%d" % (i % 2)])
                S.dma("sp", self.y[i * 128:(i + 1) * 128, :], o[:], reads=["fo%d" % (i % 2)], writes=["y%d" % i])
            S.barrier()


def host_packs(cfg, inp, b):
    D, KC, DEPTH = cfg.D, cfg.KC, cfg.DEPTH
    cols = np.zeros((128, cfg.NCOL), np.float32)

    def colset(name, v):
        o, w = cfg.coff[name]
        cols[:, o:o + w] = np.asarray(v, np.float32).reshape(w, 128).T
    colset("c", inp["c"][b])
    colset("cctx", inp["c_ctx"])
    for l in range(DEPTH):
        colset("bada%d" % l, inp["b_ada"][l])
        colset("n1%d" % l, inp["norm1_w"][l])
        colset("n2%d" % l, inp["norm2_w"][l])
        colset("conv%d" % l, np.asarray(inp["mlstm_conv_w"][l]).reshape(-1))
        qn = np.asarray(inp["gqa_qnorm_w"][l], np.float32)
        kn = np.asarray(inp["gqa_knorm_w"][l], np.float32)
        perm = np.concatenate([np.arange(32, 64), np.arange(0, 32)])
        o, _ = cfg.coff["qnc%d" % l]
        cols[:, o] = np.tile(qn, 2)
        cols[:, o + 1] = np.tile(qn[perm], 2)
        o, _ = cfg.coff["knc%d" % l]
        cols[:, o] = np.tile(kn, 2)
        cols[:, o + 1] = np.tile(kn[perm], 2)
    cols[:, cfg.coff["iop"][0]] = np.arange(128)
    cols[:, cfg.coff["eps"][0]] = EPS
    rowsL = np.zeros((DEPTH, 128, cfg.NROWL), np.float32)
    rowsG = np.zeros((128, cfg.NROWG), np.float32)
    brows = np.zeros((DEPTH, 128, 6 * D), np.float32)

    def rowset(arr, name, v):
        o, w = cfg.roff[name]
        arr[:, o:o + w] = np.asarray(v, np.float32).reshape(1, w)
    for l in range(DEPTH):
        rowset(rowsL[l], "sub", inp["diff_subln_w"][l])
        rowset(rowsL[l], "cn", inp["mlstm_norm_w"][l])
        rowset(rowsL[l], "gb", inp["mlstm_gate_b"][l])
        rowset(rowsL[l], "lam", np.asarray(inp["diff_lambda"][l]).reshape(-1))
        brows[l] = np.asarray(inp["b_ada"][l], np.float32).reshape(1, 6 * D)
    rowset(rowsG, "iof", np.arange(256))
    rows = (rowsL, rowsG, brows)
    return cols, rows


def make_in_maps(cfg, inp, cores):
    cosT, sinT = rope_tables(cfg)
    ropeT = np.concatenate([cosT, sinT], axis=1)
    consts = const_pack()
    E = cfg.E
    esel = np.zeros((E, E * 128), np.float32)
    for e in range(E):
        esel[e, e * 128:(e + 1) * 128] = 1.0
    shared = {k: np.ascontiguousarray(np.asarray(inp[k], np.float32)) for k in
              ("w_ada", "w_in", "w_branch_a", "w_branch_b", "w_branch_c", "w_out", "w_router",
               "w_exp_gate", "w_exp_up", "w_exp_down")}
    maps = []
    for b in cores:
        cols, rows = host_packs(cfg, inp, b)
        m = {"x": np.ascontiguousarray(inp["x"][b], np.float32), "ctx": np.ascontiguousarray(inp["ctx"][b], np.float32),
             "cols": cols, "rowsL": rows[0], "rowsG": rows[1], "brows": rows[2], "fnrow": np.ascontiguousarray(np.broadcast_to(np.asarray(inp["final_norm_w"], np.float32).reshape(1, -1), (128, cfg.D))), "consts": consts, "ropeT": ropeT, "esel": esel}
        m.update(shared)
        maps.append(m)
    return maps


_CACHE = {}


def kernel(**inputs):
    cfg = Cfg()
    if "nc" not in _CACHE:
        _CACHE["nc"] = Builder(cfg).build()
    nc = _CACHE["nc"]
    inp = {k: np.asarray(v) for k, v in inputs.items()}
    n = inp["x"].shape[0]
    in_maps = make_in_maps(cfg, inp, list(range(n)))
    res = run_bass_kernel_spmd(nc, in_maps, core_ids=list(range(n)))
    return np.stack([np.asarray(r["y"], np.float32) for r in res.results], axis=0)
```

```python
import math
from contextlib import ExitStack

import numpy as np
import concourse.bass as bass
import concourse.mybir as mybir
from concourse.bass_utils import run_bass_kernel_spmd

F32 = mybir.dt.float32
BF16 = mybir.dt.bfloat16
AF = mybir.ActivationFunctionType
ALU = mybir.AluOpType
AX = mybir.AxisListType
EPS = 1e-6


class Sched:
    def __init__(self, nc, n_dma_sems=32, same_engine_sync=True):
        self.nc = nc
        self.eng = {"pe": nc.tensor, "dve": nc.vector, "act": nc.scalar, "pool": nc.gpsimd, "sp": nc.sync}
        self.sem = {k: nc.alloc_semaphore(name="s_" + k) for k in self.eng}
        self.cnt = {k: 0 for k in self.eng}
        self.waited = {k: {} for k in self.eng}
        self.dsem = [nc.alloc_semaphore(name="d%d" % i) for i in range(2 * n_dma_sems)]
        self.dcnt = [0] * (2 * n_dma_sems)
        self.nds = n_dma_sems
        self.drr = [0, 0]
        self.tok = {}
        self.same = same_engine_sync
        self.n_inst = 0
        self.n_wait = 0

    def _st(self, t):
        s = self.tok.get(t)
        if s is None:
            s = self.tok[t] = [None, []]
        return s

    def _wait(self, engname, deps):
        need = {}
        for ev, skip_same in deps:
            if ev is None:
                continue
            sem, val, src = ev
            if src == engname and (skip_same or not self.same or engname == "pe"):
                continue
            k = sem.num
            if k not in need or need[k][1] < val:
                need[k] = (sem, val)
        e = self.eng[engname]
        w = self.waited[engname]
        for k, (sem, val) in need.items():
            if w.get(k, 0) < val:
                e.wait_ge(sem, val)
                w[k] = val
                self.n_wait += 1

    def _deps(self, reads, writes, excl):
        deps = []
        for t in reads:
            deps.append((self._st(t)[0], False))
        for t in writes:
            s = self._st(t)
            deps.append((s[0], False))
            deps.extend((r, False) for r in s[1])
        for t in excl:
            s = self._st(t)
            deps.append((s[0], True))
        return deps

    def _commit(self, ev, reads, writes, excl):
        for t in reads:
            self._st(t)[1].append(ev)
        for t in writes:
            s = self._st(t)
            s[0] = ev
            s[1] = []
        for t in excl:
            s = self._st(t)
            s[0] = ev
            s[1] = []

    def op(self, engname, fn, reads=(), writes=(), excl=()):
        self._wait(engname, self._deps(reads, writes, excl))
        inst = fn(self.eng[engname])
        self.cnt[engname] += 1
        ev = (self.sem[engname], self.cnt[engname], engname)
        inst.then_inc(ev[0], 1)
        self._commit(ev, reads, writes, excl)
        self.n_inst += 1
        return ev

    def dma(self, queue, out, in_, reads=(), writes=(), **kw):
        self._wait(queue, self._deps(reads, writes, ()))
        q = 1 if queue == "pool" else 0
        i = q * self.nds + self.drr[q]
        self.drr[q] = (self.drr[q] + 1) % self.nds
        inst = self.eng[queue].dma_start(out=out, in_=in_, **kw)
        self.dcnt[i] += 16
        ev = (self.dsem[i], self.dcnt[i], "dma")
        inst.then_inc(ev[0], 16)
        self._commit(ev, reads, writes, ())
        self.n_inst += 1
        return ev

    def wait_all(self, engname):
        deps = [((self.sem[k], self.cnt[k], k), False) for k in self.eng if self.cnt[k] > 0 and k != engname]
        deps += [((self.dsem[i], self.dcnt[i], "dma"), False) for i in range(len(self.dsem)) if self.dcnt[i] > 0]
        self._wait(engname, deps)

    def barrier(self):
        for k in ("pe", "dve", "act", "pool", "sp"):
            self.wait_all(k)
        self.tok = {}


class Cfg:
    def __init__(s, D=1024, L=2048, LC=256, E=16, FF=2048, DEPTH=2, GW=64):
        s.D, s.L, s.LC, s.E, s.FF, s.DEPTH, s.GW = D, L, LC, E, FF, DEPTH, GW
        s.KC = D // 128
        s.NT = L // 128
        s.NTC = LC // 128
        s.NTT = s.NT + s.NTC
        s.NTOK = s.NTT * 128
        MW = s.MW = D // 2
        s.HA = MW // 128
        s.HBQ = MW // 64
        s.GRP = s.HBQ // 2
        s.HC = MW // 128
        s.FC = FF // 128
        s.CAPL = 2 * L // E
        s.CAPC = 2 * LC // E
        s.oAq, s.oAk, s.oAv, s.oBq = 0, MW, 2 * MW, 3 * MW
        s.oBk, s.oBv = 4 * MW, 4 * MW + 128
        s.oCq, s.oCk, s.oCv, s.oCo = 4 * MW + 256, 5 * MW + 256, 6 * MW + 256, 7 * MW + 256
        s.oG = 8 * MW + 256
        s.oMG = s.oG + 4 * s.HC
        s.INC = s.oMG + 3 * D
        off = {}
        n = 0

        def add(name, w):
            nonlocal n
            off[name] = (n, w)
            n += w
        add("c", s.KC)
        add("cctx", s.KC)
        for l in range(DEPTH):
            add("bada%d" % l, 6 * s.KC)
            add("n1%d" % l, s.KC)
            add("n2%d" % l, s.KC)
            add("conv%d" % l, 3 * 2 * s.HC)
            add("qnc%d" % l, 2)
            add("knc%d" % l, 2)
        add("cosT", 0)
        add("iop", 1)
        add("eps", 1)
        s.coff, s.NCOL = off, n
        roff = {}
        n = 0

        def addr(name, w):
            nonlocal n
            roff[name] = (n, w)
            n += w
        addr("sub", 128)
        addr("cn", MW)
        addr("gb", 4 * s.HC)
        addr("lam", 256)
        s.NROWL = n
        n = 0
        addr("iof", 256)
        s.roff, s.NROWG = roff, n


def rope_tables(cfg):
    L, GW = cfg.L, cfg.GW
    t = np.arange(L)
    rows = (t // GW).astype(np.float32)
    cols = (t % GW).astype(np.float32)
    nf = 16
    inv = (10000.0 ** (-np.arange(nf, dtype=np.float32) / nf)).astype(np.float32)
    ang = np.concatenate([rows[:, None] * inv, cols[:, None] * inv], axis=-1).astype(np.float32)
    cos = np.cos(ang).astype(np.float32)
    sin = np.sin(ang).astype(np.float32)
    cosT = np.zeros((128, L), np.float32)
    sinT = np.zeros((128, L), np.float32)
    for p in range(128):
        d = p % 64
        f = d % 32
        cosT[p] = cos[:, f]
        sinT[p] = -sin[:, f] if d < 32 else sin[:, f]
    return cosT, sinT


def const_pack():
    r = np.arange(128)
    ident = np.eye(128, dtype=np.float32)
    triU = (r[:, None] <= r[None, :]).astype(np.float32)
    triL = (r[:, None] >= r[None, :]).astype(np.float32)
    sU = (r[:, None] < r[None, :]).astype(np.float32)
    sL = (r[:, None] > r[None, :]).astype(np.float32)
    ones = np.ones((128, 128), np.float32)
    blk = (r[:, None] // 64 == r[None, :] // 64).astype(np.float32)
    return np.concatenate([ident, triU, triL, sU, sL, ones, blk], axis=1)


CI, CTU, CTL, CSU, CSL, CON, CBK = range(7)


class Builder:
    def __init__(self, cfg, taps=None, stop_after=None):
        self.cfg = cfg
        self.taps = taps or {}
        self.stop_after = stop_after
        self.nc = bass.Bass("TRN2", target_bir_lowering=False)
        self.S = None

    def sb(self, es, name, shape, dt):
        self._uid = getattr(self, "_uid", 0) + 1
        return es.enter_context(self.nc.sbuf_tensor("%s_%d" % (name, self._uid), list(shape), dt))

    def V(self, fn, r=(), w=(), x=()):
        return self.S.op("dve", fn, r, w, x)

    def A(self, fn, r=(), w=(), x=()):
        return self.S.op("act", fn, r, w, x)

    def G(self, fn, r=(), w=(), x=()):
        return self.S.op("pool", fn, r, w, x)

    def P(self, fn, r=(), w=(), x=()):
        return self.S.op("pe", fn, r, w, x)

    def bk(self, i):
        return "pb%d" % i

    def cst(self, k):
        return self.consts[:, k * 128:(k + 1) * 128]

    def col(self, name, a=0, b=None):
        o, w = self.cfg.coff[name]
        if b is None:
            b = w
        return self.cols[:, o + a:o + b]

    def row(self, name, a=0, b=None):
        o, w = self.cfg.roff[name]
        if b is None:
            b = w
        t = self.rowsG if name in ("iof",) else self.rowsL
        return t[:, o + a:o + b]

    def tap(self, name, ap_sb, reads):
        if name not in self.taps:
            return
        shape = list(ap_sb.shape)
        d = self.nc.dram_tensor("tap_" + name, shape, ap_sb.dtype, kind="ExternalOutput").ap()
        self.S.dma("sp", d, ap_sb, reads=reads, writes=["tapd_" + name])

    def build(self):
        cfg, nc = self.cfg, self.nc
        D, L, LC, E, FF, DEPTH = cfg.D, cfg.L, cfg.LC, cfg.E, cfg.FF, cfg.DEPTH
        KC, NT, NTC, NTT = cfg.KC, cfg.NT, cfg.NTC, cfg.NTT
        dr = {}

        def din(name, shape, dt=F32):
            dr[name] = nc.dram_tensor(name, list(shape), dt, kind="ExternalInput").ap()
            return dr[name]
        din("x", [L, D])
        din("ctx", [LC, D])
        din("cols", [128, cfg.NCOL])
        din("rowsL", [DEPTH, 128, cfg.NROWL])
        din("rowsG", [128, cfg.NROWG])
        din("brows", [DEPTH, 128, 6 * D])
        din("fnrow", [128, D])
        din("consts", [128, 7 * 128])
        din("ropeT", [128, 2 * L])
        din("esel", [E, E * 128])
        din("w_ada", [DEPTH, D, 6 * D])
        din("w_in", [DEPTH, D, cfg.INC])
        din("w_branch_a", [DEPTH, cfg.MW, D])
        din("w_branch_b", [DEPTH, cfg.MW, D])
        din("w_branch_c", [DEPTH, cfg.MW, D])
        din("w_out", [DEPTH, D, D])
        din("w_router", [DEPTH, D, E])
        din("w_exp_gate", [DEPTH, E, D, FF])
        din("w_exp_up", [DEPTH, E, D, FF])
        din("w_exp_down", [DEPTH, E, FF, D])
        self.dr = dr
        self.y = nc.dram_tensor("y", [L, D], F32, kind="ExternalOutput").ap()
        self.S = Sched(nc)
        S = self.S
        with ExitStack() as es:
            self.pb = [es.enter_context(nc.psum_tensor("pb%d" % i, [128, 512], F32)) for i in range(8)]
            self.x_sb = self.sb(es, "x_sb", [128, NT, D], F32)
            self.c_sb = self.sb(es, "c_sb", [128, NTC, D], F32)
            self.cols = self.sb(es, "cols_sb", [128, cfg.NCOL], F32)
            self.rowsG = self.sb(es, "rowsG_sb", [128, cfg.NROWG], F32)
            self.consts = self.sb(es, "consts_sb", [128, 7 * 128], F32)
            self.identb = self.sb(es, "identb", [128, 128], BF16)
            self.silc = self.sb(es, "silc", [128, KC, 2], F32)
            self.modc = self.sb(es, "modc", [128, 6 * KC, 2], F32)
            self.modA = self.sb(es, "modA", [128, 2, KC, 2], F32)
            self.grow = self.sb(es, "grow", [128, 2, D], F32)
            S.dma("sp", self.cols[:], dr["cols"], writes=["cols"])
            S.dma("sp", self.rowsG[:], dr["rowsG"], writes=["rowsG"])
            S.dma("sp", self.consts[:], dr["consts"], writes=["consts"])
            for i in range(NT):
                S.dma("sp", self.x_sb[:, i, :], dr["x"][i * 128:(i + 1) * 128, :], writes=["x%d" % i])
            for i in range(NTC):
                S.dma("sp", self.c_sb[:, i, :], dr["ctx"][i * 128:(i + 1) * 128, :], writes=["x%d" % (NT + i)])
            self.V(lambda e: e.tensor_copy(out=self.identb[:], in_=self.cst(CI)), r=["consts"], w=["identb"])
            self.A(lambda e: e.activation(out=self.silc[:, :, 0], in_=self.col("c"), func=AF.Silu), r=["cols"], w=["silc"])
            self.A(lambda e: e.activation(out=self.silc[:, :, 1], in_=self.col("cctx"), func=AF.Silu), r=["cols"], w=["silc"])
            for l in range(DEPTH):
                self.layer(l)
                if self.stop_after is not None and self.stop_after[0] == l:
                    break
            self.final()
            S.barrier()
        return nc

    def src_tile(self, i):
        return self.x_sb[:, i, :] if i < self.cfg.NT else self.c_sb[:, i - self.cfg.NT, :]

    def phase_mod(self, l, rowsec, do_cols):
        cfg, S = self.cfg, self.S
        D, KC = cfg.D, cfg.KC
        wa_d = self.dr["w_ada"][l].rearrange("(kc p) f -> p kc f", p=128)
        npiece = 6 * D // 512
        with ExitStack() as es:
            wa = [self.sb(es, "wa%d" % i, [128, KC, 512], F32) for i in range(2)]
            rep = self.sb(es, "rep", [128, KC, 2, 128], F32)
            brow = self.sb(es, "brow", [128, D], F32)
            S.dma("sp", brow[:], self.dr["brows"][l][:, rowsec * D:(rowsec + 1) * D], writes=["brow"])
            for kc in range(KC):
                for w_ in range(2):
                    self.V(lambda e: e.tensor_copy(out=rep[:, kc, w_, :], in_=self.silc[:, kc, w_:w_ + 1].to_broadcast([128, 128])),
                           r=["silc"], w=["rep"])
            jj = 0
            for j in range(npiece):
                sec = (j * 512) // D
                off = j * 512 - sec * D
                if not do_cols and sec != rowsec:
                    continue
                buf = wa[jj % 2]
                tk = "wa%d" % (jj % 2)
                pbk = self.pb[jj % 2]
                bkt = self.bk(jj % 2)
                jj += 1
                S.dma("sp", buf[:], wa_d[:, :, j * 512:(j + 1) * 512], writes=[tk])
                if do_cols:
                    for s_ in range(4):
                        for kc in range(KC):
                            self.P(lambda e: e.matmul(pbk[:, s_ * 2:s_ * 2 + 2], lhsT=buf[:, kc, s_ * 128:(s_ + 1) * 128],
                                                      rhs=self.silc[:, kc, :], start=(kc == 0), stop=(kc == KC - 1)),
                                   r=[tk, "silc"], x=[bkt])
                    o, _ = cfg.coff["bada%d" % l]
                    self.V(lambda e: e.tensor_tensor(
                        out=self.modc[:, j * 4:(j + 1) * 4, :],
                        in0=pbk[:, 0:8].rearrange("p (a b) -> p a b", b=2),
                        in1=self.cols[:, o + j * 4:o + (j + 1) * 4].unsqueeze(2).to_broadcast([128, 4, 2]),
                        op=ALU.add), r=["cols"], w=["modc"], x=[bkt])
                if sec == rowsec:
                    for w_ in range(2):
                        pb2 = self.pb[2 + w_]
                        for kc in range(KC):
                            self.P(lambda e: e.matmul(pb2[:, :], lhsT=rep[:, kc, w_, :], rhs=buf[:, kc, :],
                                                      start=(kc == 0), stop=(kc == KC - 1)),
                                   r=[tk, "rep"], x=[self.bk(2 + w_)])
                        self.V(lambda e: e.tensor_tensor(out=self.grow[:, w_, off:off + 512], in0=pb2[:, :],
                                                         in1=brow[:, off:off + 512], op=ALU.add),
                               r=["brow"], w=["grow"], x=[self.bk(2 + w_)])
            if do_cols:
                for ni, (nname, scsec) in enumerate((("n1%d" % l, 1), ("n2%d" % l, 4))):
                    self.V(lambda e: e.scalar_tensor_tensor(
                        out=self.modA[:, ni, :, :], in0=self.modc[:, scsec * KC:(scsec + 1) * KC, :], scalar=1.0,
                        in1=self.col(nname).unsqueeze(2).to_broadcast([128, KC, 2]), op0=ALU.add, op1=ALU.mult),
                        r=["modc", "cols"], w=["modA"])
            S.barrier()

    def phase_norm(self, es, l, ni, xs, hT, with_ctx=True):
        cfg, S = self.cfg, self.S
        D, KC, NT, NTT = cfg.D, cfg.KC, cfg.NT, cfg.NTT
        shsec = 0 if ni == 0 else 3
        ntl = NTT if with_ctx else NT
        with ExitStack() as es2:
            ss = self.sb(es2, "nss", [128, NTT], F32)
            rstd = self.sb(es2, "nrstd", [128, NTT], F32)
            junk = self.sb(es2, "njunk", [128, D], BF16)
            for i in range(ntl):
                self.A(lambda e: e.activation(out=junk[:], in_=self.src_tile(i), func=AF.Square,
                                              accum_out=ss[:, i:i + 1]), r=["x%d" % i], w=["njunk", "nss"])
            self.V(lambda e: e.tensor_scalar(out=rstd[:, :ntl], in0=ss[:, :ntl], scalar1=1.0 / D, scalar2=EPS,
                                             op0=ALU.mult, op1=ALU.add), r=["nss"], w=["nrstd"])
            self.A(lambda e: e.activation(out=rstd[:, :ntl], in_=rstd[:, :ntl], func=AF.Sqrt), r=[], w=["nrstd"])
            self.V(lambda e: e.reciprocal(out=rstd[:, :ntl], in_=rstd[:, :ntl]), r=[], w=["nrstd"])
            for i in range(ntl):
                self.V(lambda e: e.tensor_scalar(out=xs[:, i, :], in0=self.src_tile(i), scalar1=rstd[:, i:i + 1],
                                                 scalar2=None, op0=ALU.mult), r=["x%d" % i, "nrstd"], w=["xs%d" % i])
            groups = [(g, min(4, NT - g), 0) for g in range(0, NT, 4)]
            if with_ctx:
                groups += [(NT + g, min(4, cfg.NTC - g), 1) for g in range(0, cfg.NTC, 4)]
            n = 0
            for fc in range(KC):
                for (t0, nt_, w_) in groups:
                    b = n % 2
                    n += 1
                    pv = self.pb[b][:].bitcast(BF16)
                    for k in range(nt_):
                        self.P(lambda e: e.transpose(out=pv[:, k * 128:(k + 1) * 128],
                                                     in_=xs[:, t0 + k, fc * 128:(fc + 1) * 128], identity=self.identb[:]),
                               r=["xs%d" % (t0 + k), "identb"], x=[self.bk(b)])
                    dst = hT[:, fc, t0 * 128:(t0 + nt_) * 128]
                    sc = self.modA[:, ni, fc, w_:w_ + 1]
                    bi = self.modc[:, shsec * KC + fc, w_:w_ + 1]
                    if n % 2 == 0:
                        self.A(lambda e: e.activation(out=dst, in_=pv[:, :nt_ * 128], func=AF.Identity, scale=sc, bias=bi),
                               r=["modA", "modc"], w=["hT%d" % fc], x=[self.bk(b)])
                    else:
                        self.V(lambda e: e.tensor_scalar(out=dst, in0=pv[:, :nt_ * 128], scalar1=sc, scalar2=bi,
                                                         op0=ALU.mult, op1=ALU.add),
                               r=["modA", "modc"], w=["hT%d" % fc], x=[self.bk(b)])
            S.barrier()

    def layer(self, l):
        cfg = self.cfg
        with_ctx = l < cfg.DEPTH - 1
        self.phase_mod(l, 2, True)
        if self.stop_after == (l, "mod"):
            return
        with ExitStack() as es:
            hT = self.sb(es, "hT", [128, cfg.KC, cfg.NTOK], BF16)
            with ExitStack() as e0:
                xs = self.sb(e0, "xs", [128, cfg.NTT, cfg.D], BF16)
                self.phase_norm(e0, l, 0, xs, hT, True)
            self.tap("hT%d" % l, hT[:], ["hT%d" % fc for fc in range(cfg.KC)])
            if self.stop_after == (l, "norm1"):
                return
            self.phase_mix(l, hT, with_ctx)
        if self.stop_after is not None and self.stop_after[0] == l and self.stop_after[1] != "ffn":
            return
        self.phase_ffn(l, with_ctx)

    def wload(self, dst, win, slices, tok):
        a = 0
        for (c0, n) in slices:
            self.S.dma("pool", dst[:, :, a:a + n], win[:, :, c0:c0 + n], writes=[tok])
            a += n

    def proj_fm(self, W, wtok, hT, col0, ncols, banks, consume):
        KC = self.cfg.KC
        gi = 0
        for g0 in range(col0, col0 + ncols, 512):
            gn = min(512, col0 + ncols - g0)
            b = banks[gi % len(banks)]
            gi += 1
            for kc in range(KC):
                self.P(lambda e: e.matmul(self.pb[b][:, :gn], lhsT=W[:, kc, :], rhs=hT[:, kc, g0:g0 + gn],
                                          start=(kc == 0), stop=(kc == KC - 1)),
                       r=[wtok, "hT%d" % kc], x=[self.bk(b)])
            consume(self.pb[b][:, :gn], g0, gn, b)

    def proj_tm(self, W, wtok, hT, ti, ncols, b):
        KC = self.cfg.KC
        for kc in range(KC):
            self.P(lambda e: e.matmul(self.pb[b][:, :ncols], lhsT=hT[:, kc, ti * 128:(ti + 1) * 128], rhs=W[:, kc, :ncols],
                                      start=(kc == 0), stop=(kc == KC - 1)),
                   r=[wtok, "hT%d" % kc], x=[self.bk(b)])

    def qk_chunk(self, l, es, hT, win, nat, dst, dtok, nrm, nq_cols, pf, mode="both"):
        cfg = self.cfg
        L, KC = cfg.L, cfg.KC
        if "qk_t1" not in es:
            for nm in ("qk_t1", "qk_t2", "qk_sq", "qk_rs"):
                es[nm] = self.sb(es["es"], nm, [128, 512], F32)
        if pf + "W" not in es:
            es[pf + "W"] = self.sb(es["es"], pf + "qkW", [128, KC, 128], BF16)
            es[pf + "Wp"] = self.sb(es["es"], pf + "qkWp", [128, KC, 128], BF16)
        W, Wp = es[pf + "W"], es[pf + "Wp"]
        wtok, wptok = pf + "qkW", pf + "qkWp"
        pf = ""
        perm = []
        for (c0, n) in nat:
            for a in range(0, n, 64):
                perm += [(c0 + a + 32, 32), (c0 + a, 32)]
        if mode in ("both", "load"):
            self.wload(W, win, nat, wtok)
            self.wload(Wp, win, perm, wptok)
        if mode == "load":
            return
        t1, t2, sq, rs = es["qk_t1"], es["qk_t2"], es["qk_sq"], es["qk_rs"]
        cosT, sinT = self.ropeT[:, 0:L], self.ropeT[:, L:2 * L]
        for g0 in range(0, nq_cols, 512):
            gn = min(512, nq_cols - g0)
            lat = g0 < L
            gi_ = g0 // 512
            bq, bp, bs_ = [0, 2, 4][gi_ % 3], [1, 3, 5][gi_ % 3], 6 + gi_ % 2
            for kc in range(KC):
                self.P(lambda e: e.matmul(self.pb[bq][:, :gn], lhsT=W[:, kc, :], rhs=hT[:, kc, g0:g0 + gn],
                                          start=(kc == 0), stop=(kc == KC - 1)), r=[wtok, "hT%d" % kc], x=[self.bk(bq)])
            if lat:
                for kc in range(KC):
                    self.P(lambda e: e.matmul(self.pb[bp][:, :gn], lhsT=Wp[:, kc, :], rhs=hT[:, kc, g0:g0 + gn],
                                              start=(kc == 0), stop=(kc == KC - 1)), r=[wptok, "hT%d" % kc], x=[self.bk(bp)])
            pq, pp = self.pb[bq][:, :gn], self.pb[bp][:, :gn]
            if nrm is not None:
                self.A(lambda e: e.activation(out=sq[:, :gn], in_=pq, func=AF.Square), w=[pf + "qk_sq"], x=[self.bk(bq)])
                self.P(lambda e: e.matmul(self.pb[bs_][:, :gn], lhsT=self.cst(CBK), rhs=sq[:, :gn], start=True, stop=True),
                       r=["consts", pf + "qk_sq"], x=[self.bk(bs_)])
                self.V(lambda e: e.tensor_scalar(out=rs[:, :gn], in0=self.pb[bs_][:, :gn], scalar1=1.0 / 64, scalar2=EPS,
                                                 op0=ALU.mult, op1=ALU.add), w=[pf + "qk_rs"], x=[self.bk(bs_)])
                self.A(lambda e: e.activation(out=rs[:, :gn], in_=rs[:, :gn], func=AF.Sqrt), w=[pf + "qk_rs"])
                self.V(lambda e: e.reciprocal(out=rs[:, :gn], in_=rs[:, :gn]), w=[pf + "qk_rs"])
                wc = self.col(nrm + "%d" % l)
                if lat:
                    self.V(lambda e: e.scalar_tensor_tensor(out=t1[:, :gn], in0=pq, scalar=wc[:, 0:1], in1=cosT[:, g0:g0 + gn],
                                                            op0=ALU.mult, op1=ALU.mult), r=["cols", "ropeT"], w=[pf + "qk_t1"], x=[self.bk(bq)])
                    self.V(lambda e: e.scalar_tensor_tensor(out=t2[:, :gn], in0=pp, scalar=wc[:, 1:2], in1=sinT[:, g0:g0 + gn],
                                                            op0=ALU.mult, op1=ALU.mult), r=["cols", "ropeT"], w=[pf + "qk_t2"], x=[self.bk(bp)])
                    self.G(lambda e: e.tensor_tensor(out=t1[:, :gn], in0=t1[:, :gn], in1=t2[:, :gn], op=ALU.add),
                           r=[pf + "qk_t2"], w=[pf + "qk_t1"])
                    self.V(lambda e: e.tensor_tensor(out=dst[:, g0:g0 + gn], in0=t1[:, :gn], in1=rs[:, :gn], op=ALU.mult),
                           r=[pf + "qk_t1", pf + "qk_rs"], w=[dtok])
                else:
                    self.V(lambda e: e.scalar_tensor_tensor(out=dst[:, g0:g0 + gn], in0=pq, scalar=wc[:, 0:1], in1=rs[:, :gn],
                                                            op0=ALU.mult, op1=ALU.mult), r=["cols", pf + "qk_rs"], w=[dtok], x=[self.bk(bq)])
            else:
                if lat:
                    self.V(lambda e: e.tensor_tensor(out=t1[:, :gn], in0=pq, in1=cosT[:, g0:g0 + gn], op=ALU.mult),
                           r=["ropeT"], w=[pf + "qk_t1"], x=[self.bk(bq)])
                    self.V(lambda e: e.tensor_tensor(out=t2[:, :gn], in0=pp, in1=sinT[:, g0:g0 + gn], op=ALU.mult),
                           r=["ropeT"], w=[pf + "qk_t2"], x=[self.bk(bp)])
                    self.G(lambda e: e.tensor_tensor(out=dst[:, g0:g0 + gn], in0=t1[:, :gn], in1=t2[:, :gn], op=ALU.add),
                           r=[pf + "qk_t1", pf + "qk_t2"], w=[dtok])
                else:
                    self.A(lambda e: e.copy(out=dst[:, g0:g0 + gn], in_=pq), w=[dtok], x=[self.bk(bq)])

    def attention(self, PT, QT, KT, Vaug, vw, qcol0, nq, ktiles, finish, tokQ, tokK, tokV, sbanks, accsets, dist, state, gq=512):
        spb = 512 // (vw + 1)
        for g0 in range(qcol0, qcol0 + nq, gq):
            gn = min(gq, qcol0 + nq - g0)
            nqt = gn // 128
            aset = accsets[state["gi"] % len(accsets)]
            state["gi"] += 1

            def acc(j, qt, aset=aset, nqt=nqt):
                s_ = j * nqt + qt
                bnk = aset[s_ // spb]
                return self.pb[bnk][:, (s_ % spb) * (vw + 1):(s_ % spb + 1) * (vw + 1)], bnk
            nb = (2 * nqt + spb - 1) // spb
            for b in range(nb):
                self.V(lambda e: e.memset(self.pb[aset[b]][:], 0.0), w=[self.bk(aset[b])])
            steps = [(kt, j) for kt in ktiles for j in range(2)]
            n = len(steps)

            def issue_S(i):
                kt, j = steps[i]
                sbk = sbanks[i % len(sbanks)]
                pbuf = i % len(PT)
                self.P(lambda e: e.matmul(self.pb[sbk][:, :gn], lhsT=KT[:, j, kt * 128:(kt + 1) * 128],
                                          rhs=QT[:, g0:g0 + gn], start=True, stop=True),
                       r=[tokQ, tokK], x=[self.bk(sbk)])
                self.A(lambda e: e.activation(out=PT[pbuf][:, :gn], in_=self.pb[sbk][:, :gn], func=AF.Exp, scale=0.125),
                       w=["PT%d" % pbuf], x=[self.bk(sbk)])

            def issue_PV(i):
                kt, j = steps[i]
                pbuf = i % len(PT)
                for qt in range(nqt):
                    ap_, bnk = acc(j, qt)
                    self.P(lambda e: e.matmul(ap_, lhsT=PT[pbuf][:, qt * 128:(qt + 1) * 128], rhs=Vaug(kt),
                                              start=False, stop=False, skip_group_check=True),
                           r=["PT%d" % pbuf, tokV], x=[self.bk(bnk)])
            for i in range(min(dist, n)):
                issue_S(i)
            pend = state.get("pending")
            for i in range(n):
                if i + dist < n:
                    issue_S(i + dist)
                issue_PV(i)
                if pend is not None and i == min(1, n - 1):
                    pend(1)
                if pend is not None and i == min(max(n // 2, 2), n - 1):
                    pend(2)

            def fin(phase, g0=g0, nqt=nqt, acc=acc):
                for qt in range(nqt):
                    (a0, b0), (a1, b1) = acc(0, qt), acc(1, qt)
                    finish(g0 // 128 + qt, a0, a1, [self.bk(b0), self.bk(b1)], qt, phase)
            state["pending"] = fin

    def pad_k(self, es, KT):
        NTOK = self.cfg.NTOK
        KTz = self.sb(es, "KTz", [128, 2, NTOK], BF16)
        self.G(lambda e: e.memset(KTz[64:128, 0, :], 0.0), w=["KTz"])
        self.G(lambda e: e.memset(KTz[0:64, 1, :], 0.0), w=["KTz"])
        self.A(lambda e: e.copy(out=KTz[0:64, 0, :], in_=KT[0:64, :]), r=["KT"], w=["KTz"])
        self.G(lambda e: e.tensor_copy(out=KTz[64:128, 1, :], in_=KT[64:128, :]), r=["KT"], w=["KTz"])
        return KTz

    def attention_flush(self, state):
        if state.get("pending") is not None:
            state["pending"](1)
            state["pending"](2)
            state["pending"] = None

    def out_transpose(self, tok_ap, ttok, oT, chunk, ti, bank=7):
        pv = self.pb[bank][:].bitcast(BF16)
        k = self._otn % 3
        self._otn += 1
        st = self.otst[k]
        self.P(lambda e: e.transpose(out=pv[:, 0:128], in_=tok_ap, identity=self.identb[:]), r=[ttok, "identb"], x=[self.bk(bank)])
        self.A(lambda e: e.copy(out=st[:], in_=pv[:, 0:128]), w=["otst%d" % k], x=[self.bk(bank)])
        self.S.dma("sp", self.oTd[chunk, :, ti * 128:(ti + 1) * 128], st[:], reads=["otst%d" % k], writes=["oTd"])

    def rstd_col(self, ss, n, rs_tok, r):
        self.V(lambda e: e.tensor_scalar(out=ss, in0=ss, scalar1=1.0 / n, scalar2=EPS, op0=ALU.mult, op1=ALU.add), r=r, w=[rs_tok])
        self.A(lambda e: e.activation(out=ss, in_=ss, func=AF.Ln), w=[rs_tok])
        self.A(lambda e: e.activation(out=ss, in_=ss, func=AF.Exp, scale=-0.5), w=[rs_tok])

    def phase_mix(self, l, hT, with_ctx):
        cfg, S = self.cfg, self.S
        D, L, LC, KC, NT, NTC, NTT, NTOK = cfg.D, cfg.L, cfg.LC, cfg.KC, cfg.NT, cfg.NTC, cfg.NTT, cfg.NTOK
        NB = cfg.HA
        win = self.dr["w_in"][l].rearrange("(kc p) c -> p kc c", p=128)
        lam_init = 0.8 - 0.6 * math.exp(-0.3 * l)
        nqc = NTOK if with_ctx else L
        allk = list(range(NTT))
        ctxk = list(range(NT, NTT))
        hTt = ["hT%d" % kc for kc in range(KC)]
        with ExitStack() as es:
            oT = None
            self.oTd = self.nc.dram_tensor("oTd%d" % l, [3 * NB, 128, NTOK], BF16).ap()
            self.otst = [self.sb(es, "otst%d" % i, [128, 128], BF16) for i in range(3)]
            self._otn = 0
            self.rowsL = self.sb(es, "rowsL", [128, cfg.NROWL], F32)
            S.dma("sp", self.rowsL[:], self.dr["rowsL"][l], writes=["rows"])
            lam = self.sb(es, "lam", [128, 4], F32)
            subw = self.sb(es, "subw", [128, 128], F32)
            ljunk = self.sb(es, "ljunk", [128, 64], F32)
            lr = self.row("lam")
            for i in range(2):
                self.V(lambda e: e.tensor_tensor(out=ljunk[:], in0=lr[:, 128 * i:128 * i + 64], in1=lr[:, 128 * i + 64:128 * i + 128],
                                                 op=ALU.mult), r=["rows"], w=["ljunk"])
                self.V(lambda e: e.reduce_sum(out=lam[:, i:i + 1], in_=ljunk[:], axis=AX.X), r=["ljunk"], w=["lam"])
            self.A(lambda e: e.activation(out=lam[:, 0:2], in_=lam[:, 0:2], func=AF.Exp), w=["lam"])
            self.V(lambda e: e.tensor_tensor(out=lam[:, 2:3], in0=lam[:, 1:2], in1=lam[:, 0:1], op=ALU.subtract), w=["lam"])
            self.V(lambda e: e.tensor_scalar(out=lam[:, 2:3], in0=lam[:, 2:3], scalar1=-lam_init, scalar2=None, op0=ALU.add), w=["lam"])
            self.V(lambda e: e.tensor_scalar(out=subw[:], in0=self.row("sub"), scalar1=1.0 - lam_init, scalar2=None,
                                             op0=ALU.mult), r=["rows"], w=["subw"])
            e_rope = ExitStack()
            self.ropeT = self.sb(e_rope, "ropeT", [128, 2 * L], F32)
            S.dma("sp", self.ropeT[:], self.dr["ropeT"], writes=["ropeT"])
            for h in range(cfg.HA):
                with ExitStack() as e2:
                    QT = self.sb(e2, "QT", [128, NTOK], BF16)
                    KT = self.sb(e2, "KT", [128, NTOK], BF16)
                    Va = self.sb(e2, "Va", [128, NTT, 129], BF16)
                    Wv = self.sb(e2, "Wv", [128, KC, 128], BF16)
                    qsc = {"es": e2}
                    self.qk_chunk(l, qsc, hT, win, [(cfg.oAq + h * 128, 128)], QT, "QT", None, nqc, "q", mode="load")
                    self.qk_chunk(l, qsc, hT, win, [(cfg.oAk + h * 128, 128)], KT, "KT", None, NTOK, "k", mode="load")
                    self.wload(Wv, win, [(cfg.oAv + h * 128, 128)], "Wv")
                    self.qk_chunk(l, qsc, hT, win, [(cfg.oAq + h * 128, 128)], QT, "QT", None, nqc, "q", mode="compute")
                    self.qk_chunk(l, qsc, hT, win, [(cfg.oAk + h * 128, 128)], KT, "KT", None, NTOK, "k", mode="compute")
                    self.G(lambda e: e.memset(Va[:, :, 128:129], 1.0), w=["Va"])
                    for ti in range(NTT):
                        b = 5 + ti % 2
                        self.proj_tm(Wv, "Wv", hT, ti, 128, b)
                        self.A(lambda e: e.copy(out=Va[:, ti, 0:128], in_=self.pb[b][:, :128]), w=["Va"], x=[self.bk(b)])
                    fs = [self.sb(e2, "fin%d" % i, [128, 132], F32) for i in range(4)]
                    ft = [self.sb(e2, "fint%d" % i, [128, 128], F32) for i in range(4)]
                    fo = [self.sb(e2, "fino%d" % i, [128, 128], BF16) for i in range(4)]
                    fj = self.sb(e2, "finj", [128, 128], F32)

                    def finishA(ti, a0, a1, btoks, k, phase, h=h):
                        sm, t1, ob = fs[k], ft[k], fo[k]
                        stok, ttok, otok = "fin%d" % k, "fint%d" % k, "fino%d" % k
                        if phase == 2:
                            self.out_transpose(ob[:], otok, oT, h, ti)
                            return
                        self.V(lambda e: e.reciprocal(out=sm[:, 128:129], in_=a0[:, 128:129]), w=[stok], x=btoks)
                        self.V(lambda e: e.reciprocal(out=sm[:, 129:130], in_=a1[:, 128:129]), w=[stok], x=btoks)
                        self.V(lambda e: e.tensor_tensor(out=sm[:, 129:130], in0=sm[:, 129:130], in1=lam[:, 2:3], op=ALU.mult),
                               r=["lam"], w=[stok])
                        self.V(lambda e: e.tensor_scalar(out=t1[:], in0=a1[:, 0:128], scalar1=sm[:, 129:130], scalar2=None,
                                                         op0=ALU.mult), r=[stok], w=[ttok], x=btoks)
                        self.V(lambda e: e.scalar_tensor_tensor(out=sm[:, 0:128], in0=a0[:, 0:128], scalar=sm[:, 128:129], in1=t1[:],
                                                                op0=ALU.mult, op1=ALU.add), r=[ttok], w=[stok], x=btoks)
                        self.V(lambda e: e.tensor_tensor(out=fj[:], in0=sm[:, 0:128], in1=sm[:, 0:128], op=ALU.mult), r=[stok], w=["finj"])
                        self.V(lambda e: e.reduce_sum(out=sm[:, 130:131], in_=fj[:], axis=AX.X), r=["finj"], w=[stok + "s"])
                        self.rstd_col(sm[:, 130:131], 128, stok + "s", [])
                        self.V(lambda e: e.scalar_tensor_tensor(out=ob[:], in0=sm[:, 0:128], scalar=sm[:, 130:131], in1=subw[:],
                                                                op0=ALU.mult, op1=ALU.mult), r=[stok, stok + "s", "subw"], w=[otok])
                    PT = [self.sb(e2, "PT%d" % i, [128, 512], BF16) for i in range(4)]
                    KTz = self.pad_k(e2, KT)
                    ast = {"gi": 0, "pending": None}
                    akw = dict(sbanks=[0, 1, 6], accsets=[[2, 3], [4, 5]], dist=2, state=ast, gq=384)
                    self.attention(PT, QT, KTz, lambda kt: Va[:, kt, :], 128, 0, L, allk, finishA, "QT", "KTz", "Va", **akw)
                    if with_ctx:
                        self.attention(PT, QT, KTz, lambda kt: Va[:, kt, :], 128, L, LC, ctxk, finishA, "QT", "KTz", "Va", **akw)
                    self.attention_flush(ast)
                    S.barrier()
            self.tap("oTa%d" % l, self.oTd[0:NB], ["oTd"])
            if self.stop_after == (l, "mixA"):
                e_rope.close()
                return
            c2_per_hk = max(1, cfg.GRP // 2)
            for hk in range(2):
                with ExitStack() as e2:
                    QT = self.sb(e2, "QT", [128, NTOK], BF16)
                    KT = self.sb(e2, "KT", [128, NTOK], BF16)
                    Vb = self.sb(e2, "Vb", [128, NTT, 65], BF16)
                    Wv = self.sb(e2, "Wv", [128, KC, 64], BF16)
                    qsc = {"es": e2}
                    self.qk_chunk(l, qsc, hT, win, [(cfg.oBk + hk * 64, 64), (cfg.oBk + hk * 64, 64)], KT, "KT", "knc", NTOK, "k", mode="load")
                    self.wload(Wv, win, [(cfg.oBv + hk * 64, 64)], "Wv")
                    self.qk_chunk(l, qsc, hT, win, [(cfg.oBk + hk * 64, 64), (cfg.oBk + hk * 64, 64)], KT, "KT", "knc", NTOK, "k", mode="compute")
                    self.G(lambda e: e.memset(Vb[:, :, 64:65], 1.0), w=["Vb"])
                    for ti in range(NTT):
                        b = 5 + ti % 2
                        self.proj_tm(Wv, "Wv", hT, ti, 64, b)
                        self.A(lambda e: e.copy(out=Vb[:, ti, 0:64], in_=self.pb[b][:, :64]), w=["Vb"], x=[self.bk(b)])
                    fs = [self.sb(e2, "fin%d" % i, [128, 2], F32) for i in range(4)]
                    fo = [self.sb(e2, "fino%d" % i, [128, 128], BF16) for i in range(4)]
                    PT = [self.sb(e2, "PT%d" % i, [128, 512], BF16) for i in range(4)]
                    KTz = self.pad_k(e2, KT)
                    for c2 in range(hk * c2_per_hk, (hk + 1) * c2_per_hk):
                        self.qk_chunk(l, qsc, hT, win, [(cfg.oBq + c2 * 128, 128)], QT, "QT", "qnc", nqc, "q")

                        def finishB(ti, a0, a1, btoks, k, phase, c2=c2):
                            sm, ob = fs[k], fo[k]
                            stok, otok = "fin%d" % k, "fino%d" % k
                            if phase == 2:
                                self.out_transpose(ob[:], otok, oT, NB + c2, ti)
                                return
                            self.V(lambda e: e.reciprocal(out=sm[:, 0:1], in_=a0[:, 64:65]), w=[stok], x=btoks)
                            self.V(lambda e: e.reciprocal(out=sm[:, 1:2], in_=a1[:, 64:65]), w=[stok], x=btoks)
                            self.V(lambda e: e.tensor_scalar(out=ob[:, 0:64], in0=a0[:, 0:64], scalar1=sm[:, 0:1], scalar2=None,
                                                             op0=ALU.mult), r=[stok], w=[otok], x=btoks)
                            self.V(lambda e: e.tensor_scalar(out=ob[:, 64:128], in0=a1[:, 0:64], scalar1=sm[:, 1:2], scalar2=None,
                                                             op0=ALU.mult), r=[stok], w=[otok], x=btoks)
                        ast = {"gi": 0, "pending": None}
                        akw = dict(sbanks=[0, 1, 6], accsets=[[2, 3], [4, 5]], dist=2, state=ast)
                        self.attention(PT, QT, KTz, lambda kt: Vb[:, kt, :], 64, 0, L, allk, finishB, "QT", "KTz", "Vb", **akw)
                        if with_ctx:
                            self.attention(PT, QT, KTz, lambda kt: Vb[:, kt, :], 64, L, LC, ctxk, finishB, "QT", "KTz", "Vb", **akw)
                        self.attention_flush(ast)
                    S.barrier()
            self.tap("oTb%d" % l, self.oTd[NB:2 * NB], ["oTd"])
            S.barrier()
            e_rope.close()
            if self.stop_after == (l, "mixB"):
                return
            self.mix_mlstm(l, hT, win, oT, with_ctx)
            self.tap("oTc%d" % l, self.oTd[2 * NB:3 * NB], ["oTd"])
            if self.stop_after == (l, "mixC"):
                return
            self.mix_merge(l, hT, win, oT, with_ctx)

    def mix_mlstm(self, l, hT, win, oT, with_ctx):
        cfg, S = self.cfg, self.S
        D, L, LC, KC, NT, NTC, NTT, NTOK, HC = cfg.D, cfg.L, cfg.LC, cfg.KC, cfg.NT, cfg.NTC, cfg.NTT, cfg.NTOK, cfg.HC
        NB = HC
        one_col = self.cst(CON)[:, 0:1]
        with ExitStack() as es:
            Wg = self.sb(es, "Wg", [128, KC, 4 * HC], BF16)
            self.wload(Wg, win, [(cfg.oG, 4 * HC)], "Wg")
            Gt = self.sb(es, "Gt", [128, NTT, 4 * HC], F32)
            LF = self.sb(es, "LF", [128, NTT, 2, HC], F32)
            BC = self.sb(es, "BC", [128, NTT, 2, HC], F32)
            TOT = self.sb(es, "TOT", [128, NTT, 2, HC], F32)
            BIAS = self.sb(es, "BIAS", [128, NTT, 2, HC], F32)
            EB = self.sb(es, "EB", [128, NTT, 2, HC], F32)
            WC = self.sb(es, "WC", [128, NTT, 2, HC], F32)
            AC = self.sb(es, "AC", [128, NTT, 2, HC], F32)
            for ti in range(NTT):
                b = 5 + ti % 2
                self.proj_tm(Wg, "Wg", hT, ti, 4 * HC, b)
                self.V(lambda e: e.tensor_tensor(out=Gt[:, ti, :], in0=self.pb[b][:, :4 * HC], in1=self.row("gb"), op=ALU.add),
                       r=["rows"], w=["Gt"], x=[self.bk(b)])
            Gv = Gt[:].rearrange("p t (q h) -> p t q h", h=HC)
            for d in range(2):
                self.A(lambda e: e.activation(out=LF[:, :, d, :], in_=Gv[:, :, 2 * d + 1, :], func=AF.Exp, scale=-1.0), r=["Gt"], w=["LF"])
            self.A(lambda e: e.activation(out=LF[:], in_=LF[:], func=AF.Ln, bias=one_col), r=["consts"], w=["LF"])
            self.V(lambda e: e.tensor_scalar(out=LF[:], in0=LF[:], scalar1=-1.0, scalar2=None, op0=ALU.mult), w=["LF"])
            for ti in range(NTT):
                b = 5 + ti % 2
                self.P(lambda e: e.matmul(self.pb[b][:, 0:HC], lhsT=self.cst(CTU), rhs=LF[:, ti, 0, :], start=True, stop=True),
                       r=["consts", "LF"], x=[self.bk(b)])
                self.P(lambda e: e.matmul(self.pb[b][:, HC:2 * HC], lhsT=self.cst(CTL), rhs=LF[:, ti, 1, :], start=True, stop=True),
                       r=["consts", "LF"], x=[self.bk(b)])
                self.P(lambda e: e.matmul(self.pb[b][:, 2 * HC:4 * HC], lhsT=self.cst(CON), rhs=LF[:, ti, :, :].rearrange("p a b -> p (a b)"),
                                          start=True, stop=True), r=["consts", "LF"], x=[self.bk(b)])
                self.V(lambda e: e.tensor_copy(out=BC[:, ti, :, :].rearrange("p a b -> p (a b)"), in_=self.pb[b][:, 0:2 * HC]), w=["BC"], x=[self.bk(b)])
                self.V(lambda e: e.tensor_copy(out=TOT[:, ti, :, :].rearrange("p a b -> p (a b)"), in_=self.pb[b][:, 2 * HC:4 * HC]), w=["TOT"], x=[self.bk(b)])
            for d in range(2):
                self.V(lambda e: e.tensor_tensor(out=BIAS[:, :, d, :], in0=Gv[:, :, 2 * d, :], in1=BC[:, :, d, :], op=ALU.subtract),
                       r=["Gt", "BC"], w=["BIAS"])
            self.A(lambda e: e.activation(out=EB[:], in_=BC[:], func=AF.Exp), r=["BC"], w=["EB"])
            self.V(lambda e: e.tensor_tensor(out=WC[:], in0=TOT[:], in1=BIAS[:], op=ALU.add), r=["TOT", "BIAS"], w=["WC"])
            self.A(lambda e: e.activation(out=WC[:], in_=WC[:], func=AF.Exp), w=["WC"])
            self.A(lambda e: e.activation(out=AC[:], in_=TOT[:], func=AF.Exp), r=["TOT"], w=["AC"])
            S.barrier()
            for hc in range(HC):
                with ExitStack() as e2:
                    Ws = {}
                    for nm, o in (("q", cfg.oCq), ("k", cfg.oCk), ("v", cfg.oCv), ("o", cfg.oCo)):
                        Ws[nm] = self.sb(e2, "Wc" + nm, [128, KC, 128], BF16)
                        self.wload(Ws[nm], win, [(o + hc * 128, 128)], "Wc" + nm)
                    qT = self.sb(e2, "cqT", [128, NTOK], BF16)
                    kT = self.sb(e2, "ckT", [128, NTOK], BF16)
                    raw = self.sb(e2, "craw", [128, NTOK], F32)
                    acc = self.sb(e2, "cacc", [128, NTOK], F32)
                    cw = self.col("conv%d" % l)
                    for (nm, dst, dtok, scale, chunk) in (("q", qT, "cqT", 1.0, hc), ("k", kT, "ckT", 128.0 ** -0.5, HC + hc)):
                        def cons(ps_ap, g0, gn, b):
                            self.A(lambda e: e.copy(out=raw[:, g0:g0 + gn], in_=ps_ap), w=["craw"], x=[self.bk(b)])
                        self.proj_fm(Ws[nm], "Wc" + nm, hT, 0, NTOK, [5, 6], cons)
                        w0 = cw[:, 0 * 2 * HC + chunk:0 * 2 * HC + chunk + 1]
                        w1 = cw[:, 1 * 2 * HC + chunk:1 * 2 * HC + chunk + 1]
                        w2 = cw[:, 2 * 2 * HC + chunk:2 * 2 * HC + chunk + 1]
                        for (s0, n) in ((0, L), (L, LC)):
                            self.V(lambda e: e.tensor_scalar(out=acc[:, s0:s0 + n], in0=raw[:, s0:s0 + n], scalar1=w1, scalar2=None, op0=ALU.mult),
                                   r=["craw", "cols"], w=["cacc"])
                            self.V(lambda e: e.scalar_tensor_tensor(out=acc[:, s0 + 1:s0 + n], in0=raw[:, s0:s0 + n - 1], scalar=w0,
                                                                    in1=acc[:, s0 + 1:s0 + n], op0=ALU.mult, op1=ALU.add), r=["craw", "cols"], w=["cacc"])
                            self.V(lambda e: e.scalar_tensor_tensor(out=acc[:, s0:s0 + n - 1], in0=raw[:, s0 + 1:s0 + n], scalar=w2,
                                                                    in1=acc[:, s0:s0 + n - 1], op0=ALU.mult, op1=ALU.add), r=["craw", "cols"], w=["cacc"])
                        self.A(lambda e: e.activation(out=raw[:], in_=acc[:], func=AF.Sigmoid), r=["cacc"], w=["craw"])
                        self.V(lambda e: e.scalar_tensor_tensor(out=dst[:], in0=acc[:], scalar=scale, in1=raw[:], op0=ALU.mult, op1=ALU.mult),
                               r=["cacc", "craw"], w=[dtok])
                    Vc = self.sb(e2, "cVc", [128, NTT, 129], BF16)
                    OG = acc[:].rearrange("p (t d) -> p t d", d=128)
                    Kt = self.sb(e2, "cKt", [128, NTT, 128], BF16)
                    HS = self.sb(e2, "cHS", [128, NTT, 128], F32)
                    self.G(lambda e: e.memset(Vc[:, :, 128:129], 1.0), w=["cVc"])
                    self.G(lambda e: e.memset(HS[:], 0.0), w=["cHS"])
                    pv7 = self.pb[7][:].bitcast(BF16)
                    for ti in range(NTT):
                        self.proj_tm(Ws["v"], "Wcv", hT, ti, 128, 5)
                        self.A(lambda e: e.copy(out=Vc[:, ti, 0:128], in_=self.pb[5][:, :128]), w=["cVc"], x=[self.bk(5)])
                        self.P(lambda e: e.transpose(out=pv7[:, 0:128], in_=kT[:, ti * 128:(ti + 1) * 128], identity=self.identb[:]),
                               r=["ckT", "identb"], x=[self.bk(7)])
                        self.V(lambda e: e.tensor_copy(out=Kt[:, ti, :], in_=pv7[:, 0:128]), w=["cKt"], x=[self.bk(7)])
                    INTRA = [raw[:].rearrange("p (t d) -> p t d", d=128), acc[:].rearrange("p (t d) -> p t d", d=128)]
                    itok = ["craw", "cacc"]
                    DENI = self.sb(e2, "cDENI", [128, NTT, 2], F32)
                    SB = [self.sb(e2, "cSB%d" % d, [128, NTT, 129], BF16) for d in range(2)]
                    st = [self.sb(e2, "st%d" % d, [128, 129], F32) for d in range(2)]
                    rot = dict(Lt=[self.sb(e2, "Lt%d" % i, [128, 128], F32) for i in range(3)],
                               DT=[self.sb(e2, "DT%d" % i, [128, 128], F32) for i in range(3)],
                               SM=[self.sb(e2, "SM%d" % i, [128, 128], BF16) for i in range(3)],
                               VW=[self.sb(e2, "VW%d" % i, [128, 129], BF16) for i in range(3)],
                               nd=[self.sb(e2, "nd%d" % i, [128, 132], F32) for i in range(3)])
                    order = [list(range(NT, NTT)) + list(range(NT)), list(range(NTT - 1, NT - 1, -1)) + list(range(NT - 1, -1, -1))]
                    out_tiles = list(range(NTT)) if with_ctx else list(range(NT))
                    for kk in ("Lt", "DT", "SM", "VW"):
                        rot[kk].append(self.sb(e2, kk + "3", [128, 129 if kk == "VW" else 128], BF16 if kk in ("SM", "VW") else F32))
                    items = [(ti, d) for ti in out_tiles for d in range(2)]
                    nit = len(items)

                    def p1A(i):
                        ti, d = items[i]
                        cs = slice(ti * 128, (ti + 1) * 128)
                        bS = (i // 2) % 2
                        if d == 0:
                            self.P(lambda e: e.matmul(self.pb[bS][:, :128], lhsT=kT[:, cs], rhs=qT[:, cs], start=True, stop=True),
                                   r=["ckT", "cqT"], x=[self.bk(bS)])
                        k4 = i % 4
                        bL = 2 + i % 2
                        Lt = rot["Lt"][k4]
                        self.V(lambda e: e.tensor_scalar(out=Lt[:], in0=self.cst(CSL if d == 0 else CSU), scalar1=LF[:, ti, d, hc:hc + 1],
                                                         scalar2=None, op0=ALU.mult), r=["consts"], w=["Lt%d" % k4])
                        self.P(lambda e: e.matmul(self.pb[bL][:, :128], lhsT=Lt[:], rhs=self.cst(CTU if d == 0 else CTL),
                                                  start=True, stop=True), r=["Lt%d" % k4, "consts"], x=[self.bk(bL)])

                    def p1B(i):
                        ti, d = items[i]
                        bS = (i // 2) % 2
                        k4 = i % 4
                        bL = 2 + i % 2
                        DT, SM = rot["DT"][k4], rot["SM"][k4]
                        self.A(lambda e: e.activation(out=DT[:], in_=self.pb[bL][:, :128], func=AF.Exp,
                                                      bias=Gv[:, ti, 2 * d, hc:hc + 1]), w=["DT%d" % k4], x=[self.bk(bL)])
                        self.G(lambda e: e.tensor_tensor(out=DT[:], in0=DT[:], in1=self.cst(CTU if d == 0 else CTL), op=ALU.mult),
                               r=["consts"], w=["DT%d" % k4])
                        self.V(lambda e: e.tensor_tensor(out=SM[:], in0=DT[:], in1=self.pb[bS][:, :128], op=ALU.mult),
                               r=["DT%d" % k4], w=["SM%d" % k4], x=[self.bk(bS)])

                    def p1C(i):
                        ti, d = items[i]
                        k4 = i % 4
                        bI = 4 + i % 2
                        SM = rot["SM"][k4]
                        self.P(lambda e: e.matmul(self.pb[bI][:, :129], lhsT=SM[:], rhs=Vc[:, ti, :], start=True, stop=True),
                               r=["SM%d" % k4, "cVc"], x=[self.bk(bI)])
                        self.A(lambda e: e.copy(out=INTRA[d][:, ti, :], in_=self.pb[bI][:, :128]), w=[itok[d]], x=[self.bk(bI)])
                        self.V(lambda e: e.tensor_copy(out=DENI[:, ti, d:d + 1], in_=self.pb[bI][:, 128:129]), w=["cDENI"], x=[self.bk(bI)])
                    for i in range(nit + 2):
                        if i < nit:
                            p1A(i)
                        if 0 <= i - 1 < nit:
                            p1B(i - 1)
                        if 0 <= i - 2 < nit:
                            p1C(i - 2)
                    for d in range(2):
                        self.G(lambda e: e.memset(st[d][:], 0.0), w=["st%d" % d])
                        self.G(lambda e: e.memset(SB[d][:, 0, :], 0.0), w=["cSB%d" % d])
                    sitems = [(step, d) for step in range(NTT - 1) for d in range(2)]
                    nsi = len(sitems)
                    ubanks = [6, 7, 0, 1]

                    def p2A(i):
                        step, d = sitems[i]
                        ti = order[d][step]
                        k4 = i % 4
                        bU = ubanks[i % 4]
                        VW = rot["VW"][k4]
                        self.V(lambda e: e.tensor_scalar(out=VW[:], in0=Vc[:, ti, :], scalar1=WC[:, ti, d, hc:hc + 1], scalar2=None,
                                                         op0=ALU.mult), r=["cVc"], w=["VW%d" % k4])
                        self.P(lambda e: e.matmul(self.pb[bU][:, :129], lhsT=Kt[:, ti, :], rhs=VW[:], start=True, stop=True),
                               r=["cKt", "VW%d" % k4], x=[self.bk(bU)])

                    def p2B(i):
                        step, d = sitems[i]
                        ti = order[d][step]
                        bU = ubanks[i % 4]
                        self.V(lambda e: e.scalar_tensor_tensor(out=st[d][:], in0=st[d][:], scalar=AC[:, ti, d, hc:hc + 1],
                                                                in1=self.pb[bU][:, :129], op0=ALU.mult, op1=ALU.add),
                               w=["st%d" % d], x=[self.bk(bU)])
                        self.A(lambda e: e.copy(out=SB[d][:, step + 1, :], in_=st[d][:]), r=["st%d" % d], w=["cSB%d" % d])
                    for i in range(nsi + 2):
                        if i < nsi:
                            p2A(i)
                        if 0 <= i - 2 < nsi:
                            p2B(i - 2)
                    n3 = 0
                    for step in range(NTT):
                        for d in range(2):
                            ti = order[d][step]
                            if not (ti < NT or with_ctx):
                                continue
                            cs = slice(ti * 128, (ti + 1) * 128)
                            k3 = n3 % 3
                            bN = 2 + n3 % 4
                            n3 += 1
                            nd = rot["nd"][k3]
                            ntk = "nd%d" % k3
                            self.P(lambda e: e.matmul(self.pb[bN][:, :129], lhsT=qT[:, cs], rhs=SB[d][:, step, :], start=True, stop=True),
                                   r=["cqT", "cSB%d" % d], x=[self.bk(bN)])
                            self.V(lambda e: e.scalar_tensor_tensor(out=nd[:, 0:128], in0=self.pb[bN][:, 0:128], scalar=EB[:, ti, d, hc:hc + 1],
                                                                    in1=INTRA[d][:, ti, :], op0=ALU.mult, op1=ALU.add),
                                   r=[itok[d]], w=[ntk], x=[self.bk(bN)])
                            self.V(lambda e: e.scalar_tensor_tensor(out=nd[:, 128:129], in0=self.pb[bN][:, 128:129], scalar=EB[:, ti, d, hc:hc + 1],
                                                                    in1=DENI[:, ti, d:d + 1], op0=ALU.mult, op1=ALU.add),
                                   r=["cDENI"], w=[ntk], x=[self.bk(bN)])
                            self.V(lambda e: e.scalar_tensor_tensor(out=nd[:, 129:130], in0=nd[:, 128:129], scalar=-1.0, in1=nd[:, 128:129],
                                                                    op0=ALU.mult, op1=ALU.max), w=[ntk])
                            self.V(lambda e: e.tensor_scalar(out=nd[:, 129:130], in0=nd[:, 129:130], scalar1=1.0, scalar2=None, op0=ALU.max), w=[ntk])
                            self.V(lambda e: e.reciprocal(out=nd[:, 129:130], in_=nd[:, 129:130]), w=[ntk])
                            self.G(lambda e: e.scalar_tensor_tensor(out=HS[:, ti, :], in0=nd[:, 0:128], scalar=nd[:, 129:130],
                                                                    in1=HS[:, ti, :], op0=ALU.mult, op1=ALU.add), r=[ntk], w=["cHS"]) \
                                if False else self.V(lambda e: e.scalar_tensor_tensor(out=HS[:, ti, :], in0=nd[:, 0:128], scalar=nd[:, 129:130],
                                                                                      in1=HS[:, ti, :], op0=ALU.mult, op1=ALU.add), r=[ntk], w=["cHS"])
                    S.barrier()
                    for ti in (range(NTT) if with_ctx else range(NT)):
                        b = 5 + ti % 2
                        self.proj_tm(Ws["o"], "Wco", hT, ti, 128, b)
                        self.A(lambda e: e.activation(out=OG[:, ti, :], in_=self.pb[b][:, :128], func=AF.Sigmoid), w=["cacc"], x=[self.bk(b)])
                    fs = self.sb(e2, "cfs", [128, NTT], F32)
                    fj = self.sb(e2, "cfj", [128, 128], F32)
                    fo = [self.sb(e2, "cfo%d" % i, [128, 128], BF16) for i in range(2)]
                    tiles = list(range(NTT)) if with_ctx else list(range(NT))
                    for ti in tiles:
                        self.A(lambda e: e.activation(out=fj[:], in_=HS[:, ti, :], func=AF.Square, accum_out=fs[:, ti:ti + 1]), w=["cfj", "cfs"])
                    self.rstd_col(fs[:, :len(tiles)], 128, "cfs", [])
                    cn = self.row("cn", hc * 128, (hc + 1) * 128)
                    for n_, ti in enumerate(tiles):
                        k = n_ % 2
                        self.V(lambda e: e.scalar_tensor_tensor(out=HS[:, ti, :], in0=HS[:, ti, :], scalar=fs[:, ti:ti + 1], in1=cn,
                                                                op0=ALU.mult, op1=ALU.mult), r=["cfs", "rows"], w=["cHS"])
                        self.G(lambda e: e.tensor_tensor(out=fo[k][:], in0=HS[:, ti, :], in1=OG[:, ti, :], op=ALU.mult), r=["cHS", "cacc"], w=["cfo%d" % k])
                        self.out_transpose(fo[k][:], "cfo%d" % k, oT, 2 * NB + hc, ti)
                    S.barrier()

    def mix_merge(self, l, hT, win, oT, with_ctx):
        cfg, S = self.cfg, self.S
        D, L, KC, NT, NTT, NTOK, NB = cfg.D, cfg.L, cfg.KC, cfg.NT, cfg.NTT, cfg.NTOK, cfg.HA
        ntok = NTOK if with_ctx else L
        wbr = [self.dr[n][l].rearrange("(c p) d -> p c d", p=128) for n in ("w_branch_a", "w_branch_b", "w_branch_c")]
        S.barrier()
        with ExitStack() as es:
            mT = self.sb(es, "mT", [128, KC, 512], BF16)
            acc = self.sb(es, "macc", [128, 512], F32)
            sgs = [self.sb(es, "msg%d" % i, [128, 512], F32) for i in range(2)]
            Wg = [self.sb(es, "mWg%d" % i, [128, KC, 128], BF16) for i in range(6)]
            Wb = [self.sb(es, "mWb%d" % i, [128, NB, 128], BF16) for i in range(6)]
            oTg = [self.sb(es, "oTg%d" % i, [128, NB, 512], BF16) for i in range(3)]
            Wo = self.sb(es, "mWo", [128, KC, D], BF16)
            tmp = [self.sb(es, "mtmp%d" % i, [128, 512], F32) for i in range(2)]
            S.dma("pool", Wo[:], self.dr["w_out"][l].rearrange("(c p) d -> p c d", p=128), writes=["mWo"])
            n = 0
            gcnt = 0
            n2 = 0
            for g0 in range(0, ntok, 512):
                gn = min(512, ntok - g0)
                for i in range(3):
                    S.dma("sp", oTg[i][:, :, :gn], self.oTd[i * NB:(i + 1) * NB, :, g0:g0 + gn].rearrange("c p t -> p c t"),
                          reads=["oTd"], writes=["oTg%d" % i])
                for dc in range(KC):
                    for i in range(3):
                        k = n % 6
                        n += 1
                        self.wload(Wg[k], win, [(cfg.oMG + i * D + dc * 128, 128)], "mWg%d" % k)
                        S.dma("pool", Wb[k][:], wbr[i][:, :, dc * 128:(dc + 1) * 128], writes=["mWb%d" % k])
                        bg = 5 + gcnt % 2
                        bb = 0 + gcnt % 2
                        sg = sgs[gcnt % 2]
                        stok = "msg%d" % (gcnt % 2)
                        gcnt += 1
                        for kc in range(KC):
                            self.P(lambda e: e.matmul(self.pb[bg][:, :gn], lhsT=Wg[k][:, kc, :], rhs=hT[:, kc, g0:g0 + gn],
                                                      start=(kc == 0), stop=(kc == KC - 1)), r=["mWg%d" % k, "hT%d" % kc], x=[self.bk(bg)])
                        self.A(lambda e: e.activation(out=sg[:, :gn], in_=self.pb[bg][:, :gn], func=AF.Sigmoid), w=[stok], x=[self.bk(bg)])
                        for c in range(NB):
                            self.P(lambda e: e.matmul(self.pb[bb][:, :gn], lhsT=Wb[k][:, c, :], rhs=oTg[i][:, c, :gn],
                                                      start=(c == 0), stop=(c == NB - 1)), r=["mWb%d" % k, "oTg%d" % i], x=[self.bk(bb)])
                        if i == 0:
                            self.V(lambda e: e.tensor_tensor(out=acc[:, :gn], in0=sg[:, :gn], in1=self.pb[bb][:, :gn], op=ALU.mult),
                                   r=[stok], w=["macc"], x=[self.bk(bb)])
                        else:
                            self.V(lambda e: e.tensor_tensor(out=sg[:, :gn], in0=sg[:, :gn], in1=self.pb[bb][:, :gn], op=ALU.mult),
                                   w=[stok], x=[self.bk(bb)])
                            if i == 1:
                                self.G(lambda e: e.tensor_tensor(out=acc[:, :gn], in0=acc[:, :gn], in1=sg[:, :gn], op=ALU.add),
                                       r=[stok], w=["macc"])
                            else:
                                self.G(lambda e: e.tensor_tensor(out=mT[:, dc, :gn], in0=acc[:, :gn], in1=sg[:, :gn], op=ALU.add),
                                       r=[stok, "macc"], w=["mT"])
                for tt in range(gn // 128):
                    ti = g0 // 128 + tt
                    w_ = 0 if ti < NT else 1
                    for hf in range(D // 512):
                        k = n2 % 2
                        n2 += 1
                        b = 2 + k
                        for kc in range(KC):
                            self.P(lambda e: e.matmul(self.pb[b][:, :], lhsT=mT[:, kc, tt * 128:(tt + 1) * 128], rhs=Wo[:, kc, hf * 512:(hf + 1) * 512],
                                                      start=(kc == 0), stop=(kc == KC - 1)), r=["mT", "mWo"], x=[self.bk(b)])
                        self.V(lambda e: e.tensor_tensor(out=tmp[k][:], in0=self.pb[b][:, :], in1=self.grow[:, w_, hf * 512:(hf + 1) * 512], op=ALU.mult),
                               r=["grow"], w=["mtmp%d" % k], x=[self.bk(b)])
                        xt = self.src_tile(ti)
                        self.G(lambda e: e.tensor_tensor(out=xt[:, hf * 512:(hf + 1) * 512], in0=xt[:, hf * 512:(hf + 1) * 512], in1=tmp[k][:], op=ALU.add),
                               r=["mtmp%d" % k], w=["x%d" % ti])
            S.barrier()

    def phase_ffn(self, l, with_ctx):
        cfg, S = self.cfg, self.S
        D, L, LC, E, FF, FC, KC, NT, NTC, NTT, NTOK = cfg.D, cfg.L, cfg.LC, cfg.E, cfg.FF, cfg.FC, cfg.KC, cfg.NT, cfg.NTC, cfg.NTT, cfg.NTOK
        self.phase_mod(l, 5, False)
        sets = [dict(t0=0, nt=NT, cap=cfg.CAPL, w=0, s0=0)]
        if with_ctx:
            sets.append(dict(t0=NT, nt=NTC, cap=cfg.CAPC, w=1, s0=cfg.CAPL))
        NS = sum(st["cap"] for st in sets)
        ntl = NTT if with_ctx else NT
        stiles = []
        for si, st in enumerate(sets):
            assert st["s0"] % 128 == 0
            for a in range(0, st["cap"], 128):
                stiles.append((st["s0"] + a, min(128, st["cap"] - a), si))
        NST = len(stiles)
        NH = D // 512
        assert NST * NH <= 6
        identf = self.cst(CI)
        iof = self.row("iof")
        with ExitStack() as es:
            xs = self.sb(es, "xs2", [128, NTT, D], BF16)
            rankTok = self.sb(es, "rankTok", [128, NTT, E], F32)
            LG = self.sb(es, "LG", [128, NTT, E], F32)
            iopj = self.sb(es, "iopj", [128, NST], F32)
            e_row = ExitStack()
            gT = self.sb(e_row, "gT", [E, NTOK], F32)
            rankT = self.sb(e_row, "rankT", [E, NTOK], F32)
            for k, (s_start, nn, si) in enumerate(stiles):
                self.V(lambda e: e.tensor_scalar(out=iopj[:, k:k + 1], in0=self.col("iop"), scalar1=float(s_start), scalar2=None, op0=ALU.add),
                       r=["cols"], w=["iopj"])
            with ExitStack() as e1:
                hT2 = self.sb(e1, "hT2", [128, KC, NTOK], BF16)
                self.phase_norm(e1, l, 1, xs, hT2, with_ctx)
                Wr = self.sb(e1, "Wr", [128, KC, E], BF16)
                S.dma("pool", Wr[:], self.dr["w_router"][l].rearrange("(c p) e -> p c e", p=128), writes=["Wr"])
                mx = self.sb(e1, "lgmx", [128, NTT], F32)
                for ti in range(ntl):
                    b = 5 + ti % 2
                    self.proj_tm(Wr, "Wr", hT2, ti, E, b)
                    self.V(lambda e: e.tensor_copy(out=LG[:, ti, :], in_=self.pb[b][:, :E]), w=["LG"], x=[self.bk(b)])
                lg = LG[:, :ntl, :]
                self.V(lambda e: e.tensor_reduce(out=mx[:, :ntl], in_=lg, axis=AX.X, op=ALU.max), r=["LG"], w=["lgmx"])
                self.V(lambda e: e.tensor_tensor(out=lg, in0=lg, in1=mx[:, :ntl].unsqueeze(2).to_broadcast([128, ntl, E]), op=ALU.subtract),
                       r=["lgmx"], w=["LG"])
                self.A(lambda e: e.activation(out=lg, in_=lg, func=AF.Exp), w=["LG"])
                self.V(lambda e: e.reduce_sum(out=mx[:, :ntl], in_=lg, axis=AX.X), r=["LG"], w=["lgmx"])
                self.V(lambda e: e.reciprocal(out=mx[:, :ntl], in_=mx[:, :ntl]), w=["lgmx"])
                self.V(lambda e: e.tensor_tensor(out=lg, in0=lg, in1=mx[:, :ntl].unsqueeze(2).to_broadcast([128, ntl, E]), op=ALU.mult),
                       r=["lgmx"], w=["LG"])
                for t0 in range(0, ntl, 4):
                    nt_ = min(4, ntl - t0)
                    b = (t0 // 4) % 2
                    for k in range(nt_):
                        self.P(lambda e: e.transpose(out=self.pb[b][0:E, k * 128:(k + 1) * 128], in_=LG[:, t0 + k, :], identity=identf),
                               r=["LG", "consts"], x=[self.bk(b)])
                    self.A(lambda e: e.copy(out=gT[:, t0 * 128:(t0 + nt_) * 128], in_=self.pb[b][0:E, :nt_ * 128]), w=["gT"], x=[self.bk(b)])
                S.barrier()
            with ExitStack() as e1:
                nmax = max(st["nt"] for st in sets) * 128
                work = self.sb(e1, "tkw", [E, nmax], F32)
                MK = self.sb(e1, "tkm", [E, nmax], F32)
                CS = self.sb(e1, "tkc", [E, nmax], F32)
                ones = self.sb(e1, "tko", [E, nmax], F32)
                mx8 = self.sb(e1, "tk8", [E, 8], F32)
                self.G(lambda e: e.memset(ones[:], 1.0), w=["tko"])
                for st in sets:
                    c0, n, cap = st["t0"] * 128, st["nt"] * 128, st["cap"]
                    assert cap % 8 == 0
                    self.V(lambda e: e.tensor_copy(out=work[:, :n], in_=gT[:, c0:c0 + n]), r=["gT"], w=["tkw"])
                    for r_ in range(cap // 8):
                        self.V(lambda e: e.max(out=mx8[:], in_=work[:, :n]), r=["tkw"], w=["tk8"])
                        if r_ < cap // 8 - 1:
                            self.V(lambda e: e.match_replace(out=work[:, :n], in_to_replace=mx8[:], in_values=work[:, :n], imm_value=-1.0),
                                   r=["tk8"], w=["tkw"])
                    self.V(lambda e: e.tensor_scalar(out=MK[:, :n], in0=gT[:, c0:c0 + n], scalar1=mx8[:, 7:8], scalar2=None, op0=ALU.is_ge),
                           r=["gT", "tk8"], w=["tkm"])
                    self.V(lambda e: e.tensor_tensor_scan(out=CS[:, :n], data0=ones[:, :n], data1=MK[:, :n], initial=0.0, op0=ALU.mult, op1=ALU.add),
                           r=["tko", "tkm"], w=["tkc"])
                    self.V(lambda e: e.scalar_tensor_tensor(out=CS[:, :n], in0=CS[:, :n], scalar=float(st["s0"]), in1=MK[:, :n], op0=ALU.add, op1=ALU.mult),
                           r=["tkm"], w=["tkc"])
                    self.V(lambda e: e.tensor_scalar(out=rankT[:, c0:c0 + n], in0=CS[:, :n], scalar1=-1.0, scalar2=None, op0=ALU.add),
                           r=["tkc"], w=["rankT"])
                for ti in range(ntl):
                    b = ti % 2
                    self.P(lambda e: e.transpose(out=self.pb[b][:, 0:E], in_=rankT[0:E, ti * 128:(ti + 1) * 128], identity=identf[0:E, 0:E]),
                           r=["rankT", "consts"], x=[self.bk(b)])
                    self.V(lambda e: e.tensor_copy(out=rankTok[:, ti, :], in_=self.pb[b][:, 0:E]), w=["rankTok"], x=[self.bk(b)])
                S.barrier()
            self.tap("rankT%d" % l, rankT[:, :ntl * 128], [])
            self.tap("gT%d" % l, gT[:, :ntl * 128], [])
            S.barrier()
            e_row.close()
            with ExitStack() as e1:
                CAPM = max(st["cap"] for st in sets)
                Sel = self.sb(e1, "Sel", [128, NTT, CAPM], BF16)
                SelT = [self.sb(e1, "SelT%d" % i, [128, NST, 512], BF16) for i in range(2)]
                gsb = [self.sb(e1, "gsb%d" % i, [128, 512], F32) for i in range(2)]
                repR = [self.sb(e1, "repR%d" % i, [128, 128], F32) for i in range(2)]
                repG = [self.sb(e1, "repG%d" % i, [128, 128], F32) for i in range(2)]
                xeT = self.sb(e1, "xeT", [128, KC, NS], BF16)
                actT = self.sb(e1, "actT", [128, FC, NS], BF16)
                ye = self.sb(e1, "ye", [128, NST, D], BF16)
                sa = [self.sb(e1, "sa%d" % i, [128, NS], F32) for i in range(2)]
                PW = 256
                DP = 2
                NWB = 3
                Wg = [self.sb(e1, "eWg%d" % i, [128, KC, PW], BF16) for i in range(NWB)]
                Wu = [self.sb(e1, "eWu%d" % i, [128, KC, PW], BF16) for i in range(NWB)]
                Wd = [self.sb(e1, "eWd%d" % i, [128, DP, D], BF16) for i in range(NWB)]
                cn = dict(wcnt=0, dcnt=0, scnt=0, ocnt=0, ecnt=0, rcnt=0)

                def do_gather(ex):
                        for st in sets:
                            for k in range(st["nt"]):
                                ti = st["t0"] + k
                                cap = st["cap"]
                                if st["s0"] == 0:
                                    self.V(lambda e: e.tensor_scalar(out=Sel[:, ti, :cap], in0=iof[:, :cap], scalar1=rankTok[:, ti, ex:ex + 1],
                                                                     scalar2=None, op0=ALU.is_equal), r=["rankTok", "rowsG"], w=["Sel"])
                                else:
                                    self.V(lambda e: e.tensor_scalar(out=Sel[:, ti, :cap], in0=iof[:, :cap], scalar1=float(st["s0"]),
                                                                     scalar2=rankTok[:, ti, ex:ex + 1], op0=ALU.add, op1=ALU.is_equal),
                                           r=["rankTok", "rowsG"], w=["Sel"])
                        for fc in range(KC):
                            b = 6 + fc % 2
                            for st in sets:
                                s0, cap, w_ = st["s0"], st["cap"], st["w"]
                                for k in range(st["nt"]):
                                    ti = st["t0"] + k
                                    self.P(lambda e: e.matmul(self.pb[b][:, s0:s0 + cap], lhsT=xs[:, ti, fc * 128:(fc + 1) * 128], rhs=Sel[:, ti, :cap],
                                                              start=(k == 0), stop=(k == st["nt"] - 1)), r=["xs%d" % ti, "Sel"], x=[self.bk(b)])
                                sc_ = self.modA[:, 1, fc, w_:w_ + 1]
                                bi_ = self.modc[:, 3 * KC + fc, w_:w_ + 1]
                                cn["ecnt"] += 1
                                if cn["ecnt"] % 2 == 0:
                                    self.A(lambda e: e.activation(out=xeT[:, fc, s0:s0 + cap], in_=self.pb[b][:, s0:s0 + cap], func=AF.Identity,
                                                                  scale=sc_, bias=bi_), r=["modA", "modc"], w=["xeT"], x=[self.bk(b)])
                                else:
                                    self.V(lambda e: e.tensor_scalar(out=xeT[:, fc, s0:s0 + cap], in0=self.pb[b][:, s0:s0 + cap], scalar1=sc_, scalar2=bi_,
                                                                     op0=ALU.mult, op1=ALU.add), r=["modA", "modc"], w=["xeT"], x=[self.bk(b)])

                def do_gateup(ex):
                        wg_d = self.dr["w_exp_gate"][l, ex].rearrange("(kc p) f -> p kc f", p=128)
                        wu_d = self.dr["w_exp_up"][l, ex].rearrange("(kc p) f -> p kc f", p=128)
                        for pc in range(FF // PW):
                            kb = cn["wcnt"] % NWB
                            cn["wcnt"] += 1
                            S.dma("pool", Wg[kb][:], wg_d[:, :, pc * PW:(pc + 1) * PW], writes=["eWg%d" % kb])
                            S.dma("pool", Wu[kb][:], wu_d[:, :, pc * PW:(pc + 1) * PW], writes=["eWu%d" % kb])
                            for fo in range(PW // 128):
                                fidx = pc * (PW // 128) + fo
                                ba, bu = (0, 1) if fidx % 2 == 0 else (2, 3)
                                for kc in range(KC):
                                    self.P(lambda e: e.matmul(self.pb[ba][:, :NS], lhsT=Wg[kb][:, kc, fo * 128:(fo + 1) * 128], rhs=xeT[:, kc, :],
                                                              start=(kc == 0), stop=(kc == KC - 1)), r=["eWg%d" % kb, "xeT"], x=[self.bk(ba)])
                                for kc in range(KC):
                                    self.P(lambda e: e.matmul(self.pb[bu][:, :NS], lhsT=Wu[kb][:, kc, fo * 128:(fo + 1) * 128], rhs=xeT[:, kc, :],
                                                              start=(kc == 0), stop=(kc == KC - 1)), r=["eWu%d" % kb, "xeT"], x=[self.bk(bu)])
                                sa_ = sa[fidx % 2]
                                self.A(lambda e: e.activation(out=sa_[:], in_=self.pb[ba][:, :NS], func=AF.Silu), w=["sa%d" % (fidx % 2)], x=[self.bk(ba)])
                                self.V(lambda e: e.tensor_tensor(out=actT[:, fidx, :], in0=sa_[:], in1=self.pb[bu][:, :NS], op=ALU.mult),
                                       r=["sa%d" % (fidx % 2)], w=["actT"], x=[self.bk(bu)])

                def do_rest(ex):
                        wd_d = self.dr["w_exp_down"][l, ex].rearrange("(fc p) d -> p fc d", p=128)
                        for pc in range(FC // DP):
                            kb = cn["dcnt"] % NWB
                            cn["dcnt"] += 1
                            S.dma("pool", Wd[kb][:], wd_d[:, pc * DP:(pc + 1) * DP, :], writes=["eWd%d" % kb])
                            for f2 in range(DP):
                                fc = pc * DP + f2
                                for k_st, (s_start, nn, si) in enumerate(stiles):
                                    for hf in range(NH):
                                        b = k_st * NH + hf
                                        self.P(lambda e: e.matmul(self.pb[b][:nn, :512], lhsT=actT[:, fc, s_start:s_start + nn],
                                                                  rhs=Wd[kb][:, f2, hf * 512:(hf + 1) * 512], start=(fc == 0), stop=(fc == FC - 1)),
                                               r=["actT", "eWd%d" % kb], x=[self.bk(b)])
                        for k_st, (s_start, nn, si) in enumerate(stiles):
                            w_ = sets[si]["w"]
                            for hf in range(NH):
                                b = k_st * NH + hf
                                self.V(lambda e: e.tensor_tensor(out=ye[:nn, k_st, hf * 512:(hf + 1) * 512], in0=self.pb[b][:nn, :512],
                                                                 in1=self.grow[:nn, w_, hf * 512:(hf + 1) * 512], op=ALU.mult),
                                       r=["grow"], w=["ye"], x=[self.bk(b)])
                        for si, st in enumerate(sets):
                            c0, n = st["t0"] * 128, st["nt"] * 128
                            mine = [(k_st, s_start, nn) for k_st, (s_start, nn, sj) in enumerate(stiles) if sj == si]
                            for g0 in range(c0, c0 + n, 512):
                                gn = min(512, c0 + n - g0)
                                kb = cn["scnt"] % 2
                                cn["scnt"] += 1
                                for tt in range(gn // 128):
                                    ti = g0 // 128 + tt
                                    rk = cn["rcnt"] % 2
                                    cn["rcnt"] += 1
                                    self.G(lambda e: e.tensor_copy(out=repR[rk][:], in_=rankTok[:, ti, ex:ex + 1].to_broadcast([128, 128])),
                                           r=["rankTok"], w=["repR%d" % rk])
                                    self.G(lambda e: e.tensor_copy(out=repG[rk][:], in_=LG[:, ti, ex:ex + 1].to_broadcast([128, 128])),
                                           r=["LG"], w=["repG%d" % rk])
                                    self.P(lambda e: e.matmul(self.pb[6][:, tt * 128:(tt + 1) * 128], lhsT=repR[rk][:], rhs=identf, start=True, stop=True),
                                           r=["repR%d" % rk, "consts"], x=[self.bk(6)])
                                    self.P(lambda e: e.matmul(self.pb[7][:, tt * 128:(tt + 1) * 128], lhsT=repG[rk][:], rhs=identf, start=True, stop=True),
                                           r=["repG%d" % rk, "consts"], x=[self.bk(7)])
                                self.A(lambda e: e.copy(out=gsb[kb][:, :gn], in_=self.pb[7][:, :gn]), w=["gsb%d" % kb], x=[self.bk(7)])
                                for (k_st, s_start, nn) in mine:
                                    self.V(lambda e: e.scalar_tensor_tensor(out=SelT[kb][:, k_st, :gn], in0=self.pb[6][:, :gn], scalar=iopj[:, k_st:k_st + 1],
                                                                            in1=gsb[kb][:, :gn], op0=ALU.is_equal, op1=ALU.mult),
                                           r=["iopj", "gsb%d" % kb], w=["SelT%d" % kb], x=[self.bk(6)])
                                for tt in range(gn // 128):
                                    ti = g0 // 128 + tt
                                    xt = self.src_tile(ti)
                                    for hf in range(NH):
                                        b = cn["ocnt"] % 4
                                        cn["ocnt"] += 1
                                        for idx, (k_st, s_start, nn) in enumerate(mine):
                                            self.P(lambda e: e.matmul(self.pb[b][:, :512], lhsT=SelT[kb][:nn, k_st, tt * 128:(tt + 1) * 128],
                                                                      rhs=ye[:nn, k_st, hf * 512:(hf + 1) * 512], start=(idx == 0), stop=(idx == len(mine) - 1)),
                                                   r=["SelT%d" % kb, "ye"], x=[self.bk(b)])
                                        self.V(lambda e: e.tensor_tensor(out=xt[:, hf * 512:(hf + 1) * 512], in0=xt[:, hf * 512:(hf + 1) * 512],
                                                                         in1=self.pb[b][:, :512], op=ALU.add), w=["x%d" % ti], x=[self.bk(b)])

                do_gather(0)
                for ex in range(E):
                    do_gateup(ex)
                    if ex + 1 < E:
                        do_gather(ex + 1)
                    do_rest(ex)
                S.barrier()

    def final(self):
        cfg, S = self.cfg, self.S
        D, NT = cfg.D, cfg.NT
        with ExitStack() as es:
            ss = self.sb(es, "fss", [128, NT], F32)
            rstd = self.sb(es, "frstd", [128, NT], F32)
            junk = self.sb(es, "fjunk", [128, D], BF16)
            ot = [self.sb(es, "fo%d" % i, [128, D], F32) for i in range(2)]
            fnr = self.sb(es, "fnr", [128, D], F32)
            S.dma("sp", fnr[:], self.dr["fnrow"], writes=["fnr"])
            for i in range(NT):
                self.A(lambda e: e.activation(out=junk[:], in_=self.x_sb[:, i, :], func=AF.Square,
                                              accum_out=ss[:, i:i + 1]), r=["x%d" % i], w=["fjunk", "fss"])
            self.V(lambda e: e.tensor_scalar(out=rstd[:], in0=ss[:], scalar1=1.0 / D, scalar2=EPS,
                                             op0=ALU.mult, op1=ALU.add), r=["fss"], w=["frstd"])
            self.A(lambda e: e.activation(out=rstd[:], in_=rstd[:], func=AF.Sqrt), r=[], w=["frstd"])
            self.V(lambda e: e.reciprocal(out=rstd[:], in_=rstd[:]), r=[], w=["frstd"])
            for i in range(NT):
                o = ot[i % 2]
                self.V(lambda e: e.scalar_tensor_tensor(out=o[:], in0=self.x_sb[:, i, :], scalar=rstd[:, i:i + 1],
                                                        in1=fnr[:], op0=ALU.mult, op1=ALU.mult),
                       r=["x%d" % i, "frstd", "fnr"], w=["fo%d" % (i % 2)])
                S.dma("sp", self.y[i * 128:(i + 1) * 128, :], o[:], reads=["fo%d" % (i % 2)], writes=["y%d" % i])
            S.barrier()


def host_packs(cfg, inp, b):
    D, KC, DEPTH = cfg.D, cfg.KC, cfg.DEPTH
    cols = np.zeros((128, cfg.NCOL), np.float32)

    def colset(name, v):
        o, w = cfg.coff[name]
        cols[:, o:o + w] = np.asarray(v, np.float32).reshape(w, 128).T
    colset("c", inp["c"][b])
    colset("cctx", inp["c_ctx"])
    for l in range(DEPTH):
        colset("bada%d" % l, inp["b_ada"][l])
        colset("n1%d" % l, inp["norm1_w"][l])
        colset("n2%d" % l, inp["norm2_w"][l])
        colset("conv%d" % l, np.asarray(inp["mlstm_conv_w"][l]).reshape(-1))
        qn = np.asarray(inp["gqa_qnorm_w"][l], np.float32)
        kn = np.asarray(inp["gqa_knorm_w"][l], np.float32)
        perm = np.concatenate([np.arange(32, 64), np.arange(0, 32)])
        o, _ = cfg.coff["qnc%d" % l]
        cols[:, o] = np.tile(qn, 2)
        cols[:, o + 1] = np.tile(qn[perm], 2)
        o, _ = cfg.coff["knc%d" % l]
        cols[:, o] = np.tile(kn, 2)
        cols[:, o + 1] = np.tile(kn[perm], 2)
    cols[:, cfg.coff["iop"][0]] = np.arange(128)
    cols[:, cfg.coff["eps"][0]] = EPS
    rowsL = np.zeros((DEPTH, 128, cfg.NROWL), np.float32)
    rowsG = np.zeros((128, cfg.NROWG), np.float32)
    brows = np.zeros((DEPTH, 128, 6 * D), np.float32)

    def rowset(arr, name, v):
        o, w = cfg.roff[name]
        arr[:, o:o + w] = np.asarray(v, np.float32).reshape(1, w)
    for l in range(DEPTH):
        rowset(rowsL[l], "sub", inp["diff_subln_w"][l])
        rowset(rowsL[l], "cn", inp["mlstm_norm_w"][l])
        rowset(rowsL[l], "gb", inp["mlstm_gate_b"][l])
        rowset(rowsL[l], "lam", np.asarray(inp["diff_lambda"][l]).reshape(-1))
        brows[l] = np.asarray(inp["b_ada"][l], np.float32).reshape(1, 6 * D)
    rowset(rowsG, "iof", np.arange(256))
    rows = (rowsL, rowsG, brows)
    return cols, rows


def make_in_maps(cfg, inp, cores):
    cosT, sinT = rope_tables(cfg)
    ropeT = np.concatenate([cosT, sinT], axis=1)
    consts = const_pack()
    E = cfg.E
    esel = np.zeros((E, E * 128), np.float32)
    for e in range(E):
        esel[e, e * 128:(e + 1) * 128] = 1.0
    shared = {k: np.ascontiguousarray(np.asarray(inp[k], np.float32)) for k in
              ("w_ada", "w_in", "w_branch_a", "w_branch_b", "w_branch_c", "w_out", "w_router",
               "w_exp_gate", "w_exp_up", "w_exp_down")}
    maps = []
    for b in cores:
        cols, rows = host_packs(cfg, inp, b)
        m = {"x": np.ascontiguousarray(inp["x"][b], np.float32), "ctx": np.ascontiguousarray(inp["ctx"][b], np.float32),
             "cols": cols, "rowsL": rows[0], "rowsG": rows[1], "brows": rows[2], "fnrow": np.ascontiguousarray(np.broadcast_to(np.asarray(inp["final_norm_w"], np.float32).reshape(1, -1), (128, cfg.D))), "consts": consts, "ropeT": ropeT, "esel": esel}
        m.update(shared)
        maps.append(m)
    return maps


_CACHE = {}


def kernel(**inputs):
    cfg = Cfg()
    if "nc" not in _CACHE:
        _CACHE["nc"] = Builder(cfg).build()
    nc = _CACHE["nc"]
    inp = {k: np.asarray(v) for k, v in inputs.items()}
    n = inp["x"].shape[0]
    in_maps = make_in_maps(cfg, inp, list(range(n)))
    res = run_bass_kernel_spmd(nc, in_maps, core_ids=list(range(n)))
    return np.stack([np.asarray(r["y"], np.float32) for r in res.results], axis=0)
```

```python
import math
from contextlib import ExitStack

import numpy as np
import concourse.bass as bass
import concourse.mybir as mybir
from concourse.bass_utils import run_bass_kernel_spmd

F32 = mybir.dt.float32
BF16 = mybir.dt.bfloat16
AF = mybir.ActivationFunctionType
ALU = mybir.AluOpType
AX = mybir.AxisListType
EPS = 1e-6


class Sched:
    def __init__(self, nc, n_dma_sems=32, same_engine_sync=True):
        self.nc = nc
        self.eng = {"pe": nc.tensor, "dve": nc.vector, "act": nc.scalar, "pool": nc.gpsimd, "sp": nc.sync}
        self.sem = {k: nc.alloc_semaphore(name="s_" + k) for k in self.eng}
        self.cnt = {k: 0 for k in self.eng}
        self.waited = {k: {} for k in self.eng}
        self.dsem = [nc.alloc_semaphore(name="d%d" % i) for i in range(2 * n_dma_sems)]
        self.dcnt = [0] * (2 * n_dma_sems)
        self.nds = n_dma_sems
        self.drr = [0, 0]
        self.tok = {}
        self.same = same_engine_sync
        self.n_inst = 0
        self.n_wait = 0

    def _st(self, t):
        s = self.tok.get(t)
        if s is None:
            s = self.tok[t] = [None, []]
        return s

    def _wait(self, engname, deps):
        need = {}
        for ev, skip_same in deps:
            if ev is None:
                continue
            sem, val, src = ev
            if src == engname and (skip_same or not self.same or engname == "pe"):
                continue
            k = sem.num
            if k not in need or need[k][1] < val:
                need[k] = (sem, val)
        e = self.eng[engname]
        w = self.waited[engname]
        for k, (sem, val) in need.items():
            if w.get(k, 0) < val:
                e.wait_ge(sem, val)
                w[k] = val
                self.n_wait += 1

    def _deps(self, reads, writes, excl):
        deps = []
        for t in reads:
            deps.append((self._st(t)[0], False))
        for t in writes:
            s = self._st(t)
            deps.append((s[0], False))
            deps.extend((r, False) for r in s[1])
        for t in excl:
            s = self._st(t)
            deps.append((s[0], True))
        return deps

    def _commit(self, ev, reads, writes, excl):
        for t in reads:
            self._st(t)[1].append(ev)
        for t in writes:
            s = self._st(t)
            s[0] = ev
            s[1] = []
        for t in excl:
            s = self._st(t)
            s[0] = ev
            s[1] = []

    def op(self, engname, fn, reads=(), writes=(), excl=()):
        self._wait(engname, self._deps(reads, writes, excl))
        inst = fn(self.eng[engname])
        self.cnt[engname] += 1
        ev = (self.sem[engname], self.cnt[engname], engname)
        inst.then_inc(ev[0], 1)
        self._commit(ev, reads, writes, excl)
        self.n_inst += 1
        return ev

    def dma(self, queue, out, in_, reads=(), writes=(), **kw):
        self._wait(queue, self._deps(reads, writes, ()))
        q = 1 if queue == "pool" else 0
        i = q * self.nds + self.drr[q]
        self.drr[q] = (self.drr[q] + 1) % self.nds
        inst = self.eng[queue].dma_start(out=out, in_=in_, **kw)
        self.dcnt[i] += 16
        ev = (self.dsem[i], self.dcnt[i], "dma")
        inst.then_inc(ev[0], 16)
        self._commit(ev, reads, writes, ())
        self.n_inst += 1
        return ev

    def wait_all(self, engname):
        deps = [((self.sem[k], self.cnt[k], k), False) for k in self.eng if self.cnt[k] > 0 and k != engname]
        deps += [((self.dsem[i], self.dcnt[i], "dma"), False) for i in range(len(self.dsem)) if self.dcnt[i] > 0]
        self._wait(engname, deps)

    def barrier(self):
        for k in ("pe", "dve", "act", "pool", "sp"):
            self.wait_all(k)
        self.tok = {}


class Cfg:
    def __init__(s, D=1024, L=2048, LC=256, E=16, FF=2048, DEPTH=2, GW=64):
        s.D, s.L, s.LC, s.E, s.FF, s.DEPTH, s.GW = D, L, LC, E, FF, DEPTH, GW
        s.KC = D // 128
        s.NT = L // 128
        s.NTC = LC // 128
        s.NTT = s.NT + s.NTC
        s.NTOK = s.NTT * 128
        MW = s.MW = D // 2
        s.HA = MW // 128
        s.HBQ = MW // 64
        s.GRP = s.HBQ // 2
        s.HC = MW // 128
        s.FC = FF // 128
        s.CAPL = 2 * L // E
        s.CAPC = 2 * LC // E
        s.oAq, s.oAk, s.oAv, s.oBq = 0, MW, 2 * MW, 3 * MW
        s.oBk, s.oBv = 4 * MW, 4 * MW + 128
        s.oCq, s.oCk, s.oCv, s.oCo = 4 * MW + 256, 5 * MW + 256, 6 * MW + 256, 7 * MW + 256
        s.oG = 8 * MW + 256
        s.oMG = s.oG + 4 * s.HC
        s.INC = s.oMG + 3 * D
        off = {}
        n = 0

        def add(name, w):
            nonlocal n
            off[name] = (n, w)
            n += w
        add("c", s.KC)
        add("cctx", s.KC)
        for l in range(DEPTH):
            add("bada%d" % l, 6 * s.KC)
            add("n1%d" % l, s.KC)
            add("n2%d" % l, s.KC)
            add("conv%d" % l, 3 * 2 * s.HC)
            add("qnc%d" % l, 2)
            add("knc%d" % l, 2)
        add("cosT", 0)
        add("iop", 1)
        add("eps", 1)
        s.coff, s.NCOL = off, n
        roff = {}
        n = 0

        def addr(name, w):
            nonlocal n
            roff[name] = (n, w)
            n += w
        addr("sub", 128)
        addr("cn", MW)
        addr("gb", 4 * s.HC)
        addr("lam", 256)
        s.NROWL = n
        n = 0
        addr("iof", 256)
        s.roff, s.NROWG = roff, n


def rope_tables(cfg):
    L, GW = cfg.L, cfg.GW
    t = np.arange(L)
    rows = (t // GW).astype(np.float32)
    cols = (t % GW).astype(np.float32)
    nf = 16
    inv = (10000.0 ** (-np.arange(nf, dtype=np.float32) / nf)).astype(np.float32)
    ang = np.concatenate([rows[:, None] * inv, cols[:, None] * inv], axis=-1).astype(np.float32)
    cos = np.cos(ang).astype(np.float32)
    sin = np.sin(ang).astype(np.float32)
    cosT = np.zeros((128, L), np.float32)
    sinT = np.zeros((128, L), np.float32)
    for p in range(128):
        d = p % 64
        f = d % 32
        cosT[p] = cos[:, f]
        sinT[p] = -sin[:, f] if d < 32 else sin[:, f]
    return cosT, sinT


def const_pack():
    r = np.arange(128)
    ident = np.eye(128, dtype=np.float32)
    triU = (r[:, None] <= r[None, :]).astype(np.float32)
    triL = (r[:, None] >= r[None, :]).astype(np.float32)
    sU = (r[:, None] < r[None, :]).astype(np.float32)
    sL = (r[:, None] > r[None, :]).astype(np.float32)
    ones = np.ones((128, 128), np.float32)
    blk = (r[:, None] // 64 == r[None, :] // 64).astype(np.float32)
    return np.concatenate([ident, triU, triL, sU, sL, ones, blk], axis=1)


CI, CTU, CTL, CSU, CSL, CON, CBK = range(7)


class Builder:
    def __init__(self, cfg, taps=None, stop_after=None):
        self.cfg = cfg
        self.taps = taps or {}
        self.stop_after = stop_after
        self.nc = bass.Bass("TRN2", target_bir_lowering=False)
        self.S = None

    def sb(self, es, name, shape, dt):
        self._uid = getattr(self, "_uid", 0) + 1
        return es.enter_context(self.nc.sbuf_tensor("%s_%d" % (name, self._uid), list(shape), dt))

    def V(self, fn, r=(), w=(), x=()):
        return self.S.op("dve", fn, r, w, x)

    def A(self, fn, r=(), w=(), x=()):
        return self.S.op("act", fn, r, w, x)

    def G(self, fn, r=(), w=(), x=()):
        return self.S.op("pool", fn, r, w, x)

    def P(self, fn, r=(), w=(), x=()):
        return self.S.op("pe", fn, r, w, x)

    def bk(self, i):
        return "pb%d" % i

    def cst(self, k):
        return self.consts[:, k * 128:(k + 1) * 128]

    def col(self, name, a=0, b=None):
        o, w = self.cfg.coff[name]
        if b is None:
            b = w
        return self.cols[:, o + a:o + b]

    def row(self, name, a=0, b=None):
        o, w = self.cfg.roff[name]
        if b is None:
            b = w
        t = self.rowsG if name in ("iof",) else self.rowsL
        return t[:, o + a:o + b]

    def tap(self, name, ap_sb, reads):
        if name not in self.taps:
            return
        shape = list(ap_sb.shape)
        d = self.nc.dram_tensor("tap_" + name, shape, ap_sb.dtype, kind="ExternalOutput").ap()
        self.S.dma("sp", d, ap_sb, reads=reads, writes=["tapd_" + name])

    def build(self):
        cfg, nc = self.cfg, self.nc
        D, L, LC, E, FF, DEPTH = cfg.D, cfg.L, cfg.LC, cfg.E, cfg.FF, cfg.DEPTH
        KC, NT, NTC, NTT = cfg.KC, cfg.NT, cfg.NTC, cfg.NTT
        dr = {}

        def din(name, shape, dt=F32):
            dr[name] = nc.dram_tensor(name, list(shape), dt, kind="ExternalInput").ap()
            return dr[name]
        din("x", [L, D])
        din("ctx", [LC, D])
        din("cols", [128, cfg.NCOL])
        din("rowsL", [DEPTH, 128, cfg.NROWL])
        din("rowsG", [128, cfg.NROWG])
        din("brows", [DEPTH, 128, 6 * D])
        din("fnrow", [128, D])
        din("consts", [128, 7 * 128])
        din("ropeT", [128, 2 * L])
        din("esel", [E, E * 128])
        din("w_ada", [DEPTH, D, 6 * D])
        din("w_in", [DEPTH, D, cfg.INC])
        din("w_branch_a", [DEPTH, cfg.MW, D])
        din("w_branch_b", [DEPTH, cfg.MW, D])
        din("w_branch_c", [DEPTH, cfg.MW, D])
        din("w_out", [DEPTH, D, D])
        din("w_router", [DEPTH, D, E])
        din("w_exp_gate", [DEPTH, E, D, FF])
        din("w_exp_up", [DEPTH, E, D, FF])
        din("w_exp_down", [DEPTH, E, FF, D])
        self.dr = dr
        self.y = nc.dram_tensor("y", [L, D], F32, kind="ExternalOutput").ap()
        self.S = Sched(nc)
        S = self.S
        with ExitStack() as es:
            self.pb = [es.enter_context(nc.psum_tensor("pb%d" % i, [128, 512], F32)) for i in range(8)]
            self.x_sb = self.sb(es, "x_sb", [128, NT, D], F32)
            self.c_sb = self.sb(es, "c_sb", [128, NTC, D], F32)
            self.cols = self.sb(es, "cols_sb", [128, cfg.NCOL], F32)
            self.rowsG = self.sb(es, "rowsG_sb", [128, cfg.NROWG], F32)
            self.consts = self.sb(es, "consts_sb", [128, 7 * 128], F32)
            self.identb = self.sb(es, "identb", [128, 128], BF16)
            self.silc = self.sb(es, "silc", [128, KC, 2], F32)
            self.modc = self.sb(es, "modc", [128, 6 * KC, 2], F32)
            self.modA = self.sb(es, "modA", [128, 2, KC, 2], F32)
            self.grow = self.sb(es, "grow", [128, 2, D], F32)
            S.dma("sp", self.cols[:], dr["cols"], writes=["cols"])
            S.dma("sp", self.rowsG[:], dr["rowsG"], writes=["rowsG"])
            S.dma("sp", self.consts[:], dr["consts"], writes=["consts"])
            for i in range(NT):
                S.dma("sp", self.x_sb[:, i, :], dr["x"][i * 128:(i + 1) * 128, :], writes=["x%d" % i])
            for i in range(NTC):
                S.dma("sp", self.c_sb[:, i, :], dr["ctx"][i * 128:(i + 1) * 128, :], writes=["x%d" % (NT + i)])
            self.V(lambda e: e.tensor_copy(out=self.identb[:], in_=self.cst(CI)), r=["consts"], w=["identb"])
            self.A(lambda e: e.activation(out=self.silc[:, :, 0], in_=self.col("c"), func=AF.Silu), r=["cols"], w=["silc"])
            self.A(lambda e: e.activation(out=self.silc[:, :, 1], in_=self.col("cctx"), func=AF.Silu), r=["cols"], w=["silc"])
            for l in range(DEPTH):
                self.layer(l)
                if self.stop_after is not None and self.stop_after[0] == l:
                    break
            self.final()
            S.barrier()
        return nc

    def src_tile(self, i):
        return self.x_sb[:, i, :] if i < self.cfg.NT else self.c_sb[:, i - self.cfg.NT, :]

    def phase_mod(self, l, rowsec, do_cols):
        cfg, S = self.cfg, self.S
        D, KC = cfg.D, cfg.KC
        wa_d = self.dr["w_ada"][l].rearrange("(kc p) f -> p kc f", p=128)
        npiece = 6 * D // 512
        with ExitStack() as es:
            wa = [self.sb(es, "wa%d" % i, [128, KC, 512], F32) for i in range(2)]
            rep = self.sb(es, "rep", [128, KC, 2, 128], F32)
            brow = self.sb(es, "brow", [128, D], F32)
            S.dma("sp", brow[:], self.dr["brows"][l][:, rowsec * D:(rowsec + 1) * D], writes=["brow"])
            for kc in range(KC):
                for w_ in range(2):
                    self.V(lambda e: e.tensor_copy(out=rep[:, kc, w_, :], in_=self.silc[:, kc, w_:w_ + 1].to_broadcast([128, 128])),
                           r=["silc"], w=["rep"])
            jj = 0
            for j in range(npiece):
                sec = (j * 512) // D
                off = j * 512 - sec * D
                if not do_cols and sec != rowsec:
                    continue
                buf = wa[jj % 2]
                tk = "wa%d" % (jj % 2)
                pbk = self.pb[jj % 2]
                bkt = self.bk(jj % 2)
                jj += 1
                S.dma("sp", buf[:], wa_d[:, :, j * 512:(j + 1) * 512], writes=[tk])
                if do_cols:
                    for s_ in range(4):
                        for kc in range(KC):
                            self.P(lambda e: e.matmul(pbk[:, s_ * 2:s_ * 2 + 2], lhsT=buf[:, kc, s_ * 128:(s_ + 1) * 128],
                                                      rhs=self.silc[:, kc, :], start=(kc == 0), stop=(kc == KC - 1)),
                                   r=[tk, "silc"], x=[bkt])
                    o, _ = cfg.coff["bada%d" % l]
                    self.V(lambda e: e.tensor_tensor(
                        out=self.modc[:, j * 4:(j + 1) * 4, :],
                        in0=pbk[:, 0:8].rearrange("p (a b) -> p a b", b=2),
                        in1=self.cols[:, o + j * 4:o + (j + 1) * 4].unsqueeze(2).to_broadcast([128, 4, 2]),
                        op=ALU.add), r=["cols"], w=["modc"], x=[bkt])
                if sec == rowsec:
                    for w_ in range(2):
                        pb2 = self.pb[2 + w_]
                        for kc in range(KC):
                            self.P(lambda e: e.matmul(pb2[:, :], lhsT=rep[:, kc, w_, :], rhs=buf[:, kc, :],
                                                      start=(kc == 0), stop=(kc == KC - 1)),
                                   r=[tk, "rep"], x=[self.bk(2 + w_)])
                        self.V(lambda e: e.tensor_tensor(out=self.grow[:, w_, off:off + 512], in0=pb2[:, :],
                                                         in1=brow[:, off:off + 512], op=ALU.add),
                               r=["brow"], w=["grow"], x=[self.bk(2 + w_)])
            if do_cols:
                for ni, (nname, scsec) in enumerate((("n1%d" % l, 1), ("n2%d" % l, 4))):
                    self.V(lambda e: e.scalar_tensor_tensor(
                        out=self.modA[:, ni, :, :], in0=self.modc[:, scsec * KC:(scsec + 1) * KC, :], scalar=1.0,
                        in1=self.col(nname).unsqueeze(2).to_broadcast([128, KC, 2]), op0=ALU.add, op1=ALU.mult),
                        r=["modc", "cols"], w=["modA"])
            S.barrier()

    def phase_norm(self, es, l, ni, xs, hT, with_ctx=True):
        cfg, S = self.cfg, self.S
        D, KC, NT, NTT = cfg.D, cfg.KC, cfg.NT, cfg.NTT
        shsec = 0 if ni == 0 else 3
        ntl = NTT if with_ctx else NT
        with ExitStack() as es2:
            ss = self.sb(es2, "nss", [128, NTT], F32)
            rstd = self.sb(es2, "nrstd", [128, NTT], F32)
            junk = self.sb(es2, "njunk", [128, D], BF16)
            for i in range(ntl):
                self.A(lambda e: e.activation(out=junk[:], in_=self.src_tile(i), func=AF.Square,
                                              accum_out=ss[:, i:i + 1]), r=["x%d" % i], w=["njunk", "nss"])
            self.V(lambda e: e.tensor_scalar(out=rstd[:, :ntl], in0=ss[:, :ntl], scalar1=1.0 / D, scalar2=EPS,
                                             op0=ALU.mult, op1=ALU.add), r=["nss"], w=["nrstd"])
            self.A(lambda e: e.activation(out=rstd[:, :ntl], in_=rstd[:, :ntl], func=AF.Sqrt), r=[], w=["nrstd"])
            self.V(lambda e: e.reciprocal(out=rstd[:, :ntl], in_=rstd[:, :ntl]), r=[], w=["nrstd"])
            for i in range(ntl):
                self.V(lambda e: e.tensor_scalar(out=xs[:, i, :], in0=self.src_tile(i), scalar1=rstd[:, i:i + 1],
                                                 scalar2=None, op0=ALU.mult), r=["x%d" % i, "nrstd"], w=["xs%d" % i])
            groups = [(g, min(4, NT - g), 0) for g in range(0, NT, 4)]
            if with_ctx:
                groups += [(NT + g, min(4, cfg.NTC - g), 1) for g in range(0, cfg.NTC, 4)]
            n = 0
            for fc in range(KC):
                for (t0, nt_, w_) in groups:
                    b = n % 2
                    n += 1
                    pv = self.pb[b][:].bitcast(BF16)
                    for k in range(nt_):
                        self.P(lambda e: e.transpose(out=pv[:, k * 128:(k + 1) * 128],
                                                     in_=xs[:, t0 + k, fc * 128:(fc + 1) * 128], identity=self.identb[:]),
                               r=["xs%d" % (t0 + k), "identb"], x=[self.bk(b)])
                    dst = hT[:, fc, t0 * 128:(t0 + nt_) * 128]
                    sc = self.modA[:, ni, fc, w_:w_ + 1]
                    bi = self.modc[:, shsec * KC + fc, w_:w_ + 1]
                    if n % 2 == 0:
                        self.A(lambda e: e.activation(out=dst, in_=pv[:, :nt_ * 128], func=AF.Identity, scale=sc, bias=bi),
                               r=["modA", "modc"], w=["hT%d" % fc], x=[self.bk(b)])
                    else:
                        self.V(lambda e: e.tensor_scalar(out=dst, in0=pv[:, :nt_ * 128], scalar1=sc, scalar2=bi,
                                                         op0=ALU.mult, op1=ALU.add),
                               r=["modA", "modc"], w=["hT%d" % fc], x=[self.bk(b)])
            S.barrier()

    def layer(self, l):
        cfg = self.cfg
        with_ctx = l < cfg.DEPTH - 1
        self.phase_mod(l, 2, True)
        if self.stop_after == (l, "mod"):
            return
        with ExitStack() as es:
            hT = self.sb(es, "hT", [128, cfg.KC, cfg.NTOK], BF16)
            with ExitStack() as e0:
                xs = self.sb(e0, "xs", [128, cfg.NTT, cfg.D], BF16)
                self.phase_norm(e0, l, 0, xs, hT, True)
            self.tap("hT%d" % l, hT[:], ["hT%d" % fc for fc in range(cfg.KC)])
            if self.stop_after == (l, "norm1"):
                return
            self.phase_mix(l, hT, with_ctx)
        if self.stop_after is not None and self.stop_after[0] == l and self.stop_after[1] != "ffn":
            return
        self.phase_ffn(l, with_ctx)

    def wload(self, dst, win, slices, tok):
        a = 0
        for (c0, n) in slices:
            self.S.dma("pool", dst[:, :, a:a + n], win[:, :, c0:c0 + n], writes=[tok])
            a += n

    def proj_fm(self, W, wtok, hT, col0, ncols, banks, consume):
        KC = self.cfg.KC
        gi = 0
        for g0 in range(col0, col0 + ncols, 512):
            gn = min(512, col0 + ncols - g0)
            b = banks[gi % len(banks)]
            gi += 1
            for kc in range(KC):
                self.P(lambda e: e.matmul(self.pb[b][:, :gn], lhsT=W[:, kc, :], rhs=hT[:, kc, g0:g0 + gn],
                                          start=(kc == 0), stop=(kc == KC - 1)),
                       r=[wtok, "hT%d" % kc], x=[self.bk(b)])
            consume(self.pb[b][:, :gn], g0, gn, b)

    def proj_tm(self, W, wtok, hT, ti, ncols, b):
        KC = self.cfg.KC
        for kc in range(KC):
            self.P(lambda e: e.matmul(self.pb[b][:, :ncols], lhsT=hT[:, kc, ti * 128:(ti + 1) * 128], rhs=W[:, kc, :ncols],
                                      start=(kc == 0), stop=(kc == KC - 1)),
                   r=[wtok, "hT%d" % kc], x=[self.bk(b)])

    def qk_chunk(self, l, es, hT, win, nat, dst, dtok, nrm, nq_cols, pf, mode="both"):
        cfg = self.cfg
        L, KC = cfg.L, cfg.KC
        if "qk_t1" not in es:
            for nm in ("qk_t1", "qk_t2", "qk_sq", "qk_rs"):
                es[nm] = self.sb(es["es"], nm, [128, 512], F32)
        if pf + "W" not in es:
            es[pf + "W"] = self.sb(es["es"], pf + "qkW", [128, KC, 128], BF16)
            es[pf + "Wp"] = self.sb(es["es"], pf + "qkWp", [128, KC, 128], BF16)
        W, Wp = es[pf + "W"], es[pf + "Wp"]
        wtok, wptok = pf + "qkW", pf + "qkWp"
        pf = ""
        perm = []
        for (c0, n) in nat:
            for a in range(0, n, 64):
                perm += [(c0 + a + 32, 32), (c0 + a, 32)]
        if mode in ("both", "load"):
            self.wload(W, win, nat, wtok)
            self.wload(Wp, win, perm, wptok)
        if mode == "load":
            return
        t1, t2, sq, rs = es["qk_t1"], es["qk_t2"], es["qk_sq"], es["qk_rs"]
        cosT, sinT = self.ropeT[:, 0:L], self.ropeT[:, L:2 * L]
        for g0 in range(0, nq_cols, 512):
            gn = min(512, nq_cols - g0)
            lat = g0 < L
            gi_ = g0 // 512
            bq, bp, bs_ = [0, 2, 4][gi_ % 3], [1, 3, 5][gi_ % 3], 6 + gi_ % 2
            for kc in range(KC):
                self.P(lambda e: e.matmul(self.pb[bq][:, :gn], lhsT=W[:, kc, :], rhs=hT[:, kc, g0:g0 + gn],
                                          start=(kc == 0), stop=(kc == KC - 1)), r=[wtok, "hT%d" % kc], x=[self.bk(bq)])
            if lat:
                for kc in range(KC):
                    self.P(lambda e: e.matmul(self.pb[bp][:, :gn], lhsT=Wp[:, kc, :], rhs=hT[:, kc, g0:g0 + gn],
                                              start=(kc == 0), stop=(kc == KC - 1)), r=[wptok, "hT%d" % kc], x=[self.bk(bp)])
            pq, pp = self.pb[bq][:, :gn], self.pb[bp][:, :gn]
            if nrm is not None:
                self.A(lambda e: e.activation(out=sq[:, :gn], in_=pq, func=AF.Square), w=[pf + "qk_sq"], x=[self.bk(bq)])
                self.P(lambda e: e.matmul(self.pb[bs_][:, :gn], lhsT=self.cst(CBK), rhs=sq[:, :gn], start=True, stop=True),
                       r=["consts", pf + "qk_sq"], x=[self.bk(bs_)])
                self.V(lambda e: e.tensor_scalar(out=rs[:, :gn], in0=self.pb[bs_][:, :gn], scalar1=1.0 / 64, scalar2=EPS,
                                                 op0=ALU.mult, op1=ALU.add), w=[pf + "qk_rs"], x=[self.bk(bs_)])
                self.A(lambda e: e.activation(out=rs[:, :gn], in_=rs[:, :gn], func=AF.Sqrt), w=[pf + "qk_rs"])
                self.V(lambda e: e.reciprocal(out=rs[:, :gn], in_=rs[:, :gn]), w=[pf + "qk_rs"])
                wc = self.col(nrm + "%d" % l)
                if lat:
                    self.V(lambda e: e.scalar_tensor_tensor(out=t1[:, :gn], in0=pq, scalar=wc[:, 0:1], in1=cosT[:, g0:g0 + gn],
                                                            op0=ALU.mult, op1=ALU.mult), r=["cols", "ropeT"], w=[pf + "qk_t1"], x=[self.bk(bq)])
                    self.V(lambda e: e.scalar_tensor_tensor(out=t2[:, :gn], in0=pp, scalar=wc[:, 1:2], in1=sinT[:, g0:g0 + gn],
                                                            op0=ALU.mult, op1=ALU.mult), r=["cols", "ropeT"], w=[pf + "qk_t2"], x=[self.bk(bp)])
                    self.G(lambda e: e.tensor_tensor(out=t1[:, :gn], in0=t1[:, :gn], in1=t2[:, :gn], op=ALU.add),
                           r=[pf + "qk_t2"], w=[pf + "qk_t1"])
                    self.V(lambda e: e.tensor_tensor(out=dst[:, g0:g0 + gn], in0=t1[:, :gn], in1=rs[:, :gn], op=ALU.mult),
                           r=[pf + "qk_t1", pf + "qk_rs"], w=[dtok])
                else:
                    self.V(lambda e: e.scalar_tensor_tensor(out=dst[:, g0:g0 + gn], in0=pq, scalar=wc[:, 0:1], in1=rs[:, :gn],
                                                            op0=ALU.mult, op1=ALU.mult), r=["cols", pf + "qk_rs"], w=[dtok], x=[self.bk(bq)])
            else:
                if lat:
                    self.V(lambda e: e.tensor_tensor(out=t1[:, :gn], in0=pq, in1=cosT[:, g0:g0 + gn], op=ALU.mult),
                           r=["ropeT"], w=[pf + "qk_t1"], x=[self.bk(bq)])
                    self.V(lambda e: e.tensor_tensor(out=t2[:, :gn], in0=pp, in1=sinT[:, g0:g0 + gn], op=ALU.mult),
                           r=["ropeT"], w=[pf + "qk_t2"], x=[self.bk(bp)])
                    self.G(lambda e: e.tensor_tensor(out=dst[:, g0:g0 + gn], in0=t1[:, :gn], in1=t2[:, :gn], op=ALU.add),
                           r=[pf + "qk_t1", pf + "qk_t2"], w=[dtok])
                else:
                    self.A(lambda e: e.copy(out=dst[:, g0:g0 + gn], in_=pq), w=[dtok], x=[self.bk(bq)])

    def attention(self, PT, QT, KT, Vaug, vw, qcol0, nq, ktiles, finish, tokQ, tokK, tokV, sbanks, accsets, dist, state, gq=512):
        spb = 512 // (vw + 1)
        for g0 in range(qcol0, qcol0 + nq, gq):
            gn = min(gq, qcol0 + nq - g0)
            nqt = gn // 128
            aset = accsets[state["gi"] % len(accsets)]
            state["gi"] += 1

            def acc(j, qt, aset=aset, nqt=nqt):
                s_ = j * nqt + qt
                bnk = aset[s_ // spb]
                return self.pb[bnk][:, (s_ % spb) * (vw + 1):(s_ % spb + 1) * (vw + 1)], bnk
            nb = (2 * nqt + spb - 1) // spb
            for b in range(nb):
                self.V(lambda e: e.memset(self.pb[aset[b]][:], 0.0), w=[self.bk(aset[b])])
            steps = [(kt, j) for kt in ktiles for j in range(2)]
            n = len(steps)

            def issue_S(i):
                kt, j = steps[i]
                sbk = sbanks[i % len(sbanks)]
                pbuf = i % len(PT)
                self.P(lambda e: e.matmul(self.pb[sbk][:, :gn], lhsT=KT[:, j, kt * 128:(kt + 1) * 128],
                                          rhs=QT[:, g0:g0 + gn], start=True, stop=True),
                       r=[tokQ, tokK], x=[self.bk(sbk)])
                self.A(lambda e: e.activation(out=PT[pbuf][:, :gn], in_=self.pb[sbk][:, :gn], func=AF.Exp, scale=0.125),
                       w=["PT%d" % pbuf], x=[self.bk(sbk)])

            def issue_PV(i):
                kt, j = steps[i]
                pbuf = i % len(PT)
                for qt in range(nqt):
                    ap_, bnk = acc(j, qt)
                    self.P(lambda e: e.matmul(ap_, lhsT=PT[pbuf][:, qt * 128:(qt + 1) * 128], rhs=Vaug(kt),
                                              start=False, stop=False, skip_group_check=True),
                           r=["PT%d" % pbuf, tokV], x=[self.bk(bnk)])
            for i in range(min(dist, n)):
                issue_S(i)
            pend = state.get("pending")
            for i in range(n):
                if i + dist < n:
                    issue_S(i + dist)
                issue_PV(i)
                if pend is not None and i == min(1, n - 1):
                    pend(1)
                if pend is not None and i == min(max(n // 2, 2), n - 1):
                    pend(2)

            def fin(phase, g0=g0, nqt=nqt, acc=acc):
                for qt in range(nqt):
                    (a0, b0), (a1, b1) = acc(0, qt), acc(1, qt)
                    finish(g0 // 128 + qt, a0, a1, [self.bk(b0), self.bk(b1)], qt, phase)
            state["pending"] = fin

    def pad_k(self, es, KT):
        NTOK = self.cfg.NTOK
        KTz = self.sb(es, "KTz", [128, 2, NTOK], BF16)
        self.G(lambda e: e.memset(KTz[64:128, 0, :], 0.0), w=["KTz"])
        self.G(lambda e: e.memset(KTz[0:64, 1, :], 0.0), w=["KTz"])
        self.A(lambda e: e.copy(out=KTz[0:64, 0, :], in_=KT[0:64, :]), r=["KT"], w=["KTz"])
        self.G(lambda e: e.tensor_copy(out=KTz[64:128, 1, :], in_=KT[64:128, :]), r=["KT"], w=["KTz"])
        return KTz

    def attention_flush(self, state):
        if state.get("pending") is not None:
            state["pending"](1)
            state["pending"](2)
            state["pending"] = None

    def out_transpose(self, tok_ap, ttok, oT, chunk, ti, bank=7):
        pv = self.pb[bank][:].bitcast(BF16)
        k = self._otn % 3
        self._otn += 1
        st = self.otst[k]
        self.P(lambda e: e.transpose(out=pv[:, 0:128], in_=tok_ap, identity=self.identb[:]), r=[ttok, "identb"], x=[self.bk(bank)])
        self.A(lambda e: e.copy(out=st[:], in_=pv[:, 0:128]), w=["otst%d" % k], x=[self.bk(bank)])
        self.S.dma("sp", self.oTd[chunk, :, ti * 128:(ti + 1) * 128], st[:], reads=["otst%d" % k], writes=["oTd"])

    def rstd_col(self, ss, n, rs_tok, r):
        self.V(lambda e: e.tensor_scalar(out=ss, in0=ss, scalar1=1.0 / n, scalar2=EPS, op0=ALU.mult, op1=ALU.add), r=r, w=[rs_tok])
        self.A(lambda e: e.activation(out=ss, in_=ss, func=AF.Ln), w=[rs_tok])
        self.A(lambda e: e.activation(out=ss, in_=ss, func=AF.Exp, scale=-0.5), w=[rs_tok])

    def phase_mix(self, l, hT, with_ctx):
        cfg, S = self.cfg, self.S
        D, L, LC, KC, NT, NTC, NTT, NTOK = cfg.D, cfg.L, cfg.LC, cfg.KC, cfg.NT, cfg.NTC, cfg.NTT, cfg.NTOK
        NB = cfg.HA
        win = self.dr["w_in"][l].rearrange("(kc p) c -> p kc c", p=128)
        lam_init = 0.8 - 0.6 * math.exp(-0.3 * l)
        nqc = NTOK if with_ctx else L
        allk = list(range(NTT))
        ctxk = list(range(NT, NTT))
        hTt = ["hT%d" % kc for kc in range(KC)]
        with ExitStack() as es:
            oT = None
            self.oTd = self.nc.dram_tensor("oTd%d" % l, [3 * NB, 128, NTOK], BF16).ap()
            self.otst = [self.sb(es, "otst%d" % i, [128, 128], BF16) for i in range(3)]
            self._otn = 0
            self.rowsL = self.sb(es, "rowsL", [128, cfg.NROWL], F32)
            S.dma("sp", self.rowsL[:], self.dr["rowsL"][l], writes=["rows"])
            lam = self.sb(es, "lam", [128, 4], F32)
            subw = self.sb(es, "subw", [128, 128], F32)
            ljunk = self.sb(es, "ljunk", [128, 64], F32)
            lr = self.row("lam")
            for i in range(2):
                self.V(lambda e: e.tensor_tensor(out=ljunk[:], in0=lr[:, 128 * i:128 * i + 64], in1=lr[:, 128 * i + 64:128 * i + 128],
                                                 op=ALU.mult), r=["rows"], w=["ljunk"])
                self.V(lambda e: e.reduce_sum(out=lam[:, i:i + 1], in_=ljunk[:], axis=AX.X), r=["ljunk"], w=["lam"])
            self.A(lambda e: e.activation(out=lam[:, 0:2], in_=lam[:, 0:2], func=AF.Exp), w=["lam"])
            self.V(lambda e: e.tensor_tensor(out=lam[:, 2:3], in0=lam[:, 1:2], in1=lam[:, 0:1], op=ALU.subtract), w=["lam"])
            self.V(lambda e: e.tensor_scalar(out=lam[:, 2:3], in0=lam[:, 2:3], scalar1=-lam_init, scalar2=None, op0=ALU.add), w=["lam"])
            self.V(lambda e: e.tensor_scalar(out=subw[:], in0=self.row("sub"), scalar1=1.0 - lam_init, scalar2=None,
                                             op0=ALU.mult), r=["rows"], w=["subw"])
            e_rope = ExitStack()
            self.ropeT = self.sb(e_rope, "ropeT", [128, 2 * L], F32)
            S.dma("sp", self.ropeT[:], self.dr["ropeT"], writes=["ropeT"])
            for h in range(cfg.HA):
                with ExitStack() as e2:
                    QT = self.sb(e2, "QT", [128, NTOK], BF16)
                    KT = self.sb(e2, "KT", [128, NTOK], BF16)
                    Va = self.sb(e2, "Va", [128, NTT, 129], BF16)
                    Wv = self.sb(e2, "Wv", [128, KC, 128], BF16)
                    qsc = {"es": e2}
                    self.qk_chunk(l, qsc, hT, win, [(cfg.oAq + h * 128, 128)], QT, "QT", None, nqc, "q", mode="load")
                    self.qk_chunk(l, qsc, hT, win, [(cfg.oAk + h * 128, 128)], KT, "KT", None, NTOK, "k", mode="load")
                    self.wload(Wv, win, [(cfg.oAv + h * 128, 128)], "Wv")
                    self.qk_chunk(l, qsc, hT, win, [(cfg.oAq + h * 128, 128)], QT, "QT", None, nqc, "q", mode="compute")
                    self.qk_chunk(l, qsc, hT, win, [(cfg.oAk + h * 128, 128)], KT, "KT", None, NTOK, "k", mode="compute")
                    self.G(lambda e: e.memset(Va[:, :, 128:129], 1.0), w=["Va"])
                    for ti in range(NTT):
                        b = 5 + ti % 2
                        self.proj_tm(Wv, "Wv", hT, ti, 128, b)
                        self.A(lambda e: e.copy(out=Va[:, ti, 0:128], in_=self.pb[b][:, :128]), w=["Va"], x=[self.bk(b)])
                    fs = [self.sb(e2, "fin%d" % i, [128, 132], F32) for i in range(4)]
                    ft = [self.sb(e2, "fint%d" % i, [128, 128], F32) for i in range(4)]
                    fo = [self.sb(e2, "fino%d" % i, [128, 128], BF16) for i in range(4)]
                    fj = self.sb(e2, "finj", [128, 128], F32)

                    def finishA(ti, a0, a1, btoks, k, phase, h=h):
                        sm, t1, ob = fs[k], ft[k], fo[k]
                        stok, ttok, otok = "fin%d" % k, "fint%d" % k, "fino%d" % k
                        if phase == 2:
                            self.out_transpose(ob[:], otok, oT, h, ti)
                            return
                        self.V(lambda e: e.reciprocal(out=sm[:, 128:129], in_=a0[:, 128:129]), w=[stok], x=btoks)
                        self.V(lambda e: e.reciprocal(out=sm[:, 129:130], in_=a1[:, 128:129]), w=[stok], x=btoks)
                        self.V(lambda e: e.tensor_tensor(out=sm[:, 129:130], in0=sm[:, 129:130], in1=lam[:, 2:3], op=ALU.mult),
                               r=["lam"], w=[stok])
                        self.V(lambda e: e.tensor_scalar(out=t1[:], in0=a1[:, 0:128], scalar1=sm[:, 129:130], scalar2=None,
                                                         op0=ALU.mult), r=[stok], w=[ttok], x=btoks)
                        self.V(lambda e: e.scalar_tensor_tensor(out=sm[:, 0:128], in0=a0[:, 0:128], scalar=sm[:, 128:129], in1=t1[:],
                                                                op0=ALU.mult, op1=ALU.add), r=[ttok], w=[stok], x=btoks)
                        self.V(lambda e: e.tensor_tensor(out=fj[:], in0=sm[:, 0:128], in1=sm[:, 0:128], op=ALU.mult), r=[stok], w=["finj"])
                        self.V(lambda e: e.reduce_sum(out=sm[:, 130:131], in_=fj[:], axis=AX.X), r=["finj"], w=[stok + "s"])
                        self.rstd_col(sm[:, 130:131], 128, stok + "s", [])
                        self.V(lambda e: e.scalar_tensor_tensor(out=ob[:], in0=sm[:, 0:128], scalar=sm[:, 130:131], in1=subw[:],
                                                                op0=ALU.mult, op1=ALU.mult), r=[stok, stok + "s", "subw"], w=[otok])
                    PT = [self.sb(e2, "PT%d" % i, [128, 512], BF16) for i in range(4)]
                    KTz = self.pad_k(e2, KT)
                    ast = {"gi": 0, "pending": None}
                    akw = dict(sbanks=[0, 1, 6], accsets=[[2, 3], [4, 5]], dist=2, state=ast, gq=384)
                    self.attention(PT, QT, KTz, lambda kt: Va[:, kt, :], 128, 0, L, allk, finishA, "QT", "KTz", "Va", **akw)
                    if with_ctx:
                        self.attention(PT, QT, KTz, lambda kt: Va[:, kt, :], 128, L, LC, ctxk, finishA, "QT", "KTz", "Va", **akw)
                    self.attention_flush(ast)
                    S.barrier()
            self.tap("oTa%d" % l, self.oTd[0:NB], ["oTd"])
            if self.stop_after == (l, "mixA"):
                e_rope.close()
                return
            c2_per_hk = max(1, cfg.GRP // 2)
            for hk in range(2):
                with ExitStack() as e2:
                    QT = self.sb(e2, "QT", [128, NTOK], BF16)
                    KT = self.sb(e2, "KT", [128, NTOK], BF16)
                    Vb = self.sb(e2, "Vb", [128, NTT, 65], BF16)
                    Wv = self.sb(e2, "Wv", [128, KC, 64], BF16)
                    qsc = {"es": e2}
                    self.qk_chunk(l, qsc, hT, win, [(cfg.oBk + hk * 64, 64), (cfg.oBk + hk * 64, 64)], KT, "KT", "knc", NTOK, "k", mode="load")
                    self.wload(Wv, win, [(cfg.oBv + hk * 64, 64)], "Wv")
                    self.qk_chunk(l, qsc, hT, win, [(cfg.oBk + hk * 64, 64), (cfg.oBk + hk * 64, 64)], KT, "KT", "knc", NTOK, "k", mode="compute")
                    self.G(lambda e: e.memset(Vb[:, :, 64:65], 1.0), w=["Vb"])
                    for ti in range(NTT):
                        b = 5 + ti % 2
                        self.proj_tm(Wv, "Wv", hT, ti, 64, b)
                        self.A(lambda e: e.copy(out=Vb[:, ti, 0:64], in_=self.pb[b][:, :64]), w=["Vb"], x=[self.bk(b)])
                    fs = [self.sb(e2, "fin%d" % i, [128, 2], F32) for i in range(4)]
                    fo = [self.sb(e2, "fino%d" % i, [128, 128], BF16) for i in range(4)]
                    PT = [self.sb(e2, "PT%d" % i, [128, 512], BF16) for i in range(5)]
                    KTz = self.pad_k(e2, KT)
                    for c2 in range(hk * c2_per_hk, (hk + 1) * c2_per_hk):
                        self.qk_chunk(l, qsc, hT, win, [(cfg.oBq + c2 * 128, 128)], QT, "QT", "qnc", nqc, "q")

                        def finishB(ti, a0, a1, btoks, k, phase, c2=c2):
                            sm, ob = fs[k], fo[k]
                            stok, otok = "fin%d" % k, "fino%d" % k
                            if phase == 2:
                                self.out_transpose(ob[:], otok, oT, NB + c2, ti)
                                return
                            self.V(lambda e: e.reciprocal(out=sm[:, 0:1], in_=a0[:, 64:65]), w=[stok], x=btoks)
                            self.V(lambda e: e.reciprocal(out=sm[:, 1:2], in_=a1[:, 64:65]), w=[stok], x=btoks)
                            self.V(lambda e: e.tensor_scalar(out=ob[:, 0:64], in0=a0[:, 0:64], scalar1=sm[:, 0:1], scalar2=None,
                                                             op0=ALU.mult), r=[stok], w=[otok], x=btoks)
                            self.V(lambda e: e.tensor_scalar(out=ob[:, 64:128], in0=a1[:, 0:64], scalar1=sm[:, 1:2], scalar2=None,
                                                             op0=ALU.mult), r=[stok], w=[otok], x=btoks)
                        ast = {"gi": 0, "pending": None}
                        akw = dict(sbanks=[0, 1, 6, 7], accsets=[[2, 3], [4, 5]], dist=3, state=ast)
                        self.attention(PT, QT, KTz, lambda kt: Vb[:, kt, :], 64, 0, L, allk, finishB, "QT", "KTz", "Vb", **akw)
                        if with_ctx:
                            self.attention(PT, QT, KTz, lambda kt: Vb[:, kt, :], 64, L, LC, ctxk, finishB, "QT", "KTz", "Vb", **akw)
                        self.attention_flush(ast)
                    S.barrier()
            self.tap("oTb%d" % l, self.oTd[NB:2 * NB], ["oTd"])
            S.barrier()
            e_rope.close()
            if self.stop_after == (l, "mixB"):
                return
            self.mix_mlstm(l, hT, win, oT, with_ctx)
            self.tap("oTc%d" % l, self.oTd[2 * NB:3 * NB], ["oTd"])
            if self.stop_after == (l, "mixC"):
                return
            self.mix_merge(l, hT, win, oT, with_ctx)

    def mix_mlstm(self, l, hT, win, oT, with_ctx):
        cfg, S = self.cfg, self.S
        D, L, LC, KC, NT, NTC, NTT, NTOK, HC = cfg.D, cfg.L, cfg.LC, cfg.KC, cfg.NT, cfg.NTC, cfg.NTT, cfg.NTOK, cfg.HC
        NB = HC
        one_col = self.cst(CON)[:, 0:1]
        with ExitStack() as es:
            Wg = self.sb(es, "Wg", [128, KC, 4 * HC], BF16)
            self.wload(Wg, win, [(cfg.oG, 4 * HC)], "Wg")
            Gt = self.sb(es, "Gt", [128, NTT, 4 * HC], F32)
            LF = self.sb(es, "LF", [128, NTT, 2, HC], F32)
            BC = self.sb(es, "BC", [128, NTT, 2, HC], F32)
            TOT = self.sb(es, "TOT", [128, NTT, 2, HC], F32)
            BIAS = self.sb(es, "BIAS", [128, NTT, 2, HC], F32)
            EB = self.sb(es, "EB", [128, NTT, 2, HC], F32)
            WC = self.sb(es, "WC", [128, NTT, 2, HC], F32)
            AC = self.sb(es, "AC", [128, NTT, 2, HC], F32)
            for ti in range(NTT):
                b = 5 + ti % 2
                self.proj_tm(Wg, "Wg", hT, ti, 4 * HC, b)
                self.V(lambda e: e.tensor_tensor(out=Gt[:, ti, :], in0=self.pb[b][:, :4 * HC], in1=self.row("gb"), op=ALU.add),
                       r=["rows"], w=["Gt"], x=[self.bk(b)])
            Gv = Gt[:].rearrange("p t (q h) -> p t q h", h=HC)
            for d in range(2):
                self.A(lambda e: e.activation(out=LF[:, :, d, :], in_=Gv[:, :, 2 * d + 1, :], func=AF.Exp, scale=-1.0), r=["Gt"], w=["LF"])
            self.A(lambda e: e.activation(out=LF[:], in_=LF[:], func=AF.Ln, bias=one_col), r=["consts"], w=["LF"])
            self.V(lambda e: e.tensor_scalar(out=LF[:], in0=LF[:], scalar1=-1.0, scalar2=None, op0=ALU.mult), w=["LF"])
            for ti in range(NTT):
                b = 5 + ti % 2
                self.P(lambda e: e.matmul(self.pb[b][:, 0:HC], lhsT=self.cst(CTU), rhs=LF[:, ti, 0, :], start=True, stop=True),
                       r=["consts", "LF"], x=[self.bk(b)])
                self.P(lambda e: e.matmul(self.pb[b][:, HC:2 * HC], lhsT=self.cst(CTL), rhs=LF[:, ti, 1, :], start=True, stop=True),
                       r=["consts", "LF"], x=[self.bk(b)])
                self.P(lambda e: e.matmul(self.pb[b][:, 2 * HC:4 * HC], lhsT=self.cst(CON), rhs=LF[:, ti, :, :].rearrange("p a b -> p (a b)"),
                                          start=True, stop=True), r=["consts", "LF"], x=[self.bk(b)])
                self.V(lambda e: e.tensor_copy(out=BC[:, ti, :, :].rearrange("p a b -> p (a b)"), in_=self.pb[b][:, 0:2 * HC]), w=["BC"], x=[self.bk(b)])
                self.V(lambda e: e.tensor_copy(out=TOT[:, ti, :, :].rearrange("p a b -> p (a b)"), in_=self.pb[b][:, 2 * HC:4 * HC]), w=["TOT"], x=[self.bk(b)])
            for d in range(2):
                self.V(lambda e: e.tensor_tensor(out=BIAS[:, :, d, :], in0=Gv[:, :, 2 * d, :], in1=BC[:, :, d, :], op=ALU.subtract),
                       r=["Gt", "BC"], w=["BIAS"])
            self.A(lambda e: e.activation(out=EB[:], in_=BC[:], func=AF.Exp), r=["BC"], w=["EB"])
            self.V(lambda e: e.tensor_tensor(out=WC[:], in0=TOT[:], in1=BIAS[:], op=ALU.add), r=["TOT", "BIAS"], w=["WC"])
            self.A(lambda e: e.activation(out=WC[:], in_=WC[:], func=AF.Exp), w=["WC"])
            self.A(lambda e: e.activation(out=AC[:], in_=TOT[:], func=AF.Exp), r=["TOT"], w=["AC"])
            S.barrier()
            for hc in range(HC):
                with ExitStack() as e2:
                    Ws = {}
                    for nm, o in (("q", cfg.oCq), ("k", cfg.oCk), ("v", cfg.oCv), ("o", cfg.oCo)):
                        Ws[nm] = self.sb(e2, "Wc" + nm, [128, KC, 128], BF16)
                        self.wload(Ws[nm], win, [(o + hc * 128, 128)], "Wc" + nm)
                    qT = self.sb(e2, "cqT", [128, NTOK], BF16)
                    kT = self.sb(e2, "ckT", [128, NTOK], BF16)
                    raw = self.sb(e2, "craw", [128, NTOK], F32)
                    acc = self.sb(e2, "cacc", [128, NTOK], F32)
                    cw = self.col("conv%d" % l)
                    for (nm, dst, dtok, scale, chunk) in (("q", qT, "cqT", 1.0, hc), ("k", kT, "ckT", 128.0 ** -0.5, HC + hc)):
                        def cons(ps_ap, g0, gn, b):
                            self.A(lambda e: e.copy(out=raw[:, g0:g0 + gn], in_=ps_ap), w=["craw"], x=[self.bk(b)])
                        self.proj_fm(Ws[nm], "Wc" + nm, hT, 0, NTOK, [5, 6], cons)
                        w0 = cw[:, 0 * 2 * HC + chunk:0 * 2 * HC + chunk + 1]
                        w1 = cw[:, 1 * 2 * HC + chunk:1 * 2 * HC + chunk + 1]
                        w2 = cw[:, 2 * 2 * HC + chunk:2 * 2 * HC + chunk + 1]
                        for (s0, n) in ((0, L), (L, LC)):
                            self.V(lambda e: e.tensor_scalar(out=acc[:, s0:s0 + n], in0=raw[:, s0:s0 + n], scalar1=w1, scalar2=None, op0=ALU.mult),
                                   r=["craw", "cols"], w=["cacc"])
                            self.V(lambda e: e.scalar_tensor_tensor(out=acc[:, s0 + 1:s0 + n], in0=raw[:, s0:s0 + n - 1], scalar=w0,
                                                                    in1=acc[:, s0 + 1:s0 + n], op0=ALU.mult, op1=ALU.add), r=["craw", "cols"], w=["cacc"])
                            self.V(lambda e: e.scalar_tensor_tensor(out=acc[:, s0:s0 + n - 1], in0=raw[:, s0 + 1:s0 + n], scalar=w2,
                                                                    in1=acc[:, s0:s0 + n - 1], op0=ALU.mult, op1=ALU.add), r=["craw", "cols"], w=["cacc"])
                        self.A(lambda e: e.activation(out=raw[:], in_=acc[:], func=AF.Sigmoid), r=["cacc"], w=["craw"])
                        self.V(lambda e: e.scalar_tensor_tensor(out=dst[:], in0=acc[:], scalar=scale, in1=raw[:], op0=ALU.mult, op1=ALU.mult),
                               r=["cacc", "craw"], w=[dtok])
                    Vc = self.sb(e2, "cVc", [128, NTT, 129], BF16)
                    OG = acc[:].rearrange("p (t d) -> p t d", d=128)
                    Kt = self.sb(e2, "cKt", [128, NTT, 128], BF16)
                    HS = self.sb(e2, "cHS", [128, NTT, 128], F32)
                    self.G(lambda e: e.memset(Vc[:, :, 128:129], 1.0), w=["cVc"])
                    self.G(lambda e: e.memset(HS[:], 0.0), w=["cHS"])
                    pv7 = self.pb[7][:].bitcast(BF16)
                    for ti in range(NTT):
                        self.proj_tm(Ws["v"], "Wcv", hT, ti, 128, 5)
                        self.A(lambda e: e.copy(out=Vc[:, ti, 0:128], in_=self.pb[5][:, :128]), w=["cVc"], x=[self.bk(5)])
                        self.P(lambda e: e.transpose(out=pv7[:, 0:128], in_=kT[:, ti * 128:(ti + 1) * 128], identity=self.identb[:]),
                               r=["ckT", "identb"], x=[self.bk(7)])
                        self.V(lambda e: e.tensor_copy(out=Kt[:, ti, :], in_=pv7[:, 0:128]), w=["cKt"], x=[self.bk(7)])
                    INTRA = [raw[:].rearrange("p (t d) -> p t d", d=128), acc[:].rearrange("p (t d) -> p t d", d=128)]
                    itok = ["craw", "cacc"]
                    DENI = self.sb(e2, "cDENI", [128, NTT, 2], F32)
                    SB = [self.sb(e2, "cSB%d" % d, [128, NTT, 129], BF16) for d in range(2)]
                    st = [self.sb(e2, "st%d" % d, [128, 129], F32) for d in range(2)]
                    rot = dict(Lt=[self.sb(e2, "Lt%d" % i, [128, 128], F32) for i in range(3)],
                               DT=[self.sb(e2, "DT%d" % i, [128, 128], F32) for i in range(3)],
                               SM=[self.sb(e2, "SM%d" % i, [128, 128], BF16) for i in range(3)],
                               VW=[self.sb(e2, "VW%d" % i, [128, 129], BF16) for i in range(3)],
                               nd=[self.sb(e2, "nd%d" % i, [128, 132], F32) for i in range(3)])
                    order = [list(range(NT, NTT)) + list(range(NT)), list(range(NTT - 1, NT - 1, -1)) + list(range(NT - 1, -1, -1))]
                    out_tiles = list(range(NTT)) if with_ctx else list(range(NT))
                    for kk in ("Lt", "DT", "SM", "VW"):
                        rot[kk].append(self.sb(e2, kk + "3", [128, 129 if kk == "VW" else 128], BF16 if kk in ("SM", "VW") else F32))
                    items = [(ti, d) for ti in out_tiles for d in range(2)]
                    nit = len(items)

                    def p1A(i):
                        ti, d = items[i]
                        cs = slice(ti * 128, (ti + 1) * 128)
                        bS = (i // 2) % 2
                        if d == 0:
                            self.P(lambda e: e.matmul(self.pb[bS][:, :128], lhsT=kT[:, cs], rhs=qT[:, cs], start=True, stop=True),
                                   r=["ckT", "cqT"], x=[self.bk(bS)])
                        k4 = i % 4
                        bL = 2 + i % 2
                        Lt = rot["Lt"][k4]
                        self.V(lambda e: e.tensor_scalar(out=Lt[:], in0=self.cst(CSL if d == 0 else CSU), scalar1=LF[:, ti, d, hc:hc + 1],
                                                         scalar2=None, op0=ALU.mult), r=["consts"], w=["Lt%d" % k4])
                        self.P(lambda e: e.matmul(self.pb[bL][:, :128], lhsT=Lt[:], rhs=self.cst(CTU if d == 0 else CTL),
                                                  start=True, stop=True), r=["Lt%d" % k4, "consts"], x=[self.bk(bL)])

                    def p1B(i):
                        ti, d = items[i]
                        bS = (i // 2) % 2
                        k4 = i % 4
                        bL = 2 + i % 2
                        DT, SM = rot["DT"][k4], rot["SM"][k4]
                        self.A(lambda e: e.activation(out=DT[:], in_=self.pb[bL][:, :128], func=AF.Exp,
                                                      bias=Gv[:, ti, 2 * d, hc:hc + 1]), w=["DT%d" % k4], x=[self.bk(bL)])
                        self.G(lambda e: e.tensor_tensor(out=DT[:], in0=DT[:], in1=self.cst(CTU if d == 0 else CTL), op=ALU.mult),
                               r=["consts"], w=["DT%d" % k4])
                        self.V(lambda e: e.tensor_tensor(out=SM[:], in0=DT[:], in1=self.pb[bS][:, :128], op=ALU.mult),
                               r=["DT%d" % k4], w=["SM%d" % k4], x=[self.bk(bS)])

                    def p1C(i):
                        ti, d = items[i]
                        k4 = i % 4
                        bI = 4 + i % 2
                        SM = rot["SM"][k4]
                        self.P(lambda e: e.matmul(self.pb[bI][:, :129], lhsT=SM[:], rhs=Vc[:, ti, :], start=True, stop=True),
                               r=["SM%d" % k4, "cVc"], x=[self.bk(bI)])
                        self.A(lambda e: e.copy(out=INTRA[d][:, ti, :], in_=self.pb[bI][:, :128]), w=[itok[d]], x=[self.bk(bI)])
                        self.V(lambda e: e.tensor_copy(out=DENI[:, ti, d:d + 1], in_=self.pb[bI][:, 128:129]), w=["cDENI"], x=[self.bk(bI)])
                    for i in range(nit + 2):
                        if i < nit:
                            p1A(i)
                        if 0 <= i - 1 < nit:
                            p1B(i - 1)
                        if 0 <= i - 2 < nit:
                            p1C(i - 2)
                    for d in range(2):
                        self.G(lambda e: e.memset(st[d][:], 0.0), w=["st%d" % d])
                        self.G(lambda e: e.memset(SB[d][:, 0, :], 0.0), w=["cSB%d" % d])
                    sitems = [(step, d) for step in range(NTT - 1) for d in range(2)]
                    nsi = len(sitems)
                    ubanks = [6, 7, 0, 1]

                    def p2A(i):
                        step, d = sitems[i]
                        ti = order[d][step]
                        k4 = i % 4
                        bU = ubanks[i % 4]
                        VW = rot["VW"][k4]
                        self.V(lambda e: e.tensor_scalar(out=VW[:], in0=Vc[:, ti, :], scalar1=WC[:, ti, d, hc:hc + 1], scalar2=None,
                                                         op0=ALU.mult), r=["cVc"], w=["VW%d" % k4])
                        self.P(lambda e: e.matmul(self.pb[bU][:, :129], lhsT=Kt[:, ti, :], rhs=VW[:], start=True, stop=True),
                               r=["cKt", "VW%d" % k4], x=[self.bk(bU)])

                    def p2B(i):
                        step, d = sitems[i]
                        ti = order[d][step]
                        bU = ubanks[i % 4]
                        self.V(lambda e: e.scalar_tensor_tensor(out=st[d][:], in0=st[d][:], scalar=AC[:, ti, d, hc:hc + 1],
                                                                in1=self.pb[bU][:, :129], op0=ALU.mult, op1=ALU.add),
                               w=["st%d" % d], x=[self.bk(bU)])
                        self.A(lambda e: e.copy(out=SB[d][:, step + 1, :], in_=st[d][:]), r=["st%d" % d], w=["cSB%d" % d])
                    for i in range(nsi + 2):
                        if i < nsi:
                            p2A(i)
                        if 0 <= i - 2 < nsi:
                            p2B(i - 2)
                    n3 = 0
                    for step in range(NTT):
                        for d in range(2):
                            ti = order[d][step]
                            if not (ti < NT or with_ctx):
                                continue
                            cs = slice(ti * 128, (ti + 1) * 128)
                            k3 = n3 % 3
                            bN = 2 + n3 % 4
                            n3 += 1
                            nd = rot["nd"][k3]
                            ntk = "nd%d" % k3
                            self.P(lambda e: e.matmul(self.pb[bN][:, :129], lhsT=qT[:, cs], rhs=SB[d][:, step, :], start=True, stop=True),
                                   r=["cqT", "cSB%d" % d], x=[self.bk(bN)])
                            self.V(lambda e: e.scalar_tensor_tensor(out=nd[:, 0:128], in0=self.pb[bN][:, 0:128], scalar=EB[:, ti, d, hc:hc + 1],
                                                                    in1=INTRA[d][:, ti, :], op0=ALU.mult, op1=ALU.add),
                                   r=[itok[d]], w=[ntk], x=[self.bk(bN)])
                            self.V(lambda e: e.scalar_tensor_tensor(out=nd[:, 128:129], in0=self.pb[bN][:, 128:129], scalar=EB[:, ti, d, hc:hc + 1],
                                                                    in1=DENI[:, ti, d:d + 1], op0=ALU.mult, op1=ALU.add),
                                   r=["cDENI"], w=[ntk], x=[self.bk(bN)])
                            self.V(lambda e: e.scalar_tensor_tensor(out=nd[:, 129:130], in0=nd[:, 128:129], scalar=-1.0, in1=nd[:, 128:129],
                                                                    op0=ALU.mult, op1=ALU.max), w=[ntk])
                            self.V(lambda e: e.tensor_scalar(out=nd[:, 129:130], in0=nd[:, 129:130], scalar1=1.0, scalar2=None, op0=ALU.max), w=[ntk])
                            self.V(lambda e: e.reciprocal(out=nd[:, 129:130], in_=nd[:, 129:130]), w=[ntk])
                            self.G(lambda e: e.scalar_tensor_tensor(out=HS[:, ti, :], in0=nd[:, 0:128], scalar=nd[:, 129:130],
                                                                    in1=HS[:, ti, :], op0=ALU.mult, op1=ALU.add), r=[ntk], w=["cHS"]) \
                                if False else self.V(lambda e: e.scalar_tensor_tensor(out=HS[:, ti, :], in0=nd[:, 0:128], scalar=nd[:, 129:130],
                                                                                      in1=HS[:, ti, :], op0=ALU.mult, op1=ALU.add), r=[ntk], w=["cHS"])
                    S.barrier()
                    for ti in (range(NTT) if with_ctx else range(NT)):
                        b = 5 + ti % 2
                        self.proj_tm(Ws["o"], "Wco", hT, ti, 128, b)
                        self.A(lambda e: e.activation(out=OG[:, ti, :], in_=self.pb[b][:, :128], func=AF.Sigmoid), w=["cacc"], x=[self.bk(b)])
                    fs = self.sb(e2, "cfs", [128, NTT], F32)
                    fj = self.sb(e2, "cfj", [128, 128], F32)
                    fo = [self.sb(e2, "cfo%d" % i, [128, 128], BF16) for i in range(2)]
                    tiles = list(range(NTT)) if with_ctx else list(range(NT))
                    for ti in tiles:
                        self.A(lambda e: e.activation(out=fj[:], in_=HS[:, ti, :], func=AF.Square, accum_out=fs[:, ti:ti + 1]), w=["cfj", "cfs"])
                    self.rstd_col(fs[:, :len(tiles)], 128, "cfs", [])
                    cn = self.row("cn", hc * 128, (hc + 1) * 128)
                    for n_, ti in enumerate(tiles):
                        k = n_ % 2
                        self.V(lambda e: e.scalar_tensor_tensor(out=HS[:, ti, :], in0=HS[:, ti, :], scalar=fs[:, ti:ti + 1], in1=cn,
                                                                op0=ALU.mult, op1=ALU.mult), r=["cfs", "rows"], w=["cHS"])
                        self.G(lambda e: e.tensor_tensor(out=fo[k][:], in0=HS[:, ti, :], in1=OG[:, ti, :], op=ALU.mult), r=["cHS", "cacc"], w=["cfo%d" % k])
                        self.out_transpose(fo[k][:], "cfo%d" % k, oT, 2 * NB + hc, ti)
                    S.barrier()

    def mix_merge(self, l, hT, win, oT, with_ctx):
        cfg, S = self.cfg, self.S
        D, L, KC, NT, NTT, NTOK, NB = cfg.D, cfg.L, cfg.KC, cfg.NT, cfg.NTT, cfg.NTOK, cfg.HA
        ntok = NTOK if with_ctx else L
        wbr = [self.dr[n][l].rearrange("(c p) d -> p c d", p=128) for n in ("w_branch_a", "w_branch_b", "w_branch_c")]
        S.barrier()
        with ExitStack() as es:
            mT = self.sb(es, "mT", [128, KC, 512], BF16)
            acc = self.sb(es, "macc", [128, 512], F32)
            sgs = [self.sb(es, "msg%d" % i, [128, 512], F32) for i in range(2)]
            Wg = [self.sb(es, "mWg%d" % i, [128, KC, 128], BF16) for i in range(6)]
            Wb = [self.sb(es, "mWb%d" % i, [128, NB, 128], BF16) for i in range(6)]
            oTg = [self.sb(es, "oTg%d" % i, [128, NB, 512], BF16) for i in range(3)]
            Wo = self.sb(es, "mWo", [128, KC, D], BF16)
            tmp = [self.sb(es, "mtmp%d" % i, [128, 512], F32) for i in range(2)]
            S.dma("pool", Wo[:], self.dr["w_out"][l].rearrange("(c p) d -> p c d", p=128), writes=["mWo"])
            n = 0
            gcnt = 0
            n2 = 0
            for g0 in range(0, ntok, 512):
                gn = min(512, ntok - g0)
                for i in range(3):
                    S.dma("sp", oTg[i][:, :, :gn], self.oTd[i * NB:(i + 1) * NB, :, g0:g0 + gn].rearrange("c p t -> p c t"),
                          reads=["oTd"], writes=["oTg%d" % i])
                for dc in range(KC):
                    for i in range(3):
                        k = n % 6
                        n += 1
                        self.wload(Wg[k], win, [(cfg.oMG + i * D + dc * 128, 128)], "mWg%d" % k)
                        S.dma("pool", Wb[k][:], wbr[i][:, :, dc * 128:(dc + 1) * 128], writes=["mWb%d" % k])
                        bg = 5 + gcnt % 2
                        bb = 0 + gcnt % 2
                        sg = sgs[gcnt % 2]
                        stok = "msg%d" % (gcnt % 2)
                        gcnt += 1
                        for kc in range(KC):
                            self.P(lambda e: e.matmul(self.pb[bg][:, :gn], lhsT=Wg[k][:, kc, :], rhs=hT[:, kc, g0:g0 + gn],
                                                      start=(kc == 0), stop=(kc == KC - 1)), r=["mWg%d" % k, "hT%d" % kc], x=[self.bk(bg)])
                        self.A(lambda e: e.activation(out=sg[:, :gn], in_=self.pb[bg][:, :gn], func=AF.Sigmoid), w=[stok], x=[self.bk(bg)])
                        for c in range(NB):
                            self.P(lambda e: e.matmul(self.pb[bb][:, :gn], lhsT=Wb[k][:, c, :], rhs=oTg[i][:, c, :gn],
                                                      start=(c == 0), stop=(c == NB - 1)), r=["mWb%d" % k, "oTg%d" % i], x=[self.bk(bb)])
                        if i == 0:
                            self.V(lambda e: e.tensor_tensor(out=acc[:, :gn], in0=sg[:, :gn], in1=self.pb[bb][:, :gn], op=ALU.mult),
                                   r=[stok], w=["macc"], x=[self.bk(bb)])
                        else:
                            self.V(lambda e: e.tensor_tensor(out=sg[:, :gn], in0=sg[:, :gn], in1=self.pb[bb][:, :gn], op=ALU.mult),
                                   w=[stok], x=[self.bk(bb)])
                            if i == 1:
                                self.G(lambda e: e.tensor_tensor(out=acc[:, :gn], in0=acc[:, :gn], in1=sg[:, :gn], op=ALU.add),
                                       r=[stok], w=["macc"])
                            else:
                                self.G(lambda e: e.tensor_tensor(out=mT[:, dc, :gn], in0=acc[:, :gn], in1=sg[:, :gn], op=ALU.add),
                                       r=[stok, "macc"], w=["mT"])
                for tt in range(gn // 128):
                    ti = g0 // 128 + tt
                    w_ = 0 if ti < NT else 1
                    for hf in range(D // 512):
                        k = n2 % 2
                        n2 += 1
                        b = 2 + k
                        for kc in range(KC):
                            self.P(lambda e: e.matmul(self.pb[b][:, :], lhsT=mT[:, kc, tt * 128:(tt + 1) * 128], rhs=Wo[:, kc, hf * 512:(hf + 1) * 512],
                                                      start=(kc == 0), stop=(kc == KC - 1)), r=["mT", "mWo"], x=[self.bk(b)])
                        self.V(lambda e: e.tensor_tensor(out=tmp[k][:], in0=self.pb[b][:, :], in1=self.grow[:, w_, hf * 512:(hf + 1) * 512], op=ALU.mult),
                               r=["grow"], w=["mtmp%d" % k], x=[self.bk(b)])
                        xt = self.src_tile(ti)
                        self.G(lambda e: e.tensor_tensor(out=xt[:, hf * 512:(hf + 1) * 512], in0=xt[:, hf * 512:(hf + 1) * 512], in1=tmp[k][:], op=ALU.add),
                               r=["mtmp%d" % k], w=["x%d" % ti])
            S.barrier()

    def phase_ffn(self, l, with_ctx):
        cfg, S = self.cfg, self.S
        D, L, LC, E, FF, FC, KC, NT, NTC, NTT, NTOK = cfg.D, cfg.L, cfg.LC, cfg.E, cfg.FF, cfg.FC, cfg.KC, cfg.NT, cfg.NTC, cfg.NTT, cfg.NTOK
        self.phase_mod(l, 5, False)
        sets = [dict(t0=0, nt=NT, cap=cfg.CAPL, w=0, s0=0)]
        if with_ctx:
            sets.append(dict(t0=NT, nt=NTC, cap=cfg.CAPC, w=1, s0=cfg.CAPL))
        NS = sum(st["cap"] for st in sets)
        ntl = NTT if with_ctx else NT
        stiles = []
        for si, st in enumerate(sets):
            assert st["s0"] % 128 == 0
            for a in range(0, st["cap"], 128):
                stiles.append((st["s0"] + a, min(128, st["cap"] - a), si))
        NST = len(stiles)
        NH = D // 512
        assert NST * NH <= 6
        identf = self.cst(CI)
        iof = self.row("iof")
        with ExitStack() as es:
            xs = self.sb(es, "xs2", [128, NTT, D], BF16)
            rankTok = self.sb(es, "rankTok", [128, NTT, E], F32)
            LG = self.sb(es, "LG", [128, NTT, E], F32)
            iopj = self.sb(es, "iopj", [128, NST], F32)
            e_row = ExitStack()
            gT = self.sb(e_row, "gT", [E, NTOK], F32)
            rankT = self.sb(e_row, "rankT", [E, NTOK], F32)
            for k, (s_start, nn, si) in enumerate(stiles):
                self.V(lambda e: e.tensor_scalar(out=iopj[:, k:k + 1], in0=self.col("iop"), scalar1=float(s_start), scalar2=None, op0=ALU.add),
                       r=["cols"], w=["iopj"])
            with ExitStack() as e1:
                hT2 = self.sb(e1, "hT2", [128, KC, NTOK], BF16)
                self.phase_norm(e1, l, 1, xs, hT2, with_ctx)
                Wr = self.sb(e1, "Wr", [128, KC, E], BF16)
                S.dma("pool", Wr[:], self.dr["w_router"][l].rearrange("(c p) e -> p c e", p=128), writes=["Wr"])
                mx = self.sb(e1, "lgmx", [128, NTT], F32)
                for ti in range(ntl):
                    b = 5 + ti % 2
                    self.proj_tm(Wr, "Wr", hT2, ti, E, b)
                    self.V(lambda e: e.tensor_copy(out=LG[:, ti, :], in_=self.pb[b][:, :E]), w=["LG"], x=[self.bk(b)])
                lg = LG[:, :ntl, :]
                self.V(lambda e: e.tensor_reduce(out=mx[:, :ntl], in_=lg, axis=AX.X, op=ALU.max), r=["LG"], w=["lgmx"])
                self.V(lambda e: e.tensor_tensor(out=lg, in0=lg, in1=mx[:, :ntl].unsqueeze(2).to_broadcast([128, ntl, E]), op=ALU.subtract),
                       r=["lgmx"], w=["LG"])
                self.A(lambda e: e.activation(out=lg, in_=lg, func=AF.Exp), w=["LG"])
                self.V(lambda e: e.reduce_sum(out=mx[:, :ntl], in_=lg, axis=AX.X), r=["LG"], w=["lgmx"])
                self.V(lambda e: e.reciprocal(out=mx[:, :ntl], in_=mx[:, :ntl]), w=["lgmx"])
                self.V(lambda e: e.tensor_tensor(out=lg, in0=lg, in1=mx[:, :ntl].unsqueeze(2).to_broadcast([128, ntl, E]), op=ALU.mult),
                       r=["lgmx"], w=["LG"])
                for t0 in range(0, ntl, 4):
                    nt_ = min(4, ntl - t0)
                    b = (t0 // 4) % 2
                    for k in range(nt_):
                        self.P(lambda e: e.transpose(out=self.pb[b][0:E, k * 128:(k + 1) * 128], in_=LG[:, t0 + k, :], identity=identf),
                               r=["LG", "consts"], x=[self.bk(b)])
                    self.A(lambda e: e.copy(out=gT[:, t0 * 128:(t0 + nt_) * 128], in_=self.pb[b][0:E, :nt_ * 128]), w=["gT"], x=[self.bk(b)])
                S.barrier()
            with ExitStack() as e1:
                nmax = max(st["nt"] for st in sets) * 128
                work = self.sb(e1, "tkw", [E, nmax], F32)
                MK = self.sb(e1, "tkm", [E, nmax], F32)
                CS = self.sb(e1, "tkc", [E, nmax], F32)
                ones = self.sb(e1, "tko", [E, nmax], F32)
                mx8 = self.sb(e1, "tk8", [E, 8], F32)
                self.G(lambda e: e.memset(ones[:], 1.0), w=["tko"])
                for st in sets:
                    c0, n, cap = st["t0"] * 128, st["nt"] * 128, st["cap"]
                    assert cap % 8 == 0
                    self.V(lambda e: e.tensor_copy(out=work[:, :n], in_=gT[:, c0:c0 + n]), r=["gT"], w=["tkw"])
                    for r_ in range(cap // 8):
                        self.V(lambda e: e.max(out=mx8[:], in_=work[:, :n]), r=["tkw"], w=["tk8"])
                        if r_ < cap // 8 - 1:
                            self.V(lambda e: e.match_replace(out=work[:, :n], in_to_replace=mx8[:], in_values=work[:, :n], imm_value=-1.0),
                                   r=["tk8"], w=["tkw"])
                    self.V(lambda e: e.tensor_scalar(out=MK[:, :n], in0=gT[:, c0:c0 + n], scalar1=mx8[:, 7:8], scalar2=None, op0=ALU.is_ge),
                           r=["gT", "tk8"], w=["tkm"])
                    self.V(lambda e: e.tensor_tensor_scan(out=CS[:, :n], data0=ones[:, :n], data1=MK[:, :n], initial=0.0, op0=ALU.mult, op1=ALU.add),
                           r=["tko", "tkm"], w=["tkc"])
                    self.V(lambda e: e.scalar_tensor_tensor(out=CS[:, :n], in0=CS[:, :n], scalar=float(st["s0"]), in1=MK[:, :n], op0=ALU.add, op1=ALU.mult),
                           r=["tkm"], w=["tkc"])
                    self.V(lambda e: e.tensor_scalar(out=rankT[:, c0:c0 + n], in0=CS[:, :n], scalar1=-1.0, scalar2=None, op0=ALU.add),
                           r=["tkc"], w=["rankT"])
                for ti in range(ntl):
                    b = ti % 2
                    self.P(lambda e: e.transpose(out=self.pb[b][:, 0:E], in_=rankT[0:E, ti * 128:(ti + 1) * 128], identity=identf[0:E, 0:E]),
                           r=["rankT", "consts"], x=[self.bk(b)])
                    self.V(lambda e: e.tensor_copy(out=rankTok[:, ti, :], in_=self.pb[b][:, 0:E]), w=["rankTok"], x=[self.bk(b)])
                S.barrier()
            self.tap("rankT%d" % l, rankT[:, :ntl * 128], [])
            self.tap("gT%d" % l, gT[:, :ntl * 128], [])
            S.barrier()
            e_row.close()
            with ExitStack() as e1:
                CAPM = max(st["cap"] for st in sets)
                Sel = self.sb(e1, "Sel", [128, NTT, CAPM], BF16)
                SelT = [self.sb(e1, "SelT%d" % i, [128, NST, 512], BF16) for i in range(2)]
                gsb = [self.sb(e1, "gsb%d" % i, [128, 512], F32) for i in range(2)]
                repR = [self.sb(e1, "repR%d" % i, [128, 128], F32) for i in range(2)]
                repG = [self.sb(e1, "repG%d" % i, [128, 128], F32) for i in range(2)]
                xeT = self.sb(e1, "xeT", [128, KC, NS], BF16)
                actT = self.sb(e1, "actT", [128, FC, NS], BF16)
                ye = self.sb(e1, "ye", [128, NST, D], BF16)
                sa = [self.sb(e1, "sa%d" % i, [128, NS], F32) for i in range(2)]
                PW = 256
                DP = 2
                NWB = 3
                Wg = [self.sb(e1, "eWg%d" % i, [128, KC, PW], BF16) for i in range(NWB)]
                Wu = [self.sb(e1, "eWu%d" % i, [128, KC, PW], BF16) for i in range(NWB)]
                Wd = [self.sb(e1, "eWd%d" % i, [128, DP, D], BF16) for i in range(NWB)]
                cn = dict(wcnt=0, dcnt=0, scnt=0, ocnt=0, ecnt=0, rcnt=0)

                def do_gather(ex):
                        for st in sets:
                            for k in range(st["nt"]):
                                ti = st["t0"] + k
                                cap = st["cap"]
                                if st["s0"] == 0:
                                    self.V(lambda e: e.tensor_scalar(out=Sel[:, ti, :cap], in0=iof[:, :cap], scalar1=rankTok[:, ti, ex:ex + 1],
                                                                     scalar2=None, op0=ALU.is_equal), r=["rankTok", "rowsG"], w=["Sel"])
                                else:
                                    self.V(lambda e: e.tensor_scalar(out=Sel[:, ti, :cap], in0=iof[:, :cap], scalar1=float(st["s0"]),
                                                                     scalar2=rankTok[:, ti, ex:ex + 1], op0=ALU.add, op1=ALU.is_equal),
                                           r=["rankTok", "rowsG"], w=["Sel"])
                        for fc in range(KC):
                            b = 6 + fc % 2
                            for st in sets:
                                s0, cap, w_ = st["s0"], st["cap"], st["w"]
                                for k in range(st["nt"]):
                                    ti = st["t0"] + k
                                    self.P(lambda e: e.matmul(self.pb[b][:, s0:s0 + cap], lhsT=xs[:, ti, fc * 128:(fc + 1) * 128], rhs=Sel[:, ti, :cap],
                                                              start=(k == 0), stop=(k == st["nt"] - 1)), r=["xs%d" % ti, "Sel"], x=[self.bk(b)])
                                sc_ = self.modA[:, 1, fc, w_:w_ + 1]
                                bi_ = self.modc[:, 3 * KC + fc, w_:w_ + 1]
                                cn["ecnt"] += 1
                                if cn["ecnt"] % 2 == 0:
                                    self.A(lambda e: e.activation(out=xeT[:, fc, s0:s0 + cap], in_=self.pb[b][:, s0:s0 + cap], func=AF.Identity,
                                                                  scale=sc_, bias=bi_), r=["modA", "modc"], w=["xeT"], x=[self.bk(b)])
                                else:
                                    self.V(lambda e: e.tensor_scalar(out=xeT[:, fc, s0:s0 + cap], in0=self.pb[b][:, s0:s0 + cap], scalar1=sc_, scalar2=bi_,
                                                                     op0=ALU.mult, op1=ALU.add), r=["modA", "modc"], w=["xeT"], x=[self.bk(b)])

                def do_gateup(ex):
                        wg_d = self.dr["w_exp_gate"][l, ex].rearrange("(kc p) f -> p kc f", p=128)
                        wu_d = self.dr["w_exp_up"][l, ex].rearrange("(kc p) f -> p kc f", p=128)
                        for pc in range(FF // PW):
                            kb = cn["wcnt"] % NWB
                            cn["wcnt"] += 1
                            S.dma("pool", Wg[kb][:], wg_d[:, :, pc * PW:(pc + 1) * PW], writes=["eWg%d" % kb])
                            S.dma("pool", Wu[kb][:], wu_d[:, :, pc * PW:(pc + 1) * PW], writes=["eWu%d" % kb])
                            for fo in range(PW // 128):
                                fidx = pc * (PW // 128) + fo
                                ba, bu = (0, 1) if fidx % 2 == 0 else (2, 3)
                                for kc in range(KC):
                                    self.P(lambda e: e.matmul(self.pb[ba][:, :NS], lhsT=Wg[kb][:, kc, fo * 128:(fo + 1) * 128], rhs=xeT[:, kc, :],
                                                              start=(kc == 0), stop=(kc == KC - 1)), r=["eWg%d" % kb, "xeT"], x=[self.bk(ba)])
                                for kc in range(KC):
                                    self.P(lambda e: e.matmul(self.pb[bu][:, :NS], lhsT=Wu[kb][:, kc, fo * 128:(fo + 1) * 128], rhs=xeT[:, kc, :],
                                                              start=(kc == 0), stop=(kc == KC - 1)), r=["eWu%d" % kb, "xeT"], x=[self.bk(bu)])
                                sa_ = sa[fidx % 2]
                                self.A(lambda e: e.activation(out=sa_[:], in_=self.pb[ba][:, :NS], func=AF.Silu), w=["sa%d" % (fidx % 2)], x=[self.bk(ba)])
                                self.V(lambda e: e.tensor_tensor(out=actT[:, fidx, :], in0=sa_[:], in1=self.pb[bu][:, :NS], op=ALU.mult),
                                       r=["sa%d" % (fidx % 2)], w=["actT"], x=[self.bk(bu)])

                def do_rest(ex):
                        wd_d = self.dr["w_exp_down"][l, ex].rearrange("(fc p) d -> p fc d", p=128)
                        for pc in range(FC // DP):
                            kb = cn["dcnt"] % NWB
                            cn["dcnt"] += 1
                            S.dma("pool", Wd[kb][:], wd_d[:, pc * DP:(pc + 1) * DP, :], writes=["eWd%d" % kb])
                            for f2 in range(DP):
                                fc = pc * DP + f2
                                for k_st, (s_start, nn, si) in enumerate(stiles):
                                    for hf in range(NH):
                                        b = k_st * NH + hf
                                        self.P(lambda e: e.matmul(self.pb[b][:nn, :512], lhsT=actT[:, fc, s_start:s_start + nn],
                                                                  rhs=Wd[kb][:, f2, hf * 512:(hf + 1) * 512], start=(fc == 0), stop=(fc == FC - 1)),
                                               r=["actT", "eWd%d" % kb], x=[self.bk(b)])
                        for k_st, (s_start, nn, si) in enumerate(stiles):
                            w_ = sets[si]["w"]
                            for hf in range(NH):
                                b = k_st * NH + hf
                                self.V(lambda e: e.tensor_tensor(out=ye[:nn, k_st, hf * 512:(hf + 1) * 512], in0=self.pb[b][:nn, :512],
                                                                 in1=self.grow[:nn, w_, hf * 512:(hf + 1) * 512], op=ALU.mult),
                                       r=["grow"], w=["ye"], x=[self.bk(b)])
                        for si, st in enumerate(sets):
                            c0, n = st["t0"] * 128, st["nt"] * 128
                            mine = [(k_st, s_start, nn) for k_st, (s_start, nn, sj) in enumerate(stiles) if sj == si]
                            for g0 in range(c0, c0 + n, 512):
                                gn = min(512, c0 + n - g0)
                                kb = cn["scnt"] % 2
                                cn["scnt"] += 1
                                for tt in range(gn // 128):
                                    ti = g0 // 128 + tt
                                    rk = cn["rcnt"] % 2
                                    cn["rcnt"] += 1
                                    self.G(lambda e: e.tensor_copy(out=repR[rk][:], in_=rankTok[:, ti, ex:ex + 1].to_broadcast([128, 128])),
                                           r=["rankTok"], w=["repR%d" % rk])
                                    self.G(lambda e: e.tensor_copy(out=repG[rk][:], in_=LG[:, ti, ex:ex + 1].to_broadcast([128, 128])),
                                           r=["LG"], w=["repG%d" % rk])
                                    self.P(lambda e: e.matmul(self.pb[6][:, tt * 128:(tt + 1) * 128], lhsT=repR[rk][:], rhs=identf, start=True, stop=True),
                                           r=["repR%d" % rk, "consts"], x=[self.bk(6)])
                                    self.P(lambda e: e.matmul(self.pb[7][:, tt * 128:(tt + 1) * 128], lhsT=repG[rk][:], rhs=identf, start=True, stop=True),
                                           r=["repG%d" % rk, "consts"], x=[self.bk(7)])
                                self.A(lambda e: e.copy(out=gsb[kb][:, :gn], in_=self.pb[7][:, :gn]), w=["gsb%d" % kb], x=[self.bk(7)])
                                for (k_st, s_start, nn) in mine:
                                    self.V(lambda e: e.scalar_tensor_tensor(out=SelT[kb][:, k_st, :gn], in0=self.pb[6][:, :gn], scalar=iopj[:, k_st:k_st + 1],
                                                                            in1=gsb[kb][:, :gn], op0=ALU.is_equal, op1=ALU.mult),
                                           r=["iopj", "gsb%d" % kb], w=["SelT%d" % kb], x=[self.bk(6)])
                                for tt in range(gn // 128):
                                    ti = g0 // 128 + tt
                                    xt = self.src_tile(ti)
                                    for hf in range(NH):
                                        b = cn["ocnt"] % 6
                                        cn["ocnt"] += 1
                                        for idx, (k_st, s_start, nn) in enumerate(mine):
                                            self.P(lambda e: e.matmul(self.pb[b][:, :512], lhsT=SelT[kb][:nn, k_st, tt * 128:(tt + 1) * 128],
                                                                      rhs=ye[:nn, k_st, hf * 512:(hf + 1) * 512], start=(idx == 0), stop=(idx == len(mine) - 1)),
                                                   r=["SelT%d" % kb, "ye"], x=[self.bk(b)])
                                        self.V(lambda e: e.tensor_tensor(out=xt[:, hf * 512:(hf + 1) * 512], in0=xt[:, hf * 512:(hf + 1) * 512],
                                                                         in1=self.pb[b][:, :512], op=ALU.add), w=["x%d" % ti], x=[self.bk(b)])

                do_gather(0)
                for ex in range(E):
                    do_gateup(ex)
                    if ex + 1 < E:
                        do_gather(ex + 1)
                    do_rest(ex)
                S.barrier()

    def final(self):
        cfg, S = self.cfg, self.S
        D, NT = cfg.D, cfg.NT
        with ExitStack() as es:
            ss = self.sb(es, "fss", [128, NT], F32)
            rstd = self.sb(es, "frstd", [128, NT], F32)
            junk = self.sb(es, "fjunk", [128, D], BF16)
            ot = [self.sb(es, "fo%d" % i, [128, D], F32) for i in range(2)]
            fnr = self.sb(es, "fnr", [128, D], F32)
            S.dma("sp", fnr[:], self.dr["fnrow"], writes=["fnr"])
            for i in range(NT):
                self.A(lambda e: e.activation(out=junk[:], in_=self.x_sb[:, i, :], func=AF.Square,
                                              accum_out=ss[:, i:i + 1]), r=["x%d" % i], w=["fjunk", "fss"])
            self.V(lambda e: e.tensor_scalar(out=rstd[:], in0=ss[:], scalar1=1.0 / D, scalar2=EPS,
                                             op0=ALU.mult, op1=ALU.add), r=["fss"], w=["frstd"])
            self.A(lambda e: e.activation(out=rstd[:], in_=rstd[:], func=AF.Sqrt), r=[], w=["frstd"])
            self.V(lambda e: e.reciprocal(out=rstd[:], in_=rstd[:]), r=[], w=["frstd"])
            for i in range(NT):
                o = ot[i % 2]
                self.V(lambda e: e.scalar_tensor_tensor(out=o[:], in0=self.x_sb[:, i, :], scalar=rstd[:, i:i + 1],
                                                        in1=fnr[:], op0=ALU.mult, op1=ALU.mult),
                       r=["x%d" % i, "frstd", "fnr"], w=["fo%d" % (i % 2)])
                S.dma("sp", self.y[i * 128:(i + 1) * 128, :], o[:], reads=["fo%d" % (i % 2)], writes=["y%d" % i])
            S.barrier()


def host_packs(cfg, inp, b):
    D, KC, DEPTH = cfg.D, cfg.KC, cfg.DEPTH
    cols = np.zeros((128, cfg.NCOL), np.float32)

    def colset(name, v):
        o, w = cfg.coff[name]
        cols[:, o:o + w] = np.asarray(v, np.float32).reshape(w, 128).T
    colset("c", inp["c"][b])
    colset("cctx", inp["c_ctx"])
    for l in range(DEPTH):
        colset("bada%d" % l, inp["b_ada"][l])
        colset("n1%d" % l, inp["norm1_w"][l])
        colset("n2%d" % l, inp["norm2_w"][l])
        colset("conv%d" % l, np.asarray(inp["mlstm_conv_w"][l]).reshape(-1))
        qn = np.asarray(inp["gqa_qnorm_w"][l], np.float32)
        kn = np.asarray(inp["gqa_knorm_w"][l], np.float32)
        perm = np.concatenate([np.arange(32, 64), np.arange(0, 32)])
        o, _ = cfg.coff["qnc%d" % l]
        cols[:, o] = np.tile(qn, 2)
        cols[:, o + 1] = np.tile(qn[perm], 2)
        o, _ = cfg.coff["knc%d" % l]
        cols[:, o] = np.tile(kn, 2)
        cols[:, o + 1] = np.tile(kn[perm], 2)
    cols[:, cfg.coff["iop"][0]] = np.arange(128)
    cols[:, cfg.coff["eps"][0]] = EPS
    rowsL = np.zeros((DEPTH, 128, cfg.NROWL), np.float32)
    rowsG = np.zeros((128, cfg.NROWG), np.float32)
    brows = np.zeros((DEPTH, 128, 6 * D), np.float32)

    def rowset(arr, name, v):
        o, w = cfg.roff[name]
        arr[:, o:o + w] = np.asarray(v, np.float32).reshape(1, w)
    for l in range(DEPTH):
        rowset(rowsL[l], "sub", inp["diff_subln_w"][l])
        rowset(rowsL[l], "cn", inp["mlstm_norm_w"][l])
        rowset(rowsL[l], "gb", inp["mlstm_gate_b"][l])
        rowset(rowsL[l], "lam", np.asarray(inp["diff_lambda"][l]).reshape(-1))
        brows[l] = np.asarray(inp["b_ada"][l], np.float32).reshape(1, 6 * D)
    rowset(rowsG, "iof", np.arange(256))
    rows = (rowsL, rowsG, brows)
    return cols, rows


def make_in_maps(cfg, inp, cores):
    cosT, sinT = rope_tables(cfg)
    ropeT = np.concatenate([cosT, sinT], axis=1)
    consts = const_pack()
    E = cfg.E
    esel = np.zeros((E, E * 128), np.float32)
    for e in range(E):
        esel[e, e * 128:(e + 1) * 128] = 1.0
    shared = {k: np.ascontiguousarray(np.asarray(inp[k], np.float32)) for k in
              ("w_ada", "w_in", "w_branch_a", "w_branch_b", "w_branch_c", "w_out", "w_router",
               "w_exp_gate", "w_exp_up", "w_exp_down")}
    maps = []
    for b in cores:
        cols, rows = host_packs(cfg, inp, b)
        m = {"x": np.ascontiguousarray(inp["x"][b], np.float32), "ctx": np.ascontiguousarray(inp["ctx"][b], np.float32),
             "cols": cols, "rowsL": rows[0], "rowsG": rows[1], "brows": rows[2], "fnrow": np.ascontiguousarray(np.broadcast_to(np.asarray(inp["final_norm_w"], np.float32).reshape(1, -1), (128, cfg.D))), "consts": consts, "ropeT": ropeT, "esel": esel}
        m.update(shared)
        maps.append(m)
    return maps


_CACHE = {}


def kernel(**inputs):
    cfg = Cfg()
    if "nc" not in _CACHE:
        _CACHE["nc"] = Builder(cfg).build()
    nc = _CACHE["nc"]
    inp = {k: np.asarray(v) for k, v in inputs.items()}
    n = inp["x"].shape[0]
    in_maps = make_in_maps(cfg, inp, list(range(n)))
    res = run_bass_kernel_spmd(nc, in_maps, core_ids=list(range(n)))
    return np.stack([np.asarray(r["y"], np.float32) for r in res.results], axis=0)
```

```python
import math
from contextlib import ExitStack

import numpy as np
import concourse.bass as bass
import concourse.mybir as mybir
from concourse.bass_utils import run_bass_kernel_spmd

F32 = mybir.dt.float32
BF16 = mybir.dt.bfloat16
AF = mybir.ActivationFunctionType
ALU = mybir.AluOpType
AX = mybir.AxisListType
EPS = 1e-6


class Sched:
    def __init__(self, nc, n_dma_sems=32, same_engine_sync=True):
        self.nc = nc
        self.eng = {"pe": nc.tensor, "dve": nc.vector, "act": nc.scalar, "pool": nc.gpsimd, "sp": nc.sync}
        self.sem = {k: nc.alloc_semaphore(name="s_" + k) for k in self.eng}
        self.cnt = {k: 0 for k in self.eng}
        self.waited = {k: {} for k in self.eng}
        self.dsem = [nc.alloc_semaphore(name="d%d" % i) for i in range(2 * n_dma_sems)]
        self.dcnt = [0] * (2 * n_dma_sems)
        self.nds = n_dma_sems
        self.drr = [0, 0]
        self.tok = {}
        self.same = same_engine_sync
        self.n_inst = 0
        self.n_wait = 0

    def _st(self, t):
        s = self.tok.get(t)
        if s is None:
            s = self.tok[t] = [None, []]
        return s

    def _wait(self, engname, deps):
        need = {}
        for ev, skip_same in deps:
            if ev is None:
                continue
            sem, val, src = ev
            if src == engname and (skip_same or not self.same or engname == "pe"):
                continue
            k = sem.num
            if k not in need or need[k][1] < val:
                need[k] = (sem, val)
        e = self.eng[engname]
        w = self.waited[engname]
        for k, (sem, val) in need.items():
            if w.get(k, 0) < val:
                e.wait_ge(sem, val)
                w[k] = val
                self.n_wait += 1

    def _deps(self, reads, writes, excl):
        deps = []
        for t in reads:
            deps.append((self._st(t)[0], False))
        for t in writes:
            s = self._st(t)
            deps.append((s[0], False))
            deps.extend((r, False) for r in s[1])
        for t in excl:
            s = self._st(t)
            deps.append((s[0], True))
        return deps

    def _commit(self, ev, reads, writes, excl):
        for t in reads:
            self._st(t)[1].append(ev)
        for t in writes:
            s = self._st(t)
            s[0] = ev
            s[1] = []
        for t in excl:
            s = self._st(t)
            s[0] = ev
            s[1] = []

    def op(self, engname, fn, reads=(), writes=(), excl=()):
        self._wait(engname, self._deps(reads, writes, excl))
        inst = fn(self.eng[engname])
        self.cnt[engname] += 1
        ev = (self.sem[engname], self.cnt[engname], engname)
        inst.then_inc(ev[0], 1)
        self._commit(ev, reads, writes, excl)
        self.n_inst += 1
        return ev

    def dma(self, queue, out, in_, reads=(), writes=(), **kw):
        self._wait(queue, self._deps(reads, writes, ()))
        q = 1 if queue == "pool" else 0
        i = q * self.nds + self.drr[q]
        self.drr[q] = (self.drr[q] + 1) % self.nds
        inst = self.eng[queue].dma_start(out=out, in_=in_, **kw)
        self.dcnt[i] += 16
        ev = (self.dsem[i], self.dcnt[i], "dma")
        inst.then_inc(ev[0], 16)
        self._commit(ev, reads, writes, ())
        self.n_inst += 1
        return ev

    def wait_all(self, engname):
        deps = [((self.sem[k], self.cnt[k], k), False) for k in self.eng if self.cnt[k] > 0 and k != engname]
        deps += [((self.dsem[i], self.dcnt[i], "dma"), False) for i in range(len(self.dsem)) if self.dcnt[i] > 0]
        self._wait(engname, deps)

    def barrier(self):
        for k in ("pe", "dve", "act", "pool", "sp"):
            self.wait_all(k)
        self.tok = {}


class Cfg:
    def __init__(s, D=1024, L=2048, LC=256, E=16, FF=2048, DEPTH=2, GW=64):
        s.D, s.L, s.LC, s.E, s.FF, s.DEPTH, s.GW = D, L, LC, E, FF, DEPTH, GW
        s.KC = D // 128
        s.NT = L // 128
        s.NTC = LC // 128
        s.NTT = s.NT + s.NTC
        s.NTOK = s.NTT * 128
        MW = s.MW = D // 2
        s.HA = MW // 128
        s.HBQ = MW // 64
        s.GRP = s.HBQ // 2
        s.HC = MW // 128
        s.FC = FF // 128
        s.CAPL = 2 * L // E
        s.CAPC = 2 * LC // E
        s.oAq, s.oAk, s.oAv, s.oBq = 0, MW, 2 * MW, 3 * MW
        s.oBk, s.oBv = 4 * MW, 4 * MW + 128
        s.oCq, s.oCk, s.oCv, s.oCo = 4 * MW + 256, 5 * MW + 256, 6 * MW + 256, 7 * MW + 256
        s.oG = 8 * MW + 256
        s.oMG = s.oG + 4 * s.HC
        s.INC = s.oMG + 3 * D
        off = {}
        n = 0

        def add(name, w):
            nonlocal n
            off[name] = (n, w)
            n += w
        add("c", s.KC)
        add("cctx", s.KC)
        for l in range(DEPTH):
            add("bada%d" % l, 6 * s.KC)
            add("n1%d" % l, s.KC)
            add("n2%d" % l, s.KC)
            add("conv%d" % l, 3 * 2 * s.HC)
            add("qnc%d" % l, 2)
            add("knc%d" % l, 2)
        add("cosT", 0)
        add("iop", 1)
        add("eps", 1)
        s.coff, s.NCOL = off, n
        roff = {}
        n = 0

        def addr(name, w):
            nonlocal n
            roff[name] = (n, w)
            n += w
        addr("sub", 128)
        addr("cn", MW)
        addr("gb", 4 * s.HC)
        addr("lam", 256)
        s.NROWL = n
        n = 0
        addr("iof", 256)
        s.roff, s.NROWG = roff, n


def rope_tables(cfg):
    L, GW = cfg.L, cfg.GW
    t = np.arange(L)
    rows = (t // GW).astype(np.float32)
    cols = (t % GW).astype(np.float32)
    nf = 16
    inv = (10000.0 ** (-np.arange(nf, dtype=np.float32) / nf)).astype(np.float32)
    ang = np.concatenate([rows[:, None] * inv, cols[:, None] * inv], axis=-1).astype(np.float32)
    cos = np.cos(ang).astype(np.float32)
    sin = np.sin(ang).astype(np.float32)
    cosT = np.zeros((128, L), np.float32)
    sinT = np.zeros((128, L), np.float32)
    for p in range(128):
        d = p % 64
        f = d % 32
        cosT[p] = cos[:, f]
        sinT[p] = -sin[:, f] if d < 32 else sin[:, f]
    return cosT, sinT


def const_pack():
    r = np.arange(128)
    ident = np.eye(128, dtype=np.float32)
    triU = (r[:, None] <= r[None, :]).astype(np.float32)
    triL = (r[:, None] >= r[None, :]).astype(np.float32)
    sU = (r[:, None] < r[None, :]).astype(np.float32)
    sL = (r[:, None] > r[None, :]).astype(np.float32)
    ones = np.ones((128, 128), np.float32)
    blk = (r[:, None] // 64 == r[None, :] // 64).astype(np.float32)
    return np.concatenate([ident, triU, triL, sU, sL, ones, blk], axis=1)


CI, CTU, CTL, CSU, CSL, CON, CBK = range(7)


class Builder:
    def __init__(self, cfg, taps=None, stop_after=None):
        self.cfg = cfg
        self.taps = taps or {}
        self.stop_after = stop_after
        self.nc = bass.Bass("TRN2", target_bir_lowering=False)
        self.S = None

    def sb(self, es, name, shape, dt):
        self._uid = getattr(self, "_uid", 0) + 1
        return es.enter_context(self.nc.sbuf_tensor("%s_%d" % (name, self._uid), list(shape), dt))

    def V(self, fn, r=(), w=(), x=()):
        return self.S.op("dve", fn, r, w, x)

    def A(self, fn, r=(), w=(), x=()):
        return self.S.op("act", fn, r, w, x)

    def G(self, fn, r=(), w=(), x=()):
        return self.S.op("pool", fn, r, w, x)

    def P(self, fn, r=(), w=(), x=()):
        return self.S.op("pe", fn, r, w, x)

    def bk(self, i):
        return "pb%d" % i

    def cst(self, k):
        return self.consts[:, k * 128:(k + 1) * 128]

    def col(self, name, a=0, b=None):
        o, w = self.cfg.coff[name]
        if b is None:
            b = w
        return self.cols[:, o + a:o + b]

    def row(self, name, a=0, b=None):
        o, w = self.cfg.roff[name]
        if b is None:
            b = w
        t = self.rowsG if name in ("iof",) else self.rowsL
        return t[:, o + a:o + b]

    def tap(self, name, ap_sb, reads):
        if name not in self.taps:
            return
        shape = list(ap_sb.shape)
        d = self.nc.dram_tensor("tap_" + name, shape, ap_sb.dtype, kind="ExternalOutput").ap()
        self.S.dma("sp", d, ap_sb, reads=reads, writes=["tapd_" + name])

    def build(self):
        cfg, nc = self.cfg, self.nc
        D, L, LC, E, FF, DEPTH = cfg.D, cfg.L, cfg.LC, cfg.E, cfg.FF, cfg.DEPTH
        KC, NT, NTC, NTT = cfg.KC, cfg.NT, cfg.NTC, cfg.NTT
        dr = {}

        def din(name, shape, dt=F32):
            dr[name] = nc.dram_tensor(name, list(shape), dt, kind="ExternalInput").ap()
            return dr[name]
        din("x", [L, D])
        din("ctx", [LC, D])
        din("cols", [128, cfg.NCOL])
        din("rowsL", [DEPTH, 128, cfg.NROWL])
        din("rowsG", [128, cfg.NROWG])
        din("brows", [DEPTH, 128, 6 * D])
        din("fnrow", [128, D])
        din("consts", [128, 7 * 128])
        din("ropeT", [128, 2 * L])
        din("esel", [E, E * 128])
        din("w_ada", [DEPTH, D, 6 * D])
        din("w_in", [DEPTH, D, cfg.INC])
        din("w_branch_a", [DEPTH, cfg.MW, D])
        din("w_branch_b", [DEPTH, cfg.MW, D])
        din("w_branch_c", [DEPTH, cfg.MW, D])
        din("w_out", [DEPTH, D, D])
        din("w_router", [DEPTH, D, E])
        din("w_exp_gate", [DEPTH, E, D, FF])
        din("w_exp_up", [DEPTH, E, D, FF])
        din("w_exp_down", [DEPTH, E, FF, D])
        self.dr = dr
        self.y = nc.dram_tensor("y", [L, D], F32, kind="ExternalOutput").ap()
        self.S = Sched(nc)
        S = self.S
        with ExitStack() as es:
            self.pb = [es.enter_context(nc.psum_tensor("pb%d" % i, [128, 512], F32)) for i in range(8)]
            self.x_sb = self.sb(es, "x_sb", [128, NT, D], F32)
            self.c_sb = self.sb(es, "c_sb", [128, NTC, D], F32)
            self.cols = self.sb(es, "cols_sb", [128, cfg.NCOL], F32)
            self.rowsG = self.sb(es, "rowsG_sb", [128, cfg.NROWG], F32)
            self.consts = self.sb(es, "consts_sb", [128, 7 * 128], F32)
            self.identb = self.sb(es, "identb", [128, 128], BF16)
            self.silc = self.sb(es, "silc", [128, KC, 2], F32)
            self.modc = self.sb(es, "modc", [128, 6 * KC, 2], F32)
            self.modA = self.sb(es, "modA", [128, 2, KC, 2], F32)
            self.grow = self.sb(es, "grow", [128, 2, D], F32)
            S.dma("sp", self.cols[:], dr["cols"], writes=["cols"])
            S.dma("sp", self.rowsG[:], dr["rowsG"], writes=["rowsG"])
            S.dma("sp", self.consts[:], dr["consts"], writes=["consts"])
            for i in range(NT):
                S.dma("sp", self.x_sb[:, i, :], dr["x"][i * 128:(i + 1) * 128, :], writes=["x%d" % i])
            for i in range(NTC):
                S.dma("sp", self.c_sb[:, i, :], dr["ctx"][i * 128:(i + 1) * 128, :], writes=["x%d" % (NT + i)])
            self.V(lambda e: e.tensor_copy(out=self.identb[:], in_=self.cst(CI)), r=["consts"], w=["identb"])
            self.A(lambda e: e.activation(out=self.silc[:, :, 0], in_=self.col("c"), func=AF.Silu), r=["cols"], w=["silc"])
            self.A(lambda e: e.activation(out=self.silc[:, :, 1], in_=self.col("cctx"), func=AF.Silu), r=["cols"], w=["silc"])
            for l in range(DEPTH):
                self.layer(l)
                if self.stop_after is not None and self.stop_after[0] == l:
                    break
            self.final()
            S.barrier()
        return nc

    def src_tile(self, i):
        return self.x_sb[:, i, :] if i < self.cfg.NT else self.c_sb[:, i - self.cfg.NT, :]

    def phase_mod(self, l, rowsec, do_cols):
        cfg, S = self.cfg, self.S
        D, KC = cfg.D, cfg.KC
        wa_d = self.dr["w_ada"][l].rearrange("(kc p) f -> p kc f", p=128)
        npiece = 6 * D // 512
        with ExitStack() as es:
            wa = [self.sb(es, "wa%d" % i, [128, KC, 512], F32) for i in range(2)]
            wab = [self.sb(es, "wab%d" % i, [128, KC, 512], BF16) for i in range(2)]
            silcb = self.sb(es, "silcb", [128, KC, 2], BF16)
            self.V(lambda e: e.tensor_copy(out=silcb[:], in_=self.silc[:]), r=["silc"], w=["silcb"])
            rep = self.sb(es, "rep", [128, KC, 2, 128], F32)
            brow = self.sb(es, "brow", [128, D], F32)
            S.dma("sp", brow[:], self.dr["brows"][l][:, rowsec * D:(rowsec + 1) * D], writes=["brow"])
            for kc in range(KC):
                for w_ in range(2):
                    self.V(lambda e: e.tensor_copy(out=rep[:, kc, w_, :], in_=self.silc[:, kc, w_:w_ + 1].to_broadcast([128, 128])),
                           r=["silc"], w=["rep"])
            jj = 0
            for j in range(npiece):
                sec = (j * 512) // D
                off = j * 512 - sec * D
                if not do_cols and sec != rowsec:
                    continue
                buf = wa[jj % 2]
                tk = "wa%d" % (jj % 2)
                pbk = self.pb[jj % 2]
                bkt = self.bk(jj % 2)
                jj += 1
                if sec == rowsec:
                    S.dma("sp", buf[:], wa_d[:, :, j * 512:(j + 1) * 512], writes=[tk])
                if do_cols:
                    bufb = wab[(jj - 1) % 2]
                    tkb = "wab%d" % ((jj - 1) % 2)
                    S.dma("pool", bufb[:], wa_d[:, :, j * 512:(j + 1) * 512], writes=[tkb])
                    for s_ in range(4):
                        for kc in range(KC):
                            self.P(lambda e: e.matmul(pbk[:, s_ * 2:s_ * 2 + 2], lhsT=bufb[:, kc, s_ * 128:(s_ + 1) * 128],
                                                      rhs=silcb[:, kc, :], start=(kc == 0), stop=(kc == KC - 1)),
                                   r=[tkb, "silcb"], x=[bkt])
                    o, _ = cfg.coff["bada%d" % l]
                    self.V(lambda e: e.tensor_tensor(
                        out=self.modc[:, j * 4:(j + 1) * 4, :],
                        in0=pbk[:, 0:8].rearrange("p (a b) -> p a b", b=2),
                        in1=self.cols[:, o + j * 4:o + (j + 1) * 4].unsqueeze(2).to_broadcast([128, 4, 2]),
                        op=ALU.add), r=["cols"], w=["modc"], x=[bkt])
                if sec == rowsec:
                    for w_ in range(2):
                        pb2 = self.pb[2 + w_]
                        for kc in range(KC):
                            self.P(lambda e: e.matmul(pb2[:, :], lhsT=rep[:, kc, w_, :], rhs=buf[:, kc, :],
                                                      start=(kc == 0), stop=(kc == KC - 1)),
                                   r=[tk, "rep"], x=[self.bk(2 + w_)])
                        self.V(lambda e: e.tensor_tensor(out=self.grow[:, w_, off:off + 512], in0=pb2[:, :],
                                                         in1=brow[:, off:off + 512], op=ALU.add),
                               r=["brow"], w=["grow"], x=[self.bk(2 + w_)])
            if do_cols:
                for ni, (nname, scsec) in enumerate((("n1%d" % l, 1), ("n2%d" % l, 4))):
                    self.V(lambda e: e.scalar_tensor_tensor(
                        out=self.modA[:, ni, :, :], in0=self.modc[:, scsec * KC:(scsec + 1) * KC, :], scalar=1.0,
                        in1=self.col(nname).unsqueeze(2).to_broadcast([128, KC, 2]), op0=ALU.add, op1=ALU.mult),
                        r=["modc", "cols"], w=["modA"])
            S.barrier()

    def phase_norm(self, es, l, ni, xs, hT, with_ctx=True):
        cfg, S = self.cfg, self.S
        D, KC, NT, NTT = cfg.D, cfg.KC, cfg.NT, cfg.NTT
        shsec = 0 if ni == 0 else 3
        ntl = NTT if with_ctx else NT
        with ExitStack() as es2:
            ss = self.sb(es2, "nss", [128, NTT], F32)
            rstd = self.sb(es2, "nrstd", [128, NTT], F32)
            junk = self.sb(es2, "njunk", [128, D], BF16)
            for i in range(ntl):
                self.A(lambda e: e.activation(out=junk[:], in_=self.src_tile(i), func=AF.Square,
                                              accum_out=ss[:, i:i + 1]), r=["x%d" % i], w=["njunk", "nss"])
            self.V(lambda e: e.tensor_scalar(out=rstd[:, :ntl], in0=ss[:, :ntl], scalar1=1.0 / D, scalar2=EPS,
                                             op0=ALU.mult, op1=ALU.add), r=["nss"], w=["nrstd"])
            self.A(lambda e: e.activation(out=rstd[:, :ntl], in_=rstd[:, :ntl], func=AF.Sqrt), r=[], w=["nrstd"])
            self.V(lambda e: e.reciprocal(out=rstd[:, :ntl], in_=rstd[:, :ntl]), r=[], w=["nrstd"])
            for i in range(ntl):
                self.V(lambda e: e.tensor_scalar(out=xs[:, i, :], in0=self.src_tile(i), scalar1=rstd[:, i:i + 1],
                                                 scalar2=None, op0=ALU.mult), r=["x%d" % i, "nrstd"], w=["xs%d" % i])
            groups = [(g, min(4, NT - g), 0) for g in range(0, NT, 4)]
            if with_ctx:
                groups += [(NT + g, min(4, cfg.NTC - g), 1) for g in range(0, cfg.NTC, 4)]
            n = 0
            for fc in range(KC):
                for (t0, nt_, w_) in groups:
                    b = n % 2
                    n += 1
                    pv = self.pb[b][:].bitcast(BF16)
                    for k in range(nt_):
                        self.P(lambda e: e.transpose(out=pv[:, k * 128:(k + 1) * 128],
                                                     in_=xs[:, t0 + k, fc * 128:(fc + 1) * 128], identity=self.identb[:]),
                               r=["xs%d" % (t0 + k), "identb"], x=[self.bk(b)])
                    dst = hT[:, fc, t0 * 128:(t0 + nt_) * 128]
                    sc = self.modA[:, ni, fc, w_:w_ + 1]
                    bi = self.modc[:, shsec * KC + fc, w_:w_ + 1]
                    if n % 2 == 0:
                        self.A(lambda e: e.activation(out=dst, in_=pv[:, :nt_ * 128], func=AF.Identity, scale=sc, bias=bi),
                               r=["modA", "modc"], w=["hT%d" % fc], x=[self.bk(b)])
                    else:
                        self.V(lambda e: e.tensor_scalar(out=dst, in0=pv[:, :nt_ * 128], scalar1=sc, scalar2=bi,
                                                         op0=ALU.mult, op1=ALU.add),
                               r=["modA", "modc"], w=["hT%d" % fc], x=[self.bk(b)])
            S.barrier()

    def layer(self, l):
        cfg = self.cfg
        with_ctx = l < cfg.DEPTH - 1
        self.phase_mod(l, 2, True)
        if self.stop_after == (l, "mod"):
            return
        with ExitStack() as es:
            hT = self.sb(es, "hT", [128, cfg.KC, cfg.NTOK], BF16)
            with ExitStack() as e0:
                xs = self.sb(e0, "xs", [128, cfg.NTT, cfg.D], BF16)
                self.phase_norm(e0, l, 0, xs, hT, True)
            self.tap("hT%d" % l, hT[:], ["hT%d" % fc for fc in range(cfg.KC)])
            if self.stop_after == (l, "norm1"):
                return
            self.phase_mix(l, hT, with_ctx)
        if self.stop_after is not None and self.stop_after[0] == l and self.stop_after[1] != "ffn":
            return
        self.phase_ffn(l, with_ctx)

    def wload(self, dst, win, slices, tok):
        a = 0
        for (c0, n) in slices:
            self.S.dma("pool", dst[:, :, a:a + n], win[:, :, c0:c0 + n], writes=[tok])
            a += n

    def proj_fm(self, W, wtok, hT, col0, ncols, banks, consume):
        KC = self.cfg.KC
        gi = 0
        for g0 in range(col0, col0 + ncols, 512):
            gn = min(512, col0 + ncols - g0)
            b = banks[gi % len(banks)]
            gi += 1
            for kc in range(KC):
                self.P(lambda e: e.matmul(self.pb[b][:, :gn], lhsT=W[:, kc, :], rhs=hT[:, kc, g0:g0 + gn],
                                          start=(kc == 0), stop=(kc == KC - 1)),
                       r=[wtok, "hT%d" % kc], x=[self.bk(b)])
            consume(self.pb[b][:, :gn], g0, gn, b)

    def proj_tm(self, W, wtok, hT, ti, ncols, b):
        KC = self.cfg.KC
        for kc in range(KC):
            self.P(lambda e: e.matmul(self.pb[b][:, :ncols], lhsT=hT[:, kc, ti * 128:(ti + 1) * 128], rhs=W[:, kc, :ncols],
                                      start=(kc == 0), stop=(kc == KC - 1)),
                   r=[wtok, "hT%d" % kc], x=[self.bk(b)])

    def qk_chunk(self, l, es, hT, win, nat, dst, dtok, nrm, nq_cols, pf, mode="both"):
        cfg = self.cfg
        L, KC = cfg.L, cfg.KC
        if "qk_t1" not in es:
            for nm in ("qk_t1", "qk_t2", "qk_sq", "qk_rs"):
                es[nm] = self.sb(es["es"], nm, [128, 512], F32)
        if pf + "W" not in es:
            es[pf + "W"] = self.sb(es["es"], pf + "qkW", [128, KC, 128], BF16)
            es[pf + "Wp"] = self.sb(es["es"], pf + "qkWp", [128, KC, 128], BF16)
        W, Wp = es[pf + "W"], es[pf + "Wp"]
        wtok, wptok = pf + "qkW", pf + "qkWp"
        pf = ""
        perm = []
        for (c0, n) in nat:
            for a in range(0, n, 64):
                perm += [(c0 + a + 32, 32), (c0 + a, 32)]
        if mode in ("both", "load"):
            self.wload(W, win, nat, wtok)
            self.wload(Wp, win, perm, wptok)
        if mode == "load":
            return
        t1, t2, sq, rs = es["qk_t1"], es["qk_t2"], es["qk_sq"], es["qk_rs"]
        cosT, sinT = self.ropeT[:, 0:L], self.ropeT[:, L:2 * L]
        for g0 in range(0, nq_cols, 512):
            gn = min(512, nq_cols - g0)
            lat = g0 < L
            gi_ = g0 // 512
            bq, bp, bs_ = [0, 2, 4][gi_ % 3], [1, 3, 5][gi_ % 3], 6 + gi_ % 2
            for kc in range(KC):
                self.P(lambda e: e.matmul(self.pb[bq][:, :gn], lhsT=W[:, kc, :], rhs=hT[:, kc, g0:g0 + gn],
                                          start=(kc == 0), stop=(kc == KC - 1)), r=[wtok, "hT%d" % kc], x=[self.bk(bq)])
            if lat:
                for kc in range(KC):
                    self.P(lambda e: e.matmul(self.pb[bp][:, :gn], lhsT=Wp[:, kc, :], rhs=hT[:, kc, g0:g0 + gn],
                                              start=(kc == 0), stop=(kc == KC - 1)), r=[wptok, "hT%d" % kc], x=[self.bk(bp)])
            pq, pp = self.pb[bq][:, :gn], self.pb[bp][:, :gn]
            if nrm is not None:
                self.A(lambda e: e.activation(out=sq[:, :gn], in_=pq, func=AF.Square), w=[pf + "qk_sq"], x=[self.bk(bq)])
                self.P(lambda e: e.matmul(self.pb[bs_][:, :gn], lhsT=self.cst(CBK), rhs=sq[:, :gn], start=True, stop=True),
                       r=["consts", pf + "qk_sq"], x=[self.bk(bs_)])
                self.V(lambda e: e.tensor_scalar(out=rs[:, :gn], in0=self.pb[bs_][:, :gn], scalar1=1.0 / 64, scalar2=EPS,
                                                 op0=ALU.mult, op1=ALU.add), w=[pf + "qk_rs"], x=[self.bk(bs_)])
                self.A(lambda e: e.activation(out=rs[:, :gn], in_=rs[:, :gn], func=AF.Sqrt), w=[pf + "qk_rs"])
                self.V(lambda e: e.reciprocal(out=rs[:, :gn], in_=rs[:, :gn]), w=[pf + "qk_rs"])
                wc = self.col(nrm + "%d" % l)
                if lat:
                    self.V(lambda e: e.scalar_tensor_tensor(out=t1[:, :gn], in0=pq, scalar=wc[:, 0:1], in1=cosT[:, g0:g0 + gn],
                                                            op0=ALU.mult, op1=ALU.mult), r=["cols", "ropeT"], w=[pf + "qk_t1"], x=[self.bk(bq)])
                    self.V(lambda e: e.scalar_tensor_tensor(out=t2[:, :gn], in0=pp, scalar=wc[:, 1:2], in1=sinT[:, g0:g0 + gn],
                                                            op0=ALU.mult, op1=ALU.mult), r=["cols", "ropeT"], w=[pf + "qk_t2"], x=[self.bk(bp)])
                    self.G(lambda e: e.tensor_tensor(out=t1[:, :gn], in0=t1[:, :gn], in1=t2[:, :gn], op=ALU.add),
                           r=[pf + "qk_t2"], w=[pf + "qk_t1"])
                    self.V(lambda e: e.tensor_tensor(out=dst[:, g0:g0 + gn], in0=t1[:, :gn], in1=rs[:, :gn], op=ALU.mult),
                           r=[pf + "qk_t1", pf + "qk_rs"], w=[dtok])
                else:
                    self.V(lambda e: e.scalar_tensor_tensor(out=dst[:, g0:g0 + gn], in0=pq, scalar=wc[:, 0:1], in1=rs[:, :gn],
                                                            op0=ALU.mult, op1=ALU.mult), r=["cols", pf + "qk_rs"], w=[dtok], x=[self.bk(bq)])
            else:
                if lat:
                    self.V(lambda e: e.tensor_tensor(out=t1[:, :gn], in0=pq, in1=cosT[:, g0:g0 + gn], op=ALU.mult),
                           r=["ropeT"], w=[pf + "qk_t1"], x=[self.bk(bq)])
                    self.V(lambda e: e.tensor_tensor(out=t2[:, :gn], in0=pp, in1=sinT[:, g0:g0 + gn], op=ALU.mult),
                           r=["ropeT"], w=[pf + "qk_t2"], x=[self.bk(bp)])
                    self.G(lambda e: e.tensor_tensor(out=dst[:, g0:g0 + gn], in0=t1[:, :gn], in1=t2[:, :gn], op=ALU.add),
                           r=[pf + "qk_t1", pf + "qk_t2"], w=[dtok])
                else:
                    self.A(lambda e: e.copy(out=dst[:, g0:g0 + gn], in_=pq), w=[dtok], x=[self.bk(bq)])

    def attention(self, PT, QT, KT, Vaug, vw, qcol0, nq, ktiles, finish, tokQ, tokK, tokV, sbanks, accsets, dist, state, gq=512):
        spb = 512 // (vw + 1)
        for g0 in range(qcol0, qcol0 + nq, gq):
            gn = min(gq, qcol0 + nq - g0)
            nqt = gn // 128
            aset = accsets[state["gi"] % len(accsets)]
            state["gi"] += 1

            def acc(j, qt, aset=aset, nqt=nqt):
                s_ = j * nqt + qt
                bnk = aset[s_ // spb]
                return self.pb[bnk][:, (s_ % spb) * (vw + 1):(s_ % spb + 1) * (vw + 1)], bnk
            nb = (2 * nqt + spb - 1) // spb
            for b in range(nb):
                self.V(lambda e: e.memset(self.pb[aset[b]][:], 0.0), w=[self.bk(aset[b])])
            steps = [(kt, j) for kt in ktiles for j in range(2)]
            n = len(steps)

            def issue_S(i):
                kt, j = steps[i]
                sbk = sbanks[i % len(sbanks)]
                pbuf = i % len(PT)
                self.P(lambda e: e.matmul(self.pb[sbk][:, :gn], lhsT=KT[:, j, kt * 128:(kt + 1) * 128],
                                          rhs=QT[:, g0:g0 + gn], start=True, stop=True),
                       r=[tokQ, tokK], x=[self.bk(sbk)])
                self.A(lambda e: e.activation(out=PT[pbuf][:, :gn], in_=self.pb[sbk][:, :gn], func=AF.Exp, scale=0.125),
                       w=["PT%d" % pbuf], x=[self.bk(sbk)])

            def issue_PV(i):
                kt, j = steps[i]
                pbuf = i % len(PT)
                for qt in range(nqt):
                    ap_, bnk = acc(j, qt)
                    self.P(lambda e: e.matmul(ap_, lhsT=PT[pbuf][:, qt * 128:(qt + 1) * 128], rhs=Vaug(kt),
                                              start=False, stop=False, skip_group_check=True),
                           r=["PT%d" % pbuf, tokV], x=[self.bk(bnk)])
            for i in range(min(dist, n)):
                issue_S(i)
            pend = state.get("pending")
            for i in range(n):
                if i + dist < n:
                    issue_S(i + dist)
                issue_PV(i)
                if pend is not None and i == min(1, n - 1):
                    pend(1)
                if pend is not None and i == min(max(n // 2, 2), n - 1):
                    pend(2)

            def fin(phase, g0=g0, nqt=nqt, acc=acc):
                for qt in range(nqt):
                    (a0, b0), (a1, b1) = acc(0, qt), acc(1, qt)
                    finish(g0 // 128 + qt, a0, a1, [self.bk(b0), self.bk(b1)], qt, phase)
            state["pending"] = fin

    def pad_k(self, es, KT):
        NTOK = self.cfg.NTOK
        KTz = self.sb(es, "KTz", [128, 2, NTOK], BF16)
        self.G(lambda e: e.memset(KTz[64:128, 0, :], 0.0), w=["KTz"])
        self.G(lambda e: e.memset(KTz[0:64, 1, :], 0.0), w=["KTz"])
        self.A(lambda e: e.copy(out=KTz[0:64, 0, :], in_=KT[0:64, :]), r=["KT"], w=["KTz"])
        self.G(lambda e: e.tensor_copy(out=KTz[64:128, 1, :], in_=KT[64:128, :]), r=["KT"], w=["KTz"])
        return KTz

    def attention_flush(self, state):
        if state.get("pending") is not None:
            state["pending"](1)
            state["pending"](2)
            state["pending"] = None

    def out_transpose(self, tok_ap, ttok, oT, chunk, ti, bank=7):
        pv = self.pb[bank][:].bitcast(BF16)
        k = self._otn % 3
        self._otn += 1
        st = self.otst[k]
        self.P(lambda e: e.transpose(out=pv[:, 0:128], in_=tok_ap, identity=self.identb[:]), r=[ttok, "identb"], x=[self.bk(bank)])
        self.A(lambda e: e.copy(out=st[:], in_=pv[:, 0:128]), w=["otst%d" % k], x=[self.bk(bank)])
        self.S.dma("sp", self.oTd[chunk, :, ti * 128:(ti + 1) * 128], st[:], reads=["otst%d" % k], writes=["oTd"])

    def rstd_col(self, ss, n, rs_tok, r):
        self.V(lambda e: e.tensor_scalar(out=ss, in0=ss, scalar1=1.0 / n, scalar2=EPS, op0=ALU.mult, op1=ALU.add), r=r, w=[rs_tok])
        self.A(lambda e: e.activation(out=ss, in_=ss, func=AF.Ln), w=[rs_tok])
        self.A(lambda e: e.activation(out=ss, in_=ss, func=AF.Exp, scale=-0.5), w=[rs_tok])

    def phase_mix(self, l, hT, with_ctx):
        cfg, S = self.cfg, self.S
        D, L, LC, KC, NT, NTC, NTT, NTOK = cfg.D, cfg.L, cfg.LC, cfg.KC, cfg.NT, cfg.NTC, cfg.NTT, cfg.NTOK
        NB = cfg.HA
        win = self.dr["w_in"][l].rearrange("(kc p) c -> p kc c", p=128)
        lam_init = 0.8 - 0.6 * math.exp(-0.3 * l)
        nqc = NTOK if with_ctx else L
        allk = list(range(NTT))
        ctxk = list(range(NT, NTT))
        hTt = ["hT%d" % kc for kc in range(KC)]
        with ExitStack() as es:
            oT = None
            self.oTd = self.nc.dram_tensor("oTd%d" % l, [3 * NB, 128, NTOK], BF16).ap()
            self.otst = [self.sb(es, "otst%d" % i, [128, 128], BF16) for i in range(3)]
            self._otn = 0
            self.rowsL = self.sb(es, "rowsL", [128, cfg.NROWL], F32)
            S.dma("sp", self.rowsL[:], self.dr["rowsL"][l], writes=["rows"])
            lam = self.sb(es, "lam", [128, 4], F32)
            subw = self.sb(es, "subw", [128, 128], F32)
            ljunk = self.sb(es, "ljunk", [128, 64], F32)
            lr = self.row("lam")
            for i in range(2):
                self.V(lambda e: e.tensor_tensor(out=ljunk[:], in0=lr[:, 128 * i:128 * i + 64], in1=lr[:, 128 * i + 64:128 * i + 128],
                                                 op=ALU.mult), r=["rows"], w=["ljunk"])
                self.V(lambda e: e.reduce_sum(out=lam[:, i:i + 1], in_=ljunk[:], axis=AX.X), r=["ljunk"], w=["lam"])
            self.A(lambda e: e.activation(out=lam[:, 0:2], in_=lam[:, 0:2], func=AF.Exp), w=["lam"])
            self.V(lambda e: e.tensor_tensor(out=lam[:, 2:3], in0=lam[:, 1:2], in1=lam[:, 0:1], op=ALU.subtract), w=["lam"])
            self.V(lambda e: e.tensor_scalar(out=lam[:, 2:3], in0=lam[:, 2:3], scalar1=-lam_init, scalar2=None, op0=ALU.add), w=["lam"])
            self.V(lambda e: e.tensor_scalar(out=subw[:], in0=self.row("sub"), scalar1=1.0 - lam_init, scalar2=None,
                                             op0=ALU.mult), r=["rows"], w=["subw"])
            e_rope = ExitStack()
            self.ropeT = self.sb(e_rope, "ropeT", [128, 2 * L], F32)
            S.dma("sp", self.ropeT[:], self.dr["ropeT"], writes=["ropeT"])
            for h in range(cfg.HA):
                with ExitStack() as e2:
                    QT = self.sb(e2, "QT", [128, NTOK], BF16)
                    KT = self.sb(e2, "KT", [128, NTOK], BF16)
                    Va = self.sb(e2, "Va", [128, NTT, 129], BF16)
                    Wv = self.sb(e2, "Wv", [128, KC, 128], BF16)
                    qsc = {"es": e2}
                    self.qk_chunk(l, qsc, hT, win, [(cfg.oAq + h * 128, 128)], QT, "QT", None, nqc, "q", mode="load")
                    self.qk_chunk(l, qsc, hT, win, [(cfg.oAk + h * 128, 128)], KT, "KT", None, NTOK, "k", mode="load")
                    self.wload(Wv, win, [(cfg.oAv + h * 128, 128)], "Wv")
                    self.qk_chunk(l, qsc, hT, win, [(cfg.oAq + h * 128, 128)], QT, "QT", None, nqc, "q", mode="compute")
                    self.qk_chunk(l, qsc, hT, win, [(cfg.oAk + h * 128, 128)], KT, "KT", None, NTOK, "k", mode="compute")
                    self.G(lambda e: e.memset(Va[:, :, 128:129], 1.0), w=["Va"])
                    for ti in range(NTT):
                        b = 5 + ti % 2
                        self.proj_tm(Wv, "Wv", hT, ti, 128, b)
                        self.A(lambda e: e.copy(out=Va[:, ti, 0:128], in_=self.pb[b][:, :128]), w=["Va"], x=[self.bk(b)])
                    fs = [self.sb(e2, "fin%d" % i, [128, 132], F32) for i in range(4)]
                    ft = [self.sb(e2, "fint%d" % i, [128, 128], F32) for i in range(4)]
                    fo = [self.sb(e2, "fino%d" % i, [128, 128], BF16) for i in range(4)]
                    fj = self.sb(e2, "finj", [128, 128], F32)

                    def finishA(ti, a0, a1, btoks, k, phase, h=h):
                        sm, t1, ob = fs[k], ft[k], fo[k]
                        stok, ttok, otok = "fin%d" % k, "fint%d" % k, "fino%d" % k
                        if phase == 2:
                            self.out_transpose(ob[:], otok, oT, h, ti)
                            return
                        self.V(lambda e: e.reciprocal(out=sm[:, 128:129], in_=a0[:, 128:129]), w=[stok], x=btoks)
                        self.V(lambda e: e.reciprocal(out=sm[:, 129:130], in_=a1[:, 128:129]), w=[stok], x=btoks)
                        self.V(lambda e: e.tensor_tensor(out=sm[:, 129:130], in0=sm[:, 129:130], in1=lam[:, 2:3], op=ALU.mult),
                               r=["lam"], w=[stok])
                        self.V(lambda e: e.tensor_scalar(out=t1[:], in0=a1[:, 0:128], scalar1=sm[:, 129:130], scalar2=None,
                                                         op0=ALU.mult), r=[stok], w=[ttok], x=btoks)
                        self.V(lambda e: e.scalar_tensor_tensor(out=sm[:, 0:128], in0=a0[:, 0:128], scalar=sm[:, 128:129], in1=t1[:],
                                                                op0=ALU.mult, op1=ALU.add), r=[ttok], w=[stok], x=btoks)
                        self.V(lambda e: e.tensor_tensor(out=fj[:], in0=sm[:, 0:128], in1=sm[:, 0:128], op=ALU.mult), r=[stok], w=["finj"])
                        self.V(lambda e: e.reduce_sum(out=sm[:, 130:131], in_=fj[:], axis=AX.X), r=["finj"], w=[stok + "s"])
                        self.rstd_col(sm[:, 130:131], 128, stok + "s", [])
                        self.V(lambda e: e.scalar_tensor_tensor(out=ob[:], in0=sm[:, 0:128], scalar=sm[:, 130:131], in1=subw[:],
                                                                op0=ALU.mult, op1=ALU.mult), r=[stok, stok + "s", "subw"], w=[otok])
                    PT = [self.sb(e2, "PT%d" % i, [128, 512], BF16) for i in range(4)]
                    KTz = self.pad_k(e2, KT)
                    ast = {"gi": 0, "pending": None}
                    akw = dict(sbanks=[0, 1, 6], accsets=[[2, 3], [4, 5]], dist=2, state=ast, gq=384)
                    self.attention(PT, QT, KTz, lambda kt: Va[:, kt, :], 128, 0, L, allk, finishA, "QT", "KTz", "Va", **akw)
                    if with_ctx:
                        self.attention(PT, QT, KTz, lambda kt: Va[:, kt, :], 128, L, LC, ctxk, finishA, "QT", "KTz", "Va", **akw)
                    self.attention_flush(ast)
                    S.barrier()
            self.tap("oTa%d" % l, self.oTd[0:NB], ["oTd"])
            if self.stop_after == (l, "mixA"):
                e_rope.close()
                return
            c2_per_hk = max(1, cfg.GRP // 2)
            for hk in range(2):
                with ExitStack() as e2:
                    QT = self.sb(e2, "QT", [128, NTOK], BF16)
                    KT = self.sb(e2, "KT", [128, NTOK], BF16)
                    Vb = self.sb(e2, "Vb", [128, NTT, 65], BF16)
                    Wv = self.sb(e2, "Wv", [128, KC, 64], BF16)
                    qsc = {"es": e2}
                    self.qk_chunk(l, qsc, hT, win, [(cfg.oBk + hk * 64, 64), (cfg.oBk + hk * 64, 64)], KT, "KT", "knc", NTOK, "k", mode="load")
                    self.wload(Wv, win, [(cfg.oBv + hk * 64, 64)], "Wv")
                    self.qk_chunk(l, qsc, hT, win, [(cfg.oBk + hk * 64, 64), (cfg.oBk + hk * 64, 64)], KT, "KT", "knc", NTOK, "k", mode="compute")
                    self.G(lambda e: e.memset(Vb[:, :, 64:65], 1.0), w=["Vb"])
                    for ti in range(NTT):
                        b = 5 + ti % 2
                        self.proj_tm(Wv, "Wv", hT, ti, 64, b)
                        self.A(lambda e: e.copy(out=Vb[:, ti, 0:64], in_=self.pb[b][:, :64]), w=["Vb"], x=[self.bk(b)])
                    fs = [self.sb(e2, "fin%d" % i, [128, 2], F32) for i in range(4)]
                    fo = [self.sb(e2, "fino%d" % i, [128, 128], BF16) for i in range(4)]
                    PT = [self.sb(e2, "PT%d" % i, [128, 512], BF16) for i in range(4)]
                    KTz = self.pad_k(e2, KT)
                    for c2 in range(hk * c2_per_hk, (hk + 1) * c2_per_hk):
                        self.qk_chunk(l, qsc, hT, win, [(cfg.oBq + c2 * 128, 128)], QT, "QT", "qnc", nqc, "q")

                        def finishB(ti, a0, a1, btoks, k, phase, c2=c2):
                            sm, ob = fs[k], fo[k]
                            stok, otok = "fin%d" % k, "fino%d" % k
                            if phase == 2:
                                self.out_transpose(ob[:], otok, oT, NB + c2, ti)
                                return
                            self.V(lambda e: e.reciprocal(out=sm[:, 0:1], in_=a0[:, 64:65]), w=[stok], x=btoks)
                            self.V(lambda e: e.reciprocal(out=sm[:, 1:2], in_=a1[:, 64:65]), w=[stok], x=btoks)
                            self.V(lambda e: e.tensor_scalar(out=ob[:, 0:64], in0=a0[:, 0:64], scalar1=sm[:, 0:1], scalar2=None,
                                                             op0=ALU.mult), r=[stok], w=[otok], x=btoks)
                            self.V(lambda e: e.tensor_scalar(out=ob[:, 64:128], in0=a1[:, 0:64], scalar1=sm[:, 1:2], scalar2=None,
                                                             op0=ALU.mult), r=[stok], w=[otok], x=btoks)
                        ast = {"gi": 0, "pending": None}
                        akw = dict(sbanks=[0, 1, 6], accsets=[[2, 3], [4, 5]], dist=2, state=ast)
                        self.attention(PT, QT, KTz, lambda kt: Vb[:, kt, :], 64, 0, L, allk, finishB, "QT", "KTz", "Vb", **akw)
                        if with_ctx:
                            self.attention(PT, QT, KTz, lambda kt: Vb[:, kt, :], 64, L, LC, ctxk, finishB, "QT", "KTz", "Vb", **akw)
                        self.attention_flush(ast)
                    S.barrier()
            self.tap("oTb%d" % l, self.oTd[NB:2 * NB], ["oTd"])
            S.barrier()
            e_rope.close()
            if self.stop_after == (l, "mixB"):
                return
            self.mix_mlstm(l, hT, win, oT, with_ctx)
            self.tap("oTc%d" % l, self.oTd[2 * NB:3 * NB], ["oTd"])
            if self.stop_after == (l, "mixC"):
                return
            self.mix_merge(l, hT, win, oT, with_ctx)

    def mix_mlstm(self, l, hT, win, oT, with_ctx):
        cfg, S = self.cfg, self.S
        D, L, LC, KC, NT, NTC, NTT, NTOK, HC = cfg.D, cfg.L, cfg.LC, cfg.KC, cfg.NT, cfg.NTC, cfg.NTT, cfg.NTOK, cfg.HC
        NB = HC
        one_col = self.cst(CON)[:, 0:1]
        with ExitStack() as es:
            Wg = self.sb(es, "Wg", [128, KC, 4 * HC], BF16)
            self.wload(Wg, win, [(cfg.oG, 4 * HC)], "Wg")
            Gt = self.sb(es, "Gt", [128, NTT, 4 * HC], F32)
            LF = self.sb(es, "LF", [128, NTT, 2, HC], F32)
            BC = self.sb(es, "BC", [128, NTT, 2, HC], F32)
            TOT = self.sb(es, "TOT", [128, NTT, 2, HC], F32)
            BIAS = self.sb(es, "BIAS", [128, NTT, 2, HC], F32)
            EB = self.sb(es, "EB", [128, NTT, 2, HC], F32)
            WC = self.sb(es, "WC", [128, NTT, 2, HC], F32)
            AC = self.sb(es, "AC", [128, NTT, 2, HC], F32)
            for ti in range(NTT):
                b = 5 + ti % 2
                self.proj_tm(Wg, "Wg", hT, ti, 4 * HC, b)
                self.V(lambda e: e.tensor_tensor(out=Gt[:, ti, :], in0=self.pb[b][:, :4 * HC], in1=self.row("gb"), op=ALU.add),
                       r=["rows"], w=["Gt"], x=[self.bk(b)])
            Gv = Gt[:].rearrange("p t (q h) -> p t q h", h=HC)
            for d in range(2):
                self.A(lambda e: e.activation(out=LF[:, :, d, :], in_=Gv[:, :, 2 * d + 1, :], func=AF.Exp, scale=-1.0), r=["Gt"], w=["LF"])
            self.A(lambda e: e.activation(out=LF[:], in_=LF[:], func=AF.Ln, bias=one_col), r=["consts"], w=["LF"])
            self.V(lambda e: e.tensor_scalar(out=LF[:], in0=LF[:], scalar1=-1.0, scalar2=None, op0=ALU.mult), w=["LF"])
            for ti in range(NTT):
                b = 5 + ti % 2
                self.P(lambda e: e.matmul(self.pb[b][:, 0:HC], lhsT=self.cst(CTU), rhs=LF[:, ti, 0, :], start=True, stop=True),
                       r=["consts", "LF"], x=[self.bk(b)])
                self.P(lambda e: e.matmul(self.pb[b][:, HC:2 * HC], lhsT=self.cst(CTL), rhs=LF[:, ti, 1, :], start=True, stop=True),
                       r=["consts", "LF"], x=[self.bk(b)])
                self.P(lambda e: e.matmul(self.pb[b][:, 2 * HC:4 * HC], lhsT=self.cst(CON), rhs=LF[:, ti, :, :].rearrange("p a b -> p (a b)"),
                                          start=True, stop=True), r=["consts", "LF"], x=[self.bk(b)])
                self.V(lambda e: e.tensor_copy(out=BC[:, ti, :, :].rearrange("p a b -> p (a b)"), in_=self.pb[b][:, 0:2 * HC]), w=["BC"], x=[self.bk(b)])
                self.V(lambda e: e.tensor_copy(out=TOT[:, ti, :, :].rearrange("p a b -> p (a b)"), in_=self.pb[b][:, 2 * HC:4 * HC]), w=["TOT"], x=[self.bk(b)])
            for d in range(2):
                self.V(lambda e: e.tensor_tensor(out=BIAS[:, :, d, :], in0=Gv[:, :, 2 * d, :], in1=BC[:, :, d, :], op=ALU.subtract),
                       r=["Gt", "BC"], w=["BIAS"])
            self.A(lambda e: e.activation(out=EB[:], in_=BC[:], func=AF.Exp), r=["BC"], w=["EB"])
            self.V(lambda e: e.tensor_tensor(out=WC[:], in0=TOT[:], in1=BIAS[:], op=ALU.add), r=["TOT", "BIAS"], w=["WC"])
            self.A(lambda e: e.activation(out=WC[:], in_=WC[:], func=AF.Exp), w=["WC"])
            self.A(lambda e: e.activation(out=AC[:], in_=TOT[:], func=AF.Exp), r=["TOT"], w=["AC"])
            S.barrier()
            for hc in range(HC):
                with ExitStack() as e2:
                    Ws = {}
                    for nm, o in (("q", cfg.oCq), ("k", cfg.oCk), ("v", cfg.oCv), ("o", cfg.oCo)):
                        Ws[nm] = self.sb(e2, "Wc" + nm, [128, KC, 128], BF16)
                        self.wload(Ws[nm], win, [(o + hc * 128, 128)], "Wc" + nm)
                    qT = self.sb(e2, "cqT", [128, NTOK], BF16)
                    kT = self.sb(e2, "ckT", [128, NTOK], BF16)
                    raw = self.sb(e2, "craw", [128, NTOK], F32)
                    acc = self.sb(e2, "cacc", [128, NTOK], F32)
                    cw = self.col("conv%d" % l)
                    for (nm, dst, dtok, scale, chunk) in (("q", qT, "cqT", 1.0, hc), ("k", kT, "ckT", 128.0 ** -0.5, HC + hc)):
                        def cons(ps_ap, g0, gn, b):
                            self.A(lambda e: e.copy(out=raw[:, g0:g0 + gn], in_=ps_ap), w=["craw"], x=[self.bk(b)])
                        self.proj_fm(Ws[nm], "Wc" + nm, hT, 0, NTOK, [5, 6], cons)
                        w0 = cw[:, 0 * 2 * HC + chunk:0 * 2 * HC + chunk + 1]
                        w1 = cw[:, 1 * 2 * HC + chunk:1 * 2 * HC + chunk + 1]
                        w2 = cw[:, 2 * 2 * HC + chunk:2 * 2 * HC + chunk + 1]
                        for (s0, n) in ((0, L), (L, LC)):
                            self.V(lambda e: e.tensor_scalar(out=acc[:, s0:s0 + n], in0=raw[:, s0:s0 + n], scalar1=w1, scalar2=None, op0=ALU.mult),
                                   r=["craw", "cols"], w=["cacc"])
                            self.V(lambda e: e.scalar_tensor_tensor(out=acc[:, s0 + 1:s0 + n], in0=raw[:, s0:s0 + n - 1], scalar=w0,
                                                                    in1=acc[:, s0 + 1:s0 + n], op0=ALU.mult, op1=ALU.add), r=["craw", "cols"], w=["cacc"])
                            self.V(lambda e: e.scalar_tensor_tensor(out=acc[:, s0:s0 + n - 1], in0=raw[:, s0 + 1:s0 + n], scalar=w2,
                                                                    in1=acc[:, s0:s0 + n - 1], op0=ALU.mult, op1=ALU.add), r=["craw", "cols"], w=["cacc"])
                        self.A(lambda e: e.activation(out=raw[:], in_=acc[:], func=AF.Sigmoid), r=["cacc"], w=["craw"])
                        self.V(lambda e: e.scalar_tensor_tensor(out=dst[:], in0=acc[:], scalar=scale, in1=raw[:], op0=ALU.mult, op1=ALU.mult),
                               r=["cacc", "craw"], w=[dtok])
                    Vc = self.sb(e2, "cVc", [128, NTT, 129], BF16)
                    OG = acc[:].rearrange("p (t d) -> p t d", d=128)
                    Kt = self.sb(e2, "cKt", [128, NTT, 128], BF16)
                    HS = self.sb(e2, "cHS", [128, NTT, 128], F32)
                    self.G(lambda e: e.memset(Vc[:, :, 128:129], 1.0), w=["cVc"])
                    self.G(lambda e: e.memset(HS[:], 0.0), w=["cHS"])
                    pv7 = self.pb[7][:].bitcast(BF16)
                    for ti in range(NTT):
                        self.proj_tm(Ws["v"], "Wcv", hT, ti, 128, 5)
                        self.A(lambda e: e.copy(out=Vc[:, ti, 0:128], in_=self.pb[5][:, :128]), w=["cVc"], x=[self.bk(5)])
                        self.P(lambda e: e.transpose(out=pv7[:, 0:128], in_=kT[:, ti * 128:(ti + 1) * 128], identity=self.identb[:]),
                               r=["ckT", "identb"], x=[self.bk(7)])
                        self.V(lambda e: e.tensor_copy(out=Kt[:, ti, :], in_=pv7[:, 0:128]), w=["cKt"], x=[self.bk(7)])
                    INTRA = [raw[:].rearrange("p (t d) -> p t d", d=128), acc[:].rearrange("p (t d) -> p t d", d=128)]
                    itok = ["craw", "cacc"]
                    DENI = self.sb(e2, "cDENI", [128, NTT, 2], F32)
                    SB = [self.sb(e2, "cSB%d" % d, [128, NTT, 129], BF16) for d in range(2)]
                    st = [self.sb(e2, "st%d" % d, [128, 129], F32) for d in range(2)]
                    rot = dict(Lt=[self.sb(e2, "Lt%d" % i, [128, 128], F32) for i in range(3)],
                               DT=[self.sb(e2, "DT%d" % i, [128, 128], F32) for i in range(3)],
                               SM=[self.sb(e2, "SM%d" % i, [128, 128], BF16) for i in range(3)],
                               VW=[self.sb(e2, "VW%d" % i, [128, 129], BF16) for i in range(3)],
                               nd=[self.sb(e2, "nd%d" % i, [128, 132], F32) for i in range(3)])
                    order = [list(range(NT, NTT)) + list(range(NT)), list(range(NTT - 1, NT - 1, -1)) + list(range(NT - 1, -1, -1))]
                    out_tiles = list(range(NTT)) if with_ctx else list(range(NT))
                    for kk in ("Lt", "DT", "SM", "VW"):
                        rot[kk].append(self.sb(e2, kk + "3", [128, 129 if kk == "VW" else 128], BF16 if kk in ("SM", "VW") else F32))
                    items = [(ti, d) for ti in out_tiles for d in range(2)]
                    nit = len(items)

                    def p1A(i):
                        ti, d = items[i]
                        cs = slice(ti * 128, (ti + 1) * 128)
                        bS = (i // 2) % 2
                        if d == 0:
                            self.P(lambda e: e.matmul(self.pb[bS][:, :128], lhsT=kT[:, cs], rhs=qT[:, cs], start=True, stop=True),
                                   r=["ckT", "cqT"], x=[self.bk(bS)])
                        k4 = i % 4
                        bL = 2 + i % 2
                        Lt = rot["Lt"][k4]
                        self.V(lambda e: e.tensor_scalar(out=Lt[:], in0=self.cst(CSL if d == 0 else CSU), scalar1=LF[:, ti, d, hc:hc + 1],
                                                         scalar2=None, op0=ALU.mult), r=["consts"], w=["Lt%d" % k4])
                        self.P(lambda e: e.matmul(self.pb[bL][:, :128], lhsT=Lt[:], rhs=self.cst(CTU if d == 0 else CTL),
                                                  start=True, stop=True), r=["Lt%d" % k4, "consts"], x=[self.bk(bL)])

                    def p1B(i):
                        ti, d = items[i]
                        bS = (i // 2) % 2
                        k4 = i % 4
                        bL = 2 + i % 2
                        DT, SM = rot["DT"][k4], rot["SM"][k4]
                        self.A(lambda e: e.activation(out=DT[:], in_=self.pb[bL][:, :128], func=AF.Exp,
                                                      bias=Gv[:, ti, 2 * d, hc:hc + 1]), w=["DT%d" % k4], x=[self.bk(bL)])
                        self.G(lambda e: e.tensor_tensor(out=DT[:], in0=DT[:], in1=self.cst(CTU if d == 0 else CTL), op=ALU.mult),
                               r=["consts"], w=["DT%d" % k4])
                        self.V(lambda e: e.tensor_tensor(out=SM[:], in0=DT[:], in1=self.pb[bS][:, :128], op=ALU.mult),
                               r=["DT%d" % k4], w=["SM%d" % k4], x=[self.bk(bS)])

                    def p1C(i):
                        ti, d = items[i]
                        k4 = i % 4
                        bI = 4 + i % 2
                        SM = rot["SM"][k4]
                        self.P(lambda e: e.matmul(self.pb[bI][:, :129], lhsT=SM[:], rhs=Vc[:, ti, :], start=True, stop=True),
                               r=["SM%d" % k4, "cVc"], x=[self.bk(bI)])
                        self.A(lambda e: e.copy(out=INTRA[d][:, ti, :], in_=self.pb[bI][:, :128]), w=[itok[d]], x=[self.bk(bI)])
                        self.V(lambda e: e.tensor_copy(out=DENI[:, ti, d:d + 1], in_=self.pb[bI][:, 128:129]), w=["cDENI"], x=[self.bk(bI)])
                    for i in range(nit + 2):
                        if i < nit:
                            p1A(i)
                        if 0 <= i - 1 < nit:
                            p1B(i - 1)
                        if 0 <= i - 2 < nit:
                            p1C(i - 2)
                    for d in range(2):
                        self.G(lambda e: e.memset(st[d][:], 0.0), w=["st%d" % d])
                        self.G(lambda e: e.memset(SB[d][:, 0, :], 0.0), w=["cSB%d" % d])
                    sitems = [(step, d) for step in range(NTT - 1) for d in range(2)]
                    nsi = len(sitems)
                    ubanks = [6, 7, 0, 1]

                    def p2A(i):
                        step, d = sitems[i]
                        ti = order[d][step]
                        k4 = i % 4
                        bU = ubanks[i % 4]
                        VW = rot["VW"][k4]
                        self.V(lambda e: e.tensor_scalar(out=VW[:], in0=Vc[:, ti, :], scalar1=WC[:, ti, d, hc:hc + 1], scalar2=None,
                                                         op0=ALU.mult), r=["cVc"], w=["VW%d" % k4])
                        self.P(lambda e: e.matmul(self.pb[bU][:, :129], lhsT=Kt[:, ti, :], rhs=VW[:], start=True, stop=True),
                               r=["cKt", "VW%d" % k4], x=[self.bk(bU)])

                    def p2B(i):
                        step, d = sitems[i]
                        ti = order[d][step]
                        bU = ubanks[i % 4]
                        self.V(lambda e: e.scalar_tensor_tensor(out=st[d][:], in0=st[d][:], scalar=AC[:, ti, d, hc:hc + 1],
                                                                in1=self.pb[bU][:, :129], op0=ALU.mult, op1=ALU.add),
                               w=["st%d" % d], x=[self.bk(bU)])
                        self.A(lambda e: e.copy(out=SB[d][:, step + 1, :], in_=st[d][:]), r=["st%d" % d], w=["cSB%d" % d])
                    for i in range(nsi + 2):
                        if i < nsi:
                            p2A(i)
                        if 0 <= i - 2 < nsi:
                            p2B(i - 2)
                    n3 = 0
                    for step in range(NTT):
                        for d in range(2):
                            ti = order[d][step]
                            if not (ti < NT or with_ctx):
                                continue
                            cs = slice(ti * 128, (ti + 1) * 128)
                            k3 = n3 % 3
                            bN = 2 + n3 % 4
                            n3 += 1
                            nd = rot["nd"][k3]
                            ntk = "nd%d" % k3
                            self.P(lambda e: e.matmul(self.pb[bN][:, :129], lhsT=qT[:, cs], rhs=SB[d][:, step, :], start=True, stop=True),
                                   r=["cqT", "cSB%d" % d], x=[self.bk(bN)])
                            self.V(lambda e: e.scalar_tensor_tensor(out=nd[:, 0:128], in0=self.pb[bN][:, 0:128], scalar=EB[:, ti, d, hc:hc + 1],
                                                                    in1=INTRA[d][:, ti, :], op0=ALU.mult, op1=ALU.add),
                                   r=[itok[d]], w=[ntk], x=[self.bk(bN)])
                            self.V(lambda e: e.scalar_tensor_tensor(out=nd[:, 128:129], in0=self.pb[bN][:, 128:129], scalar=EB[:, ti, d, hc:hc + 1],
                                                                    in1=DENI[:, ti, d:d + 1], op0=ALU.mult, op1=ALU.add),
                                   r=["cDENI"], w=[ntk], x=[self.bk(bN)])
                            self.V(lambda e: e.scalar_tensor_tensor(out=nd[:, 129:130], in0=nd[:, 128:129], scalar=-1.0, in1=nd[:, 128:129],
                                                                    op0=ALU.mult, op1=ALU.max), w=[ntk])
                            self.V(lambda e: e.tensor_scalar(out=nd[:, 129:130], in0=nd[:, 129:130], scalar1=1.0, scalar2=None, op0=ALU.max), w=[ntk])
                            self.V(lambda e: e.reciprocal(out=nd[:, 129:130], in_=nd[:, 129:130]), w=[ntk])
                            self.G(lambda e: e.scalar_tensor_tensor(out=HS[:, ti, :], in0=nd[:, 0:128], scalar=nd[:, 129:130],
                                                                    in1=HS[:, ti, :], op0=ALU.mult, op1=ALU.add), r=[ntk], w=["cHS"]) \
                                if False else self.V(lambda e: e.scalar_tensor_tensor(out=HS[:, ti, :], in0=nd[:, 0:128], scalar=nd[:, 129:130],
                                                                                      in1=HS[:, ti, :], op0=ALU.mult, op1=ALU.add), r=[ntk], w=["cHS"])
                    S.barrier()
                    for ti in (range(NTT) if with_ctx else range(NT)):
                        b = 5 + ti % 2
                        self.proj_tm(Ws["o"], "Wco", hT, ti, 128, b)
                        self.A(lambda e: e.activation(out=OG[:, ti, :], in_=self.pb[b][:, :128], func=AF.Sigmoid), w=["cacc"], x=[self.bk(b)])
                    fs = self.sb(e2, "cfs", [128, NTT], F32)
                    fj = self.sb(e2, "cfj", [128, 128], F32)
                    fo = [self.sb(e2, "cfo%d" % i, [128, 128], BF16) for i in range(2)]
                    tiles = list(range(NTT)) if with_ctx else list(range(NT))
                    for ti in tiles:
                        self.A(lambda e: e.activation(out=fj[:], in_=HS[:, ti, :], func=AF.Square, accum_out=fs[:, ti:ti + 1]), w=["cfj", "cfs"])
                    self.rstd_col(fs[:, :len(tiles)], 128, "cfs", [])
                    cn = self.row("cn", hc * 128, (hc + 1) * 128)
                    for n_, ti in enumerate(tiles):
                        k = n_ % 2
                        self.V(lambda e: e.scalar_tensor_tensor(out=HS[:, ti, :], in0=HS[:, ti, :], scalar=fs[:, ti:ti + 1], in1=cn,
                                                                op0=ALU.mult, op1=ALU.mult), r=["cfs", "rows"], w=["cHS"])
                        self.G(lambda e: e.tensor_tensor(out=fo[k][:], in0=HS[:, ti, :], in1=OG[:, ti, :], op=ALU.mult), r=["cHS", "cacc"], w=["cfo%d" % k])
                        self.out_transpose(fo[k][:], "cfo%d" % k, oT, 2 * NB + hc, ti)
                    S.barrier()

    def mix_merge(self, l, hT, win, oT, with_ctx):
        cfg, S = self.cfg, self.S
        D, L, KC, NT, NTT, NTOK, NB = cfg.D, cfg.L, cfg.KC, cfg.NT, cfg.NTT, cfg.NTOK, cfg.HA
        ntok = NTOK if with_ctx else L
        wbr = [self.dr[n][l].rearrange("(c p) d -> p c d", p=128) for n in ("w_branch_a", "w_branch_b", "w_branch_c")]
        S.barrier()
        with ExitStack() as es:
            mT = self.sb(es, "mT", [128, KC, 512], BF16)
            acc = self.sb(es, "macc", [128, 512], F32)
            sgs = [self.sb(es, "msg%d" % i, [128, 512], F32) for i in range(2)]
            Wg = [self.sb(es, "mWg%d" % i, [128, KC, 128], BF16) for i in range(6)]
            Wb = [self.sb(es, "mWb%d" % i, [128, NB, 128], BF16) for i in range(6)]
            oTg = [self.sb(es, "oTg%d" % i, [128, NB, 512], BF16) for i in range(3)]
            Wo = self.sb(es, "mWo", [128, KC, D], BF16)
            tmp = [self.sb(es, "mtmp%d" % i, [128, 512], F32) for i in range(2)]
            S.dma("pool", Wo[:], self.dr["w_out"][l].rearrange("(c p) d -> p c d", p=128), writes=["mWo"])
            n = 0
            gcnt = 0
            n2 = 0
            for g0 in range(0, ntok, 512):
                gn = min(512, ntok - g0)
                for i in range(3):
                    S.dma("sp", oTg[i][:, :, :gn], self.oTd[i * NB:(i + 1) * NB, :, g0:g0 + gn].rearrange("c p t -> p c t"),
                          reads=["oTd"], writes=["oTg%d" % i])
                for dc in range(KC):
                    for i in range(3):
                        k = n % 6
                        n += 1
                        self.wload(Wg[k], win, [(cfg.oMG + i * D + dc * 128, 128)], "mWg%d" % k)
                        S.dma("pool", Wb[k][:], wbr[i][:, :, dc * 128:(dc + 1) * 128], writes=["mWb%d" % k])
                        bg = 5 + gcnt % 2
                        bb = 0 + gcnt % 2
                        sg = sgs[gcnt % 2]
                        stok = "msg%d" % (gcnt % 2)
                        gcnt += 1
                        for kc in range(KC):
                            self.P(lambda e: e.matmul(self.pb[bg][:, :gn], lhsT=Wg[k][:, kc, :], rhs=hT[:, kc, g0:g0 + gn],
                                                      start=(kc == 0), stop=(kc == KC - 1)), r=["mWg%d" % k, "hT%d" % kc], x=[self.bk(bg)])
                        self.A(lambda e: e.activation(out=sg[:, :gn], in_=self.pb[bg][:, :gn], func=AF.Sigmoid), w=[stok], x=[self.bk(bg)])
                        for c in range(NB):
                            self.P(lambda e: e.matmul(self.pb[bb][:, :gn], lhsT=Wb[k][:, c, :], rhs=oTg[i][:, c, :gn],
                                                      start=(c == 0), stop=(c == NB - 1)), r=["mWb%d" % k, "oTg%d" % i], x=[self.bk(bb)])
                        if i == 0:
                            self.V(lambda e: e.tensor_tensor(out=acc[:, :gn], in0=sg[:, :gn], in1=self.pb[bb][:, :gn], op=ALU.mult),
                                   r=[stok], w=["macc"], x=[self.bk(bb)])
                        else:
                            self.V(lambda e: e.tensor_tensor(out=sg[:, :gn], in0=sg[:, :gn], in1=self.pb[bb][:, :gn], op=ALU.mult),
                                   w=[stok], x=[self.bk(bb)])
                            if i == 1:
                                self.G(lambda e: e.tensor_tensor(out=acc[:, :gn], in0=acc[:, :gn], in1=sg[:, :gn], op=ALU.add),
                                       r=[stok], w=["macc"])
                            else:
                                self.G(lambda e: e.tensor_tensor(out=mT[:, dc, :gn], in0=acc[:, :gn], in1=sg[:, :gn], op=ALU.add),
                                       r=[stok, "macc"], w=["mT"])
                for tt in range(gn // 128):
                    ti = g0 // 128 + tt
                    w_ = 0 if ti < NT else 1
                    for hf in range(D // 512):
                        k = n2 % 2
                        n2 += 1
                        b = 2 + k
                        for kc in range(KC):
                            self.P(lambda e: e.matmul(self.pb[b][:, :], lhsT=mT[:, kc, tt * 128:(tt + 1) * 128], rhs=Wo[:, kc, hf * 512:(hf + 1) * 512],
                                                      start=(kc == 0), stop=(kc == KC - 1)), r=["mT", "mWo"], x=[self.bk(b)])
                        self.V(lambda e: e.tensor_tensor(out=tmp[k][:], in0=self.pb[b][:, :], in1=self.grow[:, w_, hf * 512:(hf + 1) * 512], op=ALU.mult),
                               r=["grow"], w=["mtmp%d" % k], x=[self.bk(b)])
                        xt = self.src_tile(ti)
                        self.G(lambda e: e.tensor_tensor(out=xt[:, hf * 512:(hf + 1) * 512], in0=xt[:, hf * 512:(hf + 1) * 512], in1=tmp[k][:], op=ALU.add),
                               r=["mtmp%d" % k], w=["x%d" % ti])
            S.barrier()

    def phase_ffn(self, l, with_ctx):
        cfg, S = self.cfg, self.S
        D, L, LC, E, FF, FC, KC, NT, NTC, NTT, NTOK = cfg.D, cfg.L, cfg.LC, cfg.E, cfg.FF, cfg.FC, cfg.KC, cfg.NT, cfg.NTC, cfg.NTT, cfg.NTOK
        self.phase_mod(l, 5, False)
        sets = [dict(t0=0, nt=NT, cap=cfg.CAPL, w=0, s0=0)]
        if with_ctx:
            sets.append(dict(t0=NT, nt=NTC, cap=cfg.CAPC, w=1, s0=cfg.CAPL))
        NS = sum(st["cap"] for st in sets)
        ntl = NTT if with_ctx else NT
        stiles = []
        for si, st in enumerate(sets):
            assert st["s0"] % 128 == 0
            for a in range(0, st["cap"], 128):
                stiles.append((st["s0"] + a, min(128, st["cap"] - a), si))
        NST = len(stiles)
        NH = D // 512
        assert NST * NH <= 6
        identf = self.cst(CI)
        iof = self.row("iof")
        with ExitStack() as es:
            xs = self.sb(es, "xs2", [128, NTT, D], BF16)
            rankTok = self.sb(es, "rankTok", [128, NTT, E], F32)
            LG = self.sb(es, "LG", [128, NTT, E], F32)
            iopj = self.sb(es, "iopj", [128, NST], F32)
            e_row = ExitStack()
            gT = self.sb(e_row, "gT", [E, NTOK], F32)
            rankT = self.sb(e_row, "rankT", [E, NTOK], F32)
            for k, (s_start, nn, si) in enumerate(stiles):
                self.V(lambda e: e.tensor_scalar(out=iopj[:, k:k + 1], in0=self.col("iop"), scalar1=float(s_start), scalar2=None, op0=ALU.add),
                       r=["cols"], w=["iopj"])
            with ExitStack() as e1:
                hT2 = self.sb(e1, "hT2", [128, KC, NTOK], BF16)
                self.phase_norm(e1, l, 1, xs, hT2, with_ctx)
                Wr = self.sb(e1, "Wr", [128, KC, E], BF16)
                S.dma("pool", Wr[:], self.dr["w_router"][l].rearrange("(c p) e -> p c e", p=128), writes=["Wr"])
                mx = self.sb(e1, "lgmx", [128, NTT], F32)
                for ti in range(ntl):
                    b = 5 + ti % 2
                    self.proj_tm(Wr, "Wr", hT2, ti, E, b)
                    self.V(lambda e: e.tensor_copy(out=LG[:, ti, :], in_=self.pb[b][:, :E]), w=["LG"], x=[self.bk(b)])
                lg = LG[:, :ntl, :]
                self.V(lambda e: e.tensor_reduce(out=mx[:, :ntl], in_=lg, axis=AX.X, op=ALU.max), r=["LG"], w=["lgmx"])
                self.V(lambda e: e.tensor_tensor(out=lg, in0=lg, in1=mx[:, :ntl].unsqueeze(2).to_broadcast([128, ntl, E]), op=ALU.subtract),
                       r=["lgmx"], w=["LG"])
                self.A(lambda e: e.activation(out=lg, in_=lg, func=AF.Exp), w=["LG"])
                self.V(lambda e: e.reduce_sum(out=mx[:, :ntl], in_=lg, axis=AX.X), r=["LG"], w=["lgmx"])
                self.V(lambda e: e.reciprocal(out=mx[:, :ntl], in_=mx[:, :ntl]), w=["lgmx"])
                self.V(lambda e: e.tensor_tensor(out=lg, in0=lg, in1=mx[:, :ntl].unsqueeze(2).to_broadcast([128, ntl, E]), op=ALU.mult),
                       r=["lgmx"], w=["LG"])
                for t0 in range(0, ntl, 4):
                    nt_ = min(4, ntl - t0)
                    b = (t0 // 4) % 2
                    for k in range(nt_):
                        self.P(lambda e: e.transpose(out=self.pb[b][0:E, k * 128:(k + 1) * 128], in_=LG[:, t0 + k, :], identity=identf),
                               r=["LG", "consts"], x=[self.bk(b)])
                    self.A(lambda e: e.copy(out=gT[:, t0 * 128:(t0 + nt_) * 128], in_=self.pb[b][0:E, :nt_ * 128]), w=["gT"], x=[self.bk(b)])
                S.barrier()
            with ExitStack() as e1:
                nmax = max(st["nt"] for st in sets) * 128
                work = self.sb(e1, "tkw", [E, nmax], F32)
                MK = self.sb(e1, "tkm", [E, nmax], F32)
                CS = self.sb(e1, "tkc", [E, nmax], F32)
                ones = self.sb(e1, "tko", [E, nmax], F32)
                mx8 = self.sb(e1, "tk8", [E, 8], F32)
                self.G(lambda e: e.memset(ones[:], 1.0), w=["tko"])
                for st in sets:
                    c0, n, cap = st["t0"] * 128, st["nt"] * 128, st["cap"]
                    assert cap % 8 == 0
                    self.V(lambda e: e.tensor_copy(out=work[:, :n], in_=gT[:, c0:c0 + n]), r=["gT"], w=["tkw"])
                    for r_ in range(cap // 8):
                        self.V(lambda e: e.max(out=mx8[:], in_=work[:, :n]), r=["tkw"], w=["tk8"])
                        if r_ < cap // 8 - 1:
                            self.V(lambda e: e.match_replace(out=work[:, :n], in_to_replace=mx8[:], in_values=work[:, :n], imm_value=-1.0),
                                   r=["tk8"], w=["tkw"])
                    self.V(lambda e: e.tensor_scalar(out=MK[:, :n], in0=gT[:, c0:c0 + n], scalar1=mx8[:, 7:8], scalar2=None, op0=ALU.is_ge),
                           r=["gT", "tk8"], w=["tkm"])
                    self.V(lambda e: e.tensor_tensor_scan(out=CS[:, :n], data0=ones[:, :n], data1=MK[:, :n], initial=0.0, op0=ALU.mult, op1=ALU.add),
                           r=["tko", "tkm"], w=["tkc"])
                    self.V(lambda e: e.scalar_tensor_tensor(out=CS[:, :n], in0=CS[:, :n], scalar=float(st["s0"]), in1=MK[:, :n], op0=ALU.add, op1=ALU.mult),
                           r=["tkm"], w=["tkc"])
                    self.V(lambda e: e.tensor_scalar(out=rankT[:, c0:c0 + n], in0=CS[:, :n], scalar1=-1.0, scalar2=None, op0=ALU.add),
                           r=["tkc"], w=["rankT"])
                for ti in range(ntl):
                    b = ti % 2
                    self.P(lambda e: e.transpose(out=self.pb[b][:, 0:E], in_=rankT[0:E, ti * 128:(ti + 1) * 128], identity=identf[0:E, 0:E]),
                           r=["rankT", "consts"], x=[self.bk(b)])
                    self.V(lambda e: e.tensor_copy(out=rankTok[:, ti, :], in_=self.pb[b][:, 0:E]), w=["rankTok"], x=[self.bk(b)])
                S.barrier()
            self.tap("rankT%d" % l, rankT[:, :ntl * 128], [])
            self.tap("gT%d" % l, gT[:, :ntl * 128], [])
            S.barrier()
            e_row.close()
            with ExitStack() as e1:
                CAPM = max(st["cap"] for st in sets)
                Sel = self.sb(e1, "Sel", [128, NTT, CAPM], BF16)
                SelT = [self.sb(e1, "SelT%d" % i, [128, NST, 512], BF16) for i in range(2)]
                gsb = [self.sb(e1, "gsb%d" % i, [128, 512], F32) for i in range(2)]
                repR = [self.sb(e1, "repR%d" % i, [128, 128], F32) for i in range(2)]
                repG = [self.sb(e1, "repG%d" % i, [128, 128], F32) for i in range(2)]
                xeT = self.sb(e1, "xeT", [128, KC, NS], BF16)
                actT = self.sb(e1, "actT", [128, FC, NS], BF16)
                ye = self.sb(e1, "ye", [128, NST, D], BF16)
                sa = [self.sb(e1, "sa%d" % i, [128, NS], F32) for i in range(2)]
                PW = 256
                DP = 2
                NWB = 3
                Wg = [self.sb(e1, "eWg%d" % i, [128, KC, PW], BF16) for i in range(NWB)]
                Wu = [self.sb(e1, "eWu%d" % i, [128, KC, PW], BF16) for i in range(NWB)]
                Wd = [self.sb(e1, "eWd%d" % i, [128, DP, D], BF16) for i in range(NWB)]
                cn = dict(wcnt=0, dcnt=0, scnt=0, ocnt=0, ecnt=0, rcnt=0)

                def do_gather(ex):
                        for st in sets:
                            for k in range(st["nt"]):
                                ti = st["t0"] + k
                                cap = st["cap"]
                                if st["s0"] == 0:
                                    self.V(lambda e: e.tensor_scalar(out=Sel[:, ti, :cap], in0=iof[:, :cap], scalar1=rankTok[:, ti, ex:ex + 1],
                                                                     scalar2=None, op0=ALU.is_equal), r=["rankTok", "rowsG"], w=["Sel"])
                                else:
                                    self.V(lambda e: e.tensor_scalar(out=Sel[:, ti, :cap], in0=iof[:, :cap], scalar1=float(st["s0"]),
                                                                     scalar2=rankTok[:, ti, ex:ex + 1], op0=ALU.add, op1=ALU.is_equal),
                                           r=["rankTok", "rowsG"], w=["Sel"])
                        for fc in range(KC):
                            b = 6 + fc % 2
                            for st in sets:
                                s0, cap, w_ = st["s0"], st["cap"], st["w"]
                                for k in range(st["nt"]):
                                    ti = st["t0"] + k
                                    self.P(lambda e: e.matmul(self.pb[b][:, s0:s0 + cap], lhsT=xs[:, ti, fc * 128:(fc + 1) * 128], rhs=Sel[:, ti, :cap],
                                                              start=(k == 0), stop=(k == st["nt"] - 1)), r=["xs%d" % ti, "Sel"], x=[self.bk(b)])
                                sc_ = self.modA[:, 1, fc, w_:w_ + 1]
                                bi_ = self.modc[:, 3 * KC + fc, w_:w_ + 1]
                                cn["ecnt"] += 1
                                if cn["ecnt"] % 2 == 0:
                                    self.A(lambda e: e.activation(out=xeT[:, fc, s0:s0 + cap], in_=self.pb[b][:, s0:s0 + cap], func=AF.Identity,
                                                                  scale=sc_, bias=bi_), r=["modA", "modc"], w=["xeT"], x=[self.bk(b)])
                                else:
                                    self.V(lambda e: e.tensor_scalar(out=xeT[:, fc, s0:s0 + cap], in0=self.pb[b][:, s0:s0 + cap], scalar1=sc_, scalar2=bi_,
                                                                     op0=ALU.mult, op1=ALU.add), r=["modA", "modc"], w=["xeT"], x=[self.bk(b)])

                def do_gateup(ex):
                        wg_d = self.dr["w_exp_gate"][l, ex].rearrange("(kc p) f -> p kc f", p=128)
                        wu_d = self.dr["w_exp_up"][l, ex].rearrange("(kc p) f -> p kc f", p=128)
                        for pc in range(FF // PW):
                            kb = cn["wcnt"] % NWB
                            cn["wcnt"] += 1
                            S.dma("pool", Wg[kb][:], wg_d[:, :, pc * PW:(pc + 1) * PW], writes=["eWg%d" % kb])
                            S.dma("pool", Wu[kb][:], wu_d[:, :, pc * PW:(pc + 1) * PW], writes=["eWu%d" % kb])
                            for fo in range(PW // 128):
                                fidx = pc * (PW // 128) + fo
                                ba, bu = (0, 1) if fidx % 2 == 0 else (2, 3)
                                for kc in range(KC):
                                    self.P(lambda e: e.matmul(self.pb[ba][:, :NS], lhsT=Wg[kb][:, kc, fo * 128:(fo + 1) * 128], rhs=xeT[:, kc, :],
                                                              start=(kc == 0), stop=(kc == KC - 1)), r=["eWg%d" % kb, "xeT"], x=[self.bk(ba)])
                                for kc in range(KC):
                                    self.P(lambda e: e.matmul(self.pb[bu][:, :NS], lhsT=Wu[kb][:, kc, fo * 128:(fo + 1) * 128], rhs=xeT[:, kc, :],
                                                              start=(kc == 0), stop=(kc == KC - 1)), r=["eWu%d" % kb, "xeT"], x=[self.bk(bu)])
                                sa_ = sa[fidx % 2]
                                self.A(lambda e: e.activation(out=sa_[:], in_=self.pb[ba][:, :NS], func=AF.Silu), w=["sa%d" % (fidx % 2)], x=[self.bk(ba)])
                                self.V(lambda e: e.tensor_tensor(out=actT[:, fidx, :], in0=sa_[:], in1=self.pb[bu][:, :NS], op=ALU.mult),
                                       r=["sa%d" % (fidx % 2)], w=["actT"], x=[self.bk(bu)])

                def do_rest(ex):
                        wd_d = self.dr["w_exp_down"][l, ex].rearrange("(fc p) d -> p fc d", p=128)
                        for pc in range(FC // DP):
                            kb = cn["dcnt"] % NWB
                            cn["dcnt"] += 1
                            S.dma("pool", Wd[kb][:], wd_d[:, pc * DP:(pc + 1) * DP, :], writes=["eWd%d" % kb])
                            for f2 in range(DP):
                                fc = pc * DP + f2
                                for k_st, (s_start, nn, si) in enumerate(stiles):
                                    for hf in range(NH):
                                        b = k_st * NH + hf
                                        self.P(lambda e: e.matmul(self.pb[b][:nn, :512], lhsT=actT[:, fc, s_start:s_start + nn],
                                                                  rhs=Wd[kb][:, f2, hf * 512:(hf + 1) * 512], start=(fc == 0), stop=(fc == FC - 1)),
                                               r=["actT", "eWd%d" % kb], x=[self.bk(b)])
                        for k_st, (s_start, nn, si) in enumerate(stiles):
                            w_ = sets[si]["w"]
                            for hf in range(NH):
                                b = k_st * NH + hf
                                self.V(lambda e: e.tensor_tensor(out=ye[:nn, k_st, hf * 512:(hf + 1) * 512], in0=self.pb[b][:nn, :512],
                                                                 in1=self.grow[:nn, w_, hf * 512:(hf + 1) * 512], op=ALU.mult),
                                       r=["grow"], w=["ye"], x=[self.bk(b)])
                        for si, st in enumerate(sets):
                            c0, n = st["t0"] * 128, st["nt"] * 128
                            mine = [(k_st, s_start, nn) for k_st, (s_start, nn, sj) in enumerate(stiles) if sj == si]
                            for g0 in range(c0, c0 + n, 512):
                                gn = min(512, c0 + n - g0)
                                kb = cn["scnt"] % 2
                                cn["scnt"] += 1
                                for tt in range(gn // 128):
                                    ti = g0 // 128 + tt
                                    rk = cn["rcnt"] % 2
                                    cn["rcnt"] += 1
                                    self.G(lambda e: e.tensor_copy(out=repR[rk][:], in_=rankTok[:, ti, ex:ex + 1].to_broadcast([128, 128])),
                                           r=["rankTok"], w=["repR%d" % rk])
                                    self.G(lambda e: e.tensor_copy(out=repG[rk][:], in_=LG[:, ti, ex:ex + 1].to_broadcast([128, 128])),
                                           r=["LG"], w=["repG%d" % rk])
                                    self.P(lambda e: e.matmul(self.pb[6][:, tt * 128:(tt + 1) * 128], lhsT=repR[rk][:], rhs=identf, start=True, stop=True),
                                           r=["repR%d" % rk, "consts"], x=[self.bk(6)])
                                    self.P(lambda e: e.matmul(self.pb[7][:, tt * 128:(tt + 1) * 128], lhsT=repG[rk][:], rhs=identf, start=True, stop=True),
                                           r=["repG%d" % rk, "consts"], x=[self.bk(7)])
                                self.A(lambda e: e.copy(out=gsb[kb][:, :gn], in_=self.pb[7][:, :gn]), w=["gsb%d" % kb], x=[self.bk(7)])
                                for (k_st, s_start, nn) in mine:
                                    self.V(lambda e: e.scalar_tensor_tensor(out=SelT[kb][:, k_st, :gn], in0=self.pb[6][:, :gn], scalar=iopj[:, k_st:k_st + 1],
                                                                            in1=gsb[kb][:, :gn], op0=ALU.is_equal, op1=ALU.mult),
                                           r=["iopj", "gsb%d" % kb], w=["SelT%d" % kb], x=[self.bk(6)])
                                for tt in range(gn // 128):
                                    ti = g0 // 128 + tt
                                    xt = self.src_tile(ti)
                                    for hf in range(NH):
                                        b = cn["ocnt"] % 4
                                        cn["ocnt"] += 1
                                        for idx, (k_st, s_start, nn) in enumerate(mine):
                                            self.P(lambda e: e.matmul(self.pb[b][:, :512], lhsT=SelT[kb][:nn, k_st, tt * 128:(tt + 1) * 128],
                                                                      rhs=ye[:nn, k_st, hf * 512:(hf + 1) * 512], start=(idx == 0), stop=(idx == len(mine) - 1)),
                                                   r=["SelT%d" % kb, "ye"], x=[self.bk(b)])
                                        self.V(lambda e: e.tensor_tensor(out=xt[:, hf * 512:(hf + 1) * 512], in0=xt[:, hf * 512:(hf + 1) * 512],
                                                                         in1=self.pb[b][:, :512], op=ALU.add), w=["x%d" % ti], x=[self.bk(b)])

                do_gather(0)
                for ex in range(E):
                    do_gateup(ex)
                    if ex + 1 < E:
                        do_gather(ex + 1)
                    do_rest(ex)
                S.barrier()

    def final(self):
        cfg, S = self.cfg, self.S
        D, NT = cfg.D, cfg.NT
        with ExitStack() as es:
            ss = self.sb(es, "fss", [128, NT], F32)
            rstd = self.sb(es, "frstd", [128, NT], F32)
            junk = self.sb(es, "fjunk", [128, D], BF16)
            ot = [self.sb(es, "fo%d" % i, [128, D], F32) for i in range(2)]
            fnr = self.sb(es, "fnr", [128, D], F32)
            S.dma("sp", fnr[:], self.dr["fnrow"], writes=["fnr"])
            for i in range(NT):
                self.A(lambda e: e.activation(out=junk[:], in_=self.x_sb[:, i, :], func=AF.Square,
                                              accum_out=ss[:, i:i + 1]), r=["x%d" % i], w=["fjunk", "fss"])
            self.V(lambda e: e.tensor_scalar(out=rstd[:], in0=ss[:], scalar1=1.0 / D, scalar2=EPS,
                                             op0=ALU.mult, op1=ALU.add), r=["fss"], w=["frstd"])
            self.A(lambda e: e.activation(out=rstd[:], in_=rstd[:], func=AF.Sqrt), r=[], w=["frstd"])
            self.V(lambda e: e.reciprocal(out=rstd[:], in_=rstd[:]), r=[], w=["frstd"])
            for i in range(NT):
                o = ot[i % 2]
                self.V(lambda e: e.scalar_tensor_tensor(out=o[:], in0=self.x_sb[:, i, :], scalar=rstd[:, i:i + 1],
                                                        in1=fnr[:], op0=ALU.mult, op1=ALU.mult),
                       r=["x%d" % i, "frstd", "fnr"], w=["fo%d" % (i % 2)])
                S.dma("sp", self.y[i * 128:(i + 1) * 128, :], o[:], reads=["fo%d" % (i % 2)], writes=["y%d" % i])
            S.barrier()


def host_packs(cfg, inp, b):
    D, KC, DEPTH = cfg.D, cfg.KC, cfg.DEPTH
    cols = np.zeros((128, cfg.NCOL), np.float32)

    def colset(name, v):
        o, w = cfg.coff[name]
        cols[:, o:o + w] = np.asarray(v, np.float32).reshape(w, 128).T
    colset("c", inp["c"][b])
    colset("cctx", inp["c_ctx"])
    for l in range(DEPTH):
        colset("bada%d" % l, inp["b_ada"][l])
        colset("n1%d" % l, inp["norm1_w"][l])
        colset("n2%d" % l, inp["norm2_w"][l])
        colset("conv%d" % l, np.asarray(inp["mlstm_conv_w"][l]).reshape(-1))
        qn = np.asarray(inp["gqa_qnorm_w"][l], np.float32)
        kn = np.asarray(inp["gqa_knorm_w"][l], np.float32)
        perm = np.concatenate([np.arange(32, 64), np.arange(0, 32)])
        o, _ = cfg.coff["qnc%d" % l]
        cols[:, o] = np.tile(qn, 2)
        cols[:, o + 1] = np.tile(qn[perm], 2)
        o, _ = cfg.coff["knc%d" % l]
        cols[:, o] = np.tile(kn, 2)
        cols[:, o + 1] = np.tile(kn[perm], 2)
    cols[:, cfg.coff["iop"][0]] = np.arange(128)
    cols[:, cfg.coff["eps"][0]] = EPS
    rowsL = np.zeros((DEPTH, 128, cfg.NROWL), np.float32)
    rowsG = np.zeros((128, cfg.NROWG), np.float32)
    brows = np.zeros((DEPTH, 128, 6 * D), np.float32)

    def rowset(arr, name, v):
        o, w = cfg.roff[name]
        arr[:, o:o + w] = np.asarray(v, np.float32).reshape(1, w)
    for l in range(DEPTH):
        rowset(rowsL[l], "sub", inp["diff_subln_w"][l])
        rowset(rowsL[l], "cn", inp["mlstm_norm_w"][l])
        rowset(rowsL[l], "gb", inp["mlstm_gate_b"][l])
        rowset(rowsL[l], "lam", np.asarray(inp["diff_lambda"][l]).reshape(-1))
        brows[l] = np.asarray(inp["b_ada"][l], np.float32).reshape(1, 6 * D)
    rowset(rowsG, "iof", np.arange(256))
    rows = (rowsL, rowsG, brows)
    return cols, rows


def make_in_maps(cfg, inp, cores):
    cosT, sinT = rope_tables(cfg)
    ropeT = np.concatenate([cosT, sinT], axis=1)
    consts = const_pack()
    E = cfg.E
    esel = np.zeros((E, E * 128), np.float32)
    for e in range(E):
        esel[e, e * 128:(e + 1) * 128] = 1.0
    shared = {k: np.ascontiguousarray(np.asarray(inp[k], np.float32)) for k in
              ("w_ada", "w_in", "w_branch_a", "w_branch_b", "w_branch_c", "w_out", "w_router",
               "w_exp_gate", "w_exp_up", "w_exp_down")}
    maps = []
    for b in cores:
        cols, rows = host_packs(cfg, inp, b)
        m = {"x": np.ascontiguousarray(inp["x"][b], np.float32), "ctx": np.ascontiguousarray(inp["ctx"][b], np.float32),
             "cols": cols, "rowsL": rows[0], "rowsG": rows[1], "brows": rows[2], "fnrow": np.ascontiguousarray(np.broadcast_to(np.asarray(inp["final_norm_w"], np.float32).reshape(1, -1), (128, cfg.D))), "consts": consts, "ropeT": ropeT, "esel": esel}
        m.update(shared)
        maps.append(m)
    return maps


_CACHE = {}


def kernel(**inputs):
    cfg = Cfg()
    if "nc" not in _CACHE:
        _CACHE["nc"] = Builder(cfg).build()
    nc = _CACHE["nc"]
    inp = {k: np.asarray(v) for k, v in inputs.items()}
    n = inp["x"].shape[0]
    in_maps = make_in_maps(cfg, inp, list(range(n)))
    res = run_bass_kernel_spmd(nc, in_maps, core_ids=list(range(n)))
    return np.stack([np.asarray(r["y"], np.float32) for r in res.results], axis=0)
```
